# Optimizing a Trainium2 kernel written in Bass

```python
import jax, jax.numpy as jnp
from jax import lax
import numpy as np

D_MODEL = 1024
BATCH = 1
SEQ = 16384
DEPTH = 2

CHUNK = 64
N_EVEN = (DEPTH + 1) // 2
N_ODD = DEPTH // 2
HG_HEADS = 4
HG_DK = 128
HG_DV = 128
RET_HEADS = 4
RET_DK = 128
RET_DV = 128
HG_WIDTH = HG_HEADS * HG_DK
RET_WIDTH = RET_HEADS * RET_DK
MIX_WIDTH = HG_HEADS * HG_DV + RET_HEADS * RET_DV
IN_WIDTH = 4 * HG_WIDTH + 4 * RET_WIDTH
RET_GAMMA_EXP0 = 5.0
ROPE_BASE = 10000.0
POOL_WINDOWS = (2, 4, 8, 16)
N_POOL = 4
POOL_GROUP = D_MODEL // N_POOL
N_EXPERTS = 32
TOP_K = 4
D_EXPERT = D_MODEL
SWIGLU_LIMIT = 7.0
SWIGLU_ALPHA = 1.702
MOE_BLOCK = 256
EPS = 1e-6

kernel_name = "hybrid_hgrn2_retention_pool_moe_adaln"


def rmsnorm(x):
    xf = x.astype(jnp.float32)
    return (xf * lax.rsqrt(jnp.mean(xf * xf, axis=-1, keepdims=True) + EPS)).astype(x.dtype)


def head_rmsnorm(o, gain):
    o = o * lax.rsqrt(jnp.mean(o * o, axis=-1, keepdims=True) + EPS)
    return o * gain.astype(jnp.float32)[None, :, None, None, :]


def to_chunks(a, heads):
    B, S, W = a.shape
    return a.reshape(B, S // CHUNK, CHUNK, heads, W // heads).transpose(0, 3, 1, 2, 4)


def from_chunks(a):
    B, H, N, C, d = a.shape
    return a.transpose(0, 2, 3, 1, 4).reshape(B, N * C, H * d)


def rotary(x):
    S, d = x.shape[1], x.shape[-1]
    half = d // 2
    inv = ROPE_BASE ** (-jnp.arange(half, dtype=jnp.float32) / half)
    ang = jnp.arange(S, dtype=jnp.float32)[:, None] * inv[None, :]
    cos = jnp.cos(ang)[None, :, None, :]
    sin = jnp.sin(ang)[None, :, None, :]
    x1, x2 = x[..., :half], x[..., half:]
    return jnp.concatenate([x1 * cos - x2 * sin, x1 * sin + x2 * cos], axis=-1)


def hgrn2_chunked(q, k, v, g):
    B, H, N, C, dk = q.shape
    dv = v.shape[-1]
    mask = jnp.tril(jnp.ones((C, C), dtype=bool))[:, :, None]

    def step(S, inp):
        qc, kc, vc, gc = inp
        b = jnp.cumsum(gc, axis=2)
        o_inter = jnp.einsum('bhtd,bhde->bhte', qc * jnp.exp(b), S)
        diff = b[:, :, :, None, :] - b[:, :, None, :, :]
        decay = jnp.where(mask, jnp.exp(jnp.where(mask, diff, 0.0)), 0.0)
        attn = jnp.einsum('bhtd,bhsd,bhtsd->bhts', qc, kc, decay)
        o_intra = jnp.einsum('bhts,bhse->bhte', attn, vc)
        b_last = b[:, :, -1:, :]
        S_new = jnp.exp(b_last[:, :, 0, :])[..., None] * S + jnp.einsum(
            'bhsd,bhse->bhde', kc * jnp.exp(b_last - b), vc)
        return S_new, o_inter + o_intra

    xs = tuple(jnp.moveaxis(a, 2, 0) for a in (q, k, v, g))
    S0 = jnp.zeros((B, H, dk, dv), jnp.float32)
    _, o = lax.scan(step, S0, xs)
    return jnp.moveaxis(o, 0, 2)


def retention_chunked(q, k, v):
    B, H, N, C, dk = q.shape
    dv = v.shape[-1]
    lg = jnp.log(1.0 - 2.0 ** (-RET_GAMMA_EXP0 - jnp.arange(H, dtype=jnp.float32)))
    idx = jnp.arange(C, dtype=jnp.float32)
    rel = idx[:, None] - idx[None, :]
    causal = rel >= 0
    dmat = jnp.where(causal, jnp.exp(jnp.where(causal, rel, 0.0)[None] * lg[:, None, None]), 0.0)
    scores = jnp.einsum('bhnqd,bhnkd->bhnqk', q, k) * dmat[None, :, None]
    o_intra = jnp.einsum('bhnqk,bhnke->bhnqe', scores, v)
    k_dec = k * jnp.exp((C - 1 - idx)[None, :] * lg[:, None])[None, :, None, :, None]
    kv = jnp.einsum('bhnsd,bhnse->nbhde', k_dec, v)
    chunk_decay = jnp.exp(C * lg)[None, :, None, None]

    def step(R, kv_n):
        return chunk_decay * R + kv_n, R

    _, R_prev = lax.scan(step, jnp.zeros((B, H, dk, dv), jnp.float32), kv)
    q_dec = q * jnp.exp((idx + 1.0)[None, :] * lg[:, None])[None, :, None, :, None]
    o_inter = jnp.einsum('bhnqd,nbhde->bhnqe', q_dec, R_prev)
    return o_intra + o_inter


def hgrn2_retention_mixer(h, w_in, lower_bound, hg_gain, ret_gain, w_out):
    B, S, _ = h.shape
    proj = (h @ w_in).astype(jnp.float32)
    sizes = [HG_WIDTH] * 4 + [RET_WIDTH] * 4
    split_at = np.cumsum(sizes)[:-1].tolist()
    hq, hf, hi, hog, rq, rk, rv, rg = jnp.split(proj, split_at, axis=-1)
    lb = lower_bound.astype(jnp.float32)
    fgate = lb + (1.0 - lb) * jax.nn.sigmoid(hf)
    o_hg = hgrn2_chunked(to_chunks(jax.nn.silu(hq), HG_HEADS),
                         to_chunks(1.0 - fgate, HG_HEADS),
                         to_chunks(hi, HG_HEADS),
                         to_chunks(jnp.log(fgate), HG_HEADS))
    o_hg = from_chunks(head_rmsnorm(o_hg, hg_gain)) * jax.nn.silu(hog)
    q = rotary(rq.reshape(B, S, RET_HEADS, RET_DK)).reshape(B, S, RET_WIDTH)
    k = (rotary(rk.reshape(B, S, RET_HEADS, RET_DK)) * RET_DK ** -0.5).reshape(B, S, RET_WIDTH)
    o_ret = retention_chunked(to_chunks(q, RET_HEADS), to_chunks(k, RET_HEADS),
                              to_chunks(rv, RET_HEADS))
    o_ret = from_chunks(head_rmsnorm(o_ret, ret_gain)) * jax.nn.silu(rg)
    cat = jnp.concatenate([o_hg, o_ret], axis=-1).astype(h.dtype)
    return cat @ w_out


def pool_mixer(h, pool_w, pool_b, pool_scale):
    B, S, D = h.shape
    hf = h.astype(jnp.float32).reshape(B, S, N_POOL, POOL_GROUP)
    csp = jnp.concatenate([jnp.zeros((B, 1, N_POOL, POOL_GROUP), jnp.float32),
                           jnp.cumsum(hf, axis=1)], axis=1)
    t = jnp.arange(S)
    outs = []
    for gi, w in enumerate(POOL_WINDOWS):
        cg = csp[:, :, gi]
        lo = jnp.maximum(t + 1 - w, 0)
        win_sum = cg[:, 1:] - cg[:, lo]
        cnt = jnp.minimum(t + 1, w).astype(jnp.float32)
        outs.append(win_sum / cnt[None, :, None] - hf[:, :, gi])
    p = jnp.stack(outs, axis=2).astype(h.dtype)
    y = jnp.einsum('bsgc,gcd->bsgd', p, pool_w) + pool_b
    return (y.reshape(B, S, D) * pool_scale).astype(h.dtype)


def moe_ffn(h, w_router, b_router, w_gu, b_gu, w_down, b_down):
    B, S, D = h.shape
    T = B * S
    hf = h.reshape(T, D)
    logits = (hf @ w_router + b_router).astype(jnp.float32)
    top_val, top_idx = lax.top_k(logits, TOP_K)
    gates = jax.nn.softmax(top_val, axis=-1)
    flat_e = top_idx.reshape(-1).astype(jnp.int32)
    flat_tok = jnp.arange(T * TOP_K, dtype=jnp.int32) // TOP_K
    flat_w = gates.reshape(-1)
    order = jnp.argsort(flat_e)
    sorted_e = flat_e[order]
    counts = jnp.bincount(flat_e, length=N_EXPERTS).astype(jnp.int32)
    padded = (counts + MOE_BLOCK - 1) // MOE_BLOCK * MOE_BLOCK
    pad_end = jnp.cumsum(padded)
    pad_start = pad_end - padded
    grp_start = jnp.cumsum(counts) - counts
    pos = jnp.arange(T * TOP_K, dtype=jnp.int32)
    dest = pad_start[sorted_e] + pos - grp_start[sorted_e]
    n_rows = T * TOP_K + N_EXPERTS * MOE_BLOCK
    n_blocks = n_rows // MOE_BLOCK
    row_tok = jnp.zeros((n_rows,), jnp.int32).at[dest].set(flat_tok[order])
    row_w = jnp.zeros((n_rows,), jnp.float32).at[dest].set(flat_w[order])
    blk_start = jnp.arange(n_blocks, dtype=jnp.int32) * MOE_BLOCK
    blk_e = jnp.minimum(jnp.searchsorted(pad_end, blk_start, side='right'), N_EXPERTS - 1)
    xs = hf[row_tok].reshape(n_blocks, MOE_BLOCK, D)

    def expert_block(args):
        xb, e = args
        gu = xb @ w_gu[e] + b_gu[e]
        x_glu = jnp.minimum(gu[:, :D_EXPERT], SWIGLU_LIMIT)
        x_lin = jnp.clip(gu[:, D_EXPERT:], -SWIGLU_LIMIT, SWIGLU_LIMIT)
        act = x_glu * jax.nn.sigmoid(SWIGLU_ALPHA * x_glu) * (x_lin + 1.0)
        return act @ w_down[e] + b_down[e]

    ys = lax.map(expert_block, (xs, blk_e)).reshape(n_rows, D)
    out = jax.ops.segment_sum(ys * row_w[:, None], row_tok, num_segments=T)
    return out.reshape(B, S, D).astype(h.dtype)


def setup_inputs(seed: int = 0) -> dict:
    key = jax.random.key(seed)
    ks = jax.random.split(key, 19)

    def nrm(k, shape, scale):
        return jax.random.normal(k, shape, jnp.float32) * scale

    return {
        "x": nrm(ks[0], (BATCH, SEQ, D_MODEL), 1.0),
        "c": nrm(ks[1], (BATCH, D_MODEL), 1.0),
        "w_ada": nrm(ks[2], (DEPTH, D_MODEL, 6 * D_MODEL), 0.5 * D_MODEL ** -0.5),
        "b_ada": nrm(ks[3], (DEPTH, 6 * D_MODEL), 0.02),
        "w_in": nrm(ks[4], (N_EVEN, D_MODEL, IN_WIDTH), D_MODEL ** -0.5),
        "hg_lower_bounds": nrm(ks[5], (N_EVEN + 1, HG_WIDTH), 0.5),
        "hg_norm": 1.0 + nrm(ks[6], (N_EVEN, HG_HEADS, HG_DV), 0.02),
        "ret_norm": 1.0 + nrm(ks[7], (N_EVEN, RET_HEADS, RET_DV), 0.02),
        "w_out": nrm(ks[8], (N_EVEN, MIX_WIDTH, D_MODEL), MIX_WIDTH ** -0.5),
        "pool_w": nrm(ks[9], (N_ODD, N_POOL, POOL_GROUP, POOL_GROUP), POOL_GROUP ** -0.5),
        "pool_b": nrm(ks[10], (N_ODD, N_POOL, POOL_GROUP), 0.02),
        "pool_scale": 1.0 + nrm(ks[11], (N_ODD, D_MODEL), 0.1),
        "w_router": nrm(ks[12], (DEPTH, D_MODEL, N_EXPERTS), D_MODEL ** -0.5),
        "b_router": nrm(ks[13], (DEPTH, N_EXPERTS), 0.01),
        "w_gu": nrm(ks[14], (DEPTH, N_EXPERTS, D_MODEL, 2 * D_EXPERT), D_MODEL ** -0.5),
        "b_gu": nrm(ks[15], (DEPTH, N_EXPERTS, 2 * D_EXPERT), 0.02),
        "w_down": nrm(ks[16], (DEPTH, N_EXPERTS, D_EXPERT, D_MODEL), D_EXPERT ** -0.5),
        "b_down": nrm(ks[17], (DEPTH, N_EXPERTS, D_MODEL), 0.02),
        "final_norm": 1.0 + nrm(ks[18], (D_MODEL,), 0.02),
    }


def reference(x, c, w_ada, b_ada, w_in, hg_lower_bounds, hg_norm, ret_norm, w_out,
              pool_w, pool_b, pool_scale, w_router, b_router, w_gu, b_gu, w_down,
              b_down, final_norm):
    cond = jax.nn.silu(c.astype(jnp.float32)).astype(x.dtype)
    lb_all = jnp.cumsum(jax.nn.softmax(hg_lower_bounds.astype(jnp.float32), axis=0), axis=0)
    for layer in range(DEPTH):
        mod = (cond @ w_ada[layer] + b_ada[layer])[:, None, :]
        sh_m, sc_m, g_m, sh_f, sc_f, g_f = jnp.split(mod, 6, axis=-1)
        hn = rmsnorm(x) * (1.0 + sc_m) + sh_m
        j = layer // 2
        if layer % 2 == 0:
            mix = hgrn2_retention_mixer(hn, w_in[j], lb_all[j], hg_norm[j], ret_norm[j], w_out[j])
        else:
            mix = pool_mixer(hn, pool_w[j], pool_b[j], pool_scale[j])
        x = x + g_m * mix
        hn = rmsnorm(x) * (1.0 + sc_f) + sh_f
        x = x + g_f * moe_ffn(hn, w_router[layer], b_router[layer], w_gu[layer],
                              b_gu[layer], w_down[layer], b_down[layer])
    return rmsnorm(x) * final_norm
```

```python
from contextlib import ExitStack
import numpy as np
import concourse.bass as bass
import concourse.mybir as mybir
from concourse.bass_utils import run_bass_kernel_spmd

F32 = mybir.dt.float32
BF16 = mybir.dt.bfloat16
ALU = mybir.AluOpType
AF = mybir.ActivationFunctionType
AX = mybir.AxisListType
ENGS = ("pe", "act", "dve", "pool", "sp")
EPS = 1e-6
HALO = 64


class Sched:
    def __init__(self, nc, es):
        self.nc, self.es = nc, es
        self.items = {e: [] for e in ENGS}
        self.cnt = {e: 0 for e in ENGS}
        self.clock = {e: {} for e in ENGS}
        self.snap, self.key_w, self.key_r, self.dma_cnt, self.sems = {}, {}, {}, {}, {}
        self.out_events = []
        self.epoch = 0
        self.ek = {e: "E:" + e for e in ENGS}
        for e in ENGS:
            self._sem(self.ek[e])

    def _sem(self, k):
        if k not in self.sems:
            self.sems[k] = self.es.enter_context(self.nc.semaphore("s%d" % len(self.sems)))
        return self.sems[k]

    def _need(self, eng, ev, deps):
        if ev is None:
            return
        sk, val = ev
        if eng == "pe" and sk.startswith("E:pe"):
            return
        if self.clock[eng].get(sk, 0) >= val:
            return
        if deps.get(sk, 0) < val:
            deps[sk] = val

    def _apply(self, eng, deps):
        clk = self.clock[eng]
        for sk, val in deps.items():
            self.items[eng].append(("w", sk, val))
            for a, b in self.snap.get((sk, val), {}).items():
                if clk.get(a, 0) < b:
                    clk[a] = b
            if clk.get(sk, 0) < val:
                clk[sk] = val

    def op(self, eng, fn, reads=(), writes=(), dma_key=None, is_out=False, dma_inc=16):
        writes = [k.split("_")[0] if k.startswith("ps") else k for k in writes]
        writes += [k.split("_")[0] for k in reads if k.startswith("ps")]
        reads = [k for k in reads if not k.startswith("ps")]
        deps = {}
        for k in reads:
            self._need(eng, self.key_w.get(k), deps)
        for k in writes:
            self._need(eng, self.key_w.get(k), deps)
            for ev in self.key_r.get(k, ()):
                self._need(eng, ev, deps)
        self._apply(eng, deps)
        if dma_key is not None:
            sk = "D:" + str(dma_key)
            self._sem(sk)
            self.dma_cnt[sk] = self.dma_cnt.get(sk, 0) + 1
            ev = (sk, self._dval(sk, dma_inc))
            self.items[eng].append(("i", fn, sk, dma_inc))
            if is_out:
                self.out_events.append(ev)
        else:
            self.cnt[eng] += 1
            ev = (self.ek[eng], self.cnt[eng])
            self.items[eng].append(("i", fn, self.ek[eng], 1))
        self.snap[ev] = dict(self.clock[eng])
        for k in writes:
            self.key_w[k] = ev
            self.key_r[k] = []
        for k in reads:
            self.key_r.setdefault(k, []).append(ev)
        return ev

    def _dval(self, sk, inc):
        self.dma_val = getattr(self, "dma_val", {})
        self.dma_val[sk] = self.dma_val.get(sk, 0) + inc
        return self.dma_val[sk]

    def barrier(self):
        evs = [(self.ek[e], self.cnt[e]) for e in ENGS if self.cnt[e] > 0]
        evs += [(sk, v) for sk, v in getattr(self, "dma_val", {}).items()]
        for eng in ENGS:
            deps = {}
            for sk, val in evs:
                if self.clock[eng].get(sk, 0) < val:
                    deps[sk] = val
            self._apply(eng, deps)
        self.key_w, self.key_r = {}, {}
        self.epoch += 1
        for e in ENGS:
            if self.cnt[e] > 12000:
                self.ek[e] = "E:%s:%d" % (e, self.epoch)
                self._sem(self.ek[e])
                self.cnt[e] = 0

    def finish(self):
        deps = {}
        for ev in self.out_events:
            self._need("sp", ev, deps)
        self._apply("sp", deps)

    def emit(self):
        nc = self.nc
        with nc.Block() as block:
            def run(name):
                def body(engine):
                    for it in self.items[name]:
                        if it[0] == "w":
                            engine.wait_ge(self.sems[it[1]], it[2])
                        else:
                            it[1](engine).then_inc(self.sems[it[2]], it[3])
                return body
            block.tensor(run("pe"))
            block.scalar(run("act"))
            block.vector(run("dve"))
            block.gpsimd(run("pool"))
            block.sync(run("sp"))


class Stop(Exception):
    pass


def tiles(lo, hi, step=512):
    return [(t, min(step, hi - t)) for t in range(lo, hi, step)]


def build(TP, mode, ncores=8, dbg=()):
    T = TP + HALO
    NCH = T // 64
    nc = bass.Bass("TRN2", target_bir_lowering=False)

    in_names = []

    def din(name, shape):
        in_names.append(name)
        return nc.dram_tensor(name, list(shape), F32, kind="ExternalInput").ap()

    def dout(name, shape):
        return nc.dram_tensor(name, list(shape), F32, kind="ExternalOutput").ap()

    NW = 8 if mode == "F2" else 1
    xTw = din("xT", [NW, 1024, T])
    flag_d = din("flag", [128, 8])
    valid_d = din("valid", [128, 8])
    cT_d = din("cT", [128, 8])
    w_ada = din("w_ada", [2, 1024, 6144])
    badaT_d = din("b_adaT", [128, 96])
    w_in = din("w_in", [1024, 4096])
    w_in_sw = din("w_in_sw", [1024, 1024])
    lbraw_d = din("lbraw", [128, 8])
    gains_d = din("gains", [128, 8])
    w_out = din("w_out", [1024, 1024]) if mode != "A" else None
    pool_w = din("pool_w", [4, 256, 256]) if mode != "A" else None
    pool_bT_d = din("pool_bT", [128, 8])
    pool_sT_d = din("pool_sT", [128, 8])
    w_router = din("w_router", [2, 1024, 32]) if mode != "A" else None
    b_router = din("b_router", [2, 32]) if mode != "A" else None
    w_gu = din("w_gu", [2, 32, 1024, 2048]) if mode != "A" else None
    b_guT_d = din("b_guT", [128, 2 * 32 * 16]) if mode != "A" else None
    w_down = din("w_down", [2, 32, 1024, 1024]) if mode != "A" else None
    b_down = din("b_down", [2, 32, 1024]) if mode != "A" else None
    fnT_d = din("fnT", [128, 8])
    ident_d = din("ident", [128, 128])
    masks_d = din("masks", [64, 5 * 64])
    kscale_d = din("kscale", [64, 8])
    cos_d = din("cosT", [NW, 128, T])
    sin_d = din("sinT", [NW, 128, T])
    gdec_d = din("gdec", [128, 4 * 64])
    scanm_d = din("scanm", [128, T])
    invc_d = din("invc", [128, 4 * 16])
    seli_d = din("seli", [128, 8])
    if mode == "A":
        Lst_o = dout("Lst", [128, 8 * 128])
        Dst_o = dout("Dst", [128, 8])
    if mode == "B":
        Lall_d = din("Lall", [ncores, 128, 8 * 128])
        Dall_d = din("Dall", [ncores, 128, 8])
    if mode in ("B", "F", "F2"):
        yT = dout("yT", [1024, TP])
    dbg_outs = {}

    gam = [1.0 - 2.0 ** (-5 - h) for h in range(4)]

    with ExitStack() as es:
        S = Sched(nc, es)

        uniq = [0]

        def sbt(stack, name, shape, dt=F32):
            uniq[0] += 1
            return stack.enter_context(nc.sbuf_tensor("sb%d_%s" % (uniq[0], name), list(shape), dt))

        def DMA(eng, out, in_, reads=(), writes=(), key=None, is_out=False):
            S.op(eng, lambda e: e.dma_start(out=out, in_=in_), reads, writes, dma_key=key or writes[0], is_out=is_out)

        def MM(out, lhsT, rhs, start=True, stop=True, reads=(), writes=()):
            S.op("pe", lambda e: e.matmul(out, lhsT, rhs, start=start, stop=stop), reads, writes)

        def TR(out, in_, ident, reads=(), writes=()):
            S.op("pe", lambda e: e.transpose(out, in_, ident), reads, writes)

        def ACT(out, in_, func, reads=(), writes=(), bias=None, scale=None):
            kw = {}
            if bias is not None:
                kw["bias"] = bias
            if scale is not None:
                kw["scale"] = scale
            S.op("act", lambda e: e.activation(out=out, in_=in_, func=func, **kw), reads, writes)

        def TT(eng, out, in0, in1, op, reads=(), writes=()):
            S.op(eng, lambda e: e.tensor_tensor(out=out, in0=in0, in1=in1, op=op), reads, writes)

        def TS(eng, out, in0, s1, s2, op0, op1=None, reads=(), writes=()):
            if op1 is None:
                S.op(eng, lambda e: e.tensor_scalar(out=out, in0=in0, scalar1=s1, scalar2=None, op0=op0), reads, writes)
            else:
                S.op(eng, lambda e: e.tensor_scalar(out=out, in0=in0, scalar1=s1, scalar2=s2, op0=op0, op1=op1), reads, writes)

        def STT(out, in0, scalar, in1, op0, op1, reads=(), writes=()):
            S.op("dve", lambda e: e.scalar_tensor_tensor(out=out, in0=in0, scalar=scalar, in1=in1, op0=op0, op1=op1),
                 reads, writes)

        def MEMSET(eng, ap, val, writes=()):
            S.op(eng, lambda e: e.memset(ap, val), (), writes)

        def COPY(eng, out, in_, reads=(), writes=()):
            if eng == "act":
                ACT(out, in_, AF.Identity, reads, writes)
            else:
                S.op(eng, lambda e: e.tensor_copy(out=out, in_=in_), reads, writes)

        def dump(name, ap, shape, key):
            if name in dbg:
                d = dout("dbg_" + name, shape)
                dbg_outs[name] = d
                DMA("sp", d, ap, reads=[key], writes=["dbg_" + name], is_out=True)

        G = es
        big = sbt(G, "big", [128, 8, T])
        bigb = big[:].rearrange("p k t -> p (k t)").bitcast(BF16).rearrange("p (k t) -> p k t", k=16)
        hnT = bigb[:, 0:8, :]
        catT = bigb[:, 8:16, :]
        ps = [G.enter_context(nc.psum_tensor("ps%d" % i, [128, 512], F32)) for i in range(7)]
        psb = G.enter_context(nc.psum_tensor("psb", [128, 1024], BF16))
        ident_f = sbt(G, "ident_f", [128, 128])
        ident_b = sbt(G, "ident_b", [128, 128], BF16)
        ones_b = sbt(G, "ones_b", [128, 128], BF16)
        ones_f = sbt(G, "ones_f", [128, 128])
        eps_t = sbt(G, "eps_t", [128, 1])
        flagw = sbt(G, "flag_s", [128, 8])
        W = {"w": 0, "L": None, "D": None}
        mod = sbt(G, "mod", [128, 96])
        mod1 = sbt(G, "mod1", [128, 96])
        small = sbt(G, "small", [128, 64])
        Lst = sbt(G, "Lst_s", [128, 8, 128])
        Dst = sbt(G, "Dst_s", [128, 8])
        Sin = sbt(G, "Sin_s", [128, 8, 128])
        seli = sbt(G, "seli_s", [128, 8])

        DMA("sp", ident_f[:], ident_d[:, :], writes=["ident_f"])
        DMA("pool", ident_b[:], ident_d[:, :], writes=["ident_b"])
        DMA("sp", flagw[:], flag_d[:, :], writes=["flag"])
        DMA("sp", seli[:], (valid_d if mode == "F2" else seli_d)[:, :], writes=["seli"])
        MEMSET("pool", ones_b[:], 1.0, ["ones_b"])
        MEMSET("pool", ones_f[:], 1.0, ["ones_f"])
        MEMSET("pool", eps_t[:], EPS, ["eps_t"])
        DMA("sp", small[:, 40:48], lbraw_d[:, :], writes=["small_lbraw"])
        DMA("sp", small[:, 8:16], gains_d[:, :], writes=["small_g"])
        DMA("sp", small[:, 16:24], pool_sT_d[:, :], writes=["small_ps"])
        DMA("sp", small[:, 24:32], pool_bT_d[:, :], writes=["small_pb"])
        DMA("sp", small[:, 32:40], fnT_d[:, :], writes=["small_fn"])
        TT("dve", small[:, 48:52], small[:, 40:44], small[:, 44:48], ALU.subtract, ["small_lbraw"], ["small_t"])
        ACT(small[:, 0:4], small[:, 48:52], AF.Sigmoid, ["small_t"], ["small_lb"])
        TS("dve", small[:, 4:8], small[:, 0:4], -1.0, 1.0, ALU.mult, ALU.add, ["small_lb"], ["small_oml"])

        with ExitStack() as P0:
            cT = sbt(P0, "cT_s", [128, 8])
            cond = sbt(P0, "cond", [128, 8])
            badaT = sbt(P0, "badaT", [128, 96])
            wa = [sbt(P0, "wa%d" % i, [128, 8, 768]) for i in range(2)]
            DMA("sp", cT[:], cT_d[:, :], writes=["cT"])
            DMA("sp", badaT[:], badaT_d[:, :], writes=["badaT"])
            ACT(cond[:], cT[:], AF.Silu, ["cT"], ["cond"])
            for l in range(2):
                for blk in range(8):
                    i = (l * 8 + blk) % 2
                    DMA("sp", wa[i][:], w_ada[l, :, blk * 768:(blk + 1) * 768].rearrange("(k p) c -> p k c", p=128),
                        writes=["wa%d" % i])
                    for m in range(6):
                        col = l * 48 + blk * 6 + m
                        for k in range(8):
                            MM(ps[0][:, col:col + 1], wa[i][:, k, m * 128:(m + 1) * 128], cond[:, k:k + 1],
                               start=(k == 0), stop=(k == 7), reads=["wa%d" % i, "cond"], writes=["ps0"])
            TT("dve", mod[:], ps[0][:, 0:96], badaT[:], ALU.add, ["ps0", "badaT"], ["mod"])
            TS("dve", mod1[:], mod[:], 1.0, None, ALU.add, reads=["mod"], writes=["mod1"])
            TT("dve", small[:, 16:24], small[:, 16:24], mod[:, 48 + 16:48 + 24], ALU.mult, ["small_ps", "mod"], ["small_pc"])
            S.barrier()
        dump("mod", mod[:], [128, 96], "mod")

        rot = {"pj": 0, "at": 0, "o": 0, "ds": 0}

        def ps_pj():
            rot["pj"] ^= 1
            return ps[rot["pj"]], "ps%d" % rot["pj"]

        def norm_tile(NS, xk, xkeys, n, l, ffn, out_bf, out_bf_keys, out_f32=None, out_f32_keys=None, final=False):
            base = l * 48 + (24 if ffn else 0)
            pj, pk = ps_pj()
            for k in range(8):
                ACT(NS["sq"][:, k, :n], xk[k], AF.Square, [xkeys[k]], ["sq%d" % k])
            for k in range(8):
                MM(pj[:, :n], ones_b[:], NS["sq"][:, k, :n], start=(k == 0), stop=(k == 7),
                   reads=["sq%d" % k, "ones_b"], writes=[pk])
            ACT(NS["stdt"][:, :n], pj[:, :n], AF.Sqrt, [pk, "eps_t"], ["stdt"], bias=eps_t[:, 0:1], scale=1.0 / 1024.0)
            S.op("dve", lambda e: e.reciprocal(out=NS["stdt"][:, :n], in_=NS["stdt"][:, :n]), ["stdt"], ["stdt"])
            for k in range(8):
                if final:
                    STT(out_f32[k], xk[k], small[:, 32 + k:33 + k], NS["stdt"][:, :n], ALU.mult, ALU.mult,
                        [xkeys[k], "stdt", "small_fn"], [out_f32_keys[k]])
                    continue
                STT(NS["t1"][:, k, :n], xk[k], mod1[:, base + 8 + k:base + 9 + k], NS["stdt"][:, :n], ALU.mult, ALU.mult,
                    [xkeys[k], "stdt", "mod1"], ["t1_%d" % k])
                if out_f32 is not None:
                    ACT(out_f32[k], NS["t1"][:, k, :n], AF.Identity, ["t1_%d" % k, "mod"], [out_f32_keys[k]],
                        bias=mod[:, base + k:base + k + 1])
                    COPY("pool", out_bf[k], out_f32[k], [out_f32_keys[k]], [out_bf_keys[k]])
                else:
                    ACT(out_bf[k], NS["t1"][:, k, :n], AF.Identity, ["t1_%d" % k, "mod"], [out_bf_keys[k]],
                        bias=mod[:, base + k:base + k + 1])

        def mixer_norm():
            with ExitStack() as PN:
                NS = {"sq": sbt(PN, "sq", [128, 8, 512], BF16), "stdt": sbt(PN, "stdt", [128, 512]),
                      "t1": sbt(PN, "t1", [128, 8, 512])}
                xt = [sbt(PN, "xt%d" % i, [128, 8, 512]) for i in range(2)]
                for ti, (t0, n) in enumerate(tiles(0, T)):
                    b = xt[ti % 2]
                    DMA("sp", b[:, :, :n], xTw[W["w"], :, t0:t0 + n].rearrange("(k p) t -> p k t", p=128), writes=["xt%d" % (ti % 2)])
                    norm_tile(NS, [b[:, k, :n] for k in range(8)], ["xt%d" % (ti % 2)] * 8, n, 0, False,
                              [hnT[:, k, t0:t0 + n] for k in range(8)], ["hn%d" % k for k in range(8)])
                S.barrier()

        def proj_fm(wh, qi, t0, n):
            pj, pk = ps_pj()
            for k in range(8):
                MM(pj[:, :n], wh[:, k, qi, :], hnT[:, k, t0:t0 + n], start=(k == 0), stop=(k == 7),
                   reads=["wh", "hn%d" % k], writes=[pk])
            return pj, pk

        def proj_v(wh, qi, vtok):
            for c0 in range(0, NCH, 4):
                pj, pk = ps_pj()
                ncs = min(4, NCH - c0)
                for ci in range(ncs):
                    c = c0 + ci
                    for k in range(8):
                        MM(pj[0:64, ci * 128:(ci + 1) * 128], hnT[:, k, c * 64:(c + 1) * 64], wh[:, k, qi, :],
                           start=(k == 0), stop=(k == 7), reads=["wh", "hn%d" % k], writes=[pk])
                ACT(vtok[:, c0:c0 + ncs, :], pj[0:64, 0:ncs * 128].rearrange("p (c e) -> p c e", e=128), AF.Identity,
                    [pk], ["vtok"])

        def k_transposes(kA, ktok, ksc):
            for c0 in range(0, NCH, 8):
                ncs = min(8, NCH - c0)
                for ci in range(ncs):
                    c = c0 + ci
                    TR(psb[0:64, ci * 128:(ci + 1) * 128], kA[:, c * 64:(c + 1) * 64], ident_b[:], ["kA", "ident_b"], ["psb"])
                TS("dve", ktok[:, c0:c0 + ncs, :], psb[0:64, 0:ncs * 128].rearrange("p (c e) -> p c e", e=128),
                   ksc, None, ALU.mult, reads=["psb", "kscale"], writes=["ktok"])

        def recurrence(h, HS, full, mask_ap):
            St, Sb = HS["S"], HS["Sb"]
            if full:
                COPY("pool", St[:], Sin[:, h, :], ["Sin"], ["S"])
            else:
                MEMSET("pool", St[:], 0.0, ["S"])
            nch = NCH if full else NCH - 1
            ob = None
            for c in range(nch):
                cs = slice(c * 64, (c + 1) * 64)
                if full:
                    rot["at"] ^= 1
                    pa, pak = ps[2 + rot["at"]], "ps%d" % (2 + rot["at"])
                    MM(pa[0:64, 0:64], HS["kA"][:, cs], HS["qA"][:, cs], reads=["kA", "qA"], writes=[pak])
                    atm = HS["atm"][rot["at"]]
                    TT("dve", atm[:], pa[0:64, 0:64], mask_ap, ALU.mult, [pak, "masks"], ["atm%d" % rot["at"]])
                    TS("pool", Sb[:], St[:], HS["tabM"][:, c:c + 1], None, ALU.mult, reads=["S", "tabM"], writes=["Sb"])
                    if c % 8 == 0:
                        rot["o"] ^= 1
                        ob, obk = ps[4 + rot["o"]], "ps%d" % (4 + rot["o"])
                    oc = (c % 8) * 64
                    MM(ob[:, oc:oc + 64], HS["vtok"][:, c, :], atm[:], start=True, stop=False,
                       reads=["vtok", "atm%d" % rot["at"]], writes=[obk])
                    MM(ob[:, oc:oc + 64], Sb[:], HS["qB"][:, cs], start=False, stop=True, reads=["Sb", "qB"], writes=[obk])
                    if c % 8 == 7 or c == nch - 1:
                        c0 = (c // 8) * 8
                        headnorm(h, HS, ob, obk, c0 * 64, (c + 1 - c0) * 64)
                if c < NCH - 1:
                    rot["ds"] = (rot["ds"] + 1) % 4
                    pd = ps[6][:, rot["ds"] * 128:(rot["ds"] + 1) * 128]
                    pdk = "ps6_%d" % rot["ds"]
                    MM(pd, HS["ktok"][:, c, :], HS["vtok"][:, c, :], reads=["ktok", "vtok"], writes=[pdk])
                    ACT(HS["dst"][:], pd, AF.Identity, [pdk, "tabC"], ["dst"], scale=HS["tabC"][:, c:c + 1])
                    STT(St[:], St[:], HS["tabA"][:, c:c + 1], HS["dst"][:], ALU.mult, ALU.add, ["S", "tabA", "dst"], ["S"])
            if not full:
                COPY("pool", (W["L"][:, h, :] if W["L"] is not None else Lst[:, h, :]), St[:], ["S"], ["Lst"])

        def headnorm(h, HS, ob, obk, t0, n):
            ACT(HS["osq"][:, :n], ob[:, :n], AF.Square, [obk], ["osq"])
            pj, pk = ps_pj()
            MM(pj[:, :n], ones_b[:], HS["osq"][:, :n], reads=["osq", "ones_b"], writes=[pk])
            ACT(HS["ostd"][:, :n], pj[:, :n], AF.Sqrt, [pk, "eps_t"], ["ostd"], bias=eps_t[:, 0:1], scale=1.0 / 128.0)
            S.op("dve", lambda e: e.reciprocal(out=HS["ostd"][:, :n], in_=HS["ostd"][:, :n]), ["ostd"], ["ostd"])
            STT(HS["ot"][:, :n], ob[:, :n], small[:, 8 + h:9 + h], HS["ostd"][:, :n], ALU.mult, ALU.mult,
                [obk, "ostd", "small_g"], ["ot"])
            TT("pool", catT[:, h, t0:t0 + n], HS["ot"][:, :n], HS["gate"][:, t0:t0 + n], ALU.mult, ["ot", "gate"], ["cat%d" % h])

        def head_common(PH, full):
            HS = {"kA": sbt(PH, "kA", [128, T], BF16), "ktok": sbt(PH, "ktok", [64, NCH, 128], BF16),
                  "vtok": sbt(PH, "vtok", [64, NCH, 128], BF16), "S": sbt(PH, "S", [128, 128]),
                  "Sb": sbt(PH, "Sb", [128, 128], BF16), "dst": sbt(PH, "dst", [128, 128]),
                  "tabA": sbt(PH, "tabA", [128, NCH]), "tabC": sbt(PH, "tabC", [128, NCH]), "tabM": sbt(PH, "tabM", [128, NCH]),
                  "tmp": [sbt(PH, "tmp%d" % i, [128, 512]) for i in range(3)]}
            if full:
                HS.update({"qA": sbt(PH, "qA", [128, T], BF16), "gate": sbt(PH, "gate", [128, T], BF16),
                           "atm": [sbt(PH, "atm%d" % i, [64, 64], BF16) for i in range(2)],
                           "osq": sbt(PH, "osq", [128, 512], BF16), "ostd": sbt(PH, "ostd", [128, 512]),
                           "ot": sbt(PH, "ot", [128, 512])})
            return HS

        def hg_heads(full):
            with ExitStack() as PH:
                HS = head_common(PH, full)
                if full:
                    HS["qB"] = HS["qA"]
                wh = sbt(PH, "wh", [128, 8, 4, 128], BF16)
                ff = sbt(PH, "ff", [128, T])
                bA = sbt(PH, "bA", [128, T])
                bB = sbt(PH, "bB", [128, T])
                scanm = sbt(PH, "scanm", [128, T])
                masks = sbt(PH, "masks", [64, 64])
                ksc = sbt(PH, "ksc", [64, 8])
                tmpd = sbt(PH, "tmpd", [128, NCH])
                sumbl = sbt(PH, "sumbl", [128, 1])
                DMA("sp", scanm[:], scanm_d[:, :], writes=["scanm"])
                DMA("sp", masks[:], masks_d[:, 0:64], writes=["masks"])
                DMA("sp", ksc[:], kscale_d[:, :], writes=["kscale"])
                b3 = bB[:].rearrange("p (c s) -> p c s", s=64)
                a3 = bA[:].rearrange("p (c s) -> p c s", s=64)
                for h in range(4):
                    for qi in range(4):
                        DMA("pool", wh[:, :, qi, :], w_in[:, qi * 512 + h * 128: qi * 512 + (h + 1) * 128]
                            .rearrange("(k p) c -> p k c", p=128), writes=["wh"])
                    for ti, (t0, n) in enumerate(tiles(0, T)):
                        pj, pk = proj_fm(wh, 1, t0, n)
                        tm = HS["tmp"][ti % 2]
                        ACT(tm[:, :n], pj[:, :n], AF.Sigmoid, [pk], ["tmp%d" % (ti % 2)])
                        TS("dve", ff[:, t0:t0 + n], tm[:, :n], small[:, 4 + h:5 + h], small[:, h:h + 1], ALU.mult, ALU.add,
                           reads=["tmp%d" % (ti % 2), "small_lb", "small_oml"], writes=["ff"])
                    if "h1" in dbg:
                        return
                    ACT(bA[:], ff[:], AF.Ln, ["ff"], ["bA"])
                    if "h1b" in dbg:
                        return
                    S.op("dve", lambda e: e.tensor_tensor_scan(out=bB[:], data0=scanm[:], data1=bA[:], initial=0.0,
                                                               op0=ALU.mult, op1=ALU.add), ["scanm", "bA"], ["bB"])
                    if "h1c" in dbg:
                        return
                    ACT(HS["tabA"][:], b3[:, :, 63], AF.Exp, ["bB"], ["tabA"])
                    TT("dve", tmpd[:], b3[:, :, 63], b3[:, :, 31], ALU.subtract, ["bB"], ["tmpd"])
                    ACT(HS["tabC"][:], tmpd[:], AF.Exp, ["tmpd"], ["tabC"])
                    ACT(HS["tabM"][:], b3[:, :, 31], AF.Exp, ["bB"], ["tabM"])
                    S.op("dve", lambda e: e.reduce_sum(out=sumbl[:], in_=b3[:, 0:NCH - 1, 63], axis=AX.X), ["bB"], ["sumbl"])
                    ACT((W["D"] if W["D"] is not None else Dst)[:, h:h + 1], sumbl[:], AF.Exp, ["sumbl"], ["Dst"])
                    TT("dve", HS["tabA"][:, 0:1], HS["tabA"][:, 0:1], flagw[:, W["w"]:W["w"] + 1], ALU.mult, ["tabA", "flag"], ["tabA"])
                    TT("dve", HS["tabC"][:, 0:1], HS["tabC"][:, 0:1], flagw[:, W["w"]:W["w"] + 1], ALU.mult, ["tabC", "flag"], ["tabC"])
                    if "h1d" in dbg:
                        return
                    TT("dve", a3, b3, b3[:, :, 31:32].broadcast_to([128, NCH, 64]), ALU.subtract, ["bB", "bA"], ["bA"])
                    if "h1e" in dbg:
                        return
                    ACT(bB[:], bA[:], AF.Exp, ["bA"], ["bB"])
                    ACT(bA[:], bA[:], AF.Exp, ["bA"], ["bA"], scale=-1.0)
                    TS("pool", ff[:], ff[:], -1.0, 1.0, ALU.mult, ALU.add, reads=["ff"], writes=["ff"])
                    TT("dve", HS["kA"][:], ff[:], bA[:], ALU.mult, ["ff", "bA"], ["kA"])
                    if full:
                        for ti, (t0, n) in enumerate(tiles(0, T)):
                            pj, pk = proj_fm(wh, 0, t0, n)
                            tm = HS["tmp"][ti % 2]
                            ACT(tm[:, :n], pj[:, :n], AF.Silu, [pk], ["tmp%d" % (ti % 2)])
                            TT("dve", HS["qA"][:, t0:t0 + n], tm[:, :n], bB[:, t0:t0 + n], ALU.mult,
                               ["tmp%d" % (ti % 2), "bB"], ["qA"])
                            pj, pk = proj_fm(wh, 3, t0, n)
                            ACT(HS["gate"][:, t0:t0 + n], pj[:, :n], AF.Silu, [pk], ["gate"])
                    if "h2" in dbg:
                        return
                    proj_v(wh, 2, HS["vtok"])
                    if "h3" in dbg:
                        return
                    k_transposes(HS["kA"], HS["ktok"], ksc[:, h:h + 1])
                    if "h4" in dbg:
                        return
                    recurrence(h, HS, full, masks[:])
                    if "h5" in dbg:
                        return
                S.barrier()

        def ret_heads(full):
            with ExitStack() as PH:
                HS = head_common(PH, full)
                if full:
                    HS["qB"] = sbt(PH, "qB", [128, T], BF16)
                wh = sbt(PH, "wh6", [128, 8, 6, 128], BF16)
                cosT = sbt(PH, "cosT", [128, T])
                sinT = sbt(PH, "sinT", [128, T])
                gdec = sbt(PH, "gdec", [128, 4, 64])
                masks = sbt(PH, "dmasks", [64, 4, 64])
                ksc = sbt(PH, "ksc", [64, 8])
                DMA("sp", cosT[:], cos_d[W["w"], :, :], writes=["cosT"])
                DMA("sp", sinT[:], sin_d[W["w"], :, :], writes=["sinT"])
                DMA("sp", gdec[:], gdec_d[:, :].rearrange("p (h s) -> p h s", s=64), writes=["gdec"])
                DMA("sp", masks[:], masks_d[:, 64:320].rearrange("p (h s) -> p h s", s=64), writes=["masks"])
                DMA("sp", ksc[:], kscale_d[:, :], writes=["kscale"])
                for r in range(4):
                    h = 4 + r
                    srcs = [w_in[:, 2048 + r * 128:2048 + (r + 1) * 128], w_in_sw[:, r * 128:(r + 1) * 128],
                            w_in[:, 2560 + r * 128:2560 + (r + 1) * 128], w_in_sw[:, 512 + r * 128:512 + (r + 1) * 128],
                            w_in[:, 3072 + r * 128:3072 + (r + 1) * 128], w_in[:, 3584 + r * 128:3584 + (r + 1) * 128]]
                    for qi in range(6):
                        DMA("pool", wh[:, :, qi, :], srcs[qi].rearrange("(k p) c -> p k c", p=128), writes=["wh"])
                    MEMSET("pool", HS["tabA"][:], gam[r] ** 64, ["tabA"])
                    MEMSET("pool", HS["tabC"][:], 1.0, ["tabC"])
                    MEMSET("pool", HS["tabM"][:], 1.0, ["tabM"])
                    MEMSET("pool", (W["D"] if W["D"] is not None else Dst)[:, h:h + 1], gam[r] ** (64 * (NCH - 1)), ["Dst"])
                    TT("dve", HS["tabA"][:, 0:1], HS["tabA"][:, 0:1], flagw[:, W["w"]:W["w"] + 1], ALU.mult, ["tabA", "flag"], ["tabA"])
                    TT("dve", HS["tabC"][:, 0:1], HS["tabC"][:, 0:1], flagw[:, W["w"]:W["w"] + 1], ALU.mult, ["tabC", "flag"], ["tabC"])
                    for which in ((0, 1) if full else (1,)):
                        for ti, (t0, n) in enumerate(tiles(0, T)):
                            pa, pak = proj_fm(wh, 2 * which, t0, n)
                            t1, t2 = HS["tmp"][0], HS["tmp"][1]
                            TT("dve", t1[:, :n], pa[:, :n], cosT[:, t0:t0 + n], ALU.mult, [pak, "cosT"], ["tmp0"])
                            pb, pbk = proj_fm(wh, 2 * which + 1, t0, n)
                            TT("dve", t2[:, :n], pb[:, :n], sinT[:, t0:t0 + n], ALU.mult, [pbk, "sinT"], ["tmp1"])
                            TT("pool", t1[:, :n], t1[:, :n], t2[:, :n], ALU.add, ["tmp0", "tmp1"], ["tmp0"])
                            if which == 0:
                                ACT(HS["qA"][:, t0:t0 + n], t1[:, :n], AF.Identity, ["tmp0"], ["qA"])
                                TT("dve", HS["qB"][:, t0:t0 + n].rearrange("p (c s) -> p c s", s=64),
                                   t1[:, :n].rearrange("p (c s) -> p c s", s=64),
                                   gdec[:, r, :].unsqueeze(1).broadcast_to([128, n // 64, 64]), ALU.mult,
                                   ["tmp0", "gdec"], ["qB"])
                            else:
                                ACT(HS["kA"][:, t0:t0 + n], t1[:, :n], AF.Identity, ["tmp0"], ["kA"], scale=128.0 ** -0.5)
                    if full:
                        for ti, (t0, n) in enumerate(tiles(0, T)):
                            pj, pk = proj_fm(wh, 5, t0, n)
                            ACT(HS["gate"][:, t0:t0 + n], pj[:, :n], AF.Silu, [pk], ["gate"])
                    proj_v(wh, 4, HS["vtok"])
                    k_transposes(HS["kA"], HS["ktok"], ksc[:, h:h + 1])
                    recurrence(h, HS, full, masks[:, r, :])
                S.barrier()

        def chain_states(Lall_sb, Dall_sb):
            with ExitStack() as PC:
                tmp = sbt(PC, "ctmp", [128, 128])
                MEMSET("pool", Sin[:], 0.0, ["Sin"])
                for i in (range(ncores - 2, -1, -1) if mode == "F2" else range(ncores - 1)):
                    for h in range(8):
                        STT(tmp[:], Sin[:, h, :], Dall_sb[:, i, h:h + 1], Lall_sb[:, i, h, :], ALU.mult, ALU.add,
                            ["Sin", "Lall", "Dall"], ["ctmp"])
                        TT("dve", tmp[:], tmp[:], Sin[:, h, :], ALU.subtract, ["ctmp", "Sin"], ["ctmp"])
                        STT(Sin[:, h, :], tmp[:], seli[:, i:i + 1], Sin[:, h, :], ALU.mult, ALU.add,
                            ["ctmp", "seli", "Sin"], ["Sin"])
                S.barrier()

        def wout_residual():
            with ExitStack() as PW:
                wo = sbt(PW, "wo", [128, 8, 1024], BF16)
                xhi = sbt(PW, "xhi", [128, 4, T])
                xin = [sbt(PW, "xin%d" % i, [128, 512]) for i in range(2)]
                DMA("pool", wo[:], w_out[:, :].rearrange("(k p) c -> p k c", p=128), writes=["wo"])
                i = 0
                for dm in range(8):
                    for (t0, n) in tiles(0, T):
                        pj, pk = ps_pj()
                        for h in range(8):
                            MM(pj[:, :n], wo[:, h, dm * 128:(dm + 1) * 128], catT[:, h, t0:t0 + n], start=(h == 0), stop=(h == 7),
                               reads=["wo", "cat%d" % h], writes=[pk])
                        i ^= 1
                        DMA("sp", xin[i][:, :n], xTw[0, dm * 128:(dm + 1) * 128, t0:t0 + n], writes=["xin%d" % i])
                        dest = big[:, dm, t0:t0 + n] if dm < 4 else xhi[:, dm - 4, t0:t0 + n]
                        STT(dest, pj[:, :n], mod[:, 16 + dm:17 + dm], xin[i][:, :n], ALU.mult, ALU.add,
                            [pk, "mod", "xin%d" % i], ["xdest%d" % dm])
                S.barrier()
                for dm in range(4, 8):
                    COPY("pool" if dm % 2 else "act", big[:, dm, :], xhi[:, dm - 4, :], ["xdest%d" % dm], ["x%d" % dm])
                S.barrier()

        def moe(l, lo, hi):
            halves = [(lo, (lo + hi) // 2), ((lo + hi) // 2, hi)]
            for (h0, h1) in halves:
                NH = h1 - h0
                with ExitStack() as PM:
                    hnh = sbt(PM, "hnh", [128, 8, NH], BF16)
                    gatesT = sbt(PM, "gatesT", [32, NH])
                    with ExitStack() as PN:
                        NS = {"sq": sbt(PN, "sq", [128, 8, 512], BF16), "stdt": sbt(PN, "stdt", [128, 512]),
                              "t1": sbt(PN, "t1", [128, 8, 512])}
                        hn32 = sbt(PN, "hn32", [128, 8, 512])
                        wr = sbt(PN, "wr", [128, 8, 32])
                        br = sbt(PN, "br", [1, 32])
                        lg = sbt(PN, "lg", [128, 32])
                        mx8 = sbt(PN, "mx8", [128, 8])
                        msk = sbt(PN, "msk", [128, 32])
                        ex = sbt(PN, "ex", [128, 32])
                        den = sbt(PN, "den", [128, 2])
                        DMA("sp", wr[:], w_router[l].rearrange("(k p) e -> p k e", p=128), writes=["wr"])
                        DMA("sp", br[:], b_router[l:l + 1, :], writes=["br"])
                        for (t0, n) in tiles(h0, h1):
                            norm_tile(NS, [big[:, k, t0:t0 + n] for k in range(8)], ["x%d" % k for k in range(8)], n, l, True,
                                      [hnh[:, k, t0 - h0:t0 - h0 + n] for k in range(8)], ["hnh%d" % k for k in range(8)],
                                      [hn32[:, k, :n] for k in range(8)], ["hn32_%d" % k for k in range(8)])
                            for s0 in range(0, n, 128):
                                m = min(128, n - s0)
                                pl = ps[6]
                                for k in range(8):
                                    MM(pl[:m, 0:32], hn32[:, k, s0:s0 + m], wr[:, k, :], start=(k == 0), stop=False,
                                       reads=["hn32_%d" % k, "wr"], writes=["ps6"])
                                MM(pl[:m, 0:32], ones_f[0:1, 0:m], br[0:1, :], start=False, stop=True, reads=["ones_f", "br"], writes=["ps6"])
                                COPY("dve", lg[:m, :], pl[:m, 0:32], ["ps6"], ["lg"])
                                S.op("dve", lambda e, m=m: e.max(out=mx8[:m, :], in_=lg[:m, :]), ["lg"], ["mx8"])
                                TS("dve", msk[:m, :], lg[:m, :], mx8[:m, 3:4], None, ALU.is_ge, reads=["lg", "mx8"], writes=["msk"])
                                TS("dve", den[:m, 0:1], mx8[:m, 0:1], -1.0, None, ALU.mult, reads=["mx8"], writes=["den0"])
                                ACT(ex[:m, :], lg[:m, :], AF.Exp, ["lg", "den0"], ["ex"], bias=den[:m, 0:1])
                                TT("dve", ex[:m, :], ex[:m, :], msk[:m, :], ALU.mult, ["ex", "msk"], ["ex"])
                                S.op("dve", lambda e, m=m: e.reduce_sum(out=den[:m, 1:2], in_=ex[:m, :], axis=AX.X), ["ex"], ["den1"])
                                S.op("dve", lambda e, m=m: e.reciprocal(out=den[:m, 1:2], in_=den[:m, 1:2]), ["den1"], ["den1"])
                                TS("dve", ex[:m, :], ex[:m, :], den[:m, 1:2], None, ALU.mult, reads=["ex", "den1"], writes=["ex"])
                                TR(ps[6][0:32, 128:128 + m], ex[:m, :], ident_f[:m, :m], ["ex", "ident_f"], ["ps6"])
                                c0 = t0 - h0 + s0
                                COPY("act", gatesT[:, c0:c0 + m], ps[6][0:32, 128:128 + m], ["ps6"], ["gatesT"])
                        S.barrier()
                    if "gatesT" in dbg and h0 == lo and l == 0:
                        dump("gatesT", gatesT[:], [32, NH], "gatesT")
                    with ExitStack() as PE_:
                        act = sbt(PE_, "act", [128, 8, NH], BF16)
                        gbc = sbt(PE_, "gbc", [128, NH])
                        wg = [sbt(PE_, "wg%d" % i, [128, 8, 2, 128], BF16) for i in range(4)]
                        wd = [sbt(PE_, "wd%d" % i, [128, 8, 1024], BF16) for i in range(2)]
                        tt = [[sbt(PE_, "tt%d_%d" % (i, j), [128, 512]) for j in range(3)] for i in range(2)]
                        ytmp = [sbt(PE_, "ytmp%d" % i, [128, 512]) for i in range(2)]
                        bgu = sbt(PE_, "bgu", [128, 32 * 16])
                        bdn = sbt(PE_, "bdn", [32, 1024])
                        DMA("sp", bgu[:], b_guT_d[:, l * 512:(l + 1) * 512], writes=["bgu"])
                        DMA("sp", bdn[:], b_down[l], writes=["bdn"])
                        cnt = {"g": 0, "t": 0, "y": 0, "d": 0}

                        def down_evac(py, pyk, dm, t0, n):
                            cnt["y"] ^= 1
                            yt, ytk = ytmp[cnt["y"]], "ytmp%d" % cnt["y"]
                            ACT(yt[:, :n], py[:, :n], AF.Identity, [pyk, "mod"], [ytk], scale=mod[:, l * 48 + 40 + dm:l * 48 + 41 + dm])
                            TT("pool", big[:, dm, t0:t0 + n], big[:, dm, t0:t0 + n], yt[:, :n], ALU.add, ["x%d" % dm, ytk], ["x%d" % dm])

                        for dm in range(8):
                            for (t0, n) in tiles(h0, h1):
                                cnt["d"] ^= 1
                                py, pyk = ps[4 + cnt["d"]], "ps%d" % (4 + cnt["d"])
                                MM(py[:, :n], bdn[0:32, dm * 128:(dm + 1) * 128], gatesT[0:32, t0 - h0:t0 - h0 + n],
                                   reads=["bdn", "gatesT"], writes=[pyk])
                                down_evac(py, pyk, dm, t0, n)
                        for e in range(32):
                            for (t0, n) in tiles(0, NH):
                                MM(ps[6][:, :n], ident_f[0:32, e:e + 1].broadcast_to([32, 128]), gatesT[0:32, t0:t0 + n],
                                   reads=["ident_f", "gatesT"], writes=["ps6"])
                                COPY("act", gbc[:, t0:t0 + n], ps[6][:, :n], ["ps6"], ["gbc"])
                            wdi = e % 2
                            DMA("pool", wd[wdi][:], w_down[l, e].rearrange("(k p) c -> p k c", p=128), writes=["wd%d" % wdi])
                            for fc in range(8):
                                cnt["g"] = (cnt["g"] + 1) % 4
                                wgi, wgk = wg[cnt["g"]], "wg%d" % cnt["g"]
                                for two in range(2):
                                    DMA("pool", wgi[:, :, two, :],
                                        w_gu[l, e, :, two * 1024 + fc * 128: two * 1024 + (fc + 1) * 128].rearrange("(k p) c -> p k c", p=128),
                                        writes=[wgk])
                                bcol = e * 16 + fc
                                for (t0, n) in tiles(0, NH):
                                    pg, pgk = ps[0], "ps0"
                                    plin, plk = ps[1], "ps1"
                                    if (t0 // 512) % 2:
                                        pg, pgk, plin, plk = ps[2], "ps2", ps[3], "ps3"
                                    for k in range(8):
                                        MM(pg[:, :n], wgi[:, k, 0, :], hnh[:, k, t0:t0 + n], start=(k == 0), stop=(k == 7),
                                           reads=[wgk, "hnh%d" % k], writes=[pgk])
                                    for k in range(8):
                                        MM(plin[:, :n], wgi[:, k, 1, :], hnh[:, k, t0:t0 + n], start=(k == 0), stop=(k == 7),
                                           reads=[wgk, "hnh%d" % k], writes=[plk])
                                    cnt["t"] ^= 1
                                    t1, t2, t3 = tt[cnt["t"]]
                                    k1, k2, k3 = ["tt%d_%d" % (cnt["t"], j) for j in range(3)]
                                    TS("dve", t1[:, :n], pg[:, :n], bgu[:, bcol:bcol + 1], 7.0, ALU.add, ALU.min, reads=[pgk, "bgu"], writes=[k1])
                                    ACT(t2[:, :n], t1[:, :n], AF.Sigmoid, [k1], [k2], scale=1.702)
                                    TS("dve", t3[:, :n], plin[:, :n], bgu[:, bcol + 8:bcol + 9], 7.0, ALU.add, ALU.min, reads=[plk, "bgu"], writes=[k3])
                                    TS("pool", t3[:, :n], t3[:, :n], -7.0, 1.0, ALU.max, ALU.add, reads=[k3], writes=[k3])
                                    TT("pool", t1[:, :n], t1[:, :n], t2[:, :n], ALU.mult, [k1, k2], [k1])
                                    TT("dve", t1[:, :n], t1[:, :n], t3[:, :n], ALU.mult, [k1, k3], [k1])
                                    TT("pool", act[:, fc, t0:t0 + n], t1[:, :n], gbc[:, t0:t0 + n], ALU.mult, [k1, "gbc"], ["act%d" % fc])
                            for dm in range(8):
                                for (t0, n) in tiles(0, NH):
                                    cnt["d"] ^= 1
                                    py, pyk = ps[4 + cnt["d"]], "ps%d" % (4 + cnt["d"])
                                    for fc in range(8):
                                        MM(py[:, :n], wd[wdi][:, fc, dm * 128:(dm + 1) * 128], act[:, fc, t0:t0 + n],
                                           start=(fc == 0), stop=(fc == 7), reads=["wd%d" % wdi, "act%d" % fc], writes=[pyk])
                                    down_evac(py, pyk, dm, h0 + t0, n)
                        S.barrier()

        def pool_mixer():
            l = 1
            with ExitStack() as PP:
                NS = {"sq": sbt(PP, "sq", [128, 8, 512], BF16), "stdt": sbt(PP, "stdt", [128, 512])}
                rstd = sbt(PP, "rstd", [128, T])
                hb = sbt(PP, "hb", [128, T])
                s2 = sbt(PP, "s2", [128, T])
                s4 = sbt(PP, "s4", [128, T])
                pT = sbt(PP, "pT", [128, 8, T], BF16)
                pw = sbt(PP, "pw", [128, 4, 2, 256], BF16)
                invc = sbt(PP, "invc", [128, 4, 16])
                ptmp = [sbt(PP, "ptmp%d" % i, [128, 512]) for i in range(2)]
                DMA("sp", invc[:], invc_d[:, :].rearrange("p (g s) -> p g s", s=16), writes=["invc"])
                for g in range(4):
                    DMA("pool", pw[:, g, :, :], pool_w[g].rearrange("(i p) d -> p i d", p=128), writes=["pw"])
                for (t0, n) in tiles(0, T):
                    pj, pk = ps_pj()
                    for k in range(8):
                        ACT(NS["sq"][:, k, :n], big[:, k, t0:t0 + n], AF.Square, ["x%d" % k], ["sq%d" % k])
                    for k in range(8):
                        MM(pj[:, :n], ones_b[:], NS["sq"][:, k, :n], start=(k == 0), stop=(k == 7), reads=["sq%d" % k, "ones_b"], writes=[pk])
                    ACT(rstd[:, t0:t0 + n], pj[:, :n], AF.Sqrt, [pk, "eps_t"], ["rstd"], bias=eps_t[:, 0:1], scale=1.0 / 1024.0)
                S.op("dve", lambda e: e.reciprocal(out=rstd[:], in_=rstd[:]), ["rstd"], ["rstd"])
                for k in range(8):
                    g = k // 2
                    w = (2, 4, 8, 16)[g]
                    STT(hb[:], big[:, k, :], mod1[:, 48 + 8 + k:48 + 9 + k], rstd[:], ALU.mult, ALU.mult, ["x%d" % k, "rstd", "mod1"], ["hb"])
                    TS("dve", hb[:], hb[:], mod[:, 48 + k:48 + k + 1], None, ALU.add, reads=["hb", "mod"], writes=["hb"])
                    TS("dve", hb[:, 0:64], hb[:, 0:64], flagw[:, 0:1], None, ALU.mult, reads=["hb", "flag"], writes=["hb"])
                    TT("pool", s2[:, 1:T], hb[:, 1:T], hb[:, 0:T - 1], ALU.add, ["hb"], ["s2"])
                    ws, wsk = s2, "s2"
                    if w >= 4:
                        TT("pool", s4[:, 3:T], s2[:, 3:T], s2[:, 1:T - 2], ALU.add, ["s2"], ["s4"])
                        ws, wsk = s4, "s4"
                    if w >= 8:
                        TT("pool", s2[:, 7:T], s4[:, 7:T], s4[:, 3:T - 4], ALU.add, ["s4", "s2"], ["s2"])
                        ws, wsk = s2, "s2"
                    if w >= 16:
                        TT("pool", s4[:, 15:T], s2[:, 15:T], s2[:, 7:T - 8], ALU.add, ["s2", "s4"], ["s4"])
                        ws, wsk = s4, "s4"
                    STT(pT[:, k, 16:T], ws[:, 16:T], 1.0 / w, hb[:, 16:T], ALU.mult, ALU.subtract, [wsk, "hb"], ["pT%d" % k])
                    TT("dve", ws[:, 64:80], ws[:, 64:80], invc[:, g, :], ALU.mult, [wsk, "invc", "pT%d" % k], [wsk])
                    TT("dve", pT[:, k, 64:80], ws[:, 64:80], hb[:, 64:80], ALU.subtract, [wsk, "hb"], ["pT%d" % k])
                i = 0
                for k in range(8):
                    g, j = k // 2, k % 2
                    for (t0, n) in tiles(HALO, T):
                        pj, pk = ps_pj()
                        for ii in range(2):
                            MM(pj[:, :n], pw[:, g, ii, j * 128:(j + 1) * 128], pT[:, 2 * g + ii, t0:t0 + n], start=(ii == 0), stop=(ii == 1),
                               reads=["pw", "pT%d" % (2 * g + ii)], writes=[pk])
                        i ^= 1
                        TS("dve", ptmp[i][:, :n], pj[:, :n], small[:, 24 + k:25 + k], small[:, 16 + k:17 + k], ALU.add, ALU.mult,
                           reads=[pk, "small_pb", "small_pc"], writes=["ptmp%d" % i])
                        TT("pool", big[:, k, t0:t0 + n], big[:, k, t0:t0 + n], ptmp[i][:, :n], ALU.add, ["x%d" % k, "ptmp%d" % i], ["x%d" % k])
                S.barrier()

        def final_norm():
            with ExitStack() as PF:
                NS = {"sq": sbt(PF, "sq", [128, 8, 512], BF16), "stdt": sbt(PF, "stdt", [128, 512])}
                ob = [sbt(PF, "fo%d" % i, [128, 8, 512]) for i in range(2)]
                for ti, (t0, n) in enumerate(tiles(HALO, T)):
                    o = ob[ti % 2]
                    norm_tile(NS, [big[:, k, t0:t0 + n] for k in range(8)], ["x%d" % k for k in range(8)], n, 0, False,
                              None, None, [o[:, k, :n] for k in range(8)], ["fo%d_%d" % (ti % 2, k) for k in range(8)], final=True)
                    DMA("sp", yT[:, t0 - HALO:t0 - HALO + n].rearrange("(k p) t -> p k t", p=128), o[:, :, :n],
                        reads=["fo%d_%d" % (ti % 2, k) for k in range(8)], writes=["yT%d" % ti], is_out=True)

        if "s0" not in dbg and mode != "F2":
            mixer_norm()
        if mode in ("A", "F"):
            if "s0" not in dbg and "s1" not in dbg:
                try:
                    hg_heads(False)
                    if "s2" not in dbg:
                        ret_heads(False)
                except Stop:
                    pass
        if mode == "F2":
            with ExitStack() as PX:
                Lall_sb = sbt(PX, "Lall_sb", [128, ncores - 1, 8, 128])
                Dall_sb = sbt(PX, "Dall_sb", [128, ncores - 1, 8])
                for w in range(1, ncores):
                    W["w"], W["L"], W["D"] = w, Lall_sb[:, w - 1, :, :], Dall_sb[:, w - 1, :]
                    mixer_norm()
                    hg_heads(False)
                    ret_heads(False)
                W["w"], W["L"], W["D"] = 0, None, None
                chain_states(Lall_sb, Dall_sb)
            mixer_norm()
        if mode == "A":
            DMA("sp", Lst_o[:, :], Lst[:].rearrange("p h e -> p (h e)"), reads=["Lst"], writes=["Lst_o"], is_out=True)
            DMA("sp", Dst_o[:, :], Dst[:], reads=["Dst"], writes=["Dst_o"], is_out=True)
        else:
            for _once in ([1] if mode != "F2" else []):
              with ExitStack() as PX:
                Lall_sb = sbt(PX, "Lall_sb", [128, ncores, 8, 128])
                Dall_sb = sbt(PX, "Dall_sb", [128, ncores, 8])
                if mode == "B":
                    DMA("sp", Lall_sb[:], Lall_d.rearrange("c p (h e) -> p c h e", e=128), writes=["Lall"])
                    DMA("sp", Dall_sb[:], Dall_d.rearrange("c p h -> p c h"), writes=["Dall"])
                else:
                    bounce = nc.dram_tensor("st_bounce", [128, 1032], F32)
                    gath = nc.dram_tensor("st_gath", [ncores * 128, 1032], F32)
                    DMA("sp", bounce[:, 0:1024], Lst[:].rearrange("p h e -> p (h e)"), reads=["Lst"], writes=["bounce"])
                    DMA("sp", bounce[:, 1024:1032], Dst[:], reads=["Dst"], writes=["bounce"])
                    S.barrier()
                    S.op("pool", lambda e: e.collective_compute("AllGather", ALU.bypass, replica_groups=[list(range(ncores))],
                                                                ins=[bounce.ap().opt()], outs=[gath.ap().opt()]),
                         reads=["bounce"], writes=["gath"], dma_key="gath", dma_inc=1)
                    S.barrier()
                    gv = gath.ap().rearrange("(c p) w -> p c w", p=128)
                    DMA("sp", Lall_sb[:].rearrange("p c h e -> p c (h e)"), gv[:, :, 0:1024], reads=["gath"], writes=["Lall"])
                    DMA("sp", Dall_sb[:], gv[:, :, 1024:1032], reads=["gath"], writes=["Dall"])
                chain_states(Lall_sb, Dall_sb)
            dump("Sin", Sin[:].rearrange("p h e -> p (h e)"), [128, 1024], "Sin")
            hg_heads(True)
            ret_heads(True)
            if "cat" in dbg:
                cf = sbt(es, "catf", [128, 8, T])
                for h in range(8):
                    COPY("act", cf[:, h, :], catT[:, h, :], ["cat%d" % h], ["catf"])
                dump("cat", cf[:].rearrange("p h t -> p (h t)"), [128, 8 * T], "catf")
            wout_residual()
            dump("x1", big[:].rearrange("p k t -> p (k t)"), [128, 8 * T], "x0")
            if "stop1" not in dbg:
                moe(0, 0, T)
                dump("x2", big[:].rearrange("p k t -> p (k t)"), [128, 8 * T], "x0")
                pool_mixer()
                dump("x3", big[:].rearrange("p k t -> p (k t)"), [128, 8 * T], "x0")
                moe(1, HALO, T)
            final_norm()
        S.finish()
        S.emit()
    return nc, dbg_outs, in_names


def _consts(T, tok0):
    gam = [1.0 - 2.0 ** (-5 - h) for h in range(4)]
    idx = np.arange(64)
    rel = idx[None, :] - idx[:, None]
    masks = np.zeros((64, 5, 64), np.float64)
    masks[:, 0, :] = (rel >= 0)
    for h in range(4):
        masks[:, 1 + h, :] = np.where(rel >= 0, gam[h] ** np.maximum(rel, 0), 0.0)
    kscale = np.ones((64, 8), np.float64)
    for h in range(4):
        kscale[:, 4 + h] = gam[h] ** (63 - idx)
    gdec = np.zeros((128, 4, 64), np.float64)
    for h in range(4):
        gdec[:, h, :] = (gam[h] ** (idx + 1.0))[None, :]
    half = 64
    inv = (10000.0 ** (-np.arange(half, dtype=np.float32) / half)).astype(np.float32)
    pos = (tok0 + np.arange(T)).astype(np.float32)
    ang = (pos[None, :] * inv[:, None]).astype(np.float32)
    cos = np.cos(ang.astype(np.float64))
    sin = np.sin(ang.astype(np.float64))
    cosT = np.concatenate([cos, cos], 0)
    sinT = np.concatenate([-sin, sin], 0)
    scanm = np.ones((128, T), np.float32)
    scanm[:, ::64] = 0.0
    return dict(masks=masks.reshape(64, 320).astype(np.float32), kscale=kscale.astype(np.float32),
                gdec=gdec.reshape(128, 256).astype(np.float32), cosT=cosT.astype(np.float32),
                sinT=sinT.astype(np.float32), scanm=scanm, ident=np.eye(128, dtype=np.float32))


def _pk(v, ncol):
    return np.ascontiguousarray(np.asarray(v, np.float32).reshape(ncol, 128).T)


def make_inputs(inp, ncores, nw=8):
    x = np.asarray(inp["x"], np.float32)[0]
    Sq = x.shape[0]
    TP = Sq // ncores
    T = TP + HALO
    w_in = np.asarray(inp["w_in"], np.float32)[0]
    perm = np.concatenate([np.arange(64, 128), np.arange(0, 64)])
    cols = []
    for base in (2048, 2560):
        for r in range(4):
            cols.append(base + r * 128 + perm)
    w_in_sw = np.ascontiguousarray(w_in[:, np.concatenate(cols)])
    shared = dict(
        cT=_pk(np.asarray(inp["c"])[0], 8),
        w_ada=np.asarray(inp["w_ada"], np.float32),
        b_adaT=np.concatenate([_pk(np.asarray(inp["b_ada"])[l], 48) for l in range(2)], 1),
        w_in=w_in, w_in_sw=w_in_sw,
        lbraw=np.concatenate([_pk(np.asarray(inp["hg_lower_bounds"])[r], 4) for r in range(2)], 1),
        gains=np.concatenate([_pk(np.asarray(inp["hg_norm"])[0].reshape(-1), 4),
                              _pk(np.asarray(inp["ret_norm"])[0].reshape(-1), 4)], 1),
        w_out=np.asarray(inp["w_out"], np.float32)[0],
        pool_w=np.asarray(inp["pool_w"], np.float32)[0],
        pool_bT=_pk(np.asarray(inp["pool_b"])[0].reshape(-1), 8),
        pool_sT=_pk(np.asarray(inp["pool_scale"])[0], 8),
        w_router=np.asarray(inp["w_router"], np.float32),
        b_router=np.asarray(inp["b_router"], np.float32),
        w_gu=np.asarray(inp["w_gu"], np.float32),
        b_guT=_pk(np.asarray(inp["b_gu"]).reshape(-1), 2 * 32 * 16),
        w_down=np.asarray(inp["w_down"], np.float32),
        b_down=np.asarray(inp["b_down"], np.float32),
        fnT=_pk(np.asarray(inp["final_norm"]), 8),
    )
    win = {}
    for c in range(ncores):
        tok0 = c * TP - HALO
        xs = np.zeros((T, 1024), np.float32)
        lo = max(tok0, 0)
        xs[lo - tok0:, :] = x[lo:tok0 + T, :]
        cst = _consts(T, tok0)
        win[c] = (np.ascontiguousarray(xs.T), cst["cosT"], cst["sinT"])
    zero_w = (np.zeros((1024, T), np.float32), np.zeros((128, T), np.float32), np.zeros((128, T), np.float32))
    maps = []
    for j in range(ncores):
        m = dict(shared)
        cst = _consts(T, j * TP - HALO)
        for k in ("masks", "kscale", "gdec", "scanm", "ident"):
            m[k] = cst[k]
        ws = [win[j - w] if j - w >= 0 else zero_w for w in range(nw)]
        m["xT"] = np.stack([w_[0] for w_ in ws], 0)
        m["cosT"] = np.stack([w_[1] for w_ in ws], 0)
        m["sinT"] = np.stack([w_[2] for w_ in ws], 0)
        flag = np.ones((128, 8), np.float32)
        valid = np.zeros((128, 8), np.float32)
        for w in range(8):
            if j - w <= 0:
                flag[:, w] = 0.0
            if w >= 1 and j - w >= 0:
                valid[:, w - 1] = 1.0
        m["flag"] = flag
        m["valid"] = valid
        seli = np.zeros((128, 8), np.float32)
        seli[:, :j] = 1.0
        m["seli"] = seli
        invc = np.zeros((128, 4, 16), np.float32)
        for g, w in enumerate((2, 4, 8, 16)):
            tg = j * TP + np.arange(16)
            invc[:, g, :] = (1.0 / np.minimum(tg + 1, w))[None, :]
        m["invc"] = invc.reshape(128, 64)
        maps.append(m)
    return maps, TP


_CACHE = {}


def _get(TP, mode, ncores):
    key = (TP, mode, ncores)
    if key not in _CACHE:
        r = build(TP, mode, ncores)
        _CACHE[key] = (r[0], r[2])
    return _CACHE[key]


def kernel(**inp):
    ncores = 8
    maps, TP = make_inputs(inp, ncores, 8)
    ncF, names = _get(TP, "F2", ncores)
    r = run_bass_kernel_spmd(ncF, [{k: m[k] for k in names} for m in maps], core_ids=list(range(ncores)))
    y = np.concatenate([r.results[j]["yT"].T for j in range(ncores)], 0)
    return np.ascontiguousarray(y[None].astype(np.float32))
```

```python
from contextlib import ExitStack
import numpy as np
import concourse.bass as bass
import concourse.mybir as mybir
from concourse.bass_utils import run_bass_kernel_spmd

F32 = mybir.dt.float32
BF16 = mybir.dt.bfloat16
ALU = mybir.AluOpType
AF = mybir.ActivationFunctionType
AX = mybir.AxisListType
ENGS = ("pe", "act", "dve", "pool", "sp")
EPS = 1e-6
HALO = 64


class Sched:
    def __init__(self, nc, es):
        self.nc, self.es = nc, es
        self.items = {e: [] for e in ENGS}
        self.cnt = {e: 0 for e in ENGS}
        self.clock = {e: {} for e in ENGS}
        self.snap, self.key_w, self.key_r, self.dma_cnt, self.sems = {}, {}, {}, {}, {}
        self.out_events = []
        self.epoch = 0
        self.ek = {e: "E:" + e for e in ENGS}
        for e in ENGS:
            self._sem(self.ek[e])

    def _sem(self, k):
        if k not in self.sems:
            self.sems[k] = self.es.enter_context(self.nc.semaphore("s%d" % len(self.sems)))
        return self.sems[k]

    def _need(self, eng, ev, deps):
        if ev is None:
            return
        sk, val = ev
        if eng == "pe" and sk.startswith("E:pe"):
            return
        if self.clock[eng].get(sk, 0) >= val:
            return
        if deps.get(sk, 0) < val:
            deps[sk] = val

    def _apply(self, eng, deps):
        clk = self.clock[eng]
        for sk, val in deps.items():
            self.items[eng].append(("w", sk, val))
            for a, b in self.snap.get((sk, val), {}).items():
                if clk.get(a, 0) < b:
                    clk[a] = b
            if clk.get(sk, 0) < val:
                clk[sk] = val

    def op(self, eng, fn, reads=(), writes=(), dma_key=None, is_out=False, dma_inc=16):
        writes = [k.split("_")[0] if k.startswith("ps") else k for k in writes]
        writes += [k.split("_")[0] for k in reads if k.startswith("ps")]
        reads = [k for k in reads if not k.startswith("ps")]
        deps = {}
        for k in reads:
            self._need(eng, self.key_w.get(k), deps)
        for k in writes:
            self._need(eng, self.key_w.get(k), deps)
            for ev in self.key_r.get(k, ()):
                self._need(eng, ev, deps)
        self._apply(eng, deps)
        if dma_key is not None:
            sk = "D:" + str(dma_key)
            self._sem(sk)
            self.dma_cnt[sk] = self.dma_cnt.get(sk, 0) + 1
            ev = (sk, self._dval(sk, dma_inc))
            self.items[eng].append(("i", fn, sk, dma_inc))
            if is_out:
                self.out_events.append(ev)
        else:
            self.cnt[eng] += 1
            ev = (self.ek[eng], self.cnt[eng])
            self.items[eng].append(("i", fn, self.ek[eng], 1))
        self.snap[ev] = dict(self.clock[eng])
        for k in writes:
            self.key_w[k] = ev
            self.key_r[k] = []
        for k in reads:
            self.key_r.setdefault(k, []).append(ev)
        return ev

    def _dval(self, sk, inc):
        self.dma_val = getattr(self, "dma_val", {})
        self.dma_val[sk] = self.dma_val.get(sk, 0) + inc
        return self.dma_val[sk]

    def barrier(self):
        evs = [(self.ek[e], self.cnt[e]) for e in ENGS if self.cnt[e] > 0]
        evs += [(sk, v) for sk, v in getattr(self, "dma_val", {}).items()]
        for eng in ENGS:
            deps = {}
            for sk, val in evs:
                if self.clock[eng].get(sk, 0) < val:
                    deps[sk] = val
            self._apply(eng, deps)
        self.key_w, self.key_r = {}, {}
        self.epoch += 1
        for e in ENGS:
            if self.cnt[e] > 12000:
                self.ek[e] = "E:%s:%d" % (e, self.epoch)
                self._sem(self.ek[e])
                self.cnt[e] = 0

    def finish(self):
        deps = {}
        for ev in self.out_events:
            self._need("sp", ev, deps)
        self._apply("sp", deps)

    def emit(self):
        nc = self.nc
        with nc.Block() as block:
            def run(name):
                def body(engine):
                    for it in self.items[name]:
                        if it[0] == "w":
                            engine.wait_ge(self.sems[it[1]], it[2])
                        else:
                            it[1](engine).then_inc(self.sems[it[2]], it[3])
                return body
            block.tensor(run("pe"))
            block.scalar(run("act"))
            block.vector(run("dve"))
            block.gpsimd(run("pool"))
            block.sync(run("sp"))


class Stop(Exception):
    pass


def tiles(lo, hi, step=512):
    return [(t, min(step, hi - t)) for t in range(lo, hi, step)]


def build(TP, mode, ncores=8, dbg=()):
    T = TP + HALO
    NCH = T // 64
    nc = bass.Bass("TRN2", target_bir_lowering=False)

    in_names = []

    def din(name, shape):
        in_names.append(name)
        return nc.dram_tensor(name, list(shape), F32, kind="ExternalInput").ap()

    def dout(name, shape):
        return nc.dram_tensor(name, list(shape), F32, kind="ExternalOutput").ap()

    NW = 8 if mode == "F2" else 1
    xTw = din("xT", [NW, 1024, T])
    flag_d = din("flag", [128, 8])
    valid_d = din("valid", [128, 8])
    cT_d = din("cT", [128, 8])
    w_ada = din("w_ada", [2, 1024, 6144])
    badaT_d = din("b_adaT", [128, 96])
    w_in = din("w_in", [1024, 4096])
    w_in_sw = din("w_in_sw", [1024, 1024])
    lbraw_d = din("lbraw", [128, 8])
    gains_d = din("gains", [128, 8])
    w_out = din("w_out", [1024, 1024]) if mode != "A" else None
    pool_w = din("pool_w", [4, 256, 256]) if mode != "A" else None
    pool_bT_d = din("pool_bT", [128, 8])
    pool_sT_d = din("pool_sT", [128, 8])
    w_router = din("w_router", [2, 1024, 32]) if mode != "A" else None
    b_router = din("b_router", [2, 32]) if mode != "A" else None
    w_gu = din("w_gu", [2, 32, 1024, 2048]) if mode != "A" else None
    b_guT_d = din("b_guT", [128, 2 * 32 * 16]) if mode != "A" else None
    w_down = din("w_down", [2, 32, 1024, 1024]) if mode != "A" else None
    b_down = din("b_down", [2, 32, 1024]) if mode != "A" else None
    fnT_d = din("fnT", [128, 8])
    ident_d = din("ident", [128, 128])
    masks_d = din("masks", [64, 5 * 64])
    kscale_d = din("kscale", [64, 8])
    cos_d = din("cosT", [NW, 128, T])
    sin_d = din("sinT", [NW, 128, T])
    gdec_d = din("gdec", [128, 4 * 64])
    scanm_d = din("scanm", [128, T])
    invc_d = din("invc", [128, 4 * 16])
    seli_d = din("seli", [128, 8])
    if mode == "A":
        Lst_o = dout("Lst", [128, 8 * 128])
        Dst_o = dout("Dst", [128, 8])
    if mode == "B":
        Lall_d = din("Lall", [ncores, 128, 8 * 128])
        Dall_d = din("Dall", [ncores, 128, 8])
    if mode in ("B", "F", "F2"):
        yT = dout("yT", [1024, TP])
    dbg_outs = {}

    gam = [1.0 - 2.0 ** (-5 - h) for h in range(4)]

    with ExitStack() as es:
        S = Sched(nc, es)

        uniq = [0]

        def sbt(stack, name, shape, dt=F32):
            uniq[0] += 1
            return stack.enter_context(nc.sbuf_tensor("sb%d_%s" % (uniq[0], name), list(shape), dt))

        def DMA(eng, out, in_, reads=(), writes=(), key=None, is_out=False):
            S.op(eng, lambda e: e.dma_start(out=out, in_=in_), reads, writes, dma_key=key or writes[0], is_out=is_out)

        def MM(out, lhsT, rhs, start=True, stop=True, reads=(), writes=()):
            S.op("pe", lambda e: e.matmul(out, lhsT, rhs, start=start, stop=stop), reads, writes)

        def TR(out, in_, ident, reads=(), writes=()):
            S.op("pe", lambda e: e.transpose(out, in_, ident), reads, writes)

        def ACT(out, in_, func, reads=(), writes=(), bias=None, scale=None):
            kw = {}
            if bias is not None:
                kw["bias"] = bias
            if scale is not None:
                kw["scale"] = scale
            S.op("act", lambda e: e.activation(out=out, in_=in_, func=func, **kw), reads, writes)

        def TT(eng, out, in0, in1, op, reads=(), writes=()):
            S.op(eng, lambda e: e.tensor_tensor(out=out, in0=in0, in1=in1, op=op), reads, writes)

        def TS(eng, out, in0, s1, s2, op0, op1=None, reads=(), writes=()):
            if op1 is None:
                S.op(eng, lambda e: e.tensor_scalar(out=out, in0=in0, scalar1=s1, scalar2=None, op0=op0), reads, writes)
            else:
                S.op(eng, lambda e: e.tensor_scalar(out=out, in0=in0, scalar1=s1, scalar2=s2, op0=op0, op1=op1), reads, writes)

        def STT(out, in0, scalar, in1, op0, op1, reads=(), writes=()):
            S.op("dve", lambda e: e.scalar_tensor_tensor(out=out, in0=in0, scalar=scalar, in1=in1, op0=op0, op1=op1),
                 reads, writes)

        def MEMSET(eng, ap, val, writes=()):
            S.op(eng, lambda e: e.memset(ap, val), (), writes)

        def COPY(eng, out, in_, reads=(), writes=()):
            if eng == "act":
                ACT(out, in_, AF.Identity, reads, writes)
            else:
                S.op(eng, lambda e: e.tensor_copy(out=out, in_=in_), reads, writes)

        def dump(name, ap, shape, key):
            if name in dbg:
                d = dout("dbg_" + name, shape)
                dbg_outs[name] = d
                DMA("sp", d, ap, reads=[key], writes=["dbg_" + name], is_out=True)

        G = es
        big = sbt(G, "big", [128, 8, T])
        bigb = big[:].rearrange("p k t -> p (k t)").bitcast(BF16).rearrange("p (k t) -> p k t", k=16)
        hnT = bigb[:, 0:8, :]
        catT = bigb[:, 8:16, :]
        ps = [G.enter_context(nc.psum_tensor("ps%d" % i, [128, 512], F32)) for i in range(7)]
        psb = G.enter_context(nc.psum_tensor("psb", [128, 1024], BF16))
        ident_f = sbt(G, "ident_f", [128, 128])
        ident_b = sbt(G, "ident_b", [128, 128], BF16)
        ones_b = sbt(G, "ones_b", [128, 128], BF16)
        ones_f = sbt(G, "ones_f", [128, 128])
        eps_t = sbt(G, "eps_t", [128, 1])
        flagw = sbt(G, "flag_s", [128, 8])
        W = {"w": 0, "L": None, "D": None}
        mod = sbt(G, "mod", [128, 96])
        mod1 = sbt(G, "mod1", [128, 96])
        small = sbt(G, "small", [128, 64])
        Lst = sbt(G, "Lst_s", [128, 8, 128])
        Dst = sbt(G, "Dst_s", [128, 8])
        Sin = sbt(G, "Sin_s", [128, 8, 128])
        seli = sbt(G, "seli_s", [128, 8])

        DMA("sp", ident_f[:], ident_d[:, :], writes=["ident_f"])
        DMA("pool", ident_b[:], ident_d[:, :], writes=["ident_b"])
        DMA("sp", flagw[:], flag_d[:, :], writes=["flag"])
        DMA("sp", seli[:], (valid_d if mode == "F2" else seli_d)[:, :], writes=["seli"])
        MEMSET("pool", ones_b[:], 1.0, ["ones_b"])
        MEMSET("pool", ones_f[:], 1.0, ["ones_f"])
        MEMSET("pool", eps_t[:], EPS, ["eps_t"])
        DMA("sp", small[:, 40:48], lbraw_d[:, :], writes=["small_lbraw"])
        DMA("sp", small[:, 8:16], gains_d[:, :], writes=["small_g"])
        DMA("sp", small[:, 16:24], pool_sT_d[:, :], writes=["small_ps"])
        DMA("sp", small[:, 24:32], pool_bT_d[:, :], writes=["small_pb"])
        DMA("sp", small[:, 32:40], fnT_d[:, :], writes=["small_fn"])
        TT("dve", small[:, 48:52], small[:, 40:44], small[:, 44:48], ALU.subtract, ["small_lbraw"], ["small_t"])
        ACT(small[:, 0:4], small[:, 48:52], AF.Sigmoid, ["small_t"], ["small_lb"])
        TS("dve", small[:, 4:8], small[:, 0:4], -1.0, 1.0, ALU.mult, ALU.add, ["small_lb"], ["small_oml"])

        with ExitStack() as P0:
            cT = sbt(P0, "cT_s", [128, 8])
            cond = sbt(P0, "cond", [128, 8])
            badaT = sbt(P0, "badaT", [128, 96])
            wa = [sbt(P0, "wa%d" % i, [128, 8, 768]) for i in range(2)]
            DMA("sp", cT[:], cT_d[:, :], writes=["cT"])
            DMA("sp", badaT[:], badaT_d[:, :], writes=["badaT"])
            ACT(cond[:], cT[:], AF.Silu, ["cT"], ["cond"])
            for l in range(2):
                for blk in range(8):
                    i = (l * 8 + blk) % 2
                    DMA("sp", wa[i][:], w_ada[l, :, blk * 768:(blk + 1) * 768].rearrange("(k p) c -> p k c", p=128),
                        writes=["wa%d" % i])
                    for m in range(6):
                        col = l * 48 + blk * 6 + m
                        for k in range(8):
                            MM(ps[0][:, col:col + 1], wa[i][:, k, m * 128:(m + 1) * 128], cond[:, k:k + 1],
                               start=(k == 0), stop=(k == 7), reads=["wa%d" % i, "cond"], writes=["ps0"])
            TT("dve", mod[:], ps[0][:, 0:96], badaT[:], ALU.add, ["ps0", "badaT"], ["mod"])
            TS("dve", mod1[:], mod[:], 1.0, None, ALU.add, reads=["mod"], writes=["mod1"])
            TT("dve", small[:, 16:24], small[:, 16:24], mod[:, 48 + 16:48 + 24], ALU.mult, ["small_ps", "mod"], ["small_pc"])
            S.barrier()
        dump("mod", mod[:], [128, 96], "mod")

        rot = {"pj": 0, "at": 0, "o": 0, "ds": 0}

        def ps_pj():
            rot["pj"] ^= 1
            return ps[rot["pj"]], "ps%d" % rot["pj"]

        def norm_tile(NS, xk, xkeys, n, l, ffn, out_bf, out_bf_keys, out_f32=None, out_f32_keys=None, final=False):
            base = l * 48 + (24 if ffn else 0)
            pj, pk = ps_pj()
            for k in range(8):
                ACT(NS["sq"][:, k, :n], xk[k], AF.Square, [xkeys[k]], ["sq%d" % k])
            for k in range(8):
                MM(pj[:, :n], ones_b[:], NS["sq"][:, k, :n], start=(k == 0), stop=(k == 7),
                   reads=["sq%d" % k, "ones_b"], writes=[pk])
            ACT(NS["stdt"][:, :n], pj[:, :n], AF.Sqrt, [pk, "eps_t"], ["stdt"], bias=eps_t[:, 0:1], scale=1.0 / 1024.0)
            S.op("dve", lambda e: e.reciprocal(out=NS["stdt"][:, :n], in_=NS["stdt"][:, :n]), ["stdt"], ["stdt"])
            for k in range(8):
                if final:
                    STT(out_f32[k], xk[k], small[:, 32 + k:33 + k], NS["stdt"][:, :n], ALU.mult, ALU.mult,
                        [xkeys[k], "stdt", "small_fn"], [out_f32_keys[k]])
                    continue
                STT(NS["t1"][:, k, :n], xk[k], mod1[:, base + 8 + k:base + 9 + k], NS["stdt"][:, :n], ALU.mult, ALU.mult,
                    [xkeys[k], "stdt", "mod1"], ["t1_%d" % k])
                if out_f32 is not None:
                    ACT(out_f32[k], NS["t1"][:, k, :n], AF.Identity, ["t1_%d" % k, "mod"], [out_f32_keys[k]],
                        bias=mod[:, base + k:base + k + 1])
                    COPY("pool", out_bf[k], out_f32[k], [out_f32_keys[k]], [out_bf_keys[k]])
                else:
                    ACT(out_bf[k], NS["t1"][:, k, :n], AF.Identity, ["t1_%d" % k, "mod"], [out_bf_keys[k]],
                        bias=mod[:, base + k:base + k + 1])

        def mixer_norm():
            with ExitStack() as PN:
                NS = {"sq": sbt(PN, "sq", [128, 8, 512], BF16), "stdt": sbt(PN, "stdt", [128, 512]),
                      "t1": sbt(PN, "t1", [128, 8, 512])}
                xt = [sbt(PN, "xt%d" % i, [128, 8, 512]) for i in range(2)]
                for ti, (t0, n) in enumerate(tiles(0, T)):
                    b = xt[ti % 2]
                    DMA("sp", b[:, :, :n], xTw[W["w"], :, t0:t0 + n].rearrange("(k p) t -> p k t", p=128), writes=["xt%d" % (ti % 2)])
                    norm_tile(NS, [b[:, k, :n] for k in range(8)], ["xt%d" % (ti % 2)] * 8, n, 0, False,
                              [hnT[:, k, t0:t0 + n] for k in range(8)], ["hn%d" % k for k in range(8)])
                S.barrier()

        def proj_fm(wh, qi, t0, n):
            pj, pk = ps_pj()
            for k in range(8):
                MM(pj[:, :n], wh[:, k, qi, :], hnT[:, k, t0:t0 + n], start=(k == 0), stop=(k == 7),
                   reads=["wh", "hn%d" % k], writes=[pk])
            return pj, pk

        def proj_v(wh, qi, vtok):
            for c0 in range(0, NCH, 4):
                pj, pk = ps_pj()
                ncs = min(4, NCH - c0)
                for ci in range(ncs):
                    c = c0 + ci
                    for k in range(8):
                        MM(pj[0:64, ci * 128:(ci + 1) * 128], hnT[:, k, c * 64:(c + 1) * 64], wh[:, k, qi, :],
                           start=(k == 0), stop=(k == 7), reads=["wh", "hn%d" % k], writes=[pk])
                ACT(vtok[:, c0:c0 + ncs, :], pj[0:64, 0:ncs * 128].rearrange("p (c e) -> p c e", e=128), AF.Identity,
                    [pk], ["vtok"])

        def k_transposes(kA, ktok, ksc):
            for c0 in range(0, NCH, 8):
                ncs = min(8, NCH - c0)
                for ci in range(ncs):
                    c = c0 + ci
                    TR(psb[0:64, ci * 128:(ci + 1) * 128], kA[:, c * 64:(c + 1) * 64], ident_b[:], ["kA", "ident_b"], ["psb"])
                TS("dve", ktok[:, c0:c0 + ncs, :], psb[0:64, 0:ncs * 128].rearrange("p (c e) -> p c e", e=128),
                   ksc, None, ALU.mult, reads=["psb", "kscale"], writes=["ktok"])

        def recurrence(h, HS, full, mask_ap):
            St, Sb = HS["S"], HS["Sb"]
            if full:
                COPY("pool", St[:], Sin[:, h, :], ["Sin"], ["S"])
            else:
                MEMSET("pool", St[:], 0.0, ["S"])
            nch = NCH if full else NCH - 1
            ob = None
            for c in range(nch):
                cs = slice(c * 64, (c + 1) * 64)
                if full:
                    rot["at"] ^= 1
                    pa, pak = ps[2 + rot["at"]], "ps%d" % (2 + rot["at"])
                    MM(pa[0:64, 0:64], HS["kA"][:, cs], HS["qA"][:, cs], reads=["kA", "qA"], writes=[pak])
                    atm = HS["atm"][rot["at"]]
                    TT("dve", atm[:], pa[0:64, 0:64], mask_ap, ALU.mult, [pak, "masks"], ["atm%d" % rot["at"]])
                    TS("pool", Sb[:], St[:], HS["tabM"][:, c:c + 1], None, ALU.mult, reads=["S", "tabM"], writes=["Sb"])
                    if c % 8 == 0:
                        rot["o"] ^= 1
                        ob, obk = ps[4 + rot["o"]], "ps%d" % (4 + rot["o"])
                    oc = (c % 8) * 64
                    MM(ob[:, oc:oc + 64], HS["vtok"][:, c, :], atm[:], start=True, stop=False,
                       reads=["vtok", "atm%d" % rot["at"]], writes=[obk])
                    MM(ob[:, oc:oc + 64], Sb[:], HS["qB"][:, cs], start=False, stop=True, reads=["Sb", "qB"], writes=[obk])
                    if c % 8 == 7 or c == nch - 1:
                        c0 = (c // 8) * 8
                        headnorm(h, HS, ob, obk, c0 * 64, (c + 1 - c0) * 64)
                if c < NCH - 1:
                    rot["ds"] = (rot["ds"] + 1) % 4
                    pd = ps[6][:, rot["ds"] * 128:(rot["ds"] + 1) * 128]
                    pdk = "ps6_%d" % rot["ds"]
                    MM(pd, HS["ktok"][:, c, :], HS["vtok"][:, c, :], reads=["ktok", "vtok"], writes=[pdk])
                    ACT(HS["dst"][:], pd, AF.Identity, [pdk, "tabC"], ["dst"], scale=HS["tabC"][:, c:c + 1])
                    STT(St[:], St[:], HS["tabA"][:, c:c + 1], HS["dst"][:], ALU.mult, ALU.add, ["S", "tabA", "dst"], ["S"])
            if not full:
                COPY("pool", (W["L"][:, h, :] if W["L"] is not None else Lst[:, h, :]), St[:], ["S"], ["Lst"])

        def headnorm(h, HS, ob, obk, t0, n):
            ACT(HS["osq"][:, :n], ob[:, :n], AF.Square, [obk], ["osq"])
            pj, pk = ps_pj()
            MM(pj[:, :n], ones_b[:], HS["osq"][:, :n], reads=["osq", "ones_b"], writes=[pk])
            ACT(HS["ostd"][:, :n], pj[:, :n], AF.Sqrt, [pk, "eps_t"], ["ostd"], bias=eps_t[:, 0:1], scale=1.0 / 128.0)
            S.op("dve", lambda e: e.reciprocal(out=HS["ostd"][:, :n], in_=HS["ostd"][:, :n]), ["ostd"], ["ostd"])
            STT(HS["ot"][:, :n], ob[:, :n], small[:, 8 + h:9 + h], HS["ostd"][:, :n], ALU.mult, ALU.mult,
                [obk, "ostd", "small_g"], ["ot"])
            TT("pool", catT[:, h, t0:t0 + n], HS["ot"][:, :n], HS["gate"][:, t0:t0 + n], ALU.mult, ["ot", "gate"], ["cat%d" % h])

        def head_common(PH, full):
            HS = {"kA": sbt(PH, "kA", [128, T], BF16), "ktok": sbt(PH, "ktok", [64, NCH, 128], BF16),
                  "vtok": sbt(PH, "vtok", [64, NCH, 128], BF16), "S": sbt(PH, "S", [128, 128]),
                  "Sb": sbt(PH, "Sb", [128, 128], BF16), "dst": sbt(PH, "dst", [128, 128]),
                  "tabA": sbt(PH, "tabA", [128, NCH]), "tabC": sbt(PH, "tabC", [128, NCH]), "tabM": sbt(PH, "tabM", [128, NCH]),
                  "tmp": [sbt(PH, "tmp%d" % i, [128, 512]) for i in range(3)]}
            if full:
                HS.update({"qA": sbt(PH, "qA", [128, T], BF16), "gate": sbt(PH, "gate", [128, T], BF16),
                           "atm": [sbt(PH, "atm%d" % i, [64, 64], BF16) for i in range(2)],
                           "osq": sbt(PH, "osq", [128, 512], BF16), "ostd": sbt(PH, "ostd", [128, 512]),
                           "ot": sbt(PH, "ot", [128, 512])})
            return HS

        def hg_heads(full):
            with ExitStack() as PH:
                HS = head_common(PH, full)
                if full:
                    HS["qB"] = HS["qA"]
                wh = sbt(PH, "wh", [128, 8, 4, 128], BF16)
                ff = sbt(PH, "ff", [128, T])
                bA = sbt(PH, "bA", [128, T])
                bB = sbt(PH, "bB", [128, T])
                scanm = sbt(PH, "scanm", [128, T])
                masks = sbt(PH, "masks", [64, 64])
                ksc = sbt(PH, "ksc", [64, 8])
                tmpd = sbt(PH, "tmpd", [128, NCH])
                sumbl = sbt(PH, "sumbl", [128, 1])
                DMA("sp", scanm[:], scanm_d[:, :], writes=["scanm"])
                DMA("sp", masks[:], masks_d[:, 0:64], writes=["masks"])
                DMA("sp", ksc[:], kscale_d[:, :], writes=["kscale"])
                b3 = bB[:].rearrange("p (c s) -> p c s", s=64)
                a3 = bA[:].rearrange("p (c s) -> p c s", s=64)
                for h in range(4):
                    for qi in range(4):
                        DMA("pool", wh[:, :, qi, :], w_in[:, qi * 512 + h * 128: qi * 512 + (h + 1) * 128]
                            .rearrange("(k p) c -> p k c", p=128), writes=["wh"])
                    for ti, (t0, n) in enumerate(tiles(0, T)):
                        pj, pk = proj_fm(wh, 1, t0, n)
                        tm = HS["tmp"][ti % 2]
                        ACT(tm[:, :n], pj[:, :n], AF.Sigmoid, [pk], ["tmp%d" % (ti % 2)])
                        TS("dve", ff[:, t0:t0 + n], tm[:, :n], small[:, 4 + h:5 + h], small[:, h:h + 1], ALU.mult, ALU.add,
                           reads=["tmp%d" % (ti % 2), "small_lb", "small_oml"], writes=["ff"])
                    if "h1" in dbg:
                        return
                    ACT(bA[:], ff[:], AF.Ln, ["ff"], ["bA"])
                    if "h1b" in dbg:
                        return
                    S.op("dve", lambda e: e.tensor_tensor_scan(out=bB[:], data0=scanm[:], data1=bA[:], initial=0.0,
                                                               op0=ALU.mult, op1=ALU.add), ["scanm", "bA"], ["bB"])
                    if "h1c" in dbg:
                        return
                    ACT(HS["tabA"][:], b3[:, :, 63], AF.Exp, ["bB"], ["tabA"])
                    TT("dve", tmpd[:], b3[:, :, 63], b3[:, :, 31], ALU.subtract, ["bB"], ["tmpd"])
                    ACT(HS["tabC"][:], tmpd[:], AF.Exp, ["tmpd"], ["tabC"])
                    ACT(HS["tabM"][:], b3[:, :, 31], AF.Exp, ["bB"], ["tabM"])
                    S.op("dve", lambda e: e.reduce_sum(out=sumbl[:], in_=b3[:, 0:NCH - 1, 63], axis=AX.X), ["bB"], ["sumbl"])
                    ACT((W["D"] if W["D"] is not None else Dst)[:, h:h + 1], sumbl[:], AF.Exp, ["sumbl"], ["Dst"])
                    TT("dve", HS["tabA"][:, 0:1], HS["tabA"][:, 0:1], flagw[:, W["w"]:W["w"] + 1], ALU.mult, ["tabA", "flag"], ["tabA"])
                    TT("dve", HS["tabC"][:, 0:1], HS["tabC"][:, 0:1], flagw[:, W["w"]:W["w"] + 1], ALU.mult, ["tabC", "flag"], ["tabC"])
                    if "h1d" in dbg:
                        return
                    TT("dve", a3, b3, b3[:, :, 31:32].broadcast_to([128, NCH, 64]), ALU.subtract, ["bB", "bA"], ["bA"])
                    if "h1e" in dbg:
                        return
                    ACT(bB[:], bA[:], AF.Exp, ["bA"], ["bB"])
                    ACT(bA[:], bA[:], AF.Exp, ["bA"], ["bA"], scale=-1.0)
                    TS("pool", ff[:], ff[:], -1.0, 1.0, ALU.mult, ALU.add, reads=["ff"], writes=["ff"])
                    TT("dve", HS["kA"][:], ff[:], bA[:], ALU.mult, ["ff", "bA"], ["kA"])
                    if full:
                        for ti, (t0, n) in enumerate(tiles(0, T)):
                            pj, pk = proj_fm(wh, 0, t0, n)
                            tm = HS["tmp"][ti % 2]
                            ACT(tm[:, :n], pj[:, :n], AF.Silu, [pk], ["tmp%d" % (ti % 2)])
                            TT("dve", HS["qA"][:, t0:t0 + n], tm[:, :n], bB[:, t0:t0 + n], ALU.mult,
                               ["tmp%d" % (ti % 2), "bB"], ["qA"])
                            pj, pk = proj_fm(wh, 3, t0, n)
                            ACT(HS["gate"][:, t0:t0 + n], pj[:, :n], AF.Silu, [pk], ["gate"])
                    if "h2" in dbg:
                        return
                    proj_v(wh, 2, HS["vtok"])
                    if "h3" in dbg:
                        return
                    k_transposes(HS["kA"], HS["ktok"], ksc[:, h:h + 1])
                    if "h4" in dbg:
                        return
                    recurrence(h, HS, full, masks[:])
                    if "h5" in dbg:
                        return
                S.barrier()

        def ret_heads(full):
            with ExitStack() as PH:
                HS = head_common(PH, full)
                if full:
                    HS["qB"] = sbt(PH, "qB", [128, T], BF16)
                wh = sbt(PH, "wh6", [128, 8, 6, 128], BF16)
                cosT = sbt(PH, "cosT", [128, T])
                sinT = sbt(PH, "sinT", [128, T])
                gdec = sbt(PH, "gdec", [128, 4, 64])
                masks = sbt(PH, "dmasks", [64, 4, 64])
                ksc = sbt(PH, "ksc", [64, 8])
                DMA("sp", cosT[:], cos_d[W["w"], :, :], writes=["cosT"])
                DMA("sp", sinT[:], sin_d[W["w"], :, :], writes=["sinT"])
                DMA("sp", gdec[:], gdec_d[:, :].rearrange("p (h s) -> p h s", s=64), writes=["gdec"])
                DMA("sp", masks[:], masks_d[:, 64:320].rearrange("p (h s) -> p h s", s=64), writes=["masks"])
                DMA("sp", ksc[:], kscale_d[:, :], writes=["kscale"])
                for r in range(4):
                    h = 4 + r
                    srcs = [w_in[:, 2048 + r * 128:2048 + (r + 1) * 128], w_in_sw[:, r * 128:(r + 1) * 128],
                            w_in[:, 2560 + r * 128:2560 + (r + 1) * 128], w_in_sw[:, 512 + r * 128:512 + (r + 1) * 128],
                            w_in[:, 3072 + r * 128:3072 + (r + 1) * 128], w_in[:, 3584 + r * 128:3584 + (r + 1) * 128]]
                    for qi in range(6):
                        DMA("pool", wh[:, :, qi, :], srcs[qi].rearrange("(k p) c -> p k c", p=128), writes=["wh"])
                    MEMSET("pool", HS["tabA"][:], gam[r] ** 64, ["tabA"])
                    MEMSET("pool", HS["tabC"][:], 1.0, ["tabC"])
                    MEMSET("pool", HS["tabM"][:], 1.0, ["tabM"])
                    MEMSET("pool", (W["D"] if W["D"] is not None else Dst)[:, h:h + 1], gam[r] ** (64 * (NCH - 1)), ["Dst"])
                    TT("dve", HS["tabA"][:, 0:1], HS["tabA"][:, 0:1], flagw[:, W["w"]:W["w"] + 1], ALU.mult, ["tabA", "flag"], ["tabA"])
                    TT("dve", HS["tabC"][:, 0:1], HS["tabC"][:, 0:1], flagw[:, W["w"]:W["w"] + 1], ALU.mult, ["tabC", "flag"], ["tabC"])
                    for which in ((0, 1) if full else (1,)):
                        for ti, (t0, n) in enumerate(tiles(0, T)):
                            pa, pak = proj_fm(wh, 2 * which, t0, n)
                            t1, t2 = HS["tmp"][0], HS["tmp"][1]
                            TT("dve", t1[:, :n], pa[:, :n], cosT[:, t0:t0 + n], ALU.mult, [pak, "cosT"], ["tmp0"])
                            pb, pbk = proj_fm(wh, 2 * which + 1, t0, n)
                            TT("dve", t2[:, :n], pb[:, :n], sinT[:, t0:t0 + n], ALU.mult, [pbk, "sinT"], ["tmp1"])
                            TT("pool", t1[:, :n], t1[:, :n], t2[:, :n], ALU.add, ["tmp0", "tmp1"], ["tmp0"])
                            if which == 0:
                                ACT(HS["qA"][:, t0:t0 + n], t1[:, :n], AF.Identity, ["tmp0"], ["qA"])
                                TT("dve", HS["qB"][:, t0:t0 + n].rearrange("p (c s) -> p c s", s=64),
                                   t1[:, :n].rearrange("p (c s) -> p c s", s=64),
                                   gdec[:, r, :].unsqueeze(1).broadcast_to([128, n // 64, 64]), ALU.mult,
                                   ["tmp0", "gdec"], ["qB"])
                            else:
                                ACT(HS["kA"][:, t0:t0 + n], t1[:, :n], AF.Identity, ["tmp0"], ["kA"], scale=128.0 ** -0.5)
                    if full:
                        for ti, (t0, n) in enumerate(tiles(0, T)):
                            pj, pk = proj_fm(wh, 5, t0, n)
                            ACT(HS["gate"][:, t0:t0 + n], pj[:, :n], AF.Silu, [pk], ["gate"])
                    proj_v(wh, 4, HS["vtok"])
                    k_transposes(HS["kA"], HS["ktok"], ksc[:, h:h + 1])
                    recurrence(h, HS, full, masks[:, r, :])
                S.barrier()

        def chain_states(Lall_sb, Dall_sb):
            with ExitStack() as PC:
                tmp = sbt(PC, "ctmp", [128, 128])
                MEMSET("pool", Sin[:], 0.0, ["Sin"])
                for i in (range(ncores - 2, -1, -1) if mode == "F2" else range(ncores - 1)):
                    for h in range(8):
                        STT(tmp[:], Sin[:, h, :], Dall_sb[:, i, h:h + 1], Lall_sb[:, i, h, :], ALU.mult, ALU.add,
                            ["Sin", "Lall", "Dall"], ["ctmp"])
                        TT("dve", tmp[:], tmp[:], Sin[:, h, :], ALU.subtract, ["ctmp", "Sin"], ["ctmp"])
                        STT(Sin[:, h, :], tmp[:], seli[:, i:i + 1], Sin[:, h, :], ALU.mult, ALU.add,
                            ["ctmp", "seli", "Sin"], ["Sin"])
                S.barrier()

        def wout_residual():
            with ExitStack() as PW:
                wo = sbt(PW, "wo", [128, 8, 1024], BF16)
                xhi = sbt(PW, "xhi", [128, 4, T])
                xin = [sbt(PW, "xin%d" % i, [128, 512]) for i in range(2)]
                DMA("pool", wo[:], w_out[:, :].rearrange("(k p) c -> p k c", p=128), writes=["wo"])
                i = 0
                for dm in range(8):
                    for (t0, n) in tiles(0, T):
                        pj, pk = ps_pj()
                        for h in range(8):
                            MM(pj[:, :n], wo[:, h, dm * 128:(dm + 1) * 128], catT[:, h, t0:t0 + n], start=(h == 0), stop=(h == 7),
                               reads=["wo", "cat%d" % h], writes=[pk])
                        i ^= 1
                        DMA("sp", xin[i][:, :n], xTw[0, dm * 128:(dm + 1) * 128, t0:t0 + n], writes=["xin%d" % i])
                        dest = big[:, dm, t0:t0 + n] if dm < 4 else xhi[:, dm - 4, t0:t0 + n]
                        STT(dest, pj[:, :n], mod[:, 16 + dm:17 + dm], xin[i][:, :n], ALU.mult, ALU.add,
                            [pk, "mod", "xin%d" % i], ["xdest%d" % dm])
                S.barrier()
                for dm in range(4, 8):
                    COPY("pool" if dm % 2 else "act", big[:, dm, :], xhi[:, dm - 4, :], ["xdest%d" % dm], ["x%d" % dm])
                S.barrier()

        def moe(l, lo, hi):
            halves = [(lo, (lo + hi) // 2), ((lo + hi) // 2, hi)]
            for (h0, h1) in halves:
                NH = h1 - h0
                with ExitStack() as PM:
                    hnh = sbt(PM, "hnh", [128, 8, NH], BF16)
                    gatesT = sbt(PM, "gatesT", [32, NH])
                    with ExitStack() as PN:
                        NS = {"sq": sbt(PN, "sq", [128, 8, 512], BF16), "stdt": sbt(PN, "stdt", [128, 512]),
                              "t1": sbt(PN, "t1", [128, 8, 512])}
                        hn32 = sbt(PN, "hn32", [128, 8, 512])
                        wr = sbt(PN, "wr", [128, 8, 32])
                        br = sbt(PN, "br", [1, 32])
                        lg = sbt(PN, "lg", [128, 32])
                        mx8 = sbt(PN, "mx8", [128, 8])
                        msk = sbt(PN, "msk", [128, 32])
                        ex = sbt(PN, "ex", [128, 32])
                        den = sbt(PN, "den", [128, 2])
                        DMA("sp", wr[:], w_router[l].rearrange("(k p) e -> p k e", p=128), writes=["wr"])
                        DMA("sp", br[:], b_router[l:l + 1, :], writes=["br"])
                        for (t0, n) in tiles(h0, h1):
                            norm_tile(NS, [big[:, k, t0:t0 + n] for k in range(8)], ["x%d" % k for k in range(8)], n, l, True,
                                      [hnh[:, k, t0 - h0:t0 - h0 + n] for k in range(8)], ["hnh%d" % k for k in range(8)],
                                      [hn32[:, k, :n] for k in range(8)], ["hn32_%d" % k for k in range(8)])
                            for s0 in range(0, n, 128):
                                m = min(128, n - s0)
                                pl = ps[6]
                                for k in range(8):
                                    MM(pl[:m, 0:32], hn32[:, k, s0:s0 + m], wr[:, k, :], start=(k == 0), stop=False,
                                       reads=["hn32_%d" % k, "wr"], writes=["ps6"])
                                MM(pl[:m, 0:32], ones_f[0:1, 0:m], br[0:1, :], start=False, stop=True, reads=["ones_f", "br"], writes=["ps6"])
                                COPY("dve", lg[:m, :], pl[:m, 0:32], ["ps6"], ["lg"])
                                S.op("dve", lambda e, m=m: e.max(out=mx8[:m, :], in_=lg[:m, :]), ["lg"], ["mx8"])
                                TS("dve", msk[:m, :], lg[:m, :], mx8[:m, 3:4], None, ALU.is_ge, reads=["lg", "mx8"], writes=["msk"])
                                TS("dve", den[:m, 0:1], mx8[:m, 0:1], -1.0, None, ALU.mult, reads=["mx8"], writes=["den0"])
                                ACT(ex[:m, :], lg[:m, :], AF.Exp, ["lg", "den0"], ["ex"], bias=den[:m, 0:1])
                                TT("dve", ex[:m, :], ex[:m, :], msk[:m, :], ALU.mult, ["ex", "msk"], ["ex"])
                                S.op("dve", lambda e, m=m: e.reduce_sum(out=den[:m, 1:2], in_=ex[:m, :], axis=AX.X), ["ex"], ["den1"])
                                S.op("dve", lambda e, m=m: e.reciprocal(out=den[:m, 1:2], in_=den[:m, 1:2]), ["den1"], ["den1"])
                                TS("dve", ex[:m, :], ex[:m, :], den[:m, 1:2], None, ALU.mult, reads=["ex", "den1"], writes=["ex"])
                                TR(ps[6][0:32, 128:128 + m], ex[:m, :], ident_f[:m, :m], ["ex", "ident_f"], ["ps6"])
                                c0 = t0 - h0 + s0
                                COPY("act", gatesT[:, c0:c0 + m], ps[6][0:32, 128:128 + m], ["ps6"], ["gatesT"])
                        S.barrier()
                    if "gatesT" in dbg and h0 == lo and l == 0:
                        dump("gatesT", gatesT[:], [32, NH], "gatesT")
                    with ExitStack() as PE_:
                        act = sbt(PE_, "act", [128, 8, NH], BF16)
                        gbc = sbt(PE_, "gbc", [128, NH])
                        wg = [sbt(PE_, "wg%d" % i, [128, 8, 2, 512], BF16) for i in range(2)]
                        wd = [sbt(PE_, "wd%d" % i, [128, 8, 1024], BF16) for i in range(2)]
                        tt = [[sbt(PE_, "tt%d_%d" % (i, j), [128, 512]) for j in range(3)] for i in range(2)]
                        ytmp = [sbt(PE_, "ytmp%d" % i, [128, 512]) for i in range(2)]
                        bgu = sbt(PE_, "bgu", [128, 32 * 16])
                        bdn = sbt(PE_, "bdn", [32, 1024])
                        DMA("sp", bgu[:], b_guT_d[:, l * 512:(l + 1) * 512], writes=["bgu"])
                        DMA("sp", bdn[:], b_down[l], writes=["bdn"])
                        cnt = {"g": 0, "t": 0, "y": 0, "d": 0}

                        def down_evac(py, pyk, dm, t0, n):
                            cnt["y"] ^= 1
                            yt, ytk = ytmp[cnt["y"]], "ytmp%d" % cnt["y"]
                            ACT(yt[:, :n], py[:, :n], AF.Identity, [pyk, "mod"], [ytk], scale=mod[:, l * 48 + 40 + dm:l * 48 + 41 + dm])
                            TT("pool", big[:, dm, t0:t0 + n], big[:, dm, t0:t0 + n], yt[:, :n], ALU.add, ["x%d" % dm, ytk], ["x%d" % dm])

                        for dm in range(8):
                            for (t0, n) in tiles(h0, h1):
                                cnt["d"] ^= 1
                                py, pyk = ps[4 + cnt["d"]], "ps%d" % (4 + cnt["d"])
                                MM(py[:, :n], bdn[0:32, dm * 128:(dm + 1) * 128], gatesT[0:32, t0 - h0:t0 - h0 + n],
                                   reads=["bdn", "gatesT"], writes=[pyk])
                                down_evac(py, pyk, dm, t0, n)
                        def issue_wg(si):
                            e_, fq_ = si // 2, si % 2
                            for two in range(2):
                                DMA("pool", wg[si % 2][:, :, two, :],
                                    w_gu[l, e_, :, two * 1024 + fq_ * 512: two * 1024 + (fq_ + 1) * 512].rearrange("(k p) c -> p k c", p=128),
                                    writes=["wg%d" % (si % 2)])

                        def issue_wd(e_):
                            DMA("pool", wd[e_ % 2][:], w_down[l, e_].rearrange("(k p) c -> p k c", p=128), writes=["wd%d" % (e_ % 2)])

                        issue_wg(0)
                        issue_wd(0)
                        for e in range(32):
                            for (t0, n) in tiles(0, NH):
                                MM(ps[6][:, :n], ident_f[0:32, e:e + 1].broadcast_to([32, 128]), gatesT[0:32, t0:t0 + n],
                                   reads=["ident_f", "gatesT"], writes=["ps6"])
                                COPY("act", gbc[:, t0:t0 + n], ps[6][:, :n], ["ps6"], ["gbc"])
                            wdi = e % 2
                            if e + 1 < 32:
                                issue_wd(e + 1)
                            for fc in range(8):
                                si = e * 2 + fc // 4
                                fi = fc % 4
                                if fi == 0 and si + 1 < 64:
                                    issue_wg(si + 1)
                                wgi, wgk = wg[si % 2], "wg%d" % (si % 2)
                                bcol = e * 16 + fc
                                for (t0, n) in tiles(0, NH):
                                    pg, pgk = ps[0], "ps0"
                                    plin, plk = ps[1], "ps1"
                                    if (t0 // 512) % 2:
                                        pg, pgk, plin, plk = ps[2], "ps2", ps[3], "ps3"
                                    for k in range(8):
                                        MM(pg[:, :n], wgi[:, k, 0, fi * 128:(fi + 1) * 128], hnh[:, k, t0:t0 + n], start=(k == 0), stop=(k == 7),
                                           reads=[wgk, "hnh%d" % k], writes=[pgk])
                                    for k in range(8):
                                        MM(plin[:, :n], wgi[:, k, 1, fi * 128:(fi + 1) * 128], hnh[:, k, t0:t0 + n], start=(k == 0), stop=(k == 7),
                                           reads=[wgk, "hnh%d" % k], writes=[plk])
                                    cnt["t"] ^= 1
                                    t1, t2, t3 = tt[cnt["t"]]
                                    k1, k2, k3 = ["tt%d_%d" % (cnt["t"], j) for j in range(3)]
                                    TS("dve", t1[:, :n], pg[:, :n], bgu[:, bcol:bcol + 1], 7.0, ALU.add, ALU.min, reads=[pgk, "bgu"], writes=[k1])
                                    ACT(t2[:, :n], t1[:, :n], AF.Sigmoid, [k1], [k2], scale=1.702)
                                    TS("dve", t3[:, :n], plin[:, :n], bgu[:, bcol + 8:bcol + 9], 7.0, ALU.add, ALU.min, reads=[plk, "bgu"], writes=[k3])
                                    TS("pool", t3[:, :n], t3[:, :n], -7.0, 1.0, ALU.max, ALU.add, reads=[k3], writes=[k3])
                                    TT("pool", t1[:, :n], t1[:, :n], t2[:, :n], ALU.mult, [k1, k2], [k1])
                                    TT("dve", t1[:, :n], t1[:, :n], t3[:, :n], ALU.mult, [k1, k3], [k1])
                                    TT("pool", act[:, fc, t0:t0 + n], t1[:, :n], gbc[:, t0:t0 + n], ALU.mult, [k1, "gbc"], ["act%d" % fc])
                            for dm in range(8):
                                for (t0, n) in tiles(0, NH):
                                    cnt["d"] ^= 1
                                    py, pyk = ps[4 + cnt["d"]], "ps%d" % (4 + cnt["d"])
                                    for fc in range(8):
                                        MM(py[:, :n], wd[wdi][:, fc, dm * 128:(dm + 1) * 128], act[:, fc, t0:t0 + n],
                                           start=(fc == 0), stop=(fc == 7), reads=["wd%d" % wdi, "act%d" % fc], writes=[pyk])
                                    down_evac(py, pyk, dm, h0 + t0, n)
                        S.barrier()

        def pool_mixer():
            l = 1
            with ExitStack() as PP:
                NS = {"sq": sbt(PP, "sq", [128, 8, 512], BF16), "stdt": sbt(PP, "stdt", [128, 512])}
                rstd = sbt(PP, "rstd", [128, T])
                hb = sbt(PP, "hb", [128, T])
                s2 = sbt(PP, "s2", [128, T])
                s4 = sbt(PP, "s4", [128, T])
                pT = sbt(PP, "pT", [128, 8, T], BF16)
                pw = sbt(PP, "pw", [128, 4, 2, 256], BF16)
                invc = sbt(PP, "invc", [128, 4, 16])
                ptmp = [sbt(PP, "ptmp%d" % i, [128, 512]) for i in range(2)]
                DMA("sp", invc[:], invc_d[:, :].rearrange("p (g s) -> p g s", s=16), writes=["invc"])
                for g in range(4):
                    DMA("pool", pw[:, g, :, :], pool_w[g].rearrange("(i p) d -> p i d", p=128), writes=["pw"])
                for (t0, n) in tiles(0, T):
                    pj, pk = ps_pj()
                    for k in range(8):
                        ACT(NS["sq"][:, k, :n], big[:, k, t0:t0 + n], AF.Square, ["x%d" % k], ["sq%d" % k])
                    for k in range(8):
                        MM(pj[:, :n], ones_b[:], NS["sq"][:, k, :n], start=(k == 0), stop=(k == 7), reads=["sq%d" % k, "ones_b"], writes=[pk])
                    ACT(rstd[:, t0:t0 + n], pj[:, :n], AF.Sqrt, [pk, "eps_t"], ["rstd"], bias=eps_t[:, 0:1], scale=1.0 / 1024.0)
                S.op("dve", lambda e: e.reciprocal(out=rstd[:], in_=rstd[:]), ["rstd"], ["rstd"])
                for k in range(8):
                    g = k // 2
                    w = (2, 4, 8, 16)[g]
                    STT(hb[:], big[:, k, :], mod1[:, 48 + 8 + k:48 + 9 + k], rstd[:], ALU.mult, ALU.mult, ["x%d" % k, "rstd", "mod1"], ["hb"])
                    TS("dve", hb[:], hb[:], mod[:, 48 + k:48 + k + 1], None, ALU.add, reads=["hb", "mod"], writes=["hb"])
                    TS("dve", hb[:, 0:64], hb[:, 0:64], flagw[:, 0:1], None, ALU.mult, reads=["hb", "flag"], writes=["hb"])
                    TT("pool", s2[:, 1:T], hb[:, 1:T], hb[:, 0:T - 1], ALU.add, ["hb"], ["s2"])
                    ws, wsk = s2, "s2"
                    if w >= 4:
                        TT("pool", s4[:, 3:T], s2[:, 3:T], s2[:, 1:T - 2], ALU.add, ["s2"], ["s4"])
                        ws, wsk = s4, "s4"
                    if w >= 8:
                        TT("pool", s2[:, 7:T], s4[:, 7:T], s4[:, 3:T - 4], ALU.add, ["s4", "s2"], ["s2"])
                        ws, wsk = s2, "s2"
                    if w >= 16:
                        TT("pool", s4[:, 15:T], s2[:, 15:T], s2[:, 7:T - 8], ALU.add, ["s2", "s4"], ["s4"])
                        ws, wsk = s4, "s4"
                    STT(pT[:, k, 16:T], ws[:, 16:T], 1.0 / w, hb[:, 16:T], ALU.mult, ALU.subtract, [wsk, "hb"], ["pT%d" % k])
                    TT("dve", ws[:, 64:80], ws[:, 64:80], invc[:, g, :], ALU.mult, [wsk, "invc", "pT%d" % k], [wsk])
                    TT("dve", pT[:, k, 64:80], ws[:, 64:80], hb[:, 64:80], ALU.subtract, [wsk, "hb"], ["pT%d" % k])
                i = 0
                for k in range(8):
                    g, j = k // 2, k % 2
                    for (t0, n) in tiles(HALO, T):
                        pj, pk = ps_pj()
                        for ii in range(2):
                            MM(pj[:, :n], pw[:, g, ii, j * 128:(j + 1) * 128], pT[:, 2 * g + ii, t0:t0 + n], start=(ii == 0), stop=(ii == 1),
                               reads=["pw", "pT%d" % (2 * g + ii)], writes=[pk])
                        i ^= 1
                        TS("dve", ptmp[i][:, :n], pj[:, :n], small[:, 24 + k:25 + k], small[:, 16 + k:17 + k], ALU.add, ALU.mult,
                           reads=[pk, "small_pb", "small_pc"], writes=["ptmp%d" % i])
                        TT("pool", big[:, k, t0:t0 + n], big[:, k, t0:t0 + n], ptmp[i][:, :n], ALU.add, ["x%d" % k, "ptmp%d" % i], ["x%d" % k])
                S.barrier()

        def final_norm():
            with ExitStack() as PF:
                NS = {"sq": sbt(PF, "sq", [128, 8, 512], BF16), "stdt": sbt(PF, "stdt", [128, 512])}
                ob = [sbt(PF, "fo%d" % i, [128, 8, 512]) for i in range(2)]
                for ti, (t0, n) in enumerate(tiles(HALO, T)):
                    o = ob[ti % 2]
                    norm_tile(NS, [big[:, k, t0:t0 + n] for k in range(8)], ["x%d" % k for k in range(8)], n, 0, False,
                              None, None, [o[:, k, :n] for k in range(8)], ["fo%d_%d" % (ti % 2, k) for k in range(8)], final=True)
                    DMA("sp", yT[:, t0 - HALO:t0 - HALO + n].rearrange("(k p) t -> p k t", p=128), o[:, :, :n],
                        reads=["fo%d_%d" % (ti % 2, k) for k in range(8)], writes=["yT%d" % ti], is_out=True)

        if "s0" not in dbg and mode != "F2":
            mixer_norm()
        if mode in ("A", "F"):
            if "s0" not in dbg and "s1" not in dbg:
                try:
                    hg_heads(False)
                    if "s2" not in dbg:
                        ret_heads(False)
                except Stop:
                    pass
        if mode == "F2":
            with ExitStack() as PX:
                Lall_sb = sbt(PX, "Lall_sb", [128, ncores - 1, 8, 128])
                Dall_sb = sbt(PX, "Dall_sb", [128, ncores - 1, 8])
                for w in range(1, ncores):
                    W["w"], W["L"], W["D"] = w, Lall_sb[:, w - 1, :, :], Dall_sb[:, w - 1, :]
                    mixer_norm()
                    hg_heads(False)
                    ret_heads(False)
                W["w"], W["L"], W["D"] = 0, None, None
                chain_states(Lall_sb, Dall_sb)
            mixer_norm()
        if mode == "A":
            DMA("sp", Lst_o[:, :], Lst[:].rearrange("p h e -> p (h e)"), reads=["Lst"], writes=["Lst_o"], is_out=True)
            DMA("sp", Dst_o[:, :], Dst[:], reads=["Dst"], writes=["Dst_o"], is_out=True)
        else:
            for _once in ([1] if mode != "F2" else []):
              with ExitStack() as PX:
                Lall_sb = sbt(PX, "Lall_sb", [128, ncores, 8, 128])
                Dall_sb = sbt(PX, "Dall_sb", [128, ncores, 8])
                if mode == "B":
                    DMA("sp", Lall_sb[:], Lall_d.rearrange("c p (h e) -> p c h e", e=128), writes=["Lall"])
                    DMA("sp", Dall_sb[:], Dall_d.rearrange("c p h -> p c h"), writes=["Dall"])
                else:
                    bounce = nc.dram_tensor("st_bounce", [128, 1032], F32)
                    gath = nc.dram_tensor("st_gath", [ncores * 128, 1032], F32)
                    DMA("sp", bounce[:, 0:1024], Lst[:].rearrange("p h e -> p (h e)"), reads=["Lst"], writes=["bounce"])
                    DMA("sp", bounce[:, 1024:1032], Dst[:], reads=["Dst"], writes=["bounce"])
                    S.barrier()
                    S.op("pool", lambda e: e.collective_compute("AllGather", ALU.bypass, replica_groups=[list(range(ncores))],
                                                                ins=[bounce.ap().opt()], outs=[gath.ap().opt()]),
                         reads=["bounce"], writes=["gath"], dma_key="gath", dma_inc=1)
                    S.barrier()
                    gv = gath.ap().rearrange("(c p) w -> p c w", p=128)
                    DMA("sp", Lall_sb[:].rearrange("p c h e -> p c (h e)"), gv[:, :, 0:1024], reads=["gath"], writes=["Lall"])
                    DMA("sp", Dall_sb[:], gv[:, :, 1024:1032], reads=["gath"], writes=["Dall"])
                chain_states(Lall_sb, Dall_sb)
            dump("Sin", Sin[:].rearrange("p h e -> p (h e)"), [128, 1024], "Sin")
            hg_heads(True)
            ret_heads(True)
            if "cat" in dbg:
                cf = sbt(es, "catf", [128, 8, T])
                for h in range(8):
                    COPY("act", cf[:, h, :], catT[:, h, :], ["cat%d" % h], ["catf"])
                dump("cat", cf[:].rearrange("p h t -> p (h t)"), [128, 8 * T], "catf")
            wout_residual()
            dump("x1", big[:].rearrange("p k t -> p (k t)"), [128, 8 * T], "x0")
            if "stop1" not in dbg:
                moe(0, 0, T)
                dump("x2", big[:].rearrange("p k t -> p (k t)"), [128, 8 * T], "x0")
                pool_mixer()
                dump("x3", big[:].rearrange("p k t -> p (k t)"), [128, 8 * T], "x0")
                moe(1, HALO, T)
            final_norm()
        S.finish()
        S.emit()
    return nc, dbg_outs, in_names


def _consts(T, tok0):
    gam = [1.0 - 2.0 ** (-5 - h) for h in range(4)]
    idx = np.arange(64)
    rel = idx[None, :] - idx[:, None]
    masks = np.zeros((64, 5, 64), np.float64)
    masks[:, 0, :] = (rel >= 0)
    for h in range(4):
        masks[:, 1 + h, :] = np.where(rel >= 0, gam[h] ** np.maximum(rel, 0), 0.0)
    kscale = np.ones((64, 8), np.float64)
    for h in range(4):
        kscale[:, 4 + h] = gam[h] ** (63 - idx)
    gdec = np.zeros((128, 4, 64), np.float64)
    for h in range(4):
        gdec[:, h, :] = (gam[h] ** (idx + 1.0))[None, :]
    half = 64
    inv = (10000.0 ** (-np.arange(half, dtype=np.float32) / half)).astype(np.float32)
    pos = (tok0 + np.arange(T)).astype(np.float32)
    ang = (pos[None, :] * inv[:, None]).astype(np.float32)
    cos = np.cos(ang.astype(np.float64))
    sin = np.sin(ang.astype(np.float64))
    cosT = np.concatenate([cos, cos], 0)
    sinT = np.concatenate([-sin, sin], 0)
    scanm = np.ones((128, T), np.float32)
    scanm[:, ::64] = 0.0
    return dict(masks=masks.reshape(64, 320).astype(np.float32), kscale=kscale.astype(np.float32),
                gdec=gdec.reshape(128, 256).astype(np.float32), cosT=cosT.astype(np.float32),
                sinT=sinT.astype(np.float32), scanm=scanm, ident=np.eye(128, dtype=np.float32))


def _pk(v, ncol):
    return np.ascontiguousarray(np.asarray(v, np.float32).reshape(ncol, 128).T)


def make_inputs(inp, ncores, nw=8):
    x = np.asarray(inp["x"], np.float32)[0]
    Sq = x.shape[0]
    TP = Sq // ncores
    T = TP + HALO
    w_in = np.asarray(inp["w_in"], np.float32)[0]
    perm = np.concatenate([np.arange(64, 128), np.arange(0, 64)])
    cols = []
    for base in (2048, 2560):
        for r in range(4):
            cols.append(base + r * 128 + perm)
    w_in_sw = np.ascontiguousarray(w_in[:, np.concatenate(cols)])
    shared = dict(
        cT=_pk(np.asarray(inp["c"])[0], 8),
        w_ada=np.asarray(inp["w_ada"], np.float32),
        b_adaT=np.concatenate([_pk(np.asarray(inp["b_ada"])[l], 48) for l in range(2)], 1),
        w_in=w_in, w_in_sw=w_in_sw,
        lbraw=np.concatenate([_pk(np.asarray(inp["hg_lower_bounds"])[r], 4) for r in range(2)], 1),
        gains=np.concatenate([_pk(np.asarray(inp["hg_norm"])[0].reshape(-1), 4),
                              _pk(np.asarray(inp["ret_norm"])[0].reshape(-1), 4)], 1),
        w_out=np.asarray(inp["w_out"], np.float32)[0],
        pool_w=np.asarray(inp["pool_w"], np.float32)[0],
        pool_bT=_pk(np.asarray(inp["pool_b"])[0].reshape(-1), 8),
        pool_sT=_pk(np.asarray(inp["pool_scale"])[0], 8),
        w_router=np.asarray(inp["w_router"], np.float32),
        b_router=np.asarray(inp["b_router"], np.float32),
        w_gu=np.asarray(inp["w_gu"], np.float32),
        b_guT=_pk(np.asarray(inp["b_gu"]).reshape(-1), 2 * 32 * 16),
        w_down=np.asarray(inp["w_down"], np.float32),
        b_down=np.asarray(inp["b_down"], np.float32),
        fnT=_pk(np.asarray(inp["final_norm"]), 8),
    )
    win = {}
    for c in range(ncores):
        tok0 = c * TP - HALO
        xs = np.zeros((T, 1024), np.float32)
        lo = max(tok0, 0)
        xs[lo - tok0:, :] = x[lo:tok0 + T, :]
        cst = _consts(T, tok0)
        win[c] = (np.ascontiguousarray(xs.T), cst["cosT"], cst["sinT"])
    zero_w = (np.zeros((1024, T), np.float32), np.zeros((128, T), np.float32), np.zeros((128, T), np.float32))
    maps = []
    for j in range(ncores):
        m = dict(shared)
        cst = _consts(T, j * TP - HALO)
        for k in ("masks", "kscale", "gdec", "scanm", "ident"):
            m[k] = cst[k]
        ws = [win[j - w] if j - w >= 0 else zero_w for w in range(nw)]
        m["xT"] = np.stack([w_[0] for w_ in ws], 0)
        m["cosT"] = np.stack([w_[1] for w_ in ws], 0)
        m["sinT"] = np.stack([w_[2] for w_ in ws], 0)
        flag = np.ones((128, 8), np.float32)
        valid = np.zeros((128, 8), np.float32)
        for w in range(8):
            if j - w <= 0:
                flag[:, w] = 0.0
            if w >= 1 and j - w >= 0:
                valid[:, w - 1] = 1.0
        m["flag"] = flag
        m["valid"] = valid
        seli = np.zeros((128, 8), np.float32)
        seli[:, :j] = 1.0
        m["seli"] = seli
        invc = np.zeros((128, 4, 16), np.float32)
        for g, w in enumerate((2, 4, 8, 16)):
            tg = j * TP + np.arange(16)
            invc[:, g, :] = (1.0 / np.minimum(tg + 1, w))[None, :]
        m["invc"] = invc.reshape(128, 64)
        maps.append(m)
    return maps, TP


_CACHE = {}


def _get(TP, mode, ncores):
    key = (TP, mode, ncores)
    if key not in _CACHE:
        r = build(TP, mode, ncores)
        _CACHE[key] = (r[0], r[2])
    return _CACHE[key]


def kernel(**inp):
    ncores = 8
    maps, TP = make_inputs(inp, ncores, 8)
    ncF, names = _get(TP, "F2", ncores)
    r = run_bass_kernel_spmd(ncF, [{k: m[k] for k in names} for m in maps], core_ids=list(range(ncores)))
    y = np.concatenate([r.results[j]["yT"].T for j in range(ncores)], 0)
    return np.ascontiguousarray(y[None].astype(np.float32))
```

```python
from contextlib import ExitStack
import numpy as np
import concourse.bass as bass
import concourse.mybir as mybir
from concourse.bass_utils import run_bass_kernel_spmd

F32 = mybir.dt.float32
BF16 = mybir.dt.bfloat16
ALU = mybir.AluOpType
AF = mybir.ActivationFunctionType
AX = mybir.AxisListType
ENGS = ("pe", "act", "dve", "pool", "sp")
EPS = 1e-6
HALO = 64


class Sched:
    def __init__(self, nc, es):
        self.nc, self.es = nc, es
        self.items = {e: [] for e in ENGS}
        self.cnt = {e: 0 for e in ENGS}
        self.clock = {e: {} for e in ENGS}
        self.snap, self.key_w, self.key_r, self.dma_cnt, self.sems = {}, {}, {}, {}, {}
        self.out_events = []
        self.epoch = 0
        self.ek = {e: "E:" + e for e in ENGS}
        for e in ENGS:
            self._sem(self.ek[e])

    def _sem(self, k):
        if k not in self.sems:
            self.sems[k] = self.es.enter_context(self.nc.semaphore("s%d" % len(self.sems)))
        return self.sems[k]

    def _need(self, eng, ev, deps):
        if ev is None:
            return
        sk, val = ev
        if eng == "pe" and sk.startswith("E:pe"):
            return
        if self.clock[eng].get(sk, 0) >= val:
            return
        if deps.get(sk, 0) < val:
            deps[sk] = val

    def _apply(self, eng, deps):
        clk = self.clock[eng]
        for sk, val in deps.items():
            self.items[eng].append(("w", sk, val))
            for a, b in self.snap.get((sk, val), {}).items():
                if clk.get(a, 0) < b:
                    clk[a] = b
            if clk.get(sk, 0) < val:
                clk[sk] = val

    def op(self, eng, fn, reads=(), writes=(), dma_key=None, is_out=False, dma_inc=16):
        writes = [k.split("_")[0] if k.startswith("ps") else k for k in writes]
        writes += [k.split("_")[0] for k in reads if k.startswith("ps")]
        reads = [k for k in reads if not k.startswith("ps")]
        deps = {}
        for k in reads:
            self._need(eng, self.key_w.get(k), deps)
        for k in writes:
            self._need(eng, self.key_w.get(k), deps)
            for ev in self.key_r.get(k, ()):
                self._need(eng, ev, deps)
        self._apply(eng, deps)
        if dma_key is not None:
            sk = "D:" + str(dma_key)
            self._sem(sk)
            self.dma_cnt[sk] = self.dma_cnt.get(sk, 0) + 1
            ev = (sk, self._dval(sk, dma_inc))
            self.items[eng].append(("i", fn, sk, dma_inc))
            if is_out:
                self.out_events.append(ev)
        else:
            self.cnt[eng] += 1
            ev = (self.ek[eng], self.cnt[eng])
            self.items[eng].append(("i", fn, self.ek[eng], 1))
        self.snap[ev] = dict(self.clock[eng])
        for k in writes:
            self.key_w[k] = ev
            self.key_r[k] = []
        for k in reads:
            self.key_r.setdefault(k, []).append(ev)
        return ev

    def _dval(self, sk, inc):
        self.dma_val = getattr(self, "dma_val", {})
        self.dma_val[sk] = self.dma_val.get(sk, 0) + inc
        return self.dma_val[sk]

    def barrier(self):
        evs = [(self.ek[e], self.cnt[e]) for e in ENGS if self.cnt[e] > 0]
        evs += [(sk, v) for sk, v in getattr(self, "dma_val", {}).items()]
        for eng in ENGS:
            deps = {}
            for sk, val in evs:
                if self.clock[eng].get(sk, 0) < val:
                    deps[sk] = val
            self._apply(eng, deps)
        self.key_w, self.key_r = {}, {}
        self.epoch += 1
        for e in ENGS:
            if self.cnt[e] > 12000:
                self.ek[e] = "E:%s:%d" % (e, self.epoch)
                self._sem(self.ek[e])
                self.cnt[e] = 0

    def finish(self):
        deps = {}
        for ev in self.out_events:
            self._need("sp", ev, deps)
        self._apply("sp", deps)

    def emit(self):
        nc = self.nc
        with nc.Block() as block:
            def run(name):
                def body(engine):
                    for it in self.items[name]:
                        if it[0] == "w":
                            engine.wait_ge(self.sems[it[1]], it[2])
                        else:
                            it[1](engine).then_inc(self.sems[it[2]], it[3])
                return body
            block.tensor(run("pe"))
            block.scalar(run("act"))
            block.vector(run("dve"))
            block.gpsimd(run("pool"))
            block.sync(run("sp"))


class Stop(Exception):
    pass


def tiles(lo, hi, step=512):
    return [(t, min(step, hi - t)) for t in range(lo, hi, step)]


def build(TP, mode, ncores=8, dbg=()):
    T = TP + HALO
    NCH = T // 64
    nc = bass.Bass("TRN2", target_bir_lowering=False)

    in_names = []

    def din(name, shape):
        in_names.append(name)
        return nc.dram_tensor(name, list(shape), F32, kind="ExternalInput").ap()

    def dout(name, shape):
        return nc.dram_tensor(name, list(shape), F32, kind="ExternalOutput").ap()

    NW = 8 if mode == "F2" else 1
    xTw = din("xT", [NW, 1024, T])
    flag_d = din("flag", [128, 8])
    valid_d = din("valid", [128, 8])
    cT_d = din("cT", [128, 8])
    w_ada = din("w_ada", [2, 1024, 6144])
    badaT_d = din("b_adaT", [128, 96])
    w_in = din("w_in", [1024, 4096])
    w_in_sw = din("w_in_sw", [1024, 1024])
    lbraw_d = din("lbraw", [128, 8])
    gains_d = din("gains", [128, 8])
    w_out = din("w_out", [1024, 1024]) if mode != "A" else None
    pool_w = din("pool_w", [4, 256, 256]) if mode != "A" else None
    pool_bT_d = din("pool_bT", [128, 8])
    pool_sT_d = din("pool_sT", [128, 8])
    w_router = din("w_router", [2, 1024, 32]) if mode != "A" else None
    b_router = din("b_router", [2, 32]) if mode != "A" else None
    w_gu = din("w_gu", [2, 32, 1024, 2048]) if mode != "A" else None
    b_guT_d = din("b_guT", [128, 2 * 32 * 16]) if mode != "A" else None
    w_down = din("w_down", [2, 32, 1024, 1024]) if mode != "A" else None
    b_down = din("b_down", [2, 32, 1024]) if mode != "A" else None
    fnT_d = din("fnT", [128, 8])
    ident_d = din("ident", [128, 128])
    masks_d = din("masks", [64, 5 * 64])
    kscale_d = din("kscale", [64, 8])
    cos_d = din("cosT", [NW, 128, T])
    sin_d = din("sinT", [NW, 128, T])
    gdec_d = din("gdec", [128, 4 * 64])
    scanm_d = din("scanm", [128, T])
    invc_d = din("invc", [128, 4 * 16])
    seli_d = din("seli", [128, 8])
    if mode == "A":
        Lst_o = dout("Lst", [128, 8 * 128])
        Dst_o = dout("Dst", [128, 8])
    if mode == "B":
        Lall_d = din("Lall", [ncores, 128, 8 * 128])
        Dall_d = din("Dall", [ncores, 128, 8])
    if mode in ("B", "F", "F2"):
        yT = dout("yT", [1024, TP])
    dbg_outs = {}

    gam = [1.0 - 2.0 ** (-5 - h) for h in range(4)]

    with ExitStack() as es:
        S = Sched(nc, es)

        uniq = [0]

        def sbt(stack, name, shape, dt=F32):
            uniq[0] += 1
            return stack.enter_context(nc.sbuf_tensor("sb%d_%s" % (uniq[0], name), list(shape), dt))

        def DMA(eng, out, in_, reads=(), writes=(), key=None, is_out=False):
            S.op(eng, lambda e: e.dma_start(out=out, in_=in_), reads, writes, dma_key=key or writes[0], is_out=is_out)

        def MM(out, lhsT, rhs, start=True, stop=True, reads=(), writes=()):
            S.op("pe", lambda e: e.matmul(out, lhsT, rhs, start=start, stop=stop), reads, writes)

        def TR(out, in_, ident, reads=(), writes=()):
            S.op("pe", lambda e: e.transpose(out, in_, ident), reads, writes)

        def ACT(out, in_, func, reads=(), writes=(), bias=None, scale=None):
            kw = {}
            if bias is not None:
                kw["bias"] = bias
            if scale is not None:
                kw["scale"] = scale
            S.op("act", lambda e: e.activation(out=out, in_=in_, func=func, **kw), reads, writes)

        def TT(eng, out, in0, in1, op, reads=(), writes=()):
            S.op(eng, lambda e: e.tensor_tensor(out=out, in0=in0, in1=in1, op=op), reads, writes)

        def TS(eng, out, in0, s1, s2, op0, op1=None, reads=(), writes=()):
            if op1 is None:
                S.op(eng, lambda e: e.tensor_scalar(out=out, in0=in0, scalar1=s1, scalar2=None, op0=op0), reads, writes)
            else:
                S.op(eng, lambda e: e.tensor_scalar(out=out, in0=in0, scalar1=s1, scalar2=s2, op0=op0, op1=op1), reads, writes)

        def STT(out, in0, scalar, in1, op0, op1, reads=(), writes=()):
            S.op("dve", lambda e: e.scalar_tensor_tensor(out=out, in0=in0, scalar=scalar, in1=in1, op0=op0, op1=op1),
                 reads, writes)

        def MEMSET(eng, ap, val, writes=()):
            S.op(eng, lambda e: e.memset(ap, val), (), writes)

        def COPY(eng, out, in_, reads=(), writes=()):
            if eng == "act":
                ACT(out, in_, AF.Identity, reads, writes)
            else:
                S.op(eng, lambda e: e.tensor_copy(out=out, in_=in_), reads, writes)

        def dump(name, ap, shape, key):
            if name in dbg:
                d = dout("dbg_" + name, shape)
                dbg_outs[name] = d
                DMA("sp", d, ap, reads=[key], writes=["dbg_" + name], is_out=True)

        G = es
        big = sbt(G, "big", [128, 8, T])
        bigb = big[:].rearrange("p k t -> p (k t)").bitcast(BF16).rearrange("p (k t) -> p k t", k=16)
        hnT = bigb[:, 0:8, :]
        catT = bigb[:, 8:16, :]
        ps = [G.enter_context(nc.psum_tensor("ps%d" % i, [128, 512], F32)) for i in range(7)]
        psb = G.enter_context(nc.psum_tensor("psb", [128, 1024], BF16))
        ident_f = sbt(G, "ident_f", [128, 128])
        ident_b = sbt(G, "ident_b", [128, 128], BF16)
        ones_b = sbt(G, "ones_b", [128, 128], BF16)
        ones_f = sbt(G, "ones_f", [128, 128])
        eps_t = sbt(G, "eps_t", [128, 1])
        flagw = sbt(G, "flag_s", [128, 8])
        W = {"w": 0, "L": None, "D": None}
        mod = sbt(G, "mod", [128, 96])
        mod1 = sbt(G, "mod1", [128, 96])
        small = sbt(G, "small", [128, 64])
        Lst = sbt(G, "Lst_s", [128, 8, 128])
        Dst = sbt(G, "Dst_s", [128, 8])
        Sin = sbt(G, "Sin_s", [128, 8, 128])
        seli = sbt(G, "seli_s", [128, 8])

        DMA("sp", ident_f[:], ident_d[:, :], writes=["ident_f"])
        DMA("pool", ident_b[:], ident_d[:, :], writes=["ident_b"])
        DMA("sp", flagw[:], flag_d[:, :], writes=["flag"])
        DMA("sp", seli[:], (valid_d if mode == "F2" else seli_d)[:, :], writes=["seli"])
        MEMSET("pool", ones_b[:], 1.0, ["ones_b"])
        MEMSET("pool", ones_f[:], 1.0, ["ones_f"])
        MEMSET("pool", eps_t[:], EPS, ["eps_t"])
        DMA("sp", small[:, 40:48], lbraw_d[:, :], writes=["small_lbraw"])
        DMA("sp", small[:, 8:16], gains_d[:, :], writes=["small_g"])
        DMA("sp", small[:, 16:24], pool_sT_d[:, :], writes=["small_ps"])
        DMA("sp", small[:, 24:32], pool_bT_d[:, :], writes=["small_pb"])
        DMA("sp", small[:, 32:40], fnT_d[:, :], writes=["small_fn"])
        TT("dve", small[:, 48:52], small[:, 40:44], small[:, 44:48], ALU.subtract, ["small_lbraw"], ["small_t"])
        ACT(small[:, 0:4], small[:, 48:52], AF.Sigmoid, ["small_t"], ["small_lb"])
        TS("dve", small[:, 4:8], small[:, 0:4], -1.0, 1.0, ALU.mult, ALU.add, ["small_lb"], ["small_oml"])

        with ExitStack() as P0:
            cT = sbt(P0, "cT_s", [128, 8])
            cond = sbt(P0, "cond", [128, 8])
            badaT = sbt(P0, "badaT", [128, 96])
            wa = [sbt(P0, "wa%d" % i, [128, 8, 768]) for i in range(2)]
            DMA("sp", cT[:], cT_d[:, :], writes=["cT"])
            DMA("sp", badaT[:], badaT_d[:, :], writes=["badaT"])
            ACT(cond[:], cT[:], AF.Silu, ["cT"], ["cond"])
            for l in range(2):
                for blk in range(8):
                    i = (l * 8 + blk) % 2
                    DMA("sp", wa[i][:], w_ada[l, :, blk * 768:(blk + 1) * 768].rearrange("(k p) c -> p k c", p=128),
                        writes=["wa%d" % i])
                    for m in range(6):
                        col = l * 48 + blk * 6 + m
                        for k in range(8):
                            MM(ps[0][:, col:col + 1], wa[i][:, k, m * 128:(m + 1) * 128], cond[:, k:k + 1],
                               start=(k == 0), stop=(k == 7), reads=["wa%d" % i, "cond"], writes=["ps0"])
            TT("dve", mod[:], ps[0][:, 0:96], badaT[:], ALU.add, ["ps0", "badaT"], ["mod"])
            TS("dve", mod1[:], mod[:], 1.0, None, ALU.add, reads=["mod"], writes=["mod1"])
            TT("dve", small[:, 16:24], small[:, 16:24], mod[:, 48 + 16:48 + 24], ALU.mult, ["small_ps", "mod"], ["small_pc"])
            S.barrier()
        dump("mod", mod[:], [128, 96], "mod")

        rot = {"pj": 0, "at": 0, "o": 0, "ds": 0}

        def ps_pj():
            rot["pj"] ^= 1
            return ps[rot["pj"]], "ps%d" % rot["pj"]

        def norm_tile(NS, xk, xkeys, n, l, ffn, out_bf, out_bf_keys, out_f32=None, out_f32_keys=None, final=False):
            base = l * 48 + (24 if ffn else 0)
            pj, pk = ps_pj()
            for k in range(8):
                ACT(NS["sq"][:, k, :n], xk[k], AF.Square, [xkeys[k]], ["sq%d" % k])
            for k in range(8):
                MM(pj[:, :n], ones_b[:], NS["sq"][:, k, :n], start=(k == 0), stop=(k == 7),
                   reads=["sq%d" % k, "ones_b"], writes=[pk])
            ACT(NS["stdt"][:, :n], pj[:, :n], AF.Sqrt, [pk, "eps_t"], ["stdt"], bias=eps_t[:, 0:1], scale=1.0 / 1024.0)
            S.op("dve", lambda e: e.reciprocal(out=NS["stdt"][:, :n], in_=NS["stdt"][:, :n]), ["stdt"], ["stdt"])
            for k in range(8):
                if final:
                    STT(out_f32[k], xk[k], small[:, 32 + k:33 + k], NS["stdt"][:, :n], ALU.mult, ALU.mult,
                        [xkeys[k], "stdt", "small_fn"], [out_f32_keys[k]])
                    continue
                STT(NS["t1"][:, k, :n], xk[k], mod1[:, base + 8 + k:base + 9 + k], NS["stdt"][:, :n], ALU.mult, ALU.mult,
                    [xkeys[k], "stdt", "mod1"], ["t1_%d" % k])
                if out_f32 is not None:
                    ACT(out_f32[k], NS["t1"][:, k, :n], AF.Identity, ["t1_%d" % k, "mod"], [out_f32_keys[k]],
                        bias=mod[:, base + k:base + k + 1])
                    COPY("pool", out_bf[k], out_f32[k], [out_f32_keys[k]], [out_bf_keys[k]])
                else:
                    ACT(out_bf[k], NS["t1"][:, k, :n], AF.Identity, ["t1_%d" % k, "mod"], [out_bf_keys[k]],
                        bias=mod[:, base + k:base + k + 1])

        def mixer_norm():
            with ExitStack() as PN:
                NS = {"sq": sbt(PN, "sq", [128, 8, 512], BF16), "stdt": sbt(PN, "stdt", [128, 512]),
                      "t1": sbt(PN, "t1", [128, 8, 512])}
                xt = [sbt(PN, "xt%d" % i, [128, 8, 512]) for i in range(2)]
                for ti, (t0, n) in enumerate(tiles(0, T)):
                    b = xt[ti % 2]
                    DMA("sp", b[:, :, :n], xTw[W["w"], :, t0:t0 + n].rearrange("(k p) t -> p k t", p=128), writes=["xt%d" % (ti % 2)])
                    norm_tile(NS, [b[:, k, :n] for k in range(8)], ["xt%d" % (ti % 2)] * 8, n, 0, False,
                              [hnT[:, k, t0:t0 + n] for k in range(8)], ["hn%d" % k for k in range(8)])
                S.barrier()

        def proj_fm(wh, qi, t0, n):
            pj, pk = ps_pj()
            for k in range(8):
                MM(pj[:, :n], wh[:, k, qi, :], hnT[:, k, t0:t0 + n], start=(k == 0), stop=(k == 7),
                   reads=["wh", "hn%d" % k], writes=[pk])
            return pj, pk

        def proj_v(wh, qi, vtok):
            for c0 in range(0, NCH, 4):
                pj, pk = ps_pj()
                ncs = min(4, NCH - c0)
                for ci in range(ncs):
                    c = c0 + ci
                    for k in range(8):
                        MM(pj[0:64, ci * 128:(ci + 1) * 128], hnT[:, k, c * 64:(c + 1) * 64], wh[:, k, qi, :],
                           start=(k == 0), stop=(k == 7), reads=["wh", "hn%d" % k], writes=[pk])
                ACT(vtok[:, c0:c0 + ncs, :], pj[0:64, 0:ncs * 128].rearrange("p (c e) -> p c e", e=128), AF.Identity,
                    [pk], ["vtok"])

        def k_transposes(kA, ktok, ksc):
            for c0 in range(0, NCH, 8):
                ncs = min(8, NCH - c0)
                for ci in range(ncs):
                    c = c0 + ci
                    TR(psb[0:64, ci * 128:(ci + 1) * 128], kA[:, c * 64:(c + 1) * 64], ident_b[:], ["kA", "ident_b"], ["psb"])
                TS("dve", ktok[:, c0:c0 + ncs, :], psb[0:64, 0:ncs * 128].rearrange("p (c e) -> p c e", e=128),
                   ksc, None, ALU.mult, reads=["psb", "kscale"], writes=["ktok"])

        def recurrence(h, HS, full, mask_ap):
            St, Sb = HS["S"], HS["Sb"]
            if full:
                COPY("pool", St[:], Sin[:, h, :], ["Sin"], ["S"])
            else:
                MEMSET("pool", St[:], 0.0, ["S"])
            nch = NCH if full else NCH - 1
            ob = None
            for c in range(nch):
                cs = slice(c * 64, (c + 1) * 64)
                if full:
                    rot["at"] ^= 1
                    pa, pak = ps[2 + rot["at"]], "ps%d" % (2 + rot["at"])
                    MM(pa[0:64, 0:64], HS["kA"][:, cs], HS["qA"][:, cs], reads=["kA", "qA"], writes=[pak])
                    atm = HS["atm"][rot["at"]]
                    TT("dve", atm[:], pa[0:64, 0:64], mask_ap, ALU.mult, [pak, "masks"], ["atm%d" % rot["at"]])
                    TS("pool", Sb[:], St[:], HS["tabM"][:, c:c + 1], None, ALU.mult, reads=["S", "tabM"], writes=["Sb"])
                    if c % 8 == 0:
                        rot["o"] ^= 1
                        ob, obk = ps[4 + rot["o"]], "ps%d" % (4 + rot["o"])
                    oc = (c % 8) * 64
                    MM(ob[:, oc:oc + 64], HS["vtok"][:, c, :], atm[:], start=True, stop=False,
                       reads=["vtok", "atm%d" % rot["at"]], writes=[obk])
                    MM(ob[:, oc:oc + 64], Sb[:], HS["qB"][:, cs], start=False, stop=True, reads=["Sb", "qB"], writes=[obk])
                    if c % 8 == 7 or c == nch - 1:
                        c0 = (c // 8) * 8
                        headnorm(h, HS, ob, obk, c0 * 64, (c + 1 - c0) * 64)
                if c < NCH - 1:
                    rot["ds"] = (rot["ds"] + 1) % 4
                    pd = ps[6][:, rot["ds"] * 128:(rot["ds"] + 1) * 128]
                    pdk = "ps6_%d" % rot["ds"]
                    MM(pd, HS["ktok"][:, c, :], HS["vtok"][:, c, :], reads=["ktok", "vtok"], writes=[pdk])
                    ACT(HS["dst"][:], pd, AF.Identity, [pdk, "tabC"], ["dst"], scale=HS["tabC"][:, c:c + 1])
                    STT(St[:], St[:], HS["tabA"][:, c:c + 1], HS["dst"][:], ALU.mult, ALU.add, ["S", "tabA", "dst"], ["S"])
            if not full:
                COPY("pool", (W["L"][:, h, :] if W["L"] is not None else Lst[:, h, :]), St[:], ["S"], ["Lst"])

        def headnorm(h, HS, ob, obk, t0, n):
            ACT(HS["osq"][:, :n], ob[:, :n], AF.Square, [obk], ["osq"])
            pj, pk = ps_pj()
            MM(pj[:, :n], ones_b[:], HS["osq"][:, :n], reads=["osq", "ones_b"], writes=[pk])
            ACT(HS["ostd"][:, :n], pj[:, :n], AF.Sqrt, [pk, "eps_t"], ["ostd"], bias=eps_t[:, 0:1], scale=1.0 / 128.0)
            S.op("dve", lambda e: e.reciprocal(out=HS["ostd"][:, :n], in_=HS["ostd"][:, :n]), ["ostd"], ["ostd"])
            STT(HS["ot"][:, :n], ob[:, :n], small[:, 8 + h:9 + h], HS["ostd"][:, :n], ALU.mult, ALU.mult,
                [obk, "ostd", "small_g"], ["ot"])
            TT("pool", catT[:, h, t0:t0 + n], HS["ot"][:, :n], HS["gate"][:, t0:t0 + n], ALU.mult, ["ot", "gate"], ["cat%d" % h])

        def head_common(PH, full):
            HS = {"kA": sbt(PH, "kA", [128, T], BF16), "ktok": sbt(PH, "ktok", [64, NCH, 128], BF16),
                  "vtok": sbt(PH, "vtok", [64, NCH, 128], BF16), "S": sbt(PH, "S", [128, 128]),
                  "Sb": sbt(PH, "Sb", [128, 128], BF16), "dst": sbt(PH, "dst", [128, 128]),
                  "tabA": sbt(PH, "tabA", [128, NCH]), "tabC": sbt(PH, "tabC", [128, NCH]), "tabM": sbt(PH, "tabM", [128, NCH]),
                  "tmp": [sbt(PH, "tmp%d" % i, [128, 512]) for i in range(3)]}
            if full:
                HS.update({"qA": sbt(PH, "qA", [128, T], BF16), "gate": sbt(PH, "gate", [128, T], BF16),
                           "atm": [sbt(PH, "atm%d" % i, [64, 64], BF16) for i in range(2)],
                           "osq": sbt(PH, "osq", [128, 512], BF16), "ostd": sbt(PH, "ostd", [128, 512]),
                           "ot": sbt(PH, "ot", [128, 512])})
            return HS

        def hg_heads(full):
            with ExitStack() as PH:
                HS = head_common(PH, full)
                if full:
                    HS["qB"] = HS["qA"]
                wh = sbt(PH, "wh", [128, 8, 4, 128], BF16)
                ff = sbt(PH, "ff", [128, T])
                bA = sbt(PH, "bA", [128, T])
                bB = sbt(PH, "bB", [128, T])
                scanm = sbt(PH, "scanm", [128, T])
                masks = sbt(PH, "masks", [64, 64])
                ksc = sbt(PH, "ksc", [64, 8])
                tmpd = sbt(PH, "tmpd", [128, NCH])
                sumbl = sbt(PH, "sumbl", [128, 1])
                DMA("sp", scanm[:], scanm_d[:, :], writes=["scanm"])
                DMA("sp", masks[:], masks_d[:, 0:64], writes=["masks"])
                DMA("sp", ksc[:], kscale_d[:, :], writes=["kscale"])
                b3 = bB[:].rearrange("p (c s) -> p c s", s=64)
                a3 = bA[:].rearrange("p (c s) -> p c s", s=64)
                for h in range(4):
                    for qi in range(4):
                        DMA("pool", wh[:, :, qi, :], w_in[:, qi * 512 + h * 128: qi * 512 + (h + 1) * 128]
                            .rearrange("(k p) c -> p k c", p=128), writes=["wh"])
                    for ti, (t0, n) in enumerate(tiles(0, T)):
                        pj, pk = proj_fm(wh, 1, t0, n)
                        tm = HS["tmp"][ti % 2]
                        ACT(tm[:, :n], pj[:, :n], AF.Sigmoid, [pk], ["tmp%d" % (ti % 2)])
                        TS("dve", ff[:, t0:t0 + n], tm[:, :n], small[:, 4 + h:5 + h], small[:, h:h + 1], ALU.mult, ALU.add,
                           reads=["tmp%d" % (ti % 2), "small_lb", "small_oml"], writes=["ff"])
                    if "h1" in dbg:
                        return
                    ACT(bA[:], ff[:], AF.Ln, ["ff"], ["bA"])
                    if "h1b" in dbg:
                        return
                    S.op("dve", lambda e: e.tensor_tensor_scan(out=bB[:], data0=scanm[:], data1=bA[:], initial=0.0,
                                                               op0=ALU.mult, op1=ALU.add), ["scanm", "bA"], ["bB"])
                    if "h1c" in dbg:
                        return
                    ACT(HS["tabA"][:], b3[:, :, 63], AF.Exp, ["bB"], ["tabA"])
                    TT("dve", tmpd[:], b3[:, :, 63], b3[:, :, 31], ALU.subtract, ["bB"], ["tmpd"])
                    ACT(HS["tabC"][:], tmpd[:], AF.Exp, ["tmpd"], ["tabC"])
                    ACT(HS["tabM"][:], b3[:, :, 31], AF.Exp, ["bB"], ["tabM"])
                    S.op("dve", lambda e: e.reduce_sum(out=sumbl[:], in_=b3[:, 0:NCH - 1, 63], axis=AX.X), ["bB"], ["sumbl"])
                    ACT((W["D"] if W["D"] is not None else Dst)[:, h:h + 1], sumbl[:], AF.Exp, ["sumbl"], ["Dst"])
                    TT("dve", HS["tabA"][:, 0:1], HS["tabA"][:, 0:1], flagw[:, W["w"]:W["w"] + 1], ALU.mult, ["tabA", "flag"], ["tabA"])
                    TT("dve", HS["tabC"][:, 0:1], HS["tabC"][:, 0:1], flagw[:, W["w"]:W["w"] + 1], ALU.mult, ["tabC", "flag"], ["tabC"])
                    if "h1d" in dbg:
                        return
                    TT("dve", a3, b3, b3[:, :, 31:32].broadcast_to([128, NCH, 64]), ALU.subtract, ["bB", "bA"], ["bA"])
                    if "h1e" in dbg:
                        return
                    ACT(bB[:], bA[:], AF.Exp, ["bA"], ["bB"])
                    ACT(bA[:], bA[:], AF.Exp, ["bA"], ["bA"], scale=-1.0)
                    TS("pool", ff[:], ff[:], -1.0, 1.0, ALU.mult, ALU.add, reads=["ff"], writes=["ff"])
                    TT("dve", HS["kA"][:], ff[:], bA[:], ALU.mult, ["ff", "bA"], ["kA"])
                    if full:
                        for ti, (t0, n) in enumerate(tiles(0, T)):
                            pj, pk = proj_fm(wh, 0, t0, n)
                            tm = HS["tmp"][ti % 2]
                            ACT(tm[:, :n], pj[:, :n], AF.Silu, [pk], ["tmp%d" % (ti % 2)])
                            TT("dve", HS["qA"][:, t0:t0 + n], tm[:, :n], bB[:, t0:t0 + n], ALU.mult,
                               ["tmp%d" % (ti % 2), "bB"], ["qA"])
                            pj, pk = proj_fm(wh, 3, t0, n)
                            ACT(HS["gate"][:, t0:t0 + n], pj[:, :n], AF.Silu, [pk], ["gate"])
                    if "h2" in dbg:
                        return
                    proj_v(wh, 2, HS["vtok"])
                    if "h3" in dbg:
                        return
                    k_transposes(HS["kA"], HS["ktok"], ksc[:, h:h + 1])
                    if "h4" in dbg:
                        return
                    recurrence(h, HS, full, masks[:])
                    if "h5" in dbg:
                        return
                S.barrier()

        def ret_heads(full):
            with ExitStack() as PH:
                HS = head_common(PH, full)
                if full:
                    HS["qB"] = sbt(PH, "qB", [128, T], BF16)
                wh = sbt(PH, "wh6", [128, 8, 6, 128], BF16)
                cosT = sbt(PH, "cosT", [128, T])
                sinT = sbt(PH, "sinT", [128, T])
                gdec = sbt(PH, "gdec", [128, 4, 64])
                masks = sbt(PH, "dmasks", [64, 4, 64])
                ksc = sbt(PH, "ksc", [64, 8])
                DMA("sp", cosT[:], cos_d[W["w"], :, :], writes=["cosT"])
                DMA("sp", sinT[:], sin_d[W["w"], :, :], writes=["sinT"])
                DMA("sp", gdec[:], gdec_d[:, :].rearrange("p (h s) -> p h s", s=64), writes=["gdec"])
                DMA("sp", masks[:], masks_d[:, 64:320].rearrange("p (h s) -> p h s", s=64), writes=["masks"])
                DMA("sp", ksc[:], kscale_d[:, :], writes=["kscale"])
                for r in range(4):
                    h = 4 + r
                    srcs = [w_in[:, 2048 + r * 128:2048 + (r + 1) * 128], w_in_sw[:, r * 128:(r + 1) * 128],
                            w_in[:, 2560 + r * 128:2560 + (r + 1) * 128], w_in_sw[:, 512 + r * 128:512 + (r + 1) * 128],
                            w_in[:, 3072 + r * 128:3072 + (r + 1) * 128], w_in[:, 3584 + r * 128:3584 + (r + 1) * 128]]
                    for qi in range(6):
                        DMA("pool", wh[:, :, qi, :], srcs[qi].rearrange("(k p) c -> p k c", p=128), writes=["wh"])
                    MEMSET("pool", HS["tabA"][:], gam[r] ** 64, ["tabA"])
                    MEMSET("pool", HS["tabC"][:], 1.0, ["tabC"])
                    MEMSET("pool", HS["tabM"][:], 1.0, ["tabM"])
                    MEMSET("pool", (W["D"] if W["D"] is not None else Dst)[:, h:h + 1], gam[r] ** (64 * (NCH - 1)), ["Dst"])
                    TT("dve", HS["tabA"][:, 0:1], HS["tabA"][:, 0:1], flagw[:, W["w"]:W["w"] + 1], ALU.mult, ["tabA", "flag"], ["tabA"])
                    TT("dve", HS["tabC"][:, 0:1], HS["tabC"][:, 0:1], flagw[:, W["w"]:W["w"] + 1], ALU.mult, ["tabC", "flag"], ["tabC"])
                    for which in ((0, 1) if full else (1,)):
                        for ti, (t0, n) in enumerate(tiles(0, T)):
                            pa, pak = proj_fm(wh, 2 * which, t0, n)
                            t1, t2 = HS["tmp"][0], HS["tmp"][1]
                            TT("dve", t1[:, :n], pa[:, :n], cosT[:, t0:t0 + n], ALU.mult, [pak, "cosT"], ["tmp0"])
                            pb, pbk = proj_fm(wh, 2 * which + 1, t0, n)
                            TT("dve", t2[:, :n], pb[:, :n], sinT[:, t0:t0 + n], ALU.mult, [pbk, "sinT"], ["tmp1"])
                            TT("pool", t1[:, :n], t1[:, :n], t2[:, :n], ALU.add, ["tmp0", "tmp1"], ["tmp0"])
                            if which == 0:
                                ACT(HS["qA"][:, t0:t0 + n], t1[:, :n], AF.Identity, ["tmp0"], ["qA"])
                                TT("dve", HS["qB"][:, t0:t0 + n].rearrange("p (c s) -> p c s", s=64),
                                   t1[:, :n].rearrange("p (c s) -> p c s", s=64),
                                   gdec[:, r, :].unsqueeze(1).broadcast_to([128, n // 64, 64]), ALU.mult,
                                   ["tmp0", "gdec"], ["qB"])
                            else:
                                ACT(HS["kA"][:, t0:t0 + n], t1[:, :n], AF.Identity, ["tmp0"], ["kA"], scale=128.0 ** -0.5)
                    if full:
                        for ti, (t0, n) in enumerate(tiles(0, T)):
                            pj, pk = proj_fm(wh, 5, t0, n)
                            ACT(HS["gate"][:, t0:t0 + n], pj[:, :n], AF.Silu, [pk], ["gate"])
                    proj_v(wh, 4, HS["vtok"])
                    k_transposes(HS["kA"], HS["ktok"], ksc[:, h:h + 1])
                    recurrence(h, HS, full, masks[:, r, :])
                S.barrier()

        def chain_states(Lall_sb, Dall_sb):
            with ExitStack() as PC:
                tmp = sbt(PC, "ctmp", [128, 128])
                MEMSET("pool", Sin[:], 0.0, ["Sin"])
                for i in (range(ncores - 2, -1, -1) if mode == "F2" else range(ncores - 1)):
                    for h in range(8):
                        STT(tmp[:], Sin[:, h, :], Dall_sb[:, i, h:h + 1], Lall_sb[:, i, h, :], ALU.mult, ALU.add,
                            ["Sin", "Lall", "Dall"], ["ctmp"])
                        TT("dve", tmp[:], tmp[:], Sin[:, h, :], ALU.subtract, ["ctmp", "Sin"], ["ctmp"])
                        STT(Sin[:, h, :], tmp[:], seli[:, i:i + 1], Sin[:, h, :], ALU.mult, ALU.add,
                            ["ctmp", "seli", "Sin"], ["Sin"])
                S.barrier()

        def wout_residual():
            with ExitStack() as PW:
                wo = sbt(PW, "wo", [128, 8, 1024], BF16)
                xhi = sbt(PW, "xhi", [128, 4, T])
                xin = [sbt(PW, "xin%d" % i, [128, 512]) for i in range(2)]
                DMA("pool", wo[:], w_out[:, :].rearrange("(k p) c -> p k c", p=128), writes=["wo"])
                i = 0
                for dm in range(8):
                    for (t0, n) in tiles(0, T):
                        pj, pk = ps_pj()
                        for h in range(8):
                            MM(pj[:, :n], wo[:, h, dm * 128:(dm + 1) * 128], catT[:, h, t0:t0 + n], start=(h == 0), stop=(h == 7),
                               reads=["wo", "cat%d" % h], writes=[pk])
                        i ^= 1
                        DMA("sp", xin[i][:, :n], xTw[0, dm * 128:(dm + 1) * 128, t0:t0 + n], writes=["xin%d" % i])
                        dest = big[:, dm, t0:t0 + n] if dm < 4 else xhi[:, dm - 4, t0:t0 + n]
                        STT(dest, pj[:, :n], mod[:, 16 + dm:17 + dm], xin[i][:, :n], ALU.mult, ALU.add,
                            [pk, "mod", "xin%d" % i], ["xdest%d" % dm])
                S.barrier()
                for dm in range(4, 8):
                    COPY("pool" if dm % 2 else "act", big[:, dm, :], xhi[:, dm - 4, :], ["xdest%d" % dm], ["x%d" % dm])
                S.barrier()

        def moe(l, lo, hi):
            halves = [(lo, (lo + hi) // 2), ((lo + hi) // 2, hi)]
            for (h0, h1) in halves:
                NH = h1 - h0
                with ExitStack() as PM:
                    hnh = sbt(PM, "hnh", [128, 8, NH], BF16)
                    gatesT = sbt(PM, "gatesT", [32, NH])
                    with ExitStack() as PN:
                        NS = {"sq": sbt(PN, "sq", [128, 8, 512], BF16), "stdt": sbt(PN, "stdt", [128, 512]),
                              "t1": sbt(PN, "t1", [128, 8, 512])}
                        hn32 = sbt(PN, "hn32", [128, 8, 512])
                        wr = sbt(PN, "wr", [128, 8, 32])
                        br = sbt(PN, "br", [1, 32])
                        lg = sbt(PN, "lg", [128, 32])
                        mx8 = sbt(PN, "mx8", [128, 8])
                        msk = sbt(PN, "msk", [128, 32])
                        ex = sbt(PN, "ex", [128, 32])
                        den = sbt(PN, "den", [128, 2])
                        DMA("sp", wr[:], w_router[l].rearrange("(k p) e -> p k e", p=128), writes=["wr"])
                        DMA("sp", br[:], b_router[l:l + 1, :], writes=["br"])
                        for (t0, n) in tiles(h0, h1):
                            norm_tile(NS, [big[:, k, t0:t0 + n] for k in range(8)], ["x%d" % k for k in range(8)], n, l, True,
                                      [hnh[:, k, t0 - h0:t0 - h0 + n] for k in range(8)], ["hnh%d" % k for k in range(8)],
                                      [hn32[:, k, :n] for k in range(8)], ["hn32_%d" % k for k in range(8)])
                            for s0 in range(0, n, 128):
                                m = min(128, n - s0)
                                pl = ps[6]
                                for k in range(8):
                                    MM(pl[:m, 0:32], hn32[:, k, s0:s0 + m], wr[:, k, :], start=(k == 0), stop=False,
                                       reads=["hn32_%d" % k, "wr"], writes=["ps6"])
                                MM(pl[:m, 0:32], ones_f[0:1, 0:m], br[0:1, :], start=False, stop=True, reads=["ones_f", "br"], writes=["ps6"])
                                COPY("dve", lg[:m, :], pl[:m, 0:32], ["ps6"], ["lg"])
                                S.op("dve", lambda e, m=m: e.max(out=mx8[:m, :], in_=lg[:m, :]), ["lg"], ["mx8"])
                                TS("dve", msk[:m, :], lg[:m, :], mx8[:m, 3:4], None, ALU.is_ge, reads=["lg", "mx8"], writes=["msk"])
                                TS("dve", den[:m, 0:1], mx8[:m, 0:1], -1.0, None, ALU.mult, reads=["mx8"], writes=["den0"])
                                ACT(ex[:m, :], lg[:m, :], AF.Exp, ["lg", "den0"], ["ex"], bias=den[:m, 0:1])
                                TT("dve", ex[:m, :], ex[:m, :], msk[:m, :], ALU.mult, ["ex", "msk"], ["ex"])
                                S.op("dve", lambda e, m=m: e.reduce_sum(out=den[:m, 1:2], in_=ex[:m, :], axis=AX.X), ["ex"], ["den1"])
                                S.op("dve", lambda e, m=m: e.reciprocal(out=den[:m, 1:2], in_=den[:m, 1:2]), ["den1"], ["den1"])
                                TS("dve", ex[:m, :], ex[:m, :], den[:m, 1:2], None, ALU.mult, reads=["ex", "den1"], writes=["ex"])
                                TR(ps[6][0:32, 128:128 + m], ex[:m, :], ident_f[:m, :m], ["ex", "ident_f"], ["ps6"])
                                c0 = t0 - h0 + s0
                                COPY("act", gatesT[:, c0:c0 + m], ps[6][0:32, 128:128 + m], ["ps6"], ["gatesT"])
                        S.barrier()
                    if "gatesT" in dbg and h0 == lo and l == 0:
                        dump("gatesT", gatesT[:], [32, NH], "gatesT")
                    with ExitStack() as PE_:
                        act = sbt(PE_, "act", [128, 8, NH], BF16)
                        gbc = sbt(PE_, "gbc", [128, NH])
                        wg = [sbt(PE_, "wg%d" % i, [128, 8, 2, 512], BF16) for i in range(2)]
                        wd = [sbt(PE_, "wd%d" % i, [128, 8, 1024], BF16) for i in range(2)]
                        tt = [[sbt(PE_, "tt%d_%d" % (i, j), [128, 512]) for j in range(3)] for i in range(2)]
                        ytmp = [sbt(PE_, "ytmp%d" % i, [128, 512]) for i in range(2)]
                        bgu = sbt(PE_, "bgu", [128, 32 * 16])
                        bdn = sbt(PE_, "bdn", [32, 1024])
                        DMA("sp", bgu[:], b_guT_d[:, l * 512:(l + 1) * 512], writes=["bgu"])
                        bgu1 = sbt(PE_, "bgu1", [128, 32 * 16])
                        TS("dve", bgu1[:], bgu[:], 1.0, None, ALU.add, reads=["bgu"], writes=["bgu1"])
                        DMA("sp", bdn[:], b_down[l], writes=["bdn"])
                        cnt = {"g": 0, "t": 0, "y": 0, "d": 0}

                        def down_evac(py, pyk, dm, t0, n):
                            STT(big[:, dm, t0:t0 + n], py[:, :n], mod[:, l * 48 + 40 + dm:l * 48 + 41 + dm], big[:, dm, t0:t0 + n],
                                ALU.mult, ALU.add, [pyk, "mod", "x%d" % dm], ["x%d" % dm])

                        for dm in range(8):
                            for (t0, n) in tiles(h0, h1):
                                cnt["d"] ^= 1
                                py, pyk = ps[4 + cnt["d"]], "ps%d" % (4 + cnt["d"])
                                MM(py[:, :n], bdn[0:32, dm * 128:(dm + 1) * 128], gatesT[0:32, t0 - h0:t0 - h0 + n],
                                   reads=["bdn", "gatesT"], writes=[pyk])
                                down_evac(py, pyk, dm, t0, n)
                        def issue_wg(si):
                            e_, fq_ = si // 2, si % 2
                            for two in range(2):
                                DMA("pool", wg[si % 2][:, :, two, :],
                                    w_gu[l, e_, :, two * 1024 + fq_ * 512: two * 1024 + (fq_ + 1) * 512].rearrange("(k p) c -> p k c", p=128),
                                    writes=["wg%d" % (si % 2)])

                        def issue_wd(e_):
                            DMA("pool", wd[e_ % 2][:], w_down[l, e_].rearrange("(k p) c -> p k c", p=128), writes=["wd%d" % (e_ % 2)])

                        issue_wg(0)
                        issue_wd(0)
                        for e in range(32):
                            for (t0, n) in tiles(0, NH):
                                MM(ps[6][:, :n], ident_f[0:32, e:e + 1].broadcast_to([32, 128]), gatesT[0:32, t0:t0 + n],
                                   reads=["ident_f", "gatesT"], writes=["ps6"])
                                COPY("act", gbc[:, t0:t0 + n], ps[6][:, :n], ["ps6"], ["gbc"])
                            wdi = e % 2
                            if e + 1 < 32:
                                issue_wd(e + 1)
                            for fc in range(8):
                                si = e * 2 + fc // 4
                                fi = fc % 4
                                if fi == 0 and si + 1 < 64:
                                    issue_wg(si + 1)
                                wgi, wgk = wg[si % 2], "wg%d" % (si % 2)
                                bcol = e * 16 + fc
                                for (t0, n) in tiles(0, NH):
                                    pg, pgk = ps[0], "ps0"
                                    plin, plk = ps[1], "ps1"
                                    if (t0 // 512) % 2:
                                        pg, pgk, plin, plk = ps[2], "ps2", ps[3], "ps3"
                                    for k in range(8):
                                        MM(pg[:, :n], wgi[:, k, 0, fi * 128:(fi + 1) * 128], hnh[:, k, t0:t0 + n], start=(k == 0), stop=(k == 7),
                                           reads=[wgk, "hnh%d" % k], writes=[pgk])
                                    for k in range(8):
                                        MM(plin[:, :n], wgi[:, k, 1, fi * 128:(fi + 1) * 128], hnh[:, k, t0:t0 + n], start=(k == 0), stop=(k == 7),
                                           reads=[wgk, "hnh%d" % k], writes=[plk])
                                    cnt["t"] ^= 1
                                    t1, t2, t3 = tt[cnt["t"]]
                                    k1, k2, k3 = ["tt%d_%d" % (cnt["t"], j) for j in range(3)]
                                    TS("dve", t1[:, :n], pg[:, :n], bgu[:, bcol:bcol + 1], 7.0, ALU.add, ALU.min, reads=[pgk, "bgu"], writes=[k1])
                                    ACT(t2[:, :n], t1[:, :n], AF.Sigmoid, [k1], [k2], scale=1.702)
                                    TS("dve", t3[:, :n], plin[:, :n], bgu1[:, bcol + 8:bcol + 9], 8.0, ALU.add, ALU.min, reads=[plk, "bgu1"], writes=[k3])
                                    STT(t3[:, :n], t3[:, :n], -6.0, gbc[:, t0:t0 + n], ALU.max, ALU.mult, [k3, "gbc"], [k3])
                                    TT("pool", t1[:, :n], t1[:, :n], t2[:, :n], ALU.mult, [k1, k2], [k1])
                                    TT("pool", act[:, fc, t0:t0 + n], t1[:, :n], t3[:, :n], ALU.mult, [k1, k3], ["act%d" % fc])
                            for dm in range(8):
                                for (t0, n) in tiles(0, NH):
                                    cnt["d"] ^= 1
                                    py, pyk = ps[4 + cnt["d"]], "ps%d" % (4 + cnt["d"])
                                    for fc in range(8):
                                        MM(py[:, :n], wd[wdi][:, fc, dm * 128:(dm + 1) * 128], act[:, fc, t0:t0 + n],
                                           start=(fc == 0), stop=(fc == 7), reads=["wd%d" % wdi, "act%d" % fc], writes=[pyk])
                                    down_evac(py, pyk, dm, h0 + t0, n)
                        S.barrier()

        def pool_mixer():
            l = 1
            with ExitStack() as PP:
                NS = {"sq": sbt(PP, "sq", [128, 8, 512], BF16), "stdt": sbt(PP, "stdt", [128, 512])}
                rstd = sbt(PP, "rstd", [128, T])
                hb = sbt(PP, "hb", [128, T])
                s2 = sbt(PP, "s2", [128, T])
                s4 = sbt(PP, "s4", [128, T])
                pT = sbt(PP, "pT", [128, 8, T], BF16)
                pw = sbt(PP, "pw", [128, 4, 2, 256], BF16)
                invc = sbt(PP, "invc", [128, 4, 16])
                ptmp = [sbt(PP, "ptmp%d" % i, [128, 512]) for i in range(2)]
                DMA("sp", invc[:], invc_d[:, :].rearrange("p (g s) -> p g s", s=16), writes=["invc"])
                for g in range(4):
                    DMA("pool", pw[:, g, :, :], pool_w[g].rearrange("(i p) d -> p i d", p=128), writes=["pw"])
                for (t0, n) in tiles(0, T):
                    pj, pk = ps_pj()
                    for k in range(8):
                        ACT(NS["sq"][:, k, :n], big[:, k, t0:t0 + n], AF.Square, ["x%d" % k], ["sq%d" % k])
                    for k in range(8):
                        MM(pj[:, :n], ones_b[:], NS["sq"][:, k, :n], start=(k == 0), stop=(k == 7), reads=["sq%d" % k, "ones_b"], writes=[pk])
                    ACT(rstd[:, t0:t0 + n], pj[:, :n], AF.Sqrt, [pk, "eps_t"], ["rstd"], bias=eps_t[:, 0:1], scale=1.0 / 1024.0)
                S.op("dve", lambda e: e.reciprocal(out=rstd[:], in_=rstd[:]), ["rstd"], ["rstd"])
                for k in range(8):
                    g = k // 2
                    w = (2, 4, 8, 16)[g]
                    STT(hb[:], big[:, k, :], mod1[:, 48 + 8 + k:48 + 9 + k], rstd[:], ALU.mult, ALU.mult, ["x%d" % k, "rstd", "mod1"], ["hb"])
                    TS("dve", hb[:], hb[:], mod[:, 48 + k:48 + k + 1], None, ALU.add, reads=["hb", "mod"], writes=["hb"])
                    TS("dve", hb[:, 0:64], hb[:, 0:64], flagw[:, 0:1], None, ALU.mult, reads=["hb", "flag"], writes=["hb"])
                    TT("pool", s2[:, 1:T], hb[:, 1:T], hb[:, 0:T - 1], ALU.add, ["hb"], ["s2"])
                    ws, wsk = s2, "s2"
                    if w >= 4:
                        TT("pool", s4[:, 3:T], s2[:, 3:T], s2[:, 1:T - 2], ALU.add, ["s2"], ["s4"])
                        ws, wsk = s4, "s4"
                    if w >= 8:
                        TT("pool", s2[:, 7:T], s4[:, 7:T], s4[:, 3:T - 4], ALU.add, ["s4", "s2"], ["s2"])
                        ws, wsk = s2, "s2"
                    if w >= 16:
                        TT("pool", s4[:, 15:T], s2[:, 15:T], s2[:, 7:T - 8], ALU.add, ["s2", "s4"], ["s4"])
                        ws, wsk = s4, "s4"
                    STT(pT[:, k, 16:T], ws[:, 16:T], 1.0 / w, hb[:, 16:T], ALU.mult, ALU.subtract, [wsk, "hb"], ["pT%d" % k])
                    TT("dve", ws[:, 64:80], ws[:, 64:80], invc[:, g, :], ALU.mult, [wsk, "invc", "pT%d" % k], [wsk])
                    TT("dve", pT[:, k, 64:80], ws[:, 64:80], hb[:, 64:80], ALU.subtract, [wsk, "hb"], ["pT%d" % k])
                i = 0
                for k in range(8):
                    g, j = k // 2, k % 2
                    for (t0, n) in tiles(HALO, T):
                        pj, pk = ps_pj()
                        for ii in range(2):
                            MM(pj[:, :n], pw[:, g, ii, j * 128:(j + 1) * 128], pT[:, 2 * g + ii, t0:t0 + n], start=(ii == 0), stop=(ii == 1),
                               reads=["pw", "pT%d" % (2 * g + ii)], writes=[pk])
                        i ^= 1
                        TS("dve", ptmp[i][:, :n], pj[:, :n], small[:, 24 + k:25 + k], small[:, 16 + k:17 + k], ALU.add, ALU.mult,
                           reads=[pk, "small_pb", "small_pc"], writes=["ptmp%d" % i])
                        TT("pool", big[:, k, t0:t0 + n], big[:, k, t0:t0 + n], ptmp[i][:, :n], ALU.add, ["x%d" % k, "ptmp%d" % i], ["x%d" % k])
                S.barrier()

        def final_norm():
            with ExitStack() as PF:
                NS = {"sq": sbt(PF, "sq", [128, 8, 512], BF16), "stdt": sbt(PF, "stdt", [128, 512])}
                ob = [sbt(PF, "fo%d" % i, [128, 8, 512]) for i in range(2)]
                for ti, (t0, n) in enumerate(tiles(HALO, T)):
                    o = ob[ti % 2]
                    norm_tile(NS, [big[:, k, t0:t0 + n] for k in range(8)], ["x%d" % k for k in range(8)], n, 0, False,
                              None, None, [o[:, k, :n] for k in range(8)], ["fo%d_%d" % (ti % 2, k) for k in range(8)], final=True)
                    DMA("sp", yT[:, t0 - HALO:t0 - HALO + n].rearrange("(k p) t -> p k t", p=128), o[:, :, :n],
                        reads=["fo%d_%d" % (ti % 2, k) for k in range(8)], writes=["yT%d" % ti], is_out=True)

        if "s0" not in dbg and mode != "F2":
            mixer_norm()
        if mode in ("A", "F"):
            if "s0" not in dbg and "s1" not in dbg:
                try:
                    hg_heads(False)
                    if "s2" not in dbg:
                        ret_heads(False)
                except Stop:
                    pass
        if mode == "F2":
            with ExitStack() as PX:
                Lall_sb = sbt(PX, "Lall_sb", [128, ncores - 1, 8, 128])
                Dall_sb = sbt(PX, "Dall_sb", [128, ncores - 1, 8])
                for w in range(1, ncores):
                    W["w"], W["L"], W["D"] = w, Lall_sb[:, w - 1, :, :], Dall_sb[:, w - 1, :]
                    mixer_norm()
                    hg_heads(False)
                    ret_heads(False)
                W["w"], W["L"], W["D"] = 0, None, None
                chain_states(Lall_sb, Dall_sb)
            mixer_norm()
        if mode == "A":
            DMA("sp", Lst_o[:, :], Lst[:].rearrange("p h e -> p (h e)"), reads=["Lst"], writes=["Lst_o"], is_out=True)
            DMA("sp", Dst_o[:, :], Dst[:], reads=["Dst"], writes=["Dst_o"], is_out=True)
        else:
            for _once in ([1] if mode != "F2" else []):
              with ExitStack() as PX:
                Lall_sb = sbt(PX, "Lall_sb", [128, ncores, 8, 128])
                Dall_sb = sbt(PX, "Dall_sb", [128, ncores, 8])
                if mode == "B":
                    DMA("sp", Lall_sb[:], Lall_d.rearrange("c p (h e) -> p c h e", e=128), writes=["Lall"])
                    DMA("sp", Dall_sb[:], Dall_d.rearrange("c p h -> p c h"), writes=["Dall"])
                else:
                    bounce = nc.dram_tensor("st_bounce", [128, 1032], F32)
                    gath = nc.dram_tensor("st_gath", [ncores * 128, 1032], F32)
                    DMA("sp", bounce[:, 0:1024], Lst[:].rearrange("p h e -> p (h e)"), reads=["Lst"], writes=["bounce"])
                    DMA("sp", bounce[:, 1024:1032], Dst[:], reads=["Dst"], writes=["bounce"])
                    S.barrier()
                    S.op("pool", lambda e: e.collective_compute("AllGather", ALU.bypass, replica_groups=[list(range(ncores))],
                                                                ins=[bounce.ap().opt()], outs=[gath.ap().opt()]),
                         reads=["bounce"], writes=["gath"], dma_key="gath", dma_inc=1)
                    S.barrier()
                    gv = gath.ap().rearrange("(c p) w -> p c w", p=128)
                    DMA("sp", Lall_sb[:].rearrange("p c h e -> p c (h e)"), gv[:, :, 0:1024], reads=["gath"], writes=["Lall"])
                    DMA("sp", Dall_sb[:], gv[:, :, 1024:1032], reads=["gath"], writes=["Dall"])
                chain_states(Lall_sb, Dall_sb)
            dump("Sin", Sin[:].rearrange("p h e -> p (h e)"), [128, 1024], "Sin")
            hg_heads(True)
            ret_heads(True)
            if "cat" in dbg:
                cf = sbt(es, "catf", [128, 8, T])
                for h in range(8):
                    COPY("act", cf[:, h, :], catT[:, h, :], ["cat%d" % h], ["catf"])
                dump("cat", cf[:].rearrange("p h t -> p (h t)"), [128, 8 * T], "catf")
            wout_residual()
            dump("x1", big[:].rearrange("p k t -> p (k t)"), [128, 8 * T], "x0")
            if "stop1" not in dbg:
                moe(0, 0, T)
                dump("x2", big[:].rearrange("p k t -> p (k t)"), [128, 8 * T], "x0")
                pool_mixer()
                dump("x3", big[:].rearrange("p k t -> p (k t)"), [128, 8 * T], "x0")
                moe(1, HALO, T)
            final_norm()
        S.finish()
        S.emit()
    return nc, dbg_outs, in_names


def _consts(T, tok0):
    gam = [1.0 - 2.0 ** (-5 - h) for h in range(4)]
    idx = np.arange(64)
    rel = idx[None, :] - idx[:, None]
    masks = np.zeros((64, 5, 64), np.float64)
    masks[:, 0, :] = (rel >= 0)
    for h in range(4):
        masks[:, 1 + h, :] = np.where(rel >= 0, gam[h] ** np.maximum(rel, 0), 0.0)
    kscale = np.ones((64, 8), np.float64)
    for h in range(4):
        kscale[:, 4 + h] = gam[h] ** (63 - idx)
    gdec = np.zeros((128, 4, 64), np.float64)
    for h in range(4):
        gdec[:, h, :] = (gam[h] ** (idx + 1.0))[None, :]
    half = 64
    inv = (10000.0 ** (-np.arange(half, dtype=np.float32) / half)).astype(np.float32)
    pos = (tok0 + np.arange(T)).astype(np.float32)
    ang = (pos[None, :] * inv[:, None]).astype(np.float32)
    cos = np.cos(ang.astype(np.float64))
    sin = np.sin(ang.astype(np.float64))
    cosT = np.concatenate([cos, cos], 0)
    sinT = np.concatenate([-sin, sin], 0)
    scanm = np.ones((128, T), np.float32)
    scanm[:, ::64] = 0.0
    return dict(masks=masks.reshape(64, 320).astype(np.float32), kscale=kscale.astype(np.float32),
                gdec=gdec.reshape(128, 256).astype(np.float32), cosT=cosT.astype(np.float32),
                sinT=sinT.astype(np.float32), scanm=scanm, ident=np.eye(128, dtype=np.float32))


def _pk(v, ncol):
    return np.ascontiguousarray(np.asarray(v, np.float32).reshape(ncol, 128).T)


def make_inputs(inp, ncores, nw=8):
    x = np.asarray(inp["x"], np.float32)[0]
    Sq = x.shape[0]
    TP = Sq // ncores
    T = TP + HALO
    w_in = np.asarray(inp["w_in"], np.float32)[0]
    perm = np.concatenate([np.arange(64, 128), np.arange(0, 64)])
    cols = []
    for base in (2048, 2560):
        for r in range(4):
            cols.append(base + r * 128 + perm)
    w_in_sw = np.ascontiguousarray(w_in[:, np.concatenate(cols)])
    shared = dict(
        cT=_pk(np.asarray(inp["c"])[0], 8),
        w_ada=np.asarray(inp["w_ada"], np.float32),
        b_adaT=np.concatenate([_pk(np.asarray(inp["b_ada"])[l], 48) for l in range(2)], 1),
        w_in=w_in, w_in_sw=w_in_sw,
        lbraw=np.concatenate([_pk(np.asarray(inp["hg_lower_bounds"])[r], 4) for r in range(2)], 1),
        gains=np.concatenate([_pk(np.asarray(inp["hg_norm"])[0].reshape(-1), 4),
                              _pk(np.asarray(inp["ret_norm"])[0].reshape(-1), 4)], 1),
        w_out=np.asarray(inp["w_out"], np.float32)[0],
        pool_w=np.asarray(inp["pool_w"], np.float32)[0],
        pool_bT=_pk(np.asarray(inp["pool_b"])[0].reshape(-1), 8),
        pool_sT=_pk(np.asarray(inp["pool_scale"])[0], 8),
        w_router=np.asarray(inp["w_router"], np.float32),
        b_router=np.asarray(inp["b_router"], np.float32),
        w_gu=np.asarray(inp["w_gu"], np.float32),
        b_guT=_pk(np.asarray(inp["b_gu"]).reshape(-1), 2 * 32 * 16),
        w_down=np.asarray(inp["w_down"], np.float32),
        b_down=np.asarray(inp["b_down"], np.float32),
        fnT=_pk(np.asarray(inp["final_norm"]), 8),
    )
    win = {}
    for c in range(ncores):
        tok0 = c * TP - HALO
        xs = np.zeros((T, 1024), np.float32)
        lo = max(tok0, 0)
        xs[lo - tok0:, :] = x[lo:tok0 + T, :]
        cst = _consts(T, tok0)
        win[c] = (np.ascontiguousarray(xs.T), cst["cosT"], cst["sinT"])
    zero_w = (np.zeros((1024, T), np.float32), np.zeros((128, T), np.float32), np.zeros((128, T), np.float32))
    maps = []
    for j in range(ncores):
        m = dict(shared)
        cst = _consts(T, j * TP - HALO)
        for k in ("masks", "kscale", "gdec", "scanm", "ident"):
            m[k] = cst[k]
        ws = [win[j - w] if j - w >= 0 else zero_w for w in range(nw)]
        m["xT"] = np.stack([w_[0] for w_ in ws], 0)
        m["cosT"] = np.stack([w_[1] for w_ in ws], 0)
        m["sinT"] = np.stack([w_[2] for w_ in ws], 0)
        flag = np.ones((128, 8), np.float32)
        valid = np.zeros((128, 8), np.float32)
        for w in range(8):
            if j - w <= 0:
                flag[:, w] = 0.0
            if w >= 1 and j - w >= 0:
                valid[:, w - 1] = 1.0
        m["flag"] = flag
        m["valid"] = valid
        seli = np.zeros((128, 8), np.float32)
        seli[:, :j] = 1.0
        m["seli"] = seli
        invc = np.zeros((128, 4, 16), np.float32)
        for g, w in enumerate((2, 4, 8, 16)):
            tg = j * TP + np.arange(16)
            invc[:, g, :] = (1.0 / np.minimum(tg + 1, w))[None, :]
        m["invc"] = invc.reshape(128, 64)
        maps.append(m)
    return maps, TP


_CACHE = {}


def _get(TP, mode, ncores):
    key = (TP, mode, ncores)
    if key not in _CACHE:
        r = build(TP, mode, ncores)
        _CACHE[key] = (r[0], r[2])
    return _CACHE[key]


def kernel(**inp):
    ncores = 8
    maps, TP = make_inputs(inp, ncores, 8)
    ncF, names = _get(TP, "F2", ncores)
    r = run_bass_kernel_spmd(ncF, [{k: m[k] for k in names} for m in maps], core_ids=list(range(ncores)))
    y = np.concatenate([r.results[j]["yT"].T for j in range(ncores)], 0)
    return np.ascontiguousarray(y[None].astype(np.float32))
```

```python
from contextlib import ExitStack
import numpy as np
import concourse.bass as bass
import concourse.mybir as mybir
from concourse.bass_utils import run_bass_kernel_spmd

F32 = mybir.dt.float32
BF16 = mybir.dt.bfloat16
ALU = mybir.AluOpType
AF = mybir.ActivationFunctionType
AX = mybir.AxisListType
ENGS = ("pe", "act", "dve", "pool", "sp")
EPS = 1e-6
HALO = 64


class Sched:
    def __init__(self, nc, es):
        self.nc, self.es = nc, es
        self.items = {e: [] for e in ENGS}
        self.cnt = {e: 0 for e in ENGS}
        self.clock = {e: {} for e in ENGS}
        self.snap, self.key_w, self.key_r, self.dma_cnt, self.sems = {}, {}, {}, {}, {}
        self.out_events = []
        self.epoch = 0
        self.ek = {e: "E:" + e for e in ENGS}
        for e in ENGS:
            self._sem(self.ek[e])

    def _sem(self, k):
        if k not in self.sems:
            self.sems[k] = self.es.enter_context(self.nc.semaphore("s%d" % len(self.sems)))
        return self.sems[k]

    def _need(self, eng, ev, deps):
        if ev is None:
            return
        sk, val = ev
        if eng == "pe" and sk.startswith("E:pe"):
            return
        if self.clock[eng].get(sk, 0) >= val:
            return
        if deps.get(sk, 0) < val:
            deps[sk] = val

    def _apply(self, eng, deps):
        clk = self.clock[eng]
        for sk, val in deps.items():
            self.items[eng].append(("w", sk, val))
            for a, b in self.snap.get((sk, val), {}).items():
                if clk.get(a, 0) < b:
                    clk[a] = b
            if clk.get(sk, 0) < val:
                clk[sk] = val

    def op(self, eng, fn, reads=(), writes=(), dma_key=None, is_out=False, dma_inc=16):
        writes = [k.split("_")[0] if k.startswith("ps") else k for k in writes]
        writes += [k.split("_")[0] for k in reads if k.startswith("ps")]
        reads = [k for k in reads if not k.startswith("ps")]
        deps = {}
        for k in reads:
            self._need(eng, self.key_w.get(k), deps)
        for k in writes:
            self._need(eng, self.key_w.get(k), deps)
            for ev in self.key_r.get(k, ()):
                self._need(eng, ev, deps)
        self._apply(eng, deps)
        if dma_key is not None:
            sk = "D:" + str(dma_key)
            self._sem(sk)
            self.dma_cnt[sk] = self.dma_cnt.get(sk, 0) + 1
            ev = (sk, self._dval(sk, dma_inc))
            self.items[eng].append(("i", fn, sk, dma_inc))
            if is_out:
                self.out_events.append(ev)
        else:
            self.cnt[eng] += 1
            ev = (self.ek[eng], self.cnt[eng])
            self.items[eng].append(("i", fn, self.ek[eng], 1))
        self.snap[ev] = dict(self.clock[eng])
        for k in writes:
            self.key_w[k] = ev
            self.key_r[k] = []
        for k in reads:
            self.key_r.setdefault(k, []).append(ev)
        return ev

    def _dval(self, sk, inc):
        self.dma_val = getattr(self, "dma_val", {})
        self.dma_val[sk] = self.dma_val.get(sk, 0) + inc
        return self.dma_val[sk]

    def barrier(self):
        evs = [(self.ek[e], self.cnt[e]) for e in ENGS if self.cnt[e] > 0]
        evs += [(sk, v) for sk, v in getattr(self, "dma_val", {}).items()]
        for eng in ENGS:
            deps = {}
            for sk, val in evs:
                if self.clock[eng].get(sk, 0) < val:
                    deps[sk] = val
            self._apply(eng, deps)
        self.key_w, self.key_r = {}, {}
        self.epoch += 1
        for e in ENGS:
            if self.cnt[e] > 12000:
                self.ek[e] = "E:%s:%d" % (e, self.epoch)
                self._sem(self.ek[e])
                self.cnt[e] = 0

    def finish(self):
        deps = {}
        for ev in self.out_events:
            self._need("sp", ev, deps)
        self._apply("sp", deps)

    def emit(self):
        nc = self.nc
        with nc.Block() as block:
            def run(name):
                def body(engine):
                    for it in self.items[name]:
                        if it[0] == "w":
                            engine.wait_ge(self.sems[it[1]], it[2])
                        else:
                            it[1](engine).then_inc(self.sems[it[2]], it[3])
                return body
            block.tensor(run("pe"))
            block.scalar(run("act"))
            block.vector(run("dve"))
            block.gpsimd(run("pool"))
            block.sync(run("sp"))


class Stop(Exception):
    pass


def tiles(lo, hi, step=512):
    return [(t, min(step, hi - t)) for t in range(lo, hi, step)]


def build(TP, mode, ncores=8, dbg=()):
    T = TP + HALO
    NCH = T // 64
    nc = bass.Bass("TRN2", target_bir_lowering=False)

    in_names = []

    def din(name, shape):
        in_names.append(name)
        return nc.dram_tensor(name, list(shape), F32, kind="ExternalInput").ap()

    def dout(name, shape):
        return nc.dram_tensor(name, list(shape), F32, kind="ExternalOutput").ap()

    NW = 8 if mode == "F2" else 1
    xTw = din("xT", [NW, 1024, T])
    flag_d = din("flag", [128, 8])
    valid_d = din("valid", [128, 8])
    cT_d = din("cT", [128, 8])
    w_ada = din("w_ada", [2, 1024, 6144])
    badaT_d = din("b_adaT", [128, 96])
    w_in = din("w_in", [1024, 4096])
    w_in_sw = din("w_in_sw", [1024, 1024])
    lbraw_d = din("lbraw", [128, 8])
    gains_d = din("gains", [128, 8])
    w_out = din("w_out", [1024, 1024]) if mode != "A" else None
    pool_w = din("pool_w", [4, 256, 256]) if mode != "A" else None
    pool_bT_d = din("pool_bT", [128, 8])
    pool_sT_d = din("pool_sT", [128, 8])
    w_router = din("w_router", [2, 1024, 32]) if mode != "A" else None
    b_router = din("b_router", [2, 32]) if mode != "A" else None
    w_gu = din("w_gu", [2, 32, 1024, 2048]) if mode != "A" else None
    b_guT_d = din("b_guT", [128, 2 * 32 * 16]) if mode != "A" else None
    w_down = din("w_down", [2, 32, 1024, 1024]) if mode != "A" else None
    b_down = din("b_down", [2, 32, 1024]) if mode != "A" else None
    fnT_d = din("fnT", [128, 8])
    ident_d = din("ident", [128, 128])
    masks_d = din("masks", [64, 5 * 64])
    kscale_d = din("kscale", [64, 8])
    cos_d = din("cosT", [NW, 128, T])
    sin_d = din("sinT", [NW, 128, T])
    gdec_d = din("gdec", [128, 4 * 64])
    scanm_d = din("scanm", [128, T])
    invc_d = din("invc", [128, 4 * 16])
    seli_d = din("seli", [128, 8])
    retW_d = din("retW", [128, 4 * NCH])
    if mode == "A":
        Lst_o = dout("Lst", [128, 8 * 128])
        Dst_o = dout("Dst", [128, 8])
    if mode == "B":
        Lall_d = din("Lall", [ncores, 128, 8 * 128])
        Dall_d = din("Dall", [ncores, 128, 8])
    if mode in ("B", "F", "F2"):
        yT = dout("yT", [1024, TP])
    dbg_outs = {}

    gam = [1.0 - 2.0 ** (-5 - h) for h in range(4)]

    with ExitStack() as es:
        S = Sched(nc, es)

        uniq = [0]

        def sbt(stack, name, shape, dt=F32):
            uniq[0] += 1
            return stack.enter_context(nc.sbuf_tensor("sb%d_%s" % (uniq[0], name), list(shape), dt))

        def DMA(eng, out, in_, reads=(), writes=(), key=None, is_out=False):
            S.op(eng, lambda e: e.dma_start(out=out, in_=in_), reads, writes, dma_key=key or writes[0], is_out=is_out)

        def MM(out, lhsT, rhs, start=True, stop=True, reads=(), writes=()):
            S.op("pe", lambda e: e.matmul(out, lhsT, rhs, start=start, stop=stop), reads, writes)

        def TR(out, in_, ident, reads=(), writes=()):
            S.op("pe", lambda e: e.transpose(out, in_, ident), reads, writes)

        def ACT(out, in_, func, reads=(), writes=(), bias=None, scale=None):
            kw = {}
            if bias is not None:
                kw["bias"] = bias
            if scale is not None:
                kw["scale"] = scale
            S.op("act", lambda e: e.activation(out=out, in_=in_, func=func, **kw), reads, writes)

        def TT(eng, out, in0, in1, op, reads=(), writes=()):
            S.op(eng, lambda e: e.tensor_tensor(out=out, in0=in0, in1=in1, op=op), reads, writes)

        def TS(eng, out, in0, s1, s2, op0, op1=None, reads=(), writes=()):
            if op1 is None:
                S.op(eng, lambda e: e.tensor_scalar(out=out, in0=in0, scalar1=s1, scalar2=None, op0=op0), reads, writes)
            else:
                S.op(eng, lambda e: e.tensor_scalar(out=out, in0=in0, scalar1=s1, scalar2=s2, op0=op0, op1=op1), reads, writes)

        def STT(out, in0, scalar, in1, op0, op1, reads=(), writes=()):
            S.op("dve", lambda e: e.scalar_tensor_tensor(out=out, in0=in0, scalar=scalar, in1=in1, op0=op0, op1=op1),
                 reads, writes)

        def MEMSET(eng, ap, val, writes=()):
            S.op(eng, lambda e: e.memset(ap, val), (), writes)

        def COPY(eng, out, in_, reads=(), writes=()):
            if eng == "act":
                ACT(out, in_, AF.Identity, reads, writes)
            else:
                S.op(eng, lambda e: e.tensor_copy(out=out, in_=in_), reads, writes)

        def dump(name, ap, shape, key):
            if name in dbg:
                d = dout("dbg_" + name, shape)
                dbg_outs[name] = d
                DMA("sp", d, ap, reads=[key], writes=["dbg_" + name], is_out=True)

        G = es
        big = sbt(G, "big", [128, 8, T])
        bigb = big[:].rearrange("p k t -> p (k t)").bitcast(BF16).rearrange("p (k t) -> p k t", k=16)
        hnT = bigb[:, 0:8, :]
        catT = bigb[:, 8:16, :]
        ps = [G.enter_context(nc.psum_tensor("ps%d" % i, [128, 512], F32)) for i in range(7)]
        psb = G.enter_context(nc.psum_tensor("psb", [128, 1024], BF16))
        ident_f = sbt(G, "ident_f", [128, 128])
        ident_b = sbt(G, "ident_b", [128, 128], BF16)
        ones_b = sbt(G, "ones_b", [128, 128], BF16)
        ones_f = sbt(G, "ones_f", [128, 128])
        eps_t = sbt(G, "eps_t", [128, 1])
        flagw = sbt(G, "flag_s", [128, 8])
        W = {"w": 0, "L": None, "D": None}
        mod = sbt(G, "mod", [128, 96])
        mod1 = sbt(G, "mod1", [128, 96])
        small = sbt(G, "small", [128, 64])
        Lst = sbt(G, "Lst_s", [128, 8, 128])
        Dst = sbt(G, "Dst_s", [128, 8])
        Sin = sbt(G, "Sin_s", [128, 8, 128])
        seli = sbt(G, "seli_s", [128, 8])

        DMA("sp", ident_f[:], ident_d[:, :], writes=["ident_f"])
        DMA("pool", ident_b[:], ident_d[:, :], writes=["ident_b"])
        DMA("sp", flagw[:], flag_d[:, :], writes=["flag"])
        DMA("sp", seli[:], (valid_d if mode == "F2" else seli_d)[:, :], writes=["seli"])
        MEMSET("pool", ones_b[:], 1.0, ["ones_b"])
        MEMSET("pool", ones_f[:], 1.0, ["ones_f"])
        MEMSET("pool", eps_t[:], EPS, ["eps_t"])
        DMA("sp", small[:, 40:48], lbraw_d[:, :], writes=["small_lbraw"])
        DMA("sp", small[:, 8:16], gains_d[:, :], writes=["small_g"])
        DMA("sp", small[:, 16:24], pool_sT_d[:, :], writes=["small_ps"])
        DMA("sp", small[:, 24:32], pool_bT_d[:, :], writes=["small_pb"])
        DMA("sp", small[:, 32:40], fnT_d[:, :], writes=["small_fn"])
        TT("dve", small[:, 48:52], small[:, 40:44], small[:, 44:48], ALU.subtract, ["small_lbraw"], ["small_t"])
        ACT(small[:, 0:4], small[:, 48:52], AF.Sigmoid, ["small_t"], ["small_lb"])
        TS("dve", small[:, 4:8], small[:, 0:4], -1.0, 1.0, ALU.mult, ALU.add, ["small_lb"], ["small_oml"])

        with ExitStack() as P0:
            cT = sbt(P0, "cT_s", [128, 8])
            cond = sbt(P0, "cond", [128, 8])
            badaT = sbt(P0, "badaT", [128, 96])
            wa = [sbt(P0, "wa%d" % i, [128, 8, 768]) for i in range(2)]
            DMA("sp", cT[:], cT_d[:, :], writes=["cT"])
            DMA("sp", badaT[:], badaT_d[:, :], writes=["badaT"])
            ACT(cond[:], cT[:], AF.Silu, ["cT"], ["cond"])
            for l in range(2):
                for blk in range(8):
                    i = (l * 8 + blk) % 2
                    DMA("sp", wa[i][:], w_ada[l, :, blk * 768:(blk + 1) * 768].rearrange("(k p) c -> p k c", p=128),
                        writes=["wa%d" % i])
                    for m in range(6):
                        col = l * 48 + blk * 6 + m
                        for k in range(8):
                            MM(ps[0][:, col:col + 1], wa[i][:, k, m * 128:(m + 1) * 128], cond[:, k:k + 1],
                               start=(k == 0), stop=(k == 7), reads=["wa%d" % i, "cond"], writes=["ps0"])
            TT("dve", mod[:], ps[0][:, 0:96], badaT[:], ALU.add, ["ps0", "badaT"], ["mod"])
            TS("dve", mod1[:], mod[:], 1.0, None, ALU.add, reads=["mod"], writes=["mod1"])
            TT("dve", small[:, 16:24], small[:, 16:24], mod[:, 48 + 16:48 + 24], ALU.mult, ["small_ps", "mod"], ["small_pc"])
            S.barrier()
        dump("mod", mod[:], [128, 96], "mod")

        rot = {"pj": 0, "at": 0, "o": 0, "ds": 0}

        def ps_pj():
            rot["pj"] ^= 1
            return ps[rot["pj"]], "ps%d" % rot["pj"]

        def norm_tile(NS, xk, xkeys, n, l, ffn, out_bf, out_bf_keys, out_f32=None, out_f32_keys=None, final=False):
            base = l * 48 + (24 if ffn else 0)
            pj, pk = ps_pj()
            for k in range(8):
                ACT(NS["sq"][:, k, :n], xk[k], AF.Square, [xkeys[k]], ["sq%d" % k])
            for k in range(8):
                MM(pj[:, :n], ones_b[:], NS["sq"][:, k, :n], start=(k == 0), stop=(k == 7),
                   reads=["sq%d" % k, "ones_b"], writes=[pk])
            ACT(NS["stdt"][:, :n], pj[:, :n], AF.Sqrt, [pk, "eps_t"], ["stdt"], bias=eps_t[:, 0:1], scale=1.0 / 1024.0)
            S.op("dve", lambda e: e.reciprocal(out=NS["stdt"][:, :n], in_=NS["stdt"][:, :n]), ["stdt"], ["stdt"])
            for k in range(8):
                if final:
                    STT(out_f32[k], xk[k], small[:, 32 + k:33 + k], NS["stdt"][:, :n], ALU.mult, ALU.mult,
                        [xkeys[k], "stdt", "small_fn"], [out_f32_keys[k]])
                    continue
                STT(NS["t1"][:, k, :n], xk[k], mod1[:, base + 8 + k:base + 9 + k], NS["stdt"][:, :n], ALU.mult, ALU.mult,
                    [xkeys[k], "stdt", "mod1"], ["t1_%d" % k])
                if out_f32 is not None:
                    ACT(out_f32[k], NS["t1"][:, k, :n], AF.Identity, ["t1_%d" % k, "mod"], [out_f32_keys[k]],
                        bias=mod[:, base + k:base + k + 1])
                    COPY("pool", out_bf[k], out_f32[k], [out_f32_keys[k]], [out_bf_keys[k]])
                else:
                    ACT(out_bf[k], NS["t1"][:, k, :n], AF.Identity, ["t1_%d" % k, "mod"], [out_bf_keys[k]],
                        bias=mod[:, base + k:base + k + 1])

        def mixer_norm():
            with ExitStack() as PN:
                NS = {"sq": sbt(PN, "sq", [128, 8, 512], BF16), "stdt": sbt(PN, "stdt", [128, 512]),
                      "t1": sbt(PN, "t1", [128, 8, 512])}
                xt = [sbt(PN, "xt%d" % i, [128, 8, 512]) for i in range(2)]
                for ti, (t0, n) in enumerate(tiles(0, T)):
                    b = xt[ti % 2]
                    DMA("sp", b[:, :, :n], xTw[W["w"], :, t0:t0 + n].rearrange("(k p) t -> p k t", p=128), writes=["xt%d" % (ti % 2)])
                    norm_tile(NS, [b[:, k, :n] for k in range(8)], ["xt%d" % (ti % 2)] * 8, n, 0, False,
                              [hnT[:, k, t0:t0 + n] for k in range(8)], ["hn%d" % k for k in range(8)])
                S.barrier()

        def proj_fm(wh, qi, t0, n):
            pj, pk = ps_pj()
            for k in range(8):
                MM(pj[:, :n], wh[:, k, qi, :], hnT[:, k, t0:t0 + n], start=(k == 0), stop=(k == 7),
                   reads=["wh", "hn%d" % k], writes=[pk])
            return pj, pk

        def proj_v(wh, qi, vtok):
            for c0 in range(0, NCH, 4):
                pj, pk = ps_pj()
                ncs = min(4, NCH - c0)
                for ci in range(ncs):
                    c = c0 + ci
                    for k in range(8):
                        MM(pj[0:64, ci * 128:(ci + 1) * 128], hnT[:, k, c * 64:(c + 1) * 64], wh[:, k, qi, :],
                           start=(k == 0), stop=(k == 7), reads=["wh", "hn%d" % k], writes=[pk])
                ACT(vtok[:, c0:c0 + ncs, :], pj[0:64, 0:ncs * 128].rearrange("p (c e) -> p c e", e=128), AF.Identity,
                    [pk], ["vtok"])

        def k_transposes(kA, ktok, ksc):
            for c0 in range(0, NCH, 8):
                ncs = min(8, NCH - c0)
                for ci in range(ncs):
                    c = c0 + ci
                    TR(psb[0:64, ci * 128:(ci + 1) * 128], kA[:, c * 64:(c + 1) * 64], ident_b[:], ["kA", "ident_b"], ["psb"])
                TS("dve", ktok[:, c0:c0 + ncs, :], psb[0:64, 0:ncs * 128].rearrange("p (c e) -> p c e", e=128),
                   ksc, None, ALU.mult, reads=["psb", "kscale"], writes=["ktok"])

        def recurrence(h, HS, full, mask_ap):
            St, Sb = HS["S"], HS["Sb"]
            if not full:
                for c in range(NCH - 1):
                    MM(ps[6][:, 0:128], HS["ktok"][:, c, :], HS["vtok"][:, c, :], start=(c == 0), stop=(c == NCH - 2),
                       reads=["ktok", "vtok"], writes=["ps6"])
                COPY("act", (W["L"][:, h, :] if W["L"] is not None else Lst[:, h, :]), ps[6][:, 0:128], ["ps6"], ["Lst"])
                return
            if full:
                COPY("pool", St[:], Sin[:, h, :], ["Sin"], ["S"])
            else:
                MEMSET("pool", St[:], 0.0, ["S"])
            nch = NCH if full else NCH - 1
            ob = None
            for c in range(nch):
                cs = slice(c * 64, (c + 1) * 64)
                if full:
                    rot["at"] ^= 1
                    pa, pak = ps[2 + rot["at"]], "ps%d" % (2 + rot["at"])
                    MM(pa[0:64, 0:64], HS["kA"][:, cs], HS["qA"][:, cs], reads=["kA", "qA"], writes=[pak])
                    atm = HS["atm"][rot["at"]]
                    TT("dve", atm[:], pa[0:64, 0:64], mask_ap, ALU.mult, [pak, "masks"], ["atm%d" % rot["at"]])
                    TS("pool", Sb[:], St[:], HS["tabM"][:, c:c + 1], None, ALU.mult, reads=["S", "tabM"], writes=["Sb"])
                    if c % 8 == 0:
                        rot["o"] ^= 1
                        ob, obk = ps[4 + rot["o"]], "ps%d" % (4 + rot["o"])
                    oc = (c % 8) * 64
                    MM(ob[:, oc:oc + 64], HS["vtok"][:, c, :], atm[:], start=True, stop=False,
                       reads=["vtok", "atm%d" % rot["at"]], writes=[obk])
                    MM(ob[:, oc:oc + 64], Sb[:], HS["qB"][:, cs], start=False, stop=True, reads=["Sb", "qB"], writes=[obk])
                    if c % 8 == 7 or c == nch - 1:
                        c0 = (c // 8) * 8
                        headnorm(h, HS, ob, obk, c0 * 64, (c + 1 - c0) * 64)
                if c < NCH - 1:
                    rot["ds"] = (rot["ds"] + 1) % 4
                    pd = ps[6][:, rot["ds"] * 128:(rot["ds"] + 1) * 128]
                    pdk = "ps6_%d" % rot["ds"]
                    MM(pd, HS["ktok"][:, c, :], HS["vtok"][:, c, :], reads=["ktok", "vtok"], writes=[pdk])
                    ACT(HS["dst"][:], pd, AF.Identity, [pdk, "tabC"], ["dst"], scale=HS["tabC"][:, c:c + 1])
                    STT(St[:], St[:], HS["tabA"][:, c:c + 1], HS["dst"][:], ALU.mult, ALU.add, ["S", "tabA", "dst"], ["S"])
            if not full:
                COPY("pool", (W["L"][:, h, :] if W["L"] is not None else Lst[:, h, :]), St[:], ["S"], ["Lst"])

        def headnorm(h, HS, ob, obk, t0, n):
            ACT(HS["osq"][:, :n], ob[:, :n], AF.Square, [obk], ["osq"])
            pj, pk = ps_pj()
            MM(pj[:, :n], ones_b[:], HS["osq"][:, :n], reads=["osq", "ones_b"], writes=[pk])
            ACT(HS["ostd"][:, :n], pj[:, :n], AF.Sqrt, [pk, "eps_t"], ["ostd"], bias=eps_t[:, 0:1], scale=1.0 / 128.0)
            S.op("dve", lambda e: e.reciprocal(out=HS["ostd"][:, :n], in_=HS["ostd"][:, :n]), ["ostd"], ["ostd"])
            STT(HS["ot"][:, :n], ob[:, :n], small[:, 8 + h:9 + h], HS["ostd"][:, :n], ALU.mult, ALU.mult,
                [obk, "ostd", "small_g"], ["ot"])
            TT("pool", catT[:, h, t0:t0 + n], HS["ot"][:, :n], HS["gate"][:, t0:t0 + n], ALU.mult, ["ot", "gate"], ["cat%d" % h])

        def head_common(PH, full):
            HS = {"kA": sbt(PH, "kA", [128, T], BF16), "ktok": sbt(PH, "ktok", [64, NCH, 128], BF16),
                  "vtok": sbt(PH, "vtok", [64, NCH, 128], BF16), "S": sbt(PH, "S", [128, 128]),
                  "Sb": sbt(PH, "Sb", [128, 128], BF16), "dst": sbt(PH, "dst", [128, 128]),
                  "tabA": sbt(PH, "tabA", [128, NCH]), "tabC": sbt(PH, "tabC", [128, NCH]), "tabM": sbt(PH, "tabM", [128, NCH]),
                  "tmp": [sbt(PH, "tmp%d" % i, [128, 512]) for i in range(3)]}
            if full:
                HS.update({"qA": sbt(PH, "qA", [128, T], BF16), "gate": sbt(PH, "gate", [128, T], BF16),
                           "atm": [sbt(PH, "atm%d" % i, [64, 64], BF16) for i in range(2)],
                           "osq": sbt(PH, "osq", [128, 512], BF16), "ostd": sbt(PH, "ostd", [128, 512]),
                           "ot": sbt(PH, "ot", [128, 512])})
            return HS

        def hg_heads(full):
            with ExitStack() as PH:
                HS = head_common(PH, full)
                if full:
                    HS["qB"] = HS["qA"]
                wh = sbt(PH, "wh", [128, 8, 4, 128], BF16)
                ff = sbt(PH, "ff", [128, T])
                bA = sbt(PH, "bA", [128, T])
                bB = sbt(PH, "bB", [128, T])
                scanm = sbt(PH, "scanm", [128, T])
                masks = sbt(PH, "masks", [64, 64])
                ksc = sbt(PH, "ksc", [64, 8])
                tmpd = sbt(PH, "tmpd", [128, NCH])
                pref = sbt(PH, "pref", [128, NCH])
                Wt = sbt(PH, "Wt", [128, NCH])
                onesN = sbt(PH, "onesN", [128, NCH])
                MEMSET("pool", onesN[:], 1.0, ["onesN"])
                sumbl = sbt(PH, "sumbl", [128, 1])
                DMA("sp", scanm[:], scanm_d[:, :], writes=["scanm"])
                DMA("sp", masks[:], masks_d[:, 0:64], writes=["masks"])
                DMA("sp", ksc[:], kscale_d[:, :], writes=["kscale"])
                b3 = bB[:].rearrange("p (c s) -> p c s", s=64)
                a3 = bA[:].rearrange("p (c s) -> p c s", s=64)
                for h in range(4):
                    for qi in range(4):
                        DMA("pool", wh[:, :, qi, :], w_in[:, qi * 512 + h * 128: qi * 512 + (h + 1) * 128]
                            .rearrange("(k p) c -> p k c", p=128), writes=["wh"])
                    for ti, (t0, n) in enumerate(tiles(0, T)):
                        pj, pk = proj_fm(wh, 1, t0, n)
                        tm = HS["tmp"][ti % 2]
                        ACT(tm[:, :n], pj[:, :n], AF.Sigmoid, [pk], ["tmp%d" % (ti % 2)])
                        TS("dve", ff[:, t0:t0 + n], tm[:, :n], small[:, 4 + h:5 + h], small[:, h:h + 1], ALU.mult, ALU.add,
                           reads=["tmp%d" % (ti % 2), "small_lb", "small_oml"], writes=["ff"])
                    if "h1" in dbg:
                        return
                    ACT(bA[:], ff[:], AF.Ln, ["ff"], ["bA"])
                    if "h1b" in dbg:
                        return
                    S.op("dve", lambda e: e.tensor_tensor_scan(out=bB[:], data0=scanm[:], data1=bA[:], initial=0.0,
                                                               op0=ALU.mult, op1=ALU.add), ["scanm", "bA"], ["bB"])
                    if "h1c" in dbg:
                        return
                    ACT(HS["tabA"][:], b3[:, :, 63], AF.Exp, ["bB"], ["tabA"])
                    TT("dve", tmpd[:], b3[:, :, 63], b3[:, :, 31], ALU.subtract, ["bB"], ["tmpd"])
                    ACT(HS["tabC"][:], tmpd[:], AF.Exp, ["tmpd"], ["tabC"])
                    ACT(HS["tabM"][:], b3[:, :, 31], AF.Exp, ["bB"], ["tabM"])
                    S.op("dve", lambda e: e.reduce_sum(out=sumbl[:], in_=b3[:, 0:NCH - 1, 63], axis=AX.X), ["bB"], ["sumbl"])
                    ACT((W["D"] if W["D"] is not None else Dst)[:, h:h + 1], sumbl[:], AF.Exp, ["sumbl"], ["Dst"])
                    TT("dve", HS["tabA"][:, 0:1], HS["tabA"][:, 0:1], flagw[:, W["w"]:W["w"] + 1], ALU.mult, ["tabA", "flag"], ["tabA"])
                    TT("dve", HS["tabC"][:, 0:1], HS["tabC"][:, 0:1], flagw[:, W["w"]:W["w"] + 1], ALU.mult, ["tabC", "flag"], ["tabC"])
                    if "h1d" in dbg:
                        return
                    if not full:
                        S.op("dve", lambda e: e.tensor_tensor_scan(out=pref[:], data0=onesN[:], data1=b3[:, :, 63], initial=0.0,
                                                                   op0=ALU.mult, op1=ALU.add), ["onesN", "bB"], ["pref"])
                        TT("dve", Wt[:, 0:NCH - 1], tmpd[:, 0:NCH - 1], pref[:, 0:NCH - 1], ALU.subtract, ["tmpd", "pref"], ["Wt"])
                        ACT(Wt[:, 0:NCH - 1], Wt[:, 0:NCH - 1], AF.Exp, ["Wt", "pref"], ["Wt"], bias=pref[:, NCH - 2:NCH - 1])
                        MEMSET("pool", Wt[:, NCH - 1:NCH], 0.0, ["Wt"])
                        TT("dve", Wt[:, 0:1], Wt[:, 0:1], flagw[:, W["w"]:W["w"] + 1], ALU.mult, ["Wt", "flag"], ["Wt"])
                    TT("dve", a3, b3, b3[:, :, 31:32].broadcast_to([128, NCH, 64]), ALU.subtract, ["bB", "bA"], ["bA"])
                    if "h1e" in dbg:
                        return
                    ACT(bB[:], bA[:], AF.Exp, ["bA"], ["bB"])
                    ACT(bA[:], bA[:], AF.Exp, ["bA"], ["bA"], scale=-1.0)
                    if not full:
                        TT("dve", a3, a3, Wt[:].unsqueeze(2).broadcast_to([128, NCH, 64]), ALU.mult, ["bA", "Wt"], ["bA"])
                    TS("pool", ff[:], ff[:], -1.0, 1.0, ALU.mult, ALU.add, reads=["ff"], writes=["ff"])
                    TT("dve", HS["kA"][:], ff[:], bA[:], ALU.mult, ["ff", "bA"], ["kA"])
                    if full:
                        for ti, (t0, n) in enumerate(tiles(0, T)):
                            pj, pk = proj_fm(wh, 0, t0, n)
                            tm = HS["tmp"][ti % 2]
                            ACT(tm[:, :n], pj[:, :n], AF.Silu, [pk], ["tmp%d" % (ti % 2)])
                            TT("dve", HS["qA"][:, t0:t0 + n], tm[:, :n], bB[:, t0:t0 + n], ALU.mult,
                               ["tmp%d" % (ti % 2), "bB"], ["qA"])
                            pj, pk = proj_fm(wh, 3, t0, n)
                            ACT(HS["gate"][:, t0:t0 + n], pj[:, :n], AF.Silu, [pk], ["gate"])
                    if "h2" in dbg:
                        return
                    proj_v(wh, 2, HS["vtok"])
                    if "h3" in dbg:
                        return
                    k_transposes(HS["kA"], HS["ktok"], ksc[:, h:h + 1])
                    if "h4" in dbg:
                        return
                    recurrence(h, HS, full, masks[:])
                    if "h5" in dbg:
                        return
                S.barrier()

        def ret_heads(full):
            with ExitStack() as PH:
                HS = head_common(PH, full)
                if full:
                    HS["qB"] = sbt(PH, "qB", [128, T], BF16)
                wh = sbt(PH, "wh6", [128, 8, 6, 128], BF16)
                cosT = sbt(PH, "cosT", [128, T])
                sinT = sbt(PH, "sinT", [128, T])
                gdec = sbt(PH, "gdec", [128, 4, 64])
                masks = sbt(PH, "dmasks", [64, 4, 64])
                ksc = sbt(PH, "ksc", [64, 8])
                Wr = sbt(PH, "Wr", [128, 4, NCH])
                DMA("sp", Wr[:], retW_d[:, :].rearrange("p (h c) -> p h c", c=NCH), writes=["Wr"])
                for r_ in range(4):
                    TT("dve", Wr[:, r_, 0:1], Wr[:, r_, 0:1], flagw[:, W["w"]:W["w"] + 1], ALU.mult, ["Wr", "flag"], ["Wr"])
                DMA("sp", cosT[:], cos_d[W["w"], :, :], writes=["cosT"])
                DMA("sp", sinT[:], sin_d[W["w"], :, :], writes=["sinT"])
                DMA("sp", gdec[:], gdec_d[:, :].rearrange("p (h s) -> p h s", s=64), writes=["gdec"])
                DMA("sp", masks[:], masks_d[:, 64:320].rearrange("p (h s) -> p h s", s=64), writes=["masks"])
                DMA("sp", ksc[:], kscale_d[:, :], writes=["kscale"])
                for r in range(4):
                    h = 4 + r
                    srcs = [w_in[:, 2048 + r * 128:2048 + (r + 1) * 128], w_in_sw[:, r * 128:(r + 1) * 128],
                            w_in[:, 2560 + r * 128:2560 + (r + 1) * 128], w_in_sw[:, 512 + r * 128:512 + (r + 1) * 128],
                            w_in[:, 3072 + r * 128:3072 + (r + 1) * 128], w_in[:, 3584 + r * 128:3584 + (r + 1) * 128]]
                    for qi in range(6):
                        DMA("pool", wh[:, :, qi, :], srcs[qi].rearrange("(k p) c -> p k c", p=128), writes=["wh"])
                    MEMSET("pool", HS["tabA"][:], gam[r] ** 64, ["tabA"])
                    MEMSET("pool", HS["tabC"][:], 1.0, ["tabC"])
                    MEMSET("pool", HS["tabM"][:], 1.0, ["tabM"])
                    MEMSET("pool", (W["D"] if W["D"] is not None else Dst)[:, h:h + 1], gam[r] ** (64 * (NCH - 1)), ["Dst"])
                    TT("dve", HS["tabA"][:, 0:1], HS["tabA"][:, 0:1], flagw[:, W["w"]:W["w"] + 1], ALU.mult, ["tabA", "flag"], ["tabA"])
                    TT("dve", HS["tabC"][:, 0:1], HS["tabC"][:, 0:1], flagw[:, W["w"]:W["w"] + 1], ALU.mult, ["tabC", "flag"], ["tabC"])
                    for which in ((0, 1) if full else (1,)):
                        for ti, (t0, n) in enumerate(tiles(0, T)):
                            pa, pak = proj_fm(wh, 2 * which, t0, n)
                            t1, t2 = HS["tmp"][0], HS["tmp"][1]
                            TT("dve", t1[:, :n], pa[:, :n], cosT[:, t0:t0 + n], ALU.mult, [pak, "cosT"], ["tmp0"])
                            pb, pbk = proj_fm(wh, 2 * which + 1, t0, n)
                            TT("dve", t2[:, :n], pb[:, :n], sinT[:, t0:t0 + n], ALU.mult, [pbk, "sinT"], ["tmp1"])
                            TT("pool", t1[:, :n], t1[:, :n], t2[:, :n], ALU.add, ["tmp0", "tmp1"], ["tmp0"])
                            if which == 0:
                                ACT(HS["qA"][:, t0:t0 + n], t1[:, :n], AF.Identity, ["tmp0"], ["qA"])
                                TT("dve", HS["qB"][:, t0:t0 + n].rearrange("p (c s) -> p c s", s=64),
                                   t1[:, :n].rearrange("p (c s) -> p c s", s=64),
                                   gdec[:, r, :].unsqueeze(1).broadcast_to([128, n // 64, 64]), ALU.mult,
                                   ["tmp0", "gdec"], ["qB"])
                            else:
                                ACT(HS["kA"][:, t0:t0 + n], t1[:, :n], AF.Identity, ["tmp0"], ["kA"], scale=128.0 ** -0.5)
                    if not full:
                        k3 = HS["kA"][:].rearrange("p (c s) -> p c s", s=64)
                        TT("dve", k3, k3, Wr[:, r, :].unsqueeze(2).broadcast_to([128, NCH, 64]), ALU.mult, ["kA", "Wr"], ["kA"])
                    if full:
                        for ti, (t0, n) in enumerate(tiles(0, T)):
                            pj, pk = proj_fm(wh, 5, t0, n)
                            ACT(HS["gate"][:, t0:t0 + n], pj[:, :n], AF.Silu, [pk], ["gate"])
                    proj_v(wh, 4, HS["vtok"])
                    k_transposes(HS["kA"], HS["ktok"], ksc[:, h:h + 1])
                    recurrence(h, HS, full, masks[:, r, :])
                S.barrier()

        def chain_states(Lall_sb, Dall_sb):
            with ExitStack() as PC:
                tmp = sbt(PC, "ctmp", [128, 128])
                MEMSET("pool", Sin[:], 0.0, ["Sin"])
                for i in (range(ncores - 2, -1, -1) if mode == "F2" else range(ncores - 1)):
                    for h in range(8):
                        STT(tmp[:], Sin[:, h, :], Dall_sb[:, i, h:h + 1], Lall_sb[:, i, h, :], ALU.mult, ALU.add,
                            ["Sin", "Lall", "Dall"], ["ctmp"])
                        TT("dve", tmp[:], tmp[:], Sin[:, h, :], ALU.subtract, ["ctmp", "Sin"], ["ctmp"])
                        STT(Sin[:, h, :], tmp[:], seli[:, i:i + 1], Sin[:, h, :], ALU.mult, ALU.add,
                            ["ctmp", "seli", "Sin"], ["Sin"])
                S.barrier()

        def wout_residual():
            with ExitStack() as PW:
                wo = sbt(PW, "wo", [128, 8, 1024], BF16)
                xhi = sbt(PW, "xhi", [128, 4, T])
                xin = [sbt(PW, "xin%d" % i, [128, 512]) for i in range(2)]
                DMA("pool", wo[:], w_out[:, :].rearrange("(k p) c -> p k c", p=128), writes=["wo"])
                i = 0
                for dm in range(8):
                    for (t0, n) in tiles(0, T):
                        pj, pk = ps_pj()
                        for h in range(8):
                            MM(pj[:, :n], wo[:, h, dm * 128:(dm + 1) * 128], catT[:, h, t0:t0 + n], start=(h == 0), stop=(h == 7),
                               reads=["wo", "cat%d" % h], writes=[pk])
                        i ^= 1
                        DMA("sp", xin[i][:, :n], xTw[0, dm * 128:(dm + 1) * 128, t0:t0 + n], writes=["xin%d" % i])
                        dest = big[:, dm, t0:t0 + n] if dm < 4 else xhi[:, dm - 4, t0:t0 + n]
                        STT(dest, pj[:, :n], mod[:, 16 + dm:17 + dm], xin[i][:, :n], ALU.mult, ALU.add,
                            [pk, "mod", "xin%d" % i], ["xdest%d" % dm])
                S.barrier()
                for dm in range(4, 8):
                    COPY("pool" if dm % 2 else "act", big[:, dm, :], xhi[:, dm - 4, :], ["xdest%d" % dm], ["x%d" % dm])
                S.barrier()

        def moe(l, lo, hi):
            halves = [(lo, (lo + hi) // 2), ((lo + hi) // 2, hi)]
            for (h0, h1) in halves:
                NH = h1 - h0
                with ExitStack() as PM:
                    hnh = sbt(PM, "hnh", [128, 8, NH], BF16)
                    gatesT = sbt(PM, "gatesT", [32, NH])
                    with ExitStack() as PN:
                        NS = {"sq": sbt(PN, "sq", [128, 8, 512], BF16), "stdt": sbt(PN, "stdt", [128, 512]),
                              "t1": sbt(PN, "t1", [128, 8, 512])}
                        hn32 = sbt(PN, "hn32", [128, 8, 512])
                        wr = sbt(PN, "wr", [128, 8, 32])
                        br = sbt(PN, "br", [1, 32])
                        lg = sbt(PN, "lg", [128, 32])
                        mx8 = sbt(PN, "mx8", [128, 8])
                        msk = sbt(PN, "msk", [128, 32])
                        ex = sbt(PN, "ex", [128, 32])
                        den = sbt(PN, "den", [128, 2])
                        DMA("sp", wr[:], w_router[l].rearrange("(k p) e -> p k e", p=128), writes=["wr"])
                        DMA("sp", br[:], b_router[l:l + 1, :], writes=["br"])
                        for (t0, n) in tiles(h0, h1):
                            norm_tile(NS, [big[:, k, t0:t0 + n] for k in range(8)], ["x%d" % k for k in range(8)], n, l, True,
                                      [hnh[:, k, t0 - h0:t0 - h0 + n] for k in range(8)], ["hnh%d" % k for k in range(8)],
                                      [hn32[:, k, :n] for k in range(8)], ["hn32_%d" % k for k in range(8)])
                            for s0 in range(0, n, 128):
                                m = min(128, n - s0)
                                pl = ps[6]
                                for k in range(8):
                                    MM(pl[:m, 0:32], hn32[:, k, s0:s0 + m], wr[:, k, :], start=(k == 0), stop=False,
                                       reads=["hn32_%d" % k, "wr"], writes=["ps6"])
                                MM(pl[:m, 0:32], ones_f[0:1, 0:m], br[0:1, :], start=False, stop=True, reads=["ones_f", "br"], writes=["ps6"])
                                COPY("dve", lg[:m, :], pl[:m, 0:32], ["ps6"], ["lg"])
                                S.op("dve", lambda e, m=m: e.max(out=mx8[:m, :], in_=lg[:m, :]), ["lg"], ["mx8"])
                                TS("dve", msk[:m, :], lg[:m, :], mx8[:m, 3:4], None, ALU.is_ge, reads=["lg", "mx8"], writes=["msk"])
                                TS("dve", den[:m, 0:1], mx8[:m, 0:1], -1.0, None, ALU.mult, reads=["mx8"], writes=["den0"])
                                ACT(ex[:m, :], lg[:m, :], AF.Exp, ["lg", "den0"], ["ex"], bias=den[:m, 0:1])
                                TT("dve", ex[:m, :], ex[:m, :], msk[:m, :], ALU.mult, ["ex", "msk"], ["ex"])
                                S.op("dve", lambda e, m=m: e.reduce_sum(out=den[:m, 1:2], in_=ex[:m, :], axis=AX.X), ["ex"], ["den1"])
                                S.op("dve", lambda e, m=m: e.reciprocal(out=den[:m, 1:2], in_=den[:m, 1:2]), ["den1"], ["den1"])
                                TS("dve", ex[:m, :], ex[:m, :], den[:m, 1:2], None, ALU.mult, reads=["ex", "den1"], writes=["ex"])
                                TR(ps[6][0:32, 128:128 + m], ex[:m, :], ident_f[:m, :m], ["ex", "ident_f"], ["ps6"])
                                c0 = t0 - h0 + s0
                                COPY("act", gatesT[:, c0:c0 + m], ps[6][0:32, 128:128 + m], ["ps6"], ["gatesT"])
                        S.barrier()
                    if "gatesT" in dbg and h0 == lo and l == 0:
                        dump("gatesT", gatesT[:], [32, NH], "gatesT")
                    with ExitStack() as PE_:
                        act = sbt(PE_, "act", [128, 8, NH], BF16)
                        gbc = sbt(PE_, "gbc", [128, NH])
                        wg = [sbt(PE_, "wg%d" % i, [128, 8, 2, 512], BF16) for i in range(2)]
                        wd = [sbt(PE_, "wd%d" % i, [128, 8, 1024], BF16) for i in range(2)]
                        tt = [[sbt(PE_, "tt%d_%d" % (i, j), [128, 512]) for j in range(3)] for i in range(2)]
                        ytmp = [sbt(PE_, "ytmp%d" % i, [128, 512]) for i in range(2)]
                        bgu = sbt(PE_, "bgu", [128, 32 * 16])
                        bdn = sbt(PE_, "bdn", [32, 1024])
                        DMA("sp", bgu[:], b_guT_d[:, l * 512:(l + 1) * 512], writes=["bgu"])
                        bgu1 = sbt(PE_, "bgu1", [128, 32 * 16])
                        TS("dve", bgu1[:], bgu[:], 1.0, None, ALU.add, reads=["bgu"], writes=["bgu1"])
                        DMA("sp", bdn[:], b_down[l], writes=["bdn"])
                        cnt = {"g": 0, "t": 0, "y": 0, "d": 0}

                        def down_evac(py, pyk, dm, t0, n):
                            STT(big[:, dm, t0:t0 + n], py[:, :n], mod[:, l * 48 + 40 + dm:l * 48 + 41 + dm], big[:, dm, t0:t0 + n],
                                ALU.mult, ALU.add, [pyk, "mod", "x%d" % dm], ["x%d" % dm])

                        for dm in range(8):
                            for (t0, n) in tiles(h0, h1):
                                cnt["d"] ^= 1
                                py, pyk = ps[4 + cnt["d"]], "ps%d" % (4 + cnt["d"])
                                MM(py[:, :n], bdn[0:32, dm * 128:(dm + 1) * 128], gatesT[0:32, t0 - h0:t0 - h0 + n],
                                   reads=["bdn", "gatesT"], writes=[pyk])
                                down_evac(py, pyk, dm, t0, n)
                        def issue_wg(si):
                            e_, fq_ = si // 2, si % 2
                            for two in range(2):
                                DMA("pool", wg[si % 2][:, :, two, :],
                                    w_gu[l, e_, :, two * 1024 + fq_ * 512: two * 1024 + (fq_ + 1) * 512].rearrange("(k p) c -> p k c", p=128),
                                    writes=["wg%d" % (si % 2)])

                        def issue_wd(e_):
                            DMA("pool", wd[e_ % 2][:], w_down[l, e_].rearrange("(k p) c -> p k c", p=128), writes=["wd%d" % (e_ % 2)])

                        issue_wg(0)
                        issue_wd(0)
                        for e in range(32):
                            for (t0, n) in tiles(0, NH):
                                MM(ps[6][:, :n], ident_f[0:32, e:e + 1].broadcast_to([32, 128]), gatesT[0:32, t0:t0 + n],
                                   reads=["ident_f", "gatesT"], writes=["ps6"])
                                COPY("act", gbc[:, t0:t0 + n], ps[6][:, :n], ["ps6"], ["gbc"])
                            wdi = e % 2
                            if e + 1 < 32:
                                issue_wd(e + 1)
                            for fc in range(8):
                                si = e * 2 + fc // 4
                                fi = fc % 4
                                if fi == 0 and si + 1 < 64:
                                    issue_wg(si + 1)
                                wgi, wgk = wg[si % 2], "wg%d" % (si % 2)
                                bcol = e * 16 + fc
                                for (t0, n) in tiles(0, NH):
                                    pg, pgk = ps[0], "ps0"
                                    plin, plk = ps[1], "ps1"
                                    if (t0 // 512) % 2:
                                        pg, pgk, plin, plk = ps[2], "ps2", ps[3], "ps3"
                                    for k in range(8):
                                        MM(pg[:, :n], wgi[:, k, 0, fi * 128:(fi + 1) * 128], hnh[:, k, t0:t0 + n], start=(k == 0), stop=(k == 7),
                                           reads=[wgk, "hnh%d" % k], writes=[pgk])
                                    for k in range(8):
                                        MM(plin[:, :n], wgi[:, k, 1, fi * 128:(fi + 1) * 128], hnh[:, k, t0:t0 + n], start=(k == 0), stop=(k == 7),
                                           reads=[wgk, "hnh%d" % k], writes=[plk])
                                    cnt["t"] ^= 1
                                    t1, t2, t3 = tt[cnt["t"]]
                                    k1, k2, k3 = ["tt%d_%d" % (cnt["t"], j) for j in range(3)]
                                    TS("dve", t1[:, :n], pg[:, :n], bgu[:, bcol:bcol + 1], 7.0, ALU.add, ALU.min, reads=[pgk, "bgu"], writes=[k1])
                                    ACT(t2[:, :n], t1[:, :n], AF.Sigmoid, [k1], [k2], scale=1.702)
                                    TS("dve", t3[:, :n], plin[:, :n], bgu1[:, bcol + 8:bcol + 9], 8.0, ALU.add, ALU.min, reads=[plk, "bgu1"], writes=[k3])
                                    STT(t3[:, :n], t3[:, :n], -6.0, gbc[:, t0:t0 + n], ALU.max, ALU.mult, [k3, "gbc"], [k3])
                                    TT("pool", t1[:, :n], t1[:, :n], t2[:, :n], ALU.mult, [k1, k2], [k1])
                                    TT("pool", act[:, fc, t0:t0 + n], t1[:, :n], t3[:, :n], ALU.mult, [k1, k3], ["act%d" % fc])
                            for dm in range(8):
                                for (t0, n) in tiles(0, NH):
                                    cnt["d"] ^= 1
                                    py, pyk = ps[4 + cnt["d"]], "ps%d" % (4 + cnt["d"])
                                    for fc in range(8):
                                        MM(py[:, :n], wd[wdi][:, fc, dm * 128:(dm + 1) * 128], act[:, fc, t0:t0 + n],
                                           start=(fc == 0), stop=(fc == 7), reads=["wd%d" % wdi, "act%d" % fc], writes=[pyk])
                                    down_evac(py, pyk, dm, h0 + t0, n)
                        S.barrier()

        def pool_mixer():
            l = 1
            with ExitStack() as PP:
                NS = {"sq": sbt(PP, "sq", [128, 8, 512], BF16), "stdt": sbt(PP, "stdt", [128, 512])}
                rstd = sbt(PP, "rstd", [128, T])
                hb = sbt(PP, "hb", [128, T])
                s2 = sbt(PP, "s2", [128, T])
                s4 = sbt(PP, "s4", [128, T])
                pT = sbt(PP, "pT", [128, 8, T], BF16)
                pw = sbt(PP, "pw", [128, 4, 2, 256], BF16)
                invc = sbt(PP, "invc", [128, 4, 16])
                ptmp = [sbt(PP, "ptmp%d" % i, [128, 512]) for i in range(2)]
                DMA("sp", invc[:], invc_d[:, :].rearrange("p (g s) -> p g s", s=16), writes=["invc"])
                for g in range(4):
                    DMA("pool", pw[:, g, :, :], pool_w[g].rearrange("(i p) d -> p i d", p=128), writes=["pw"])
                for (t0, n) in tiles(0, T):
                    pj, pk = ps_pj()
                    for k in range(8):
                        ACT(NS["sq"][:, k, :n], big[:, k, t0:t0 + n], AF.Square, ["x%d" % k], ["sq%d" % k])
                    for k in range(8):
                        MM(pj[:, :n], ones_b[:], NS["sq"][:, k, :n], start=(k == 0), stop=(k == 7), reads=["sq%d" % k, "ones_b"], writes=[pk])
                    ACT(rstd[:, t0:t0 + n], pj[:, :n], AF.Sqrt, [pk, "eps_t"], ["rstd"], bias=eps_t[:, 0:1], scale=1.0 / 1024.0)
                S.op("dve", lambda e: e.reciprocal(out=rstd[:], in_=rstd[:]), ["rstd"], ["rstd"])
                for k in range(8):
                    g = k // 2
                    w = (2, 4, 8, 16)[g]
                    STT(hb[:], big[:, k, :], mod1[:, 48 + 8 + k:48 + 9 + k], rstd[:], ALU.mult, ALU.mult, ["x%d" % k, "rstd", "mod1"], ["hb"])
                    TS("dve", hb[:], hb[:], mod[:, 48 + k:48 + k + 1], None, ALU.add, reads=["hb", "mod"], writes=["hb"])
                    TS("dve", hb[:, 0:64], hb[:, 0:64], flagw[:, 0:1], None, ALU.mult, reads=["hb", "flag"], writes=["hb"])
                    TT("pool", s2[:, 1:T], hb[:, 1:T], hb[:, 0:T - 1], ALU.add, ["hb"], ["s2"])
                    ws, wsk = s2, "s2"
                    if w >= 4:
                        TT("pool", s4[:, 3:T], s2[:, 3:T], s2[:, 1:T - 2], ALU.add, ["s2"], ["s4"])
                        ws, wsk = s4, "s4"
                    if w >= 8:
                        TT("pool", s2[:, 7:T], s4[:, 7:T], s4[:, 3:T - 4], ALU.add, ["s4", "s2"], ["s2"])
                        ws, wsk = s2, "s2"
                    if w >= 16:
                        TT("pool", s4[:, 15:T], s2[:, 15:T], s2[:, 7:T - 8], ALU.add, ["s2", "s4"], ["s4"])
                        ws, wsk = s4, "s4"
                    STT(pT[:, k, 16:T], ws[:, 16:T], 1.0 / w, hb[:, 16:T], ALU.mult, ALU.subtract, [wsk, "hb"], ["pT%d" % k])
                    TT("dve", ws[:, 64:80], ws[:, 64:80], invc[:, g, :], ALU.mult, [wsk, "invc", "pT%d" % k], [wsk])
                    TT("dve", pT[:, k, 64:80], ws[:, 64:80], hb[:, 64:80], ALU.subtract, [wsk, "hb"], ["pT%d" % k])
                i = 0
                for k in range(8):
                    g, j = k // 2, k % 2
                    for (t0, n) in tiles(HALO, T):
                        pj, pk = ps_pj()
                        for ii in range(2):
                            MM(pj[:, :n], pw[:, g, ii, j * 128:(j + 1) * 128], pT[:, 2 * g + ii, t0:t0 + n], start=(ii == 0), stop=(ii == 1),
                               reads=["pw", "pT%d" % (2 * g + ii)], writes=[pk])
                        i ^= 1
                        TS("dve", ptmp[i][:, :n], pj[:, :n], small[:, 24 + k:25 + k], small[:, 16 + k:17 + k], ALU.add, ALU.mult,
                           reads=[pk, "small_pb", "small_pc"], writes=["ptmp%d" % i])
                        TT("pool", big[:, k, t0:t0 + n], big[:, k, t0:t0 + n], ptmp[i][:, :n], ALU.add, ["x%d" % k, "ptmp%d" % i], ["x%d" % k])
                S.barrier()

        def final_norm():
            with ExitStack() as PF:
                NS = {"sq": sbt(PF, "sq", [128, 8, 512], BF16), "stdt": sbt(PF, "stdt", [128, 512])}
                ob = [sbt(PF, "fo%d" % i, [128, 8, 512]) for i in range(2)]
                for ti, (t0, n) in enumerate(tiles(HALO, T)):
                    o = ob[ti % 2]
                    norm_tile(NS, [big[:, k, t0:t0 + n] for k in range(8)], ["x%d" % k for k in range(8)], n, 0, False,
                              None, None, [o[:, k, :n] for k in range(8)], ["fo%d_%d" % (ti % 2, k) for k in range(8)], final=True)
                    DMA("sp", yT[:, t0 - HALO:t0 - HALO + n].rearrange("(k p) t -> p k t", p=128), o[:, :, :n],
                        reads=["fo%d_%d" % (ti % 2, k) for k in range(8)], writes=["yT%d" % ti], is_out=True)

        if "s0" not in dbg and mode != "F2":
            mixer_norm()
        if mode in ("A", "F"):
            if "s0" not in dbg and "s1" not in dbg:
                try:
                    hg_heads(False)
                    if "s2" not in dbg:
                        ret_heads(False)
                except Stop:
                    pass
        if mode == "F2":
            with ExitStack() as PX:
                Lall_sb = sbt(PX, "Lall_sb", [128, ncores - 1, 8, 128])
                Dall_sb = sbt(PX, "Dall_sb", [128, ncores - 1, 8])
                for w in range(1, ncores):
                    W["w"], W["L"], W["D"] = w, Lall_sb[:, w - 1, :, :], Dall_sb[:, w - 1, :]
                    mixer_norm()
                    hg_heads(False)
                    ret_heads(False)
                W["w"], W["L"], W["D"] = 0, None, None
                chain_states(Lall_sb, Dall_sb)
            mixer_norm()
        if mode == "A":
            DMA("sp", Lst_o[:, :], Lst[:].rearrange("p h e -> p (h e)"), reads=["Lst"], writes=["Lst_o"], is_out=True)
            DMA("sp", Dst_o[:, :], Dst[:], reads=["Dst"], writes=["Dst_o"], is_out=True)
        else:
            for _once in ([1] if mode != "F2" else []):
              with ExitStack() as PX:
                Lall_sb = sbt(PX, "Lall_sb", [128, ncores, 8, 128])
                Dall_sb = sbt(PX, "Dall_sb", [128, ncores, 8])
                if mode == "B":
                    DMA("sp", Lall_sb[:], Lall_d.rearrange("c p (h e) -> p c h e", e=128), writes=["Lall"])
                    DMA("sp", Dall_sb[:], Dall_d.rearrange("c p h -> p c h"), writes=["Dall"])
                else:
                    bounce = nc.dram_tensor("st_bounce", [128, 1032], F32)
                    gath = nc.dram_tensor("st_gath", [ncores * 128, 1032], F32)
                    DMA("sp", bounce[:, 0:1024], Lst[:].rearrange("p h e -> p (h e)"), reads=["Lst"], writes=["bounce"])
                    DMA("sp", bounce[:, 1024:1032], Dst[:], reads=["Dst"], writes=["bounce"])
                    S.barrier()
                    S.op("pool", lambda e: e.collective_compute("AllGather", ALU.bypass, replica_groups=[list(range(ncores))],
                                                                ins=[bounce.ap().opt()], outs=[gath.ap().opt()]),
                         reads=["bounce"], writes=["gath"], dma_key="gath", dma_inc=1)
                    S.barrier()
                    gv = gath.ap().rearrange("(c p) w -> p c w", p=128)
                    DMA("sp", Lall_sb[:].rearrange("p c h e -> p c (h e)"), gv[:, :, 0:1024], reads=["gath"], writes=["Lall"])
                    DMA("sp", Dall_sb[:], gv[:, :, 1024:1032], reads=["gath"], writes=["Dall"])
                chain_states(Lall_sb, Dall_sb)
            dump("Sin", Sin[:].rearrange("p h e -> p (h e)"), [128, 1024], "Sin")
            hg_heads(True)
            ret_heads(True)
            if "cat" in dbg:
                cf = sbt(es, "catf", [128, 8, T])
                for h in range(8):
                    COPY("act", cf[:, h, :], catT[:, h, :], ["cat%d" % h], ["catf"])
                dump("cat", cf[:].rearrange("p h t -> p (h t)"), [128, 8 * T], "catf")
            wout_residual()
            dump("x1", big[:].rearrange("p k t -> p (k t)"), [128, 8 * T], "x0")
            if "stop1" not in dbg:
                moe(0, 0, T)
                dump("x2", big[:].rearrange("p k t -> p (k t)"), [128, 8 * T], "x0")
                pool_mixer()
                dump("x3", big[:].rearrange("p k t -> p (k t)"), [128, 8 * T], "x0")
                moe(1, HALO, T)
            final_norm()
        S.finish()
        S.emit()
    return nc, dbg_outs, in_names


def _consts(T, tok0):
    gam = [1.0 - 2.0 ** (-5 - h) for h in range(4)]
    idx = np.arange(64)
    rel = idx[None, :] - idx[:, None]
    masks = np.zeros((64, 5, 64), np.float64)
    masks[:, 0, :] = (rel >= 0)
    for h in range(4):
        masks[:, 1 + h, :] = np.where(rel >= 0, gam[h] ** np.maximum(rel, 0), 0.0)
    kscale = np.ones((64, 8), np.float64)
    for h in range(4):
        kscale[:, 4 + h] = gam[h] ** (63 - idx)
    gdec = np.zeros((128, 4, 64), np.float64)
    for h in range(4):
        gdec[:, h, :] = (gam[h] ** (idx + 1.0))[None, :]
    half = 64
    inv = (10000.0 ** (-np.arange(half, dtype=np.float32) / half)).astype(np.float32)
    pos = (tok0 + np.arange(T)).astype(np.float32)
    ang = (pos[None, :] * inv[:, None]).astype(np.float32)
    cos = np.cos(ang.astype(np.float64))
    sin = np.sin(ang.astype(np.float64))
    cosT = np.concatenate([cos, cos], 0)
    sinT = np.concatenate([-sin, sin], 0)
    nch = T // 64
    retW = np.zeros((128, 4, nch), np.float64)
    for h in range(4):
        for c in range(nch - 1):
            retW[:, h, c] = gam[h] ** (64.0 * (nch - 2 - c))
    scanm = np.ones((128, T), np.float32)
    scanm[:, ::64] = 0.0
    return dict(masks=masks.reshape(64, 320).astype(np.float32), kscale=kscale.astype(np.float32),
                gdec=gdec.reshape(128, 256).astype(np.float32), cosT=cosT.astype(np.float32),
                sinT=sinT.astype(np.float32), scanm=scanm, ident=np.eye(128, dtype=np.float32),
                retW=retW.reshape(128, 4 * nch).astype(np.float32))


def _pk(v, ncol):
    return np.ascontiguousarray(np.asarray(v, np.float32).reshape(ncol, 128).T)


def make_inputs(inp, ncores, nw=8):
    x = np.asarray(inp["x"], np.float32)[0]
    Sq = x.shape[0]
    TP = Sq // ncores
    T = TP + HALO
    w_in = np.asarray(inp["w_in"], np.float32)[0]
    perm = np.concatenate([np.arange(64, 128), np.arange(0, 64)])
    cols = []
    for base in (2048, 2560):
        for r in range(4):
            cols.append(base + r * 128 + perm)
    w_in_sw = np.ascontiguousarray(w_in[:, np.concatenate(cols)])
    shared = dict(
        cT=_pk(np.asarray(inp["c"])[0], 8),
        w_ada=np.asarray(inp["w_ada"], np.float32),
        b_adaT=np.concatenate([_pk(np.asarray(inp["b_ada"])[l], 48) for l in range(2)], 1),
        w_in=w_in, w_in_sw=w_in_sw,
        lbraw=np.concatenate([_pk(np.asarray(inp["hg_lower_bounds"])[r], 4) for r in range(2)], 1),
        gains=np.concatenate([_pk(np.asarray(inp["hg_norm"])[0].reshape(-1), 4),
                              _pk(np.asarray(inp["ret_norm"])[0].reshape(-1), 4)], 1),
        w_out=np.asarray(inp["w_out"], np.float32)[0],
        pool_w=np.asarray(inp["pool_w"], np.float32)[0],
        pool_bT=_pk(np.asarray(inp["pool_b"])[0].reshape(-1), 8),
        pool_sT=_pk(np.asarray(inp["pool_scale"])[0], 8),
        w_router=np.asarray(inp["w_router"], np.float32),
        b_router=np.asarray(inp["b_router"], np.float32),
        w_gu=np.asarray(inp["w_gu"], np.float32),
        b_guT=_pk(np.asarray(inp["b_gu"]).reshape(-1), 2 * 32 * 16),
        w_down=np.asarray(inp["w_down"], np.float32),
        b_down=np.asarray(inp["b_down"], np.float32),
        fnT=_pk(np.asarray(inp["final_norm"]), 8),
    )
    win = {}
    for c in range(ncores):
        tok0 = c * TP - HALO
        xs = np.zeros((T, 1024), np.float32)
        lo = max(tok0, 0)
        xs[lo - tok0:, :] = x[lo:tok0 + T, :]
        cst = _consts(T, tok0)
        win[c] = (np.ascontiguousarray(xs.T), cst["cosT"], cst["sinT"])
    zero_w = (np.zeros((1024, T), np.float32), np.zeros((128, T), np.float32), np.zeros((128, T), np.float32))
    maps = []
    for j in range(ncores):
        m = dict(shared)
        cst = _consts(T, j * TP - HALO)
        for k in ("masks", "kscale", "gdec", "scanm", "ident", "retW"):
            m[k] = cst[k]
        ws = [win[j - w] if j - w >= 0 else zero_w for w in range(nw)]
        m["xT"] = np.stack([w_[0] for w_ in ws], 0)
        m["cosT"] = np.stack([w_[1] for w_ in ws], 0)
        m["sinT"] = np.stack([w_[2] for w_ in ws], 0)
        flag = np.ones((128, 8), np.float32)
        valid = np.zeros((128, 8), np.float32)
        for w in range(8):
            if j - w <= 0:
                flag[:, w] = 0.0
            if w >= 1 and j - w >= 0:
                valid[:, w - 1] = 1.0
        m["flag"] = flag
        m["valid"] = valid
        seli = np.zeros((128, 8), np.float32)
        seli[:, :j] = 1.0
        m["seli"] = seli
        invc = np.zeros((128, 4, 16), np.float32)
        for g, w in enumerate((2, 4, 8, 16)):
            tg = j * TP + np.arange(16)
            invc[:, g, :] = (1.0 / np.minimum(tg + 1, w))[None, :]
        m["invc"] = invc.reshape(128, 64)
        maps.append(m)
    return maps, TP


_CACHE = {}


def _get(TP, mode, ncores):
    key = (TP, mode, ncores)
    if key not in _CACHE:
        r = build(TP, mode, ncores)
        _CACHE[key] = (r[0], r[2])
    return _CACHE[key]


def kernel(**inp):
    ncores = 8
    maps, TP = make_inputs(inp, ncores, 8)
    ncF, names = _get(TP, "F2", ncores)
    r = run_bass_kernel_spmd(ncF, [{k: m[k] for k in names} for m in maps], core_ids=list(range(ncores)))
    y = np.concatenate([r.results[j]["yT"].T for j in range(ncores)], 0)
    return np.ascontiguousarray(y[None].astype(np.float32))
```

```python
from contextlib import ExitStack
import numpy as np
import concourse.bass as bass
import concourse.mybir as mybir
from concourse.bass_utils import run_bass_kernel_spmd

F32 = mybir.dt.float32
BF16 = mybir.dt.bfloat16
ALU = mybir.AluOpType
AF = mybir.ActivationFunctionType
AX = mybir.AxisListType
ENGS = ("pe", "act", "dve", "pool", "sp")
EPS = 1e-6
HALO = 64


class Sched:
    def __init__(self, nc, es):
        self.nc, self.es = nc, es
        self.items = {e: [] for e in ENGS}
        self.cnt = {e: 0 for e in ENGS}
        self.clock = {e: {} for e in ENGS}
        self.snap, self.key_w, self.key_r, self.dma_cnt, self.sems = {}, {}, {}, {}, {}
        self.out_events = []
        self.epoch = 0
        self.ek = {e: "E:" + e for e in ENGS}
        for e in ENGS:
            self._sem(self.ek[e])

    def _sem(self, k):
        if k not in self.sems:
            self.sems[k] = self.es.enter_context(self.nc.semaphore("s%d" % len(self.sems)))
        return self.sems[k]

    def _need(self, eng, ev, deps):
        if ev is None:
            return
        sk, val = ev
        if eng == "pe" and sk.startswith("E:pe"):
            return
        if self.clock[eng].get(sk, 0) >= val:
            return
        if deps.get(sk, 0) < val:
            deps[sk] = val

    def _apply(self, eng, deps):
        clk = self.clock[eng]
        for sk, val in deps.items():
            self.items[eng].append(("w", sk, val))
            for a, b in self.snap.get((sk, val), {}).items():
                if clk.get(a, 0) < b:
                    clk[a] = b
            if clk.get(sk, 0) < val:
                clk[sk] = val

    def op(self, eng, fn, reads=(), writes=(), dma_key=None, is_out=False, dma_inc=16):
        writes = [k.split("_")[0] if k.startswith("ps") else k for k in writes]
        writes += [k.split("_")[0] for k in reads if k.startswith("ps")]
        reads = [k for k in reads if not k.startswith("ps")]
        deps = {}
        for k in reads:
            self._need(eng, self.key_w.get(k), deps)
        for k in writes:
            self._need(eng, self.key_w.get(k), deps)
            for ev in self.key_r.get(k, ()):
                self._need(eng, ev, deps)
        self._apply(eng, deps)
        if dma_key is not None:
            sk = "D:" + str(dma_key)
            self._sem(sk)
            self.dma_cnt[sk] = self.dma_cnt.get(sk, 0) + 1
            ev = (sk, self._dval(sk, dma_inc))
            self.items[eng].append(("i", fn, sk, dma_inc))
            if is_out:
                self.out_events.append(ev)
        else:
            self.cnt[eng] += 1
            ev = (self.ek[eng], self.cnt[eng])
            self.items[eng].append(("i", fn, self.ek[eng], 1))
        self.snap[ev] = dict(self.clock[eng])
        for k in writes:
            self.key_w[k] = ev
            self.key_r[k] = []
        for k in reads:
            self.key_r.setdefault(k, []).append(ev)
        return ev

    def _dval(self, sk, inc):
        self.dma_val = getattr(self, "dma_val", {})
        self.dma_val[sk] = self.dma_val.get(sk, 0) + inc
        return self.dma_val[sk]

    def barrier(self):
        evs = [(self.ek[e], self.cnt[e]) for e in ENGS if self.cnt[e] > 0]
        evs += [(sk, v) for sk, v in getattr(self, "dma_val", {}).items()]
        for eng in ENGS:
            deps = {}
            for sk, val in evs:
                if self.clock[eng].get(sk, 0) < val:
                    deps[sk] = val
            self._apply(eng, deps)
        self.key_w, self.key_r = {}, {}
        self.epoch += 1
        for e in ENGS:
            if self.cnt[e] > 12000:
                self.ek[e] = "E:%s:%d" % (e, self.epoch)
                self._sem(self.ek[e])
                self.cnt[e] = 0

    def finish(self):
        deps = {}
        for ev in self.out_events:
            self._need("sp", ev, deps)
        self._apply("sp", deps)

    def emit(self):
        nc = self.nc
        with nc.Block() as block:
            def run(name):
                def body(engine):
                    for it in self.items[name]:
                        if it[0] == "w":
                            engine.wait_ge(self.sems[it[1]], it[2])
                        else:
                            it[1](engine).then_inc(self.sems[it[2]], it[3])
                return body
            block.tensor(run("pe"))
            block.scalar(run("act"))
            block.vector(run("dve"))
            block.gpsimd(run("pool"))
            block.sync(run("sp"))


class Stop(Exception):
    pass


def tiles(lo, hi, step=512):
    return [(t, min(step, hi - t)) for t in range(lo, hi, step)]


def build(TP, mode, ncores=8, dbg=()):
    T = TP + HALO
    NCH = T // 64
    nc = bass.Bass("TRN2", target_bir_lowering=False)

    in_names = []

    def din(name, shape):
        in_names.append(name)
        return nc.dram_tensor(name, list(shape), F32, kind="ExternalInput").ap()

    def dout(name, shape):
        return nc.dram_tensor(name, list(shape), F32, kind="ExternalOutput").ap()

    NW = 8 if mode == "F2" else 1
    xTw = din("xT", [NW, 1024, T])
    flag_d = din("flag", [128, 8])
    valid_d = din("valid", [128, 8])
    cT_d = din("cT", [128, 8])
    w_ada = din("w_ada", [2, 1024, 6144])
    badaT_d = din("b_adaT", [128, 96])
    w_in = din("w_in", [1024, 4096])
    w_in_sw = din("w_in_sw", [1024, 1024])
    lbraw_d = din("lbraw", [128, 8])
    gains_d = din("gains", [128, 8])
    w_out = din("w_out", [1024, 1024]) if mode != "A" else None
    pool_w = din("pool_w", [4, 256, 256]) if mode != "A" else None
    pool_bT_d = din("pool_bT", [128, 8])
    pool_sT_d = din("pool_sT", [128, 8])
    w_router = din("w_router", [2, 1024, 32]) if mode != "A" else None
    b_router = din("b_router", [2, 32]) if mode != "A" else None
    w_gu = din("w_gu", [2, 32, 1024, 2048]) if mode != "A" else None
    b_guT_d = din("b_guT", [128, 2 * 32 * 16]) if mode != "A" else None
    w_down = din("w_down", [2, 32, 1024, 1024]) if mode != "A" else None
    b_down = din("b_down", [2, 32, 1024]) if mode != "A" else None
    fnT_d = din("fnT", [128, 8])
    ident_d = din("ident", [128, 128])
    masks_d = din("masks", [64, 5 * 64])
    kscale_d = din("kscale", [64, 8])
    cos_d = din("cosT", [NW, 128, T])
    sin_d = din("sinT", [NW, 128, T])
    gdec_d = din("gdec", [128, 4 * 64])
    scanm_d = din("scanm", [128, T])
    invc_d = din("invc", [128, 4 * 16])
    seli_d = din("seli", [128, 8])
    retW_d = din("retW", [128, 4 * NCH])
    if mode == "A":
        Lst_o = dout("Lst", [128, 8 * 128])
        Dst_o = dout("Dst", [128, 8])
    if mode == "B":
        Lall_d = din("Lall", [ncores, 128, 8 * 128])
        Dall_d = din("Dall", [ncores, 128, 8])
    if mode in ("B", "F", "F2"):
        yT = dout("yT", [1024, TP])
    dbg_outs = {}

    gam = [1.0 - 2.0 ** (-5 - h) for h in range(4)]

    with ExitStack() as es:
        S = Sched(nc, es)

        uniq = [0]

        def sbt(stack, name, shape, dt=F32):
            uniq[0] += 1
            return stack.enter_context(nc.sbuf_tensor("sb%d_%s" % (uniq[0], name), list(shape), dt))

        def DMA(eng, out, in_, reads=(), writes=(), key=None, is_out=False):
            S.op(eng, lambda e: e.dma_start(out=out, in_=in_), reads, writes, dma_key=key or writes[0], is_out=is_out)

        def MM(out, lhsT, rhs, start=True, stop=True, reads=(), writes=()):
            S.op("pe", lambda e: e.matmul(out, lhsT, rhs, start=start, stop=stop), reads, writes)

        def TR(out, in_, ident, reads=(), writes=()):
            S.op("pe", lambda e: e.transpose(out, in_, ident), reads, writes)

        def ACT(out, in_, func, reads=(), writes=(), bias=None, scale=None):
            kw = {}
            if bias is not None:
                kw["bias"] = bias
            if scale is not None:
                kw["scale"] = scale
            S.op("act", lambda e: e.activation(out=out, in_=in_, func=func, **kw), reads, writes)

        def TT(eng, out, in0, in1, op, reads=(), writes=()):
            S.op(eng, lambda e: e.tensor_tensor(out=out, in0=in0, in1=in1, op=op), reads, writes)

        def TS(eng, out, in0, s1, s2, op0, op1=None, reads=(), writes=()):
            if op1 is None:
                S.op(eng, lambda e: e.tensor_scalar(out=out, in0=in0, scalar1=s1, scalar2=None, op0=op0), reads, writes)
            else:
                S.op(eng, lambda e: e.tensor_scalar(out=out, in0=in0, scalar1=s1, scalar2=s2, op0=op0, op1=op1), reads, writes)

        def STT(out, in0, scalar, in1, op0, op1, reads=(), writes=()):
            S.op("dve", lambda e: e.scalar_tensor_tensor(out=out, in0=in0, scalar=scalar, in1=in1, op0=op0, op1=op1),
                 reads, writes)

        def MEMSET(eng, ap, val, writes=()):
            S.op(eng, lambda e: e.memset(ap, val), (), writes)

        def COPY(eng, out, in_, reads=(), writes=()):
            if eng == "act":
                ACT(out, in_, AF.Identity, reads, writes)
            else:
                S.op(eng, lambda e: e.tensor_copy(out=out, in_=in_), reads, writes)

        def dump(name, ap, shape, key):
            if name in dbg:
                d = dout("dbg_" + name, shape)
                dbg_outs[name] = d
                DMA("sp", d, ap, reads=[key], writes=["dbg_" + name], is_out=True)

        G = es
        big = sbt(G, "big", [128, 8, T])
        bigb = big[:].rearrange("p k t -> p (k t)").bitcast(BF16).rearrange("p (k t) -> p k t", k=16)
        hnT = bigb[:, 0:8, :]
        catT = bigb[:, 8:16, :]
        ps = [G.enter_context(nc.psum_tensor("ps%d" % i, [128, 512], F32)) for i in range(7)]
        psb = G.enter_context(nc.psum_tensor("psb", [128, 1024], BF16))
        ident_f = sbt(G, "ident_f", [128, 128])
        ident_b = sbt(G, "ident_b", [128, 128], BF16)
        ones_b = sbt(G, "ones_b", [128, 128], BF16)
        ones_f = sbt(G, "ones_f", [128, 128])
        eps_t = sbt(G, "eps_t", [128, 1])
        flagw = sbt(G, "flag_s", [128, 8])
        W = {"w": 0, "L": None, "D": None}
        mod = sbt(G, "mod", [128, 96])
        mod1 = sbt(G, "mod1", [128, 96])
        small = sbt(G, "small", [128, 64])
        Lst = sbt(G, "Lst_s", [128, 8, 128])
        Dst = sbt(G, "Dst_s", [128, 8])
        Sin = sbt(G, "Sin_s", [128, 8, 128])
        seli = sbt(G, "seli_s", [128, 8])

        DMA("sp", ident_f[:], ident_d[:, :], writes=["ident_f"])
        DMA("pool", ident_b[:], ident_d[:, :], writes=["ident_b"])
        DMA("sp", flagw[:], flag_d[:, :], writes=["flag"])
        DMA("sp", seli[:], (valid_d if mode == "F2" else seli_d)[:, :], writes=["seli"])
        MEMSET("pool", ones_b[:], 1.0, ["ones_b"])
        MEMSET("pool", ones_f[:], 1.0, ["ones_f"])
        MEMSET("pool", eps_t[:], EPS, ["eps_t"])
        DMA("sp", small[:, 40:48], lbraw_d[:, :], writes=["small_lbraw"])
        DMA("sp", small[:, 8:16], gains_d[:, :], writes=["small_g"])
        DMA("sp", small[:, 16:24], pool_sT_d[:, :], writes=["small_ps"])
        DMA("sp", small[:, 24:32], pool_bT_d[:, :], writes=["small_pb"])
        DMA("sp", small[:, 32:40], fnT_d[:, :], writes=["small_fn"])
        TT("dve", small[:, 48:52], small[:, 40:44], small[:, 44:48], ALU.subtract, ["small_lbraw"], ["small_t"])
        ACT(small[:, 0:4], small[:, 48:52], AF.Sigmoid, ["small_t"], ["small_lb"])
        TS("dve", small[:, 4:8], small[:, 0:4], -1.0, 1.0, ALU.mult, ALU.add, ["small_lb"], ["small_oml"])

        with ExitStack() as P0:
            cT = sbt(P0, "cT_s", [128, 8])
            cond = sbt(P0, "cond", [128, 8])
            badaT = sbt(P0, "badaT", [128, 96])
            wa = [sbt(P0, "wa%d" % i, [128, 8, 768]) for i in range(2)]
            DMA("sp", cT[:], cT_d[:, :], writes=["cT"])
            DMA("sp", badaT[:], badaT_d[:, :], writes=["badaT"])
            ACT(cond[:], cT[:], AF.Silu, ["cT"], ["cond"])
            for l in range(2):
                for blk in range(8):
                    i = (l * 8 + blk) % 2
                    DMA("sp", wa[i][:], w_ada[l, :, blk * 768:(blk + 1) * 768].rearrange("(k p) c -> p k c", p=128),
                        writes=["wa%d" % i])
                    for m in range(6):
                        col = l * 48 + blk * 6 + m
                        for k in range(8):
                            MM(ps[0][:, col:col + 1], wa[i][:, k, m * 128:(m + 1) * 128], cond[:, k:k + 1],
                               start=(k == 0), stop=(k == 7), reads=["wa%d" % i, "cond"], writes=["ps0"])
            TT("dve", mod[:], ps[0][:, 0:96], badaT[:], ALU.add, ["ps0", "badaT"], ["mod"])
            TS("dve", mod1[:], mod[:], 1.0, None, ALU.add, reads=["mod"], writes=["mod1"])
            TT("dve", small[:, 16:24], small[:, 16:24], mod[:, 48 + 16:48 + 24], ALU.mult, ["small_ps", "mod"], ["small_pc"])
            S.barrier()
        dump("mod", mod[:], [128, 96], "mod")

        rot = {"pj": 0, "at": 0, "o": 0, "ds": 0}

        def ps_pj():
            rot["pj"] ^= 1
            return ps[rot["pj"]], "ps%d" % rot["pj"]

        def norm_tile(NS, xk, xkeys, n, l, ffn, out_bf, out_bf_keys, out_f32=None, out_f32_keys=None, final=False):
            base = l * 48 + (24 if ffn else 0)
            pj, pk = ps_pj()
            for k in range(8):
                ACT(NS["sq"][:, k, :n], xk[k], AF.Square, [xkeys[k]], ["sq%d" % k])
            for k in range(8):
                MM(pj[:, :n], ones_b[:], NS["sq"][:, k, :n], start=(k == 0), stop=(k == 7),
                   reads=["sq%d" % k, "ones_b"], writes=[pk])
            ACT(NS["stdt"][:, :n], pj[:, :n], AF.Sqrt, [pk, "eps_t"], ["stdt"], bias=eps_t[:, 0:1], scale=1.0 / 1024.0)
            S.op("dve", lambda e: e.reciprocal(out=NS["stdt"][:, :n], in_=NS["stdt"][:, :n]), ["stdt"], ["stdt"])
            for k in range(8):
                if final:
                    STT(out_f32[k], xk[k], small[:, 32 + k:33 + k], NS["stdt"][:, :n], ALU.mult, ALU.mult,
                        [xkeys[k], "stdt", "small_fn"], [out_f32_keys[k]])
                    continue
                STT(NS["t1"][:, k, :n], xk[k], mod1[:, base + 8 + k:base + 9 + k], NS["stdt"][:, :n], ALU.mult, ALU.mult,
                    [xkeys[k], "stdt", "mod1"], ["t1_%d" % k])
                if out_f32 is not None:
                    ACT(out_f32[k], NS["t1"][:, k, :n], AF.Identity, ["t1_%d" % k, "mod"], [out_f32_keys[k]],
                        bias=mod[:, base + k:base + k + 1])
                    COPY("pool", out_bf[k], out_f32[k], [out_f32_keys[k]], [out_bf_keys[k]])
                else:
                    ACT(out_bf[k], NS["t1"][:, k, :n], AF.Identity, ["t1_%d" % k, "mod"], [out_bf_keys[k]],
                        bias=mod[:, base + k:base + k + 1])

        def mixer_norm():
            with ExitStack() as PN:
                NS = {"sq": sbt(PN, "sq", [128, 8, 512], BF16), "stdt": sbt(PN, "stdt", [128, 512]),
                      "t1": sbt(PN, "t1", [128, 8, 512])}
                xt = [sbt(PN, "xt%d" % i, [128, 8, 512]) for i in range(2)]
                for ti, (t0, n) in enumerate(tiles(0, T)):
                    b = xt[ti % 2]
                    DMA("sp", b[:, :, :n], xTw[W["w"], :, t0:t0 + n].rearrange("(k p) t -> p k t", p=128), writes=["xt%d" % (ti % 2)])
                    norm_tile(NS, [b[:, k, :n] for k in range(8)], ["xt%d" % (ti % 2)] * 8, n, 0, False,
                              [hnT[:, k, t0:t0 + n] for k in range(8)], ["hn%d" % k for k in range(8)])
                S.barrier()

        def proj_fm(whb, qi, t0, n):
            wh, whk = whb
            pj, pk = ps_pj()
            for k in range(8):
                MM(pj[:, :n], wh[:, k, qi, :], hnT[:, k, t0:t0 + n], start=(k == 0), stop=(k == 7),
                   reads=[whk, "hn%d" % k], writes=[pk])
            return pj, pk

        def proj_v(whb, qi, vtok):
            wh, whk = whb
            for c0 in range(0, NCH, 4):
                pj, pk = ps_pj()
                ncs = min(4, NCH - c0)
                for ci in range(ncs):
                    c = c0 + ci
                    for k in range(8):
                        MM(pj[0:64, ci * 128:(ci + 1) * 128], hnT[:, k, c * 64:(c + 1) * 64], wh[:, k, qi, :],
                           start=(k == 0), stop=(k == 7), reads=[whk, "hn%d" % k], writes=[pk])
                ACT(vtok[:, c0:c0 + ncs, :], pj[0:64, 0:ncs * 128].rearrange("p (c e) -> p c e", e=128), AF.Identity,
                    [pk], ["vtok"])

        def k_transposes(kA, ktok, ksc):
            for c0 in range(0, NCH, 8):
                ncs = min(8, NCH - c0)
                for ci in range(ncs):
                    c = c0 + ci
                    TR(psb[0:64, ci * 128:(ci + 1) * 128], kA[:, c * 64:(c + 1) * 64], ident_b[:], ["kA", "ident_b"], ["psb"])
                TS("dve", ktok[:, c0:c0 + ncs, :], psb[0:64, 0:ncs * 128].rearrange("p (c e) -> p c e", e=128),
                   ksc, None, ALU.mult, reads=["psb", "kscale"], writes=["ktok"])

        def recurrence(h, HS, full, mask_ap):
            St, Sb = HS["S"], HS["Sb"]
            if not full:
                for c in range(NCH - 1):
                    MM(ps[6][:, 0:128], HS["ktok"][:, c, :], HS["vtok"][:, c, :], start=(c == 0), stop=(c == NCH - 2),
                       reads=["ktok", "vtok"], writes=["ps6"])
                COPY("act", (W["L"][:, h, :] if W["L"] is not None else Lst[:, h, :]), ps[6][:, 0:128], ["ps6"], ["Lst"])
                return
            if full:
                COPY("pool", St[:], Sin[:, h, :], ["Sin"], ["S"])
            else:
                MEMSET("pool", St[:], 0.0, ["S"])
            nch = NCH if full else NCH - 1
            ob = None
            for c in range(nch):
                cs = slice(c * 64, (c + 1) * 64)
                if full:
                    rot["at"] ^= 1
                    pa, pak = ps[2 + rot["at"]], "ps%d" % (2 + rot["at"])
                    MM(pa[0:64, 0:64], HS["kA"][:, cs], HS["qA"][:, cs], reads=["kA", "qA"], writes=[pak])
                    atm = HS["atm"][rot["at"]]
                    TT("dve", atm[:], pa[0:64, 0:64], mask_ap, ALU.mult, [pak, "masks"], ["atm%d" % rot["at"]])
                    TS("pool", Sb[:], St[:], HS["tabM"][:, c:c + 1], None, ALU.mult, reads=["S", "tabM"], writes=["Sb"])
                    if c % 8 == 0:
                        rot["o"] ^= 1
                        ob, obk = ps[4 + rot["o"]], "ps%d" % (4 + rot["o"])
                    oc = (c % 8) * 64
                    MM(ob[:, oc:oc + 64], HS["vtok"][:, c, :], atm[:], start=True, stop=False,
                       reads=["vtok", "atm%d" % rot["at"]], writes=[obk])
                    MM(ob[:, oc:oc + 64], Sb[:], HS["qB"][:, cs], start=False, stop=True, reads=["Sb", "qB"], writes=[obk])
                    if c % 8 == 7 or c == nch - 1:
                        c0 = (c // 8) * 8
                        headnorm(h, HS, ob, obk, c0 * 64, (c + 1 - c0) * 64)
                if c < NCH - 1:
                    rot["ds"] = (rot["ds"] + 1) % 4
                    pd = ps[6][:, rot["ds"] * 128:(rot["ds"] + 1) * 128]
                    pdk = "ps6_%d" % rot["ds"]
                    MM(pd, HS["ktok"][:, c, :], HS["vtok"][:, c, :], reads=["ktok", "vtok"], writes=[pdk])
                    ACT(HS["dst"][:], pd, AF.Identity, [pdk, "tabC"], ["dst"], scale=HS["tabC"][:, c:c + 1])
                    STT(St[:], St[:], HS["tabA"][:, c:c + 1], HS["dst"][:], ALU.mult, ALU.add, ["S", "tabA", "dst"], ["S"])
            if not full:
                COPY("pool", (W["L"][:, h, :] if W["L"] is not None else Lst[:, h, :]), St[:], ["S"], ["Lst"])

        def headnorm(h, HS, ob, obk, t0, n):
            ACT(HS["osq"][:, :n], ob[:, :n], AF.Square, [obk], ["osq"])
            pj, pk = ps_pj()
            MM(pj[:, :n], ones_b[:], HS["osq"][:, :n], reads=["osq", "ones_b"], writes=[pk])
            ACT(HS["ostd"][:, :n], pj[:, :n], AF.Sqrt, [pk, "eps_t"], ["ostd"], bias=eps_t[:, 0:1], scale=1.0 / 128.0)
            S.op("dve", lambda e: e.reciprocal(out=HS["ostd"][:, :n], in_=HS["ostd"][:, :n]), ["ostd"], ["ostd"])
            STT(HS["ot"][:, :n], ob[:, :n], small[:, 8 + h:9 + h], HS["ostd"][:, :n], ALU.mult, ALU.mult,
                [obk, "ostd", "small_g"], ["ot"])
            TT("pool", catT[:, h, t0:t0 + n], HS["ot"][:, :n], HS["gate"][:, t0:t0 + n], ALU.mult, ["ot", "gate"], ["cat%d" % h])

        def head_common(PH, full):
            HS = {"kA": sbt(PH, "kA", [128, T], BF16), "ktok": sbt(PH, "ktok", [64, NCH, 128], BF16),
                  "vtok": sbt(PH, "vtok", [64, NCH, 128], BF16), "S": sbt(PH, "S", [128, 128]),
                  "Sb": sbt(PH, "Sb", [128, 128], BF16), "dst": sbt(PH, "dst", [128, 128]),
                  "tabA": sbt(PH, "tabA", [128, NCH]), "tabC": sbt(PH, "tabC", [128, NCH]), "tabM": sbt(PH, "tabM", [128, NCH]),
                  "tmp": [sbt(PH, "tmp%d" % i, [128, 512]) for i in range(3)]}
            if full:
                HS.update({"qA": sbt(PH, "qA", [128, T], BF16), "gate": sbt(PH, "gate", [128, T], BF16),
                           "atm": [sbt(PH, "atm%d" % i, [64, 64], BF16) for i in range(2)],
                           "osq": sbt(PH, "osq", [128, 512], BF16), "ostd": sbt(PH, "ostd", [128, 512]),
                           "ot": sbt(PH, "ot", [128, 512])})
            return HS

        def hg_heads(full):
            with ExitStack() as PH:
                HS = head_common(PH, full)
                if full:
                    HS["qB"] = HS["qA"]
                whs = [sbt(PH, "wh%d" % i, [128, 8, 4, 128], BF16) for i in range(2)]

                def load_wh(h_):
                    for qi in (range(4) if full else (1, 2)):
                        DMA("pool", whs[h_ % 2][:, :, qi, :], w_in[:, qi * 512 + h_ * 128: qi * 512 + (h_ + 1) * 128]
                            .rearrange("(k p) c -> p k c", p=128), writes=["wh%d" % (h_ % 2)])
                ff = sbt(PH, "ff", [128, T])
                bA = sbt(PH, "bA", [128, T])
                bB = sbt(PH, "bB", [128, T])
                scanm = sbt(PH, "scanm", [128, T])
                masks = sbt(PH, "masks", [64, 64])
                ksc = sbt(PH, "ksc", [64, 8])
                tmpd = sbt(PH, "tmpd", [128, NCH])
                pref = sbt(PH, "pref", [128, NCH])
                Wt = sbt(PH, "Wt", [128, NCH])
                onesN = sbt(PH, "onesN", [128, NCH])
                MEMSET("pool", onesN[:], 1.0, ["onesN"])
                sumbl = sbt(PH, "sumbl", [128, 1])
                DMA("sp", scanm[:], scanm_d[:, :], writes=["scanm"])
                DMA("sp", masks[:], masks_d[:, 0:64], writes=["masks"])
                DMA("sp", ksc[:], kscale_d[:, :], writes=["kscale"])
                b3 = bB[:].rearrange("p (c s) -> p c s", s=64)
                a3 = bA[:].rearrange("p (c s) -> p c s", s=64)
                load_wh(0)
                for h in range(4):
                    if h + 1 < 4:
                        load_wh(h + 1)
                    wh = (whs[h % 2], "wh%d" % (h % 2))
                    for ti, (t0, n) in enumerate(tiles(0, T)):
                        pj, pk = proj_fm(wh, 1, t0, n)
                        tm = HS["tmp"][ti % 2]
                        ACT(tm[:, :n], pj[:, :n], AF.Sigmoid, [pk], ["tmp%d" % (ti % 2)])
                        TS("dve", ff[:, t0:t0 + n], tm[:, :n], small[:, 4 + h:5 + h], small[:, h:h + 1], ALU.mult, ALU.add,
                           reads=["tmp%d" % (ti % 2), "small_lb", "small_oml"], writes=["ff"])
                    if "h1" in dbg:
                        return
                    ACT(bA[:], ff[:], AF.Ln, ["ff"], ["bA"])
                    if "h1b" in dbg:
                        return
                    S.op("dve", lambda e: e.tensor_tensor_scan(out=bB[:], data0=scanm[:], data1=bA[:], initial=0.0,
                                                               op0=ALU.mult, op1=ALU.add), ["scanm", "bA"], ["bB"])
                    if "h1c" in dbg:
                        return
                    ACT(HS["tabA"][:], b3[:, :, 63], AF.Exp, ["bB"], ["tabA"])
                    TT("dve", tmpd[:], b3[:, :, 63], b3[:, :, 31], ALU.subtract, ["bB"], ["tmpd"])
                    ACT(HS["tabC"][:], tmpd[:], AF.Exp, ["tmpd"], ["tabC"])
                    ACT(HS["tabM"][:], b3[:, :, 31], AF.Exp, ["bB"], ["tabM"])
                    S.op("dve", lambda e: e.reduce_sum(out=sumbl[:], in_=b3[:, 0:NCH - 1, 63], axis=AX.X), ["bB"], ["sumbl"])
                    ACT((W["D"] if W["D"] is not None else Dst)[:, h:h + 1], sumbl[:], AF.Exp, ["sumbl"], ["Dst"])
                    TT("dve", HS["tabA"][:, 0:1], HS["tabA"][:, 0:1], flagw[:, W["w"]:W["w"] + 1], ALU.mult, ["tabA", "flag"], ["tabA"])
                    TT("dve", HS["tabC"][:, 0:1], HS["tabC"][:, 0:1], flagw[:, W["w"]:W["w"] + 1], ALU.mult, ["tabC", "flag"], ["tabC"])
                    if "h1d" in dbg:
                        return
                    if not full:
                        S.op("dve", lambda e: e.tensor_tensor_scan(out=pref[:], data0=onesN[:], data1=b3[:, :, 63], initial=0.0,
                                                                   op0=ALU.mult, op1=ALU.add), ["onesN", "bB"], ["pref"])
                        TT("dve", Wt[:, 0:NCH - 1], tmpd[:, 0:NCH - 1], pref[:, 0:NCH - 1], ALU.subtract, ["tmpd", "pref"], ["Wt"])
                        ACT(Wt[:, 0:NCH - 1], Wt[:, 0:NCH - 1], AF.Exp, ["Wt", "pref"], ["Wt"], bias=pref[:, NCH - 2:NCH - 1])
                        MEMSET("pool", Wt[:, NCH - 1:NCH], 0.0, ["Wt"])
                        TT("dve", Wt[:, 0:1], Wt[:, 0:1], flagw[:, W["w"]:W["w"] + 1], ALU.mult, ["Wt", "flag"], ["Wt"])
                    TT("dve", a3, b3, b3[:, :, 31:32].broadcast_to([128, NCH, 64]), ALU.subtract, ["bB", "bA"], ["bA"])
                    if "h1e" in dbg:
                        return
                    ACT(bB[:], bA[:], AF.Exp, ["bA"], ["bB"])
                    ACT(bA[:], bA[:], AF.Exp, ["bA"], ["bA"], scale=-1.0)
                    if not full:
                        TT("dve", a3, a3, Wt[:].unsqueeze(2).broadcast_to([128, NCH, 64]), ALU.mult, ["bA", "Wt"], ["bA"])
                    TS("pool", ff[:], ff[:], -1.0, 1.0, ALU.mult, ALU.add, reads=["ff"], writes=["ff"])
                    TT("dve", HS["kA"][:], ff[:], bA[:], ALU.mult, ["ff", "bA"], ["kA"])
                    if full:
                        for ti, (t0, n) in enumerate(tiles(0, T)):
                            pj, pk = proj_fm(wh, 0, t0, n)
                            tm = HS["tmp"][ti % 2]
                            ACT(tm[:, :n], pj[:, :n], AF.Silu, [pk], ["tmp%d" % (ti % 2)])
                            TT("dve", HS["qA"][:, t0:t0 + n], tm[:, :n], bB[:, t0:t0 + n], ALU.mult,
                               ["tmp%d" % (ti % 2), "bB"], ["qA"])
                            pj, pk = proj_fm(wh, 3, t0, n)
                            ACT(HS["gate"][:, t0:t0 + n], pj[:, :n], AF.Silu, [pk], ["gate"])
                    if "h2" in dbg:
                        return
                    proj_v(wh, 2, HS["vtok"])
                    if "h3" in dbg:
                        return
                    k_transposes(HS["kA"], HS["ktok"], ksc[:, h:h + 1])
                    if "h4" in dbg:
                        return
                    recurrence(h, HS, full, masks[:])
                    if "h5" in dbg:
                        return
                S.barrier()

        def ret_heads(full):
            with ExitStack() as PH:
                HS = head_common(PH, full)
                if full:
                    HS["qB"] = sbt(PH, "qB", [128, T], BF16)
                whs = [sbt(PH, "wh6_%d" % i, [128, 8, 6, 128], BF16) for i in range(2)]

                def load_wh(r_):
                    srcs = [w_in[:, 2048 + r_ * 128:2048 + (r_ + 1) * 128], w_in_sw[:, r_ * 128:(r_ + 1) * 128],
                            w_in[:, 2560 + r_ * 128:2560 + (r_ + 1) * 128], w_in_sw[:, 512 + r_ * 128:512 + (r_ + 1) * 128],
                            w_in[:, 3072 + r_ * 128:3072 + (r_ + 1) * 128], w_in[:, 3584 + r_ * 128:3584 + (r_ + 1) * 128]]
                    for qi in (range(6) if full else (2, 3, 4)):
                        DMA("pool", whs[r_ % 2][:, :, qi, :], srcs[qi].rearrange("(k p) c -> p k c", p=128), writes=["wh%d" % (r_ % 2)])
                cosT = sbt(PH, "cosT", [128, T])
                sinT = sbt(PH, "sinT", [128, T])
                gdec = sbt(PH, "gdec", [128, 4, 64])
                masks = sbt(PH, "dmasks", [64, 4, 64])
                ksc = sbt(PH, "ksc", [64, 8])
                Wr = sbt(PH, "Wr", [128, 4, NCH])
                DMA("sp", Wr[:], retW_d[:, :].rearrange("p (h c) -> p h c", c=NCH), writes=["Wr"])
                for r_ in range(4):
                    TT("dve", Wr[:, r_, 0:1], Wr[:, r_, 0:1], flagw[:, W["w"]:W["w"] + 1], ALU.mult, ["Wr", "flag"], ["Wr"])
                DMA("sp", cosT[:], cos_d[W["w"], :, :], writes=["cosT"])
                DMA("sp", sinT[:], sin_d[W["w"], :, :], writes=["sinT"])
                DMA("sp", gdec[:], gdec_d[:, :].rearrange("p (h s) -> p h s", s=64), writes=["gdec"])
                DMA("sp", masks[:], masks_d[:, 64:320].rearrange("p (h s) -> p h s", s=64), writes=["masks"])
                DMA("sp", ksc[:], kscale_d[:, :], writes=["kscale"])
                load_wh(0)
                for r in range(4):
                    h = 4 + r
                    if r + 1 < 4:
                        load_wh(r + 1)
                    wh = (whs[r % 2], "wh%d" % (r % 2))
                    MEMSET("pool", HS["tabA"][:], gam[r] ** 64, ["tabA"])
                    MEMSET("pool", HS["tabC"][:], 1.0, ["tabC"])
                    MEMSET("pool", HS["tabM"][:], 1.0, ["tabM"])
                    MEMSET("pool", (W["D"] if W["D"] is not None else Dst)[:, h:h + 1], gam[r] ** (64 * (NCH - 1)), ["Dst"])
                    TT("dve", HS["tabA"][:, 0:1], HS["tabA"][:, 0:1], flagw[:, W["w"]:W["w"] + 1], ALU.mult, ["tabA", "flag"], ["tabA"])
                    TT("dve", HS["tabC"][:, 0:1], HS["tabC"][:, 0:1], flagw[:, W["w"]:W["w"] + 1], ALU.mult, ["tabC", "flag"], ["tabC"])
                    for which in ((0, 1) if full else (1,)):
                        for ti, (t0, n) in enumerate(tiles(0, T)):
                            pa, pak = proj_fm(wh, 2 * which, t0, n)
                            t1, t2 = HS["tmp"][0], HS["tmp"][1]
                            TT("dve", t1[:, :n], pa[:, :n], cosT[:, t0:t0 + n], ALU.mult, [pak, "cosT"], ["tmp0"])
                            pb, pbk = proj_fm(wh, 2 * which + 1, t0, n)
                            TT("dve", t2[:, :n], pb[:, :n], sinT[:, t0:t0 + n], ALU.mult, [pbk, "sinT"], ["tmp1"])
                            TT("pool", t1[:, :n], t1[:, :n], t2[:, :n], ALU.add, ["tmp0", "tmp1"], ["tmp0"])
                            if which == 0:
                                ACT(HS["qA"][:, t0:t0 + n], t1[:, :n], AF.Identity, ["tmp0"], ["qA"])
                                TT("dve", HS["qB"][:, t0:t0 + n].rearrange("p (c s) -> p c s", s=64),
                                   t1[:, :n].rearrange("p (c s) -> p c s", s=64),
                                   gdec[:, r, :].unsqueeze(1).broadcast_to([128, n // 64, 64]), ALU.mult,
                                   ["tmp0", "gdec"], ["qB"])
                            else:
                                ACT(HS["kA"][:, t0:t0 + n], t1[:, :n], AF.Identity, ["tmp0"], ["kA"], scale=128.0 ** -0.5)
                    if not full:
                        k3 = HS["kA"][:].rearrange("p (c s) -> p c s", s=64)
                        TT("dve", k3, k3, Wr[:, r, :].unsqueeze(2).broadcast_to([128, NCH, 64]), ALU.mult, ["kA", "Wr"], ["kA"])
                    if full:
                        for ti, (t0, n) in enumerate(tiles(0, T)):
                            pj, pk = proj_fm(wh, 5, t0, n)
                            ACT(HS["gate"][:, t0:t0 + n], pj[:, :n], AF.Silu, [pk], ["gate"])
                    proj_v(wh, 4, HS["vtok"])
                    k_transposes(HS["kA"], HS["ktok"], ksc[:, h:h + 1])
                    recurrence(h, HS, full, masks[:, r, :])
                S.barrier()

        def chain_states(Lall_sb, Dall_sb):
            with ExitStack() as PC:
                tmp = sbt(PC, "ctmp", [128, 128])
                MEMSET("pool", Sin[:], 0.0, ["Sin"])
                for i in (range(ncores - 2, -1, -1) if mode == "F2" else range(ncores - 1)):
                    for h in range(8):
                        STT(tmp[:], Sin[:, h, :], Dall_sb[:, i, h:h + 1], Lall_sb[:, i, h, :], ALU.mult, ALU.add,
                            ["Sin", "Lall", "Dall"], ["ctmp"])
                        TT("dve", tmp[:], tmp[:], Sin[:, h, :], ALU.subtract, ["ctmp", "Sin"], ["ctmp"])
                        STT(Sin[:, h, :], tmp[:], seli[:, i:i + 1], Sin[:, h, :], ALU.mult, ALU.add,
                            ["ctmp", "seli", "Sin"], ["Sin"])
                S.barrier()

        def wout_residual():
            with ExitStack() as PW:
                wo = sbt(PW, "wo", [128, 8, 1024], BF16)
                xhi = sbt(PW, "xhi", [128, 4, T])
                xin = [sbt(PW, "xin%d" % i, [128, 512]) for i in range(2)]
                DMA("pool", wo[:], w_out[:, :].rearrange("(k p) c -> p k c", p=128), writes=["wo"])
                i = 0
                for dm in range(8):
                    for (t0, n) in tiles(0, T):
                        pj, pk = ps_pj()
                        for h in range(8):
                            MM(pj[:, :n], wo[:, h, dm * 128:(dm + 1) * 128], catT[:, h, t0:t0 + n], start=(h == 0), stop=(h == 7),
                               reads=["wo", "cat%d" % h], writes=[pk])
                        i ^= 1
                        DMA("sp", xin[i][:, :n], xTw[0, dm * 128:(dm + 1) * 128, t0:t0 + n], writes=["xin%d" % i])
                        dest = big[:, dm, t0:t0 + n] if dm < 4 else xhi[:, dm - 4, t0:t0 + n]
                        STT(dest, pj[:, :n], mod[:, 16 + dm:17 + dm], xin[i][:, :n], ALU.mult, ALU.add,
                            [pk, "mod", "xin%d" % i], ["xdest%d" % dm])
                S.barrier()
                for dm in range(4, 8):
                    COPY("pool" if dm % 2 else "act", big[:, dm, :], xhi[:, dm - 4, :], ["xdest%d" % dm], ["x%d" % dm])
                S.barrier()

        def moe(l, lo, hi):
            halves = [(lo, (lo + hi) // 2), ((lo + hi) // 2, hi)]
            for (h0, h1) in halves:
                NH = h1 - h0
                with ExitStack() as PM:
                    hnh = sbt(PM, "hnh", [128, 8, NH], BF16)
                    gatesT = sbt(PM, "gatesT", [32, NH])
                    with ExitStack() as PN:
                        NS = {"sq": sbt(PN, "sq", [128, 8, 512], BF16), "stdt": sbt(PN, "stdt", [128, 512]),
                              "t1": sbt(PN, "t1", [128, 8, 512])}
                        hn32 = sbt(PN, "hn32", [128, 8, 512])
                        wr = sbt(PN, "wr", [128, 8, 32])
                        br = sbt(PN, "br", [1, 32])
                        lg = sbt(PN, "lg", [128, 32])
                        mx8 = sbt(PN, "mx8", [128, 8])
                        msk = sbt(PN, "msk", [128, 32])
                        ex = sbt(PN, "ex", [128, 32])
                        den = sbt(PN, "den", [128, 2])
                        DMA("sp", wr[:], w_router[l].rearrange("(k p) e -> p k e", p=128), writes=["wr"])
                        DMA("sp", br[:], b_router[l:l + 1, :], writes=["br"])
                        for (t0, n) in tiles(h0, h1):
                            norm_tile(NS, [big[:, k, t0:t0 + n] for k in range(8)], ["x%d" % k for k in range(8)], n, l, True,
                                      [hnh[:, k, t0 - h0:t0 - h0 + n] for k in range(8)], ["hnh%d" % k for k in range(8)],
                                      [hn32[:, k, :n] for k in range(8)], ["hn32_%d" % k for k in range(8)])
                            for s0 in range(0, n, 128):
                                m = min(128, n - s0)
                                pl = ps[6]
                                for k in range(8):
                                    MM(pl[:m, 0:32], hn32[:, k, s0:s0 + m], wr[:, k, :], start=(k == 0), stop=False,
                                       reads=["hn32_%d" % k, "wr"], writes=["ps6"])
                                MM(pl[:m, 0:32], ones_f[0:1, 0:m], br[0:1, :], start=False, stop=True, reads=["ones_f", "br"], writes=["ps6"])
                                COPY("dve", lg[:m, :], pl[:m, 0:32], ["ps6"], ["lg"])
                                S.op("dve", lambda e, m=m: e.max(out=mx8[:m, :], in_=lg[:m, :]), ["lg"], ["mx8"])
                                TS("dve", msk[:m, :], lg[:m, :], mx8[:m, 3:4], None, ALU.is_ge, reads=["lg", "mx8"], writes=["msk"])
                                TS("dve", den[:m, 0:1], mx8[:m, 0:1], -1.0, None, ALU.mult, reads=["mx8"], writes=["den0"])
                                ACT(ex[:m, :], lg[:m, :], AF.Exp, ["lg", "den0"], ["ex"], bias=den[:m, 0:1])
                                TT("dve", ex[:m, :], ex[:m, :], msk[:m, :], ALU.mult, ["ex", "msk"], ["ex"])
                                S.op("dve", lambda e, m=m: e.reduce_sum(out=den[:m, 1:2], in_=ex[:m, :], axis=AX.X), ["ex"], ["den1"])
                                S.op("dve", lambda e, m=m: e.reciprocal(out=den[:m, 1:2], in_=den[:m, 1:2]), ["den1"], ["den1"])
                                TS("dve", ex[:m, :], ex[:m, :], den[:m, 1:2], None, ALU.mult, reads=["ex", "den1"], writes=["ex"])
                                TR(ps[6][0:32, 128:128 + m], ex[:m, :], ident_f[:m, :m], ["ex", "ident_f"], ["ps6"])
                                c0 = t0 - h0 + s0
                                COPY("act", gatesT[:, c0:c0 + m], ps[6][0:32, 128:128 + m], ["ps6"], ["gatesT"])
                        S.barrier()
                    if "gatesT" in dbg and h0 == lo and l == 0:
                        dump("gatesT", gatesT[:], [32, NH], "gatesT")
                    with ExitStack() as PE_:
                        act = sbt(PE_, "act", [128, 8, NH], BF16)
                        gbc = sbt(PE_, "gbc", [128, NH])
                        wg = [sbt(PE_, "wg%d" % i, [128, 8, 2, 512], BF16) for i in range(2)]
                        wd = [sbt(PE_, "wd%d" % i, [128, 8, 1024], BF16) for i in range(2)]
                        tt = [[sbt(PE_, "tt%d_%d" % (i, j), [128, 512]) for j in range(3)] for i in range(2)]
                        ytmp = [sbt(PE_, "ytmp%d" % i, [128, 512]) for i in range(2)]
                        bgu = sbt(PE_, "bgu", [128, 32 * 16])
                        bdn = sbt(PE_, "bdn", [32, 1024])
                        DMA("sp", bgu[:], b_guT_d[:, l * 512:(l + 1) * 512], writes=["bgu"])
                        bgu1 = sbt(PE_, "bgu1", [128, 32 * 16])
                        TS("dve", bgu1[:], bgu[:], 1.0, None, ALU.add, reads=["bgu"], writes=["bgu1"])
                        DMA("sp", bdn[:], b_down[l], writes=["bdn"])
                        cnt = {"g": 0, "t": 0, "y": 0, "d": 0}

                        def down_evac(py, pyk, dm, t0, n):
                            STT(big[:, dm, t0:t0 + n], py[:, :n], mod[:, l * 48 + 40 + dm:l * 48 + 41 + dm], big[:, dm, t0:t0 + n],
                                ALU.mult, ALU.add, [pyk, "mod", "x%d" % dm], ["x%d" % dm])

                        for dm in range(8):
                            for (t0, n) in tiles(h0, h1):
                                cnt["d"] ^= 1
                                py, pyk = ps[4 + cnt["d"]], "ps%d" % (4 + cnt["d"])
                                MM(py[:, :n], bdn[0:32, dm * 128:(dm + 1) * 128], gatesT[0:32, t0 - h0:t0 - h0 + n],
                                   reads=["bdn", "gatesT"], writes=[pyk])
                                down_evac(py, pyk, dm, t0, n)
                        def issue_wg(si):
                            e_, fq_ = si // 2, si % 2
                            for two in range(2):
                                DMA("pool", wg[si % 2][:, :, two, :],
                                    w_gu[l, e_, :, two * 1024 + fq_ * 512: two * 1024 + (fq_ + 1) * 512].rearrange("(k p) c -> p k c", p=128),
                                    writes=["wg%d" % (si % 2)])

                        def issue_wd(e_):
                            DMA("pool", wd[e_ % 2][:], w_down[l, e_].rearrange("(k p) c -> p k c", p=128), writes=["wd%d" % (e_ % 2)])

                        issue_wg(0)
                        issue_wd(0)
                        for e in range(32):
                            for (t0, n) in tiles(0, NH):
                                MM(ps[6][:, :n], ident_f[0:32, e:e + 1].broadcast_to([32, 128]), gatesT[0:32, t0:t0 + n],
                                   reads=["ident_f", "gatesT"], writes=["ps6"])
                                COPY("act", gbc[:, t0:t0 + n], ps[6][:, :n], ["ps6"], ["gbc"])
                            wdi = e % 2
                            if e + 1 < 32:
                                issue_wd(e + 1)
                            for fc in range(8):
                                si = e * 2 + fc // 4
                                fi = fc % 4
                                if fi == 0 and si + 1 < 64:
                                    issue_wg(si + 1)
                                wgi, wgk = wg[si % 2], "wg%d" % (si % 2)
                                bcol = e * 16 + fc
                                for (t0, n) in tiles(0, NH):
                                    pg, pgk = ps[0], "ps0"
                                    plin, plk = ps[1], "ps1"
                                    if (t0 // 512) % 2:
                                        pg, pgk, plin, plk = ps[2], "ps2", ps[3], "ps3"
                                    for k in range(8):
                                        MM(pg[:, :n], wgi[:, k, 0, fi * 128:(fi + 1) * 128], hnh[:, k, t0:t0 + n], start=(k == 0), stop=(k == 7),
                                           reads=[wgk, "hnh%d" % k], writes=[pgk])
                                    for k in range(8):
                                        MM(plin[:, :n], wgi[:, k, 1, fi * 128:(fi + 1) * 128], hnh[:, k, t0:t0 + n], start=(k == 0), stop=(k == 7),
                                           reads=[wgk, "hnh%d" % k], writes=[plk])
                                    cnt["t"] ^= 1
                                    t1, t2, t3 = tt[cnt["t"]]
                                    k1, k2, k3 = ["tt%d_%d" % (cnt["t"], j) for j in range(3)]
                                    TS("dve", t1[:, :n], pg[:, :n], bgu[:, bcol:bcol + 1], 7.0, ALU.add, ALU.min, reads=[pgk, "bgu"], writes=[k1])
                                    ACT(t2[:, :n], t1[:, :n], AF.Sigmoid, [k1], [k2], scale=1.702)
                                    TS("dve", t3[:, :n], plin[:, :n], bgu1[:, bcol + 8:bcol + 9], 8.0, ALU.add, ALU.min, reads=[plk, "bgu1"], writes=[k3])
                                    STT(t3[:, :n], t3[:, :n], -6.0, gbc[:, t0:t0 + n], ALU.max, ALU.mult, [k3, "gbc"], [k3])
                                    TT("pool", t1[:, :n], t1[:, :n], t2[:, :n], ALU.mult, [k1, k2], [k1])
                                    TT("pool", act[:, fc, t0:t0 + n], t1[:, :n], t3[:, :n], ALU.mult, [k1, k3], ["act%d" % fc])
                            for dm in range(8):
                                for (t0, n) in tiles(0, NH):
                                    cnt["d"] ^= 1
                                    py, pyk = ps[4 + cnt["d"]], "ps%d" % (4 + cnt["d"])
                                    for fc in range(8):
                                        MM(py[:, :n], wd[wdi][:, fc, dm * 128:(dm + 1) * 128], act[:, fc, t0:t0 + n],
                                           start=(fc == 0), stop=(fc == 7), reads=["wd%d" % wdi, "act%d" % fc], writes=[pyk])
                                    down_evac(py, pyk, dm, h0 + t0, n)
                        S.barrier()

        def pool_mixer():
            l = 1
            with ExitStack() as PP:
                NS = {"sq": sbt(PP, "sq", [128, 8, 512], BF16), "stdt": sbt(PP, "stdt", [128, 512])}
                rstd = sbt(PP, "rstd", [128, T])
                hb = sbt(PP, "hb", [128, T])
                s2 = sbt(PP, "s2", [128, T])
                s4 = sbt(PP, "s4", [128, T])
                pT = sbt(PP, "pT", [128, 8, T], BF16)
                pw = sbt(PP, "pw", [128, 4, 2, 256], BF16)
                invc = sbt(PP, "invc", [128, 4, 16])
                ptmp = [sbt(PP, "ptmp%d" % i, [128, 512]) for i in range(2)]
                DMA("sp", invc[:], invc_d[:, :].rearrange("p (g s) -> p g s", s=16), writes=["invc"])
                for g in range(4):
                    DMA("pool", pw[:, g, :, :], pool_w[g].rearrange("(i p) d -> p i d", p=128), writes=["pw"])
                for (t0, n) in tiles(0, T):
                    pj, pk = ps_pj()
                    for k in range(8):
                        ACT(NS["sq"][:, k, :n], big[:, k, t0:t0 + n], AF.Square, ["x%d" % k], ["sq%d" % k])
                    for k in range(8):
                        MM(pj[:, :n], ones_b[:], NS["sq"][:, k, :n], start=(k == 0), stop=(k == 7), reads=["sq%d" % k, "ones_b"], writes=[pk])
                    ACT(rstd[:, t0:t0 + n], pj[:, :n], AF.Sqrt, [pk, "eps_t"], ["rstd"], bias=eps_t[:, 0:1], scale=1.0 / 1024.0)
                S.op("dve", lambda e: e.reciprocal(out=rstd[:], in_=rstd[:]), ["rstd"], ["rstd"])
                for k in range(8):
                    g = k // 2
                    w = (2, 4, 8, 16)[g]
                    STT(hb[:], big[:, k, :], mod1[:, 48 + 8 + k:48 + 9 + k], rstd[:], ALU.mult, ALU.mult, ["x%d" % k, "rstd", "mod1"], ["hb"])
                    TS("dve", hb[:], hb[:], mod[:, 48 + k:48 + k + 1], None, ALU.add, reads=["hb", "mod"], writes=["hb"])
                    TS("dve", hb[:, 0:64], hb[:, 0:64], flagw[:, 0:1], None, ALU.mult, reads=["hb", "flag"], writes=["hb"])
                    TT("pool", s2[:, 1:T], hb[:, 1:T], hb[:, 0:T - 1], ALU.add, ["hb"], ["s2"])
                    ws, wsk = s2, "s2"
                    if w >= 4:
                        TT("pool", s4[:, 3:T], s2[:, 3:T], s2[:, 1:T - 2], ALU.add, ["s2"], ["s4"])
                        ws, wsk = s4, "s4"
                    if w >= 8:
                        TT("pool", s2[:, 7:T], s4[:, 7:T], s4[:, 3:T - 4], ALU.add, ["s4", "s2"], ["s2"])
                        ws, wsk = s2, "s2"
                    if w >= 16:
                        TT("pool", s4[:, 15:T], s2[:, 15:T], s2[:, 7:T - 8], ALU.add, ["s2", "s4"], ["s4"])
                        ws, wsk = s4, "s4"
                    STT(pT[:, k, 16:T], ws[:, 16:T], 1.0 / w, hb[:, 16:T], ALU.mult, ALU.subtract, [wsk, "hb"], ["pT%d" % k])
                    TT("dve", ws[:, 64:80], ws[:, 64:80], invc[:, g, :], ALU.mult, [wsk, "invc", "pT%d" % k], [wsk])
                    TT("dve", pT[:, k, 64:80], ws[:, 64:80], hb[:, 64:80], ALU.subtract, [wsk, "hb"], ["pT%d" % k])
                i = 0
                for k in range(8):
                    g, j = k // 2, k % 2
                    for (t0, n) in tiles(HALO, T):
                        pj, pk = ps_pj()
                        for ii in range(2):
                            MM(pj[:, :n], pw[:, g, ii, j * 128:(j + 1) * 128], pT[:, 2 * g + ii, t0:t0 + n], start=(ii == 0), stop=(ii == 1),
                               reads=["pw", "pT%d" % (2 * g + ii)], writes=[pk])
                        i ^= 1
                        TS("dve", ptmp[i][:, :n], pj[:, :n], small[:, 24 + k:25 + k], small[:, 16 + k:17 + k], ALU.add, ALU.mult,
                           reads=[pk, "small_pb", "small_pc"], writes=["ptmp%d" % i])
                        TT("pool", big[:, k, t0:t0 + n], big[:, k, t0:t0 + n], ptmp[i][:, :n], ALU.add, ["x%d" % k, "ptmp%d" % i], ["x%d" % k])
                S.barrier()

        def final_norm():
            with ExitStack() as PF:
                NS = {"sq": sbt(PF, "sq", [128, 8, 512], BF16), "stdt": sbt(PF, "stdt", [128, 512])}
                ob = [sbt(PF, "fo%d" % i, [128, 8, 512]) for i in range(2)]
                for ti, (t0, n) in enumerate(tiles(HALO, T)):
                    o = ob[ti % 2]
                    norm_tile(NS, [big[:, k, t0:t0 + n] for k in range(8)], ["x%d" % k for k in range(8)], n, 0, False,
                              None, None, [o[:, k, :n] for k in range(8)], ["fo%d_%d" % (ti % 2, k) for k in range(8)], final=True)
                    DMA("sp", yT[:, t0 - HALO:t0 - HALO + n].rearrange("(k p) t -> p k t", p=128), o[:, :, :n],
                        reads=["fo%d_%d" % (ti % 2, k) for k in range(8)], writes=["yT%d" % ti], is_out=True)

        if "s0" not in dbg and mode != "F2":
            mixer_norm()
        if mode in ("A", "F"):
            if "s0" not in dbg and "s1" not in dbg:
                try:
                    hg_heads(False)
                    if "s2" not in dbg:
                        ret_heads(False)
                except Stop:
                    pass
        if mode == "F2":
            with ExitStack() as PX:
                Lall_sb = sbt(PX, "Lall_sb", [128, ncores - 1, 8, 128])
                Dall_sb = sbt(PX, "Dall_sb", [128, ncores - 1, 8])
                for w in range(1, ncores):
                    W["w"], W["L"], W["D"] = w, Lall_sb[:, w - 1, :, :], Dall_sb[:, w - 1, :]
                    mixer_norm()
                    hg_heads(False)
                    ret_heads(False)
                W["w"], W["L"], W["D"] = 0, None, None
                chain_states(Lall_sb, Dall_sb)
            mixer_norm()
        if mode == "A":
            DMA("sp", Lst_o[:, :], Lst[:].rearrange("p h e -> p (h e)"), reads=["Lst"], writes=["Lst_o"], is_out=True)
            DMA("sp", Dst_o[:, :], Dst[:], reads=["Dst"], writes=["Dst_o"], is_out=True)
        else:
            for _once in ([1] if mode != "F2" else []):
              with ExitStack() as PX:
                Lall_sb = sbt(PX, "Lall_sb", [128, ncores, 8, 128])
                Dall_sb = sbt(PX, "Dall_sb", [128, ncores, 8])
                if mode == "B":
                    DMA("sp", Lall_sb[:], Lall_d.rearrange("c p (h e) -> p c h e", e=128), writes=["Lall"])
                    DMA("sp", Dall_sb[:], Dall_d.rearrange("c p h -> p c h"), writes=["Dall"])
                else:
                    bounce = nc.dram_tensor("st_bounce", [128, 1032], F32)
                    gath = nc.dram_tensor("st_gath", [ncores * 128, 1032], F32)
                    DMA("sp", bounce[:, 0:1024], Lst[:].rearrange("p h e -> p (h e)"), reads=["Lst"], writes=["bounce"])
                    DMA("sp", bounce[:, 1024:1032], Dst[:], reads=["Dst"], writes=["bounce"])
                    S.barrier()
                    S.op("pool", lambda e: e.collective_compute("AllGather", ALU.bypass, replica_groups=[list(range(ncores))],
                                                                ins=[bounce.ap().opt()], outs=[gath.ap().opt()]),
                         reads=["bounce"], writes=["gath"], dma_key="gath", dma_inc=1)
                    S.barrier()
                    gv = gath.ap().rearrange("(c p) w -> p c w", p=128)
                    DMA("sp", Lall_sb[:].rearrange("p c h e -> p c (h e)"), gv[:, :, 0:1024], reads=["gath"], writes=["Lall"])
                    DMA("sp", Dall_sb[:], gv[:, :, 1024:1032], reads=["gath"], writes=["Dall"])
                chain_states(Lall_sb, Dall_sb)
            dump("Sin", Sin[:].rearrange("p h e -> p (h e)"), [128, 1024], "Sin")
            hg_heads(True)
            ret_heads(True)
            if "cat" in dbg:
                cf = sbt(es, "catf", [128, 8, T])
                for h in range(8):
                    COPY("act", cf[:, h, :], catT[:, h, :], ["cat%d" % h], ["catf"])
                dump("cat", cf[:].rearrange("p h t -> p (h t)"), [128, 8 * T], "catf")
            wout_residual()
            dump("x1", big[:].rearrange("p k t -> p (k t)"), [128, 8 * T], "x0")
            if "stop1" not in dbg:
                moe(0, 0, T)
                dump("x2", big[:].rearrange("p k t -> p (k t)"), [128, 8 * T], "x0")
                pool_mixer()
                dump("x3", big[:].rearrange("p k t -> p (k t)"), [128, 8 * T], "x0")
                moe(1, HALO, T)
            final_norm()
        S.finish()
        S.emit()
    return nc, dbg_outs, in_names


def _consts(T, tok0):
    gam = [1.0 - 2.0 ** (-5 - h) for h in range(4)]
    idx = np.arange(64)
    rel = idx[None, :] - idx[:, None]
    masks = np.zeros((64, 5, 64), np.float64)
    masks[:, 0, :] = (rel >= 0)
    for h in range(4):
        masks[:, 1 + h, :] = np.where(rel >= 0, gam[h] ** np.maximum(rel, 0), 0.0)
    kscale = np.ones((64, 8), np.float64)
    for h in range(4):
        kscale[:, 4 + h] = gam[h] ** (63 - idx)
    gdec = np.zeros((128, 4, 64), np.float64)
    for h in range(4):
        gdec[:, h, :] = (gam[h] ** (idx + 1.0))[None, :]
    half = 64
    inv = (10000.0 ** (-np.arange(half, dtype=np.float32) / half)).astype(np.float32)
    pos = (tok0 + np.arange(T)).astype(np.float32)
    ang = (pos[None, :] * inv[:, None]).astype(np.float32)
    cos = np.cos(ang.astype(np.float64))
    sin = np.sin(ang.astype(np.float64))
    cosT = np.concatenate([cos, cos], 0)
    sinT = np.concatenate([-sin, sin], 0)
    nch = T // 64
    retW = np.zeros((128, 4, nch), np.float64)
    for h in range(4):
        for c in range(nch - 1):
            retW[:, h, c] = gam[h] ** (64.0 * (nch - 2 - c))
    scanm = np.ones((128, T), np.float32)
    scanm[:, ::64] = 0.0
    return dict(masks=masks.reshape(64, 320).astype(np.float32), kscale=kscale.astype(np.float32),
                gdec=gdec.reshape(128, 256).astype(np.float32), cosT=cosT.astype(np.float32),
                sinT=sinT.astype(np.float32), scanm=scanm, ident=np.eye(128, dtype=np.float32),
                retW=retW.reshape(128, 4 * nch).astype(np.float32))


def _pk(v, ncol):
    return np.ascontiguousarray(np.asarray(v, np.float32).reshape(ncol, 128).T)


def make_inputs(inp, ncores, nw=8):
    x = np.asarray(inp["x"], np.float32)[0]
    Sq = x.shape[0]
    TP = Sq // ncores
    T = TP + HALO
    w_in = np.asarray(inp["w_in"], np.float32)[0]
    perm = np.concatenate([np.arange(64, 128), np.arange(0, 64)])
    cols = []
    for base in (2048, 2560):
        for r in range(4):
            cols.append(base + r * 128 + perm)
    w_in_sw = np.ascontiguousarray(w_in[:, np.concatenate(cols)])
    shared = dict(
        cT=_pk(np.asarray(inp["c"])[0], 8),
        w_ada=np.asarray(inp["w_ada"], np.float32),
        b_adaT=np.concatenate([_pk(np.asarray(inp["b_ada"])[l], 48) for l in range(2)], 1),
        w_in=w_in, w_in_sw=w_in_sw,
        lbraw=np.concatenate([_pk(np.asarray(inp["hg_lower_bounds"])[r], 4) for r in range(2)], 1),
        gains=np.concatenate([_pk(np.asarray(inp["hg_norm"])[0].reshape(-1), 4),
                              _pk(np.asarray(inp["ret_norm"])[0].reshape(-1), 4)], 1),
        w_out=np.asarray(inp["w_out"], np.float32)[0],
        pool_w=np.asarray(inp["pool_w"], np.float32)[0],
        pool_bT=_pk(np.asarray(inp["pool_b"])[0].reshape(-1), 8),
        pool_sT=_pk(np.asarray(inp["pool_scale"])[0], 8),
        w_router=np.asarray(inp["w_router"], np.float32),
        b_router=np.asarray(inp["b_router"], np.float32),
        w_gu=np.asarray(inp["w_gu"], np.float32),
        b_guT=_pk(np.asarray(inp["b_gu"]).reshape(-1), 2 * 32 * 16),
        w_down=np.asarray(inp["w_down"], np.float32),
        b_down=np.asarray(inp["b_down"], np.float32),
        fnT=_pk(np.asarray(inp["final_norm"]), 8),
    )
    win = {}
    for c in range(ncores):
        tok0 = c * TP - HALO
        xs = np.zeros((T, 1024), np.float32)
        lo = max(tok0, 0)
        xs[lo - tok0:, :] = x[lo:tok0 + T, :]
        cst = _consts(T, tok0)
        win[c] = (np.ascontiguousarray(xs.T), cst["cosT"], cst["sinT"])
    zero_w = (np.zeros((1024, T), np.float32), np.zeros((128, T), np.float32), np.zeros((128, T), np.float32))
    maps = []
    for j in range(ncores):
        m = dict(shared)
        cst = _consts(T, j * TP - HALO)
        for k in ("masks", "kscale", "gdec", "scanm", "ident", "retW"):
            m[k] = cst[k]
        ws = [win[j - w] if j - w >= 0 else zero_w for w in range(nw)]
        m["xT"] = np.stack([w_[0] for w_ in ws], 0)
        m["cosT"] = np.stack([w_[1] for w_ in ws], 0)
        m["sinT"] = np.stack([w_[2] for w_ in ws], 0)
        flag = np.ones((128, 8), np.float32)
        valid = np.zeros((128, 8), np.float32)
        for w in range(8):
            if j - w <= 0:
                flag[:, w] = 0.0
            if w >= 1 and j - w >= 0:
                valid[:, w - 1] = 1.0
        m["flag"] = flag
        m["valid"] = valid
        seli = np.zeros((128, 8), np.float32)
        seli[:, :j] = 1.0
        m["seli"] = seli
        invc = np.zeros((128, 4, 16), np.float32)
        for g, w in enumerate((2, 4, 8, 16)):
            tg = j * TP + np.arange(16)
            invc[:, g, :] = (1.0 / np.minimum(tg + 1, w))[None, :]
        m["invc"] = invc.reshape(128, 64)
        maps.append(m)
    return maps, TP


_CACHE = {}


def _get(TP, mode, ncores):
    key = (TP, mode, ncores)
    if key not in _CACHE:
        r = build(TP, mode, ncores)
        _CACHE[key] = (r[0], r[2])
    return _CACHE[key]


def kernel(**inp):
    ncores = 8
    maps, TP = make_inputs(inp, ncores, 8)
    ncF, names = _get(TP, "F2", ncores)
    r = run_bass_kernel_spmd(ncF, [{k: m[k] for k in names} for m in maps], core_ids=list(range(ncores)))
    y = np.concatenate([r.results[j]["yT"].T for j in range(ncores)], 0)
    return np.ascontiguousarray(y[None].astype(np.float32))
```

```python
from contextlib import ExitStack
import numpy as np
import concourse.bass as bass
import concourse.mybir as mybir
from concourse.bass_utils import run_bass_kernel_spmd

F32 = mybir.dt.float32
BF16 = mybir.dt.bfloat16
ALU = mybir.AluOpType
AF = mybir.ActivationFunctionType
AX = mybir.AxisListType
ENGS = ("pe", "act", "dve", "pool", "sp")
EPS = 1e-6
HALO = 64


class Sched:
    def __init__(self, nc, es):
        self.nc, self.es = nc, es
        self.items = {e: [] for e in ENGS}
        self.cnt = {e: 0 for e in ENGS}
        self.clock = {e: {} for e in ENGS}
        self.snap, self.key_w, self.key_r, self.dma_cnt, self.sems = {}, {}, {}, {}, {}
        self.out_events = []
        self.epoch = 0
        self.ek = {e: "E:" + e for e in ENGS}
        for e in ENGS:
            self._sem(self.ek[e])

    def _sem(self, k):
        if k not in self.sems:
            self.sems[k] = self.es.enter_context(self.nc.semaphore("s%d" % len(self.sems)))
        return self.sems[k]

    def _need(self, eng, ev, deps):
        if ev is None:
            return
        sk, val = ev
        if eng == "pe" and sk.startswith("E:pe"):
            return
        if self.clock[eng].get(sk, 0) >= val:
            return
        if deps.get(sk, 0) < val:
            deps[sk] = val

    def _apply(self, eng, deps):
        clk = self.clock[eng]
        for sk, val in deps.items():
            self.items[eng].append(("w", sk, val))
            for a, b in self.snap.get((sk, val), {}).items():
                if clk.get(a, 0) < b:
                    clk[a] = b
            if clk.get(sk, 0) < val:
                clk[sk] = val

    def op(self, eng, fn, reads=(), writes=(), dma_key=None, is_out=False, dma_inc=16):
        writes = [k.split("_")[0] if k.startswith("ps") else k for k in writes]
        writes += [k.split("_")[0] for k in reads if k.startswith("ps")]
        reads = [k for k in reads if not k.startswith("ps")]
        deps = {}
        for k in reads:
            self._need(eng, self.key_w.get(k), deps)
        for k in writes:
            self._need(eng, self.key_w.get(k), deps)
            for ev in self.key_r.get(k, ()):
                self._need(eng, ev, deps)
        self._apply(eng, deps)
        if dma_key is not None:
            sk = "D:" + str(dma_key)
            self._sem(sk)
            self.dma_cnt[sk] = self.dma_cnt.get(sk, 0) + 1
            ev = (sk, self._dval(sk, dma_inc))
            self.items[eng].append(("i", fn, sk, dma_inc))
            if is_out:
                self.out_events.append(ev)
        else:
            self.cnt[eng] += 1
            ev = (self.ek[eng], self.cnt[eng])
            self.items[eng].append(("i", fn, self.ek[eng], 1))
        self.snap[ev] = dict(self.clock[eng])
        for k in writes:
            self.key_w[k] = ev
            self.key_r[k] = []
        for k in reads:
            self.key_r.setdefault(k, []).append(ev)
        return ev

    def _dval(self, sk, inc):
        self.dma_val = getattr(self, "dma_val", {})
        self.dma_val[sk] = self.dma_val.get(sk, 0) + inc
        return self.dma_val[sk]

    def barrier(self):
        evs = [(self.ek[e], self.cnt[e]) for e in ENGS if self.cnt[e] > 0]
        evs += [(sk, v) for sk, v in getattr(self, "dma_val", {}).items()]
        for eng in ENGS:
            deps = {}
            for sk, val in evs:
                if self.clock[eng].get(sk, 0) < val:
                    deps[sk] = val
            self._apply(eng, deps)
        self.key_w, self.key_r = {}, {}
        self.epoch += 1
        for e in ENGS:
            if self.cnt[e] > 12000:
                self.ek[e] = "E:%s:%d" % (e, self.epoch)
                self._sem(self.ek[e])
                self.cnt[e] = 0

    def finish(self):
        deps = {}
        for ev in self.out_events:
            self._need("sp", ev, deps)
        self._apply("sp", deps)

    def emit(self):
        nc = self.nc
        with nc.Block() as block:
            def run(name):
                def body(engine):
                    for it in self.items[name]:
                        if it[0] == "w":
                            engine.wait_ge(self.sems[it[1]], it[2])
                        else:
                            it[1](engine).then_inc(self.sems[it[2]], it[3])
                return body
            block.tensor(run("pe"))
            block.scalar(run("act"))
            block.vector(run("dve"))
            block.gpsimd(run("pool"))
            block.sync(run("sp"))


class Stop(Exception):
    pass


def tiles(lo, hi, step=512):
    return [(t, min(step, hi - t)) for t in range(lo, hi, step)]


def build(TP, mode, ncores=8, dbg=()):
    T = TP + HALO
    NCH = T // 64
    nc = bass.Bass("TRN2", target_bir_lowering=False)

    in_names = []

    def din(name, shape):
        in_names.append(name)
        return nc.dram_tensor(name, list(shape), F32, kind="ExternalInput").ap()

    def dout(name, shape):
        return nc.dram_tensor(name, list(shape), F32, kind="ExternalOutput").ap()

    NW = 8 if mode == "F2" else 1
    xTw = din("xT", [NW, 1024, T])
    flag_d = din("flag", [128, 8])
    valid_d = din("valid", [128, 8])
    cT_d = din("cT", [128, 8])
    w_ada = din("w_ada", [2, 1024, 6144])
    badaT_d = din("b_adaT", [128, 96])
    w_in = din("w_in", [1024, 4096])
    w_in_sw = din("w_in_sw", [1024, 1024])
    lbraw_d = din("lbraw", [128, 8])
    gains_d = din("gains", [128, 8])
    w_out = din("w_out", [1024, 1024]) if mode != "A" else None
    pool_w = din("pool_w", [4, 256, 256]) if mode != "A" else None
    pool_bT_d = din("pool_bT", [128, 8])
    pool_sT_d = din("pool_sT", [128, 8])
    w_router = din("w_router", [2, 1024, 32]) if mode != "A" else None
    b_router = din("b_router", [2, 32]) if mode != "A" else None
    w_gu = din("w_gu", [2, 32, 1024, 2048]) if mode != "A" else None
    b_guT_d = din("b_guT", [128, 2 * 32 * 16]) if mode != "A" else None
    w_down = din("w_down", [2, 32, 1024, 1024]) if mode != "A" else None
    b_down = din("b_down", [2, 32, 1024]) if mode != "A" else None
    fnT_d = din("fnT", [128, 8])
    ident_d = din("ident", [128, 128])
    masks_d = din("masks", [64, 5 * 64])
    kscale_d = din("kscale", [64, 8])
    cos_d = din("cosT", [NW, 128, T])
    sin_d = din("sinT", [NW, 128, T])
    gdec_d = din("gdec", [128, 4 * 64])
    scanm_d = din("scanm", [128, T])
    invc_d = din("invc", [128, 4 * 16])
    seli_d = din("seli", [128, 8])
    retW_d = din("retW", [128, 4 * NCH])
    if mode == "A":
        Lst_o = dout("Lst", [128, 8 * 128])
        Dst_o = dout("Dst", [128, 8])
    if mode == "B":
        Lall_d = din("Lall", [ncores, 128, 8 * 128])
        Dall_d = din("Dall", [ncores, 128, 8])
    if mode in ("B", "F", "F2"):
        yT = dout("yT", [1024, TP])
    dbg_outs = {}

    gam = [1.0 - 2.0 ** (-5 - h) for h in range(4)]

    with ExitStack() as es:
        S = Sched(nc, es)

        uniq = [0]

        def sbt(stack, name, shape, dt=F32):
            uniq[0] += 1
            return stack.enter_context(nc.sbuf_tensor("sb%d_%s" % (uniq[0], name), list(shape), dt))

        def DMA(eng, out, in_, reads=(), writes=(), key=None, is_out=False):
            S.op(eng, lambda e: e.dma_start(out=out, in_=in_), reads, writes, dma_key=key or writes[0], is_out=is_out)

        def MM(out, lhsT, rhs, start=True, stop=True, reads=(), writes=()):
            S.op("pe", lambda e: e.matmul(out, lhsT, rhs, start=start, stop=stop), reads, writes)

        def TR(out, in_, ident, reads=(), writes=()):
            S.op("pe", lambda e: e.transpose(out, in_, ident), reads, writes)

        def ACT(out, in_, func, reads=(), writes=(), bias=None, scale=None):
            kw = {}
            if bias is not None:
                kw["bias"] = bias
            if scale is not None:
                kw["scale"] = scale
            S.op("act", lambda e: e.activation(out=out, in_=in_, func=func, **kw), reads, writes)

        def TT(eng, out, in0, in1, op, reads=(), writes=()):
            S.op(eng, lambda e: e.tensor_tensor(out=out, in0=in0, in1=in1, op=op), reads, writes)

        def TS(eng, out, in0, s1, s2, op0, op1=None, reads=(), writes=()):
            if op1 is None:
                S.op(eng, lambda e: e.tensor_scalar(out=out, in0=in0, scalar1=s1, scalar2=None, op0=op0), reads, writes)
            else:
                S.op(eng, lambda e: e.tensor_scalar(out=out, in0=in0, scalar1=s1, scalar2=s2, op0=op0, op1=op1), reads, writes)

        def STT(out, in0, scalar, in1, op0, op1, reads=(), writes=()):
            S.op("dve", lambda e: e.scalar_tensor_tensor(out=out, in0=in0, scalar=scalar, in1=in1, op0=op0, op1=op1),
                 reads, writes)

        def MEMSET(eng, ap, val, writes=()):
            S.op(eng, lambda e: e.memset(ap, val), (), writes)

        def COPY(eng, out, in_, reads=(), writes=()):
            if eng == "act":
                ACT(out, in_, AF.Identity, reads, writes)
            else:
                S.op(eng, lambda e: e.tensor_copy(out=out, in_=in_), reads, writes)

        def dump(name, ap, shape, key):
            if name in dbg:
                d = dout("dbg_" + name, shape)
                dbg_outs[name] = d
                DMA("sp", d, ap, reads=[key], writes=["dbg_" + name], is_out=True)

        G = es
        big = sbt(G, "big", [128, 8, T])
        bigb = big[:].rearrange("p k t -> p (k t)").bitcast(BF16).rearrange("p (k t) -> p k t", k=16)
        hnT = bigb[:, 0:8, :]
        catT = bigb[:, 8:16, :]
        ps = [G.enter_context(nc.psum_tensor("ps%d" % i, [128, 512], F32)) for i in range(7)]
        psb = G.enter_context(nc.psum_tensor("psb", [128, 1024], BF16))
        ident_f = sbt(G, "ident_f", [128, 128])
        ident_b = sbt(G, "ident_b", [128, 128], BF16)
        ones_b = sbt(G, "ones_b", [128, 128], BF16)
        ones_f = sbt(G, "ones_f", [128, 128])
        eps_t = sbt(G, "eps_t", [128, 1])
        flagw = sbt(G, "flag_s", [128, 8])
        W = {"w": 0, "L": None, "D": None}
        mod = sbt(G, "mod", [128, 96])
        mod1 = sbt(G, "mod1", [128, 96])
        small = sbt(G, "small", [128, 64])
        Lst = sbt(G, "Lst_s", [128, 8, 128])
        Dst = sbt(G, "Dst_s", [128, 8])
        Sin = sbt(G, "Sin_s", [128, 8, 128])
        seli = sbt(G, "seli_s", [128, 8])

        DMA("sp", ident_f[:], ident_d[:, :], writes=["ident_f"])
        DMA("pool", ident_b[:], ident_d[:, :], writes=["ident_b"])
        DMA("sp", flagw[:], flag_d[:, :], writes=["flag"])
        DMA("sp", seli[:], (valid_d if mode == "F2" else seli_d)[:, :], writes=["seli"])
        MEMSET("pool", ones_b[:], 1.0, ["ones_b"])
        MEMSET("pool", ones_f[:], 1.0, ["ones_f"])
        MEMSET("pool", eps_t[:], EPS, ["eps_t"])
        DMA("sp", small[:, 40:48], lbraw_d[:, :], writes=["small_lbraw"])
        DMA("sp", small[:, 8:16], gains_d[:, :], writes=["small_g"])
        DMA("sp", small[:, 16:24], pool_sT_d[:, :], writes=["small_ps"])
        DMA("sp", small[:, 24:32], pool_bT_d[:, :], writes=["small_pb"])
        DMA("sp", small[:, 32:40], fnT_d[:, :], writes=["small_fn"])
        TT("dve", small[:, 48:52], small[:, 40:44], small[:, 44:48], ALU.subtract, ["small_lbraw"], ["small_t"])
        ACT(small[:, 0:4], small[:, 48:52], AF.Sigmoid, ["small_t"], ["small_lb"])
        TS("dve", small[:, 4:8], small[:, 0:4], -1.0, 1.0, ALU.mult, ALU.add, ["small_lb"], ["small_oml"])

        with ExitStack() as P0:
            cT = sbt(P0, "cT_s", [128, 8])
            cond = sbt(P0, "cond", [128, 8])
            badaT = sbt(P0, "badaT", [128, 96])
            wa = [sbt(P0, "wa%d" % i, [128, 8, 768]) for i in range(2)]
            DMA("sp", cT[:], cT_d[:, :], writes=["cT"])
            DMA("sp", badaT[:], badaT_d[:, :], writes=["badaT"])
            ACT(cond[:], cT[:], AF.Silu, ["cT"], ["cond"])
            for l in range(2):
                for blk in range(8):
                    i = (l * 8 + blk) % 2
                    DMA("sp", wa[i][:], w_ada[l, :, blk * 768:(blk + 1) * 768].rearrange("(k p) c -> p k c", p=128),
                        writes=["wa%d" % i])
                    for m in range(6):
                        col = l * 48 + blk * 6 + m
                        for k in range(8):
                            MM(ps[0][:, col:col + 1], wa[i][:, k, m * 128:(m + 1) * 128], cond[:, k:k + 1],
                               start=(k == 0), stop=(k == 7), reads=["wa%d" % i, "cond"], writes=["ps0"])
            TT("dve", mod[:], ps[0][:, 0:96], badaT[:], ALU.add, ["ps0", "badaT"], ["mod"])
            TS("dve", mod1[:], mod[:], 1.0, None, ALU.add, reads=["mod"], writes=["mod1"])
            TT("dve", small[:, 16:24], small[:, 16:24], mod[:, 48 + 16:48 + 24], ALU.mult, ["small_ps", "mod"], ["small_pc"])
            S.barrier()
        dump("mod", mod[:], [128, 96], "mod")

        rot = {"pj": 0, "at": 0, "o": 0, "ds": 0}

        def ps_pj():
            rot["pj"] ^= 1
            return ps[rot["pj"]], "ps%d" % rot["pj"]

        def norm_tile(NS, xk, xkeys, n, l, ffn, out_bf, out_bf_keys, out_f32=None, out_f32_keys=None, final=False):
            base = l * 48 + (24 if ffn else 0)
            pj, pk = ps_pj()
            for k in range(8):
                ACT(NS["sq"][:, k, :n], xk[k], AF.Square, [xkeys[k]], ["sq%d" % k])
            for k in range(8):
                MM(pj[:, :n], ones_b[:], NS["sq"][:, k, :n], start=(k == 0), stop=(k == 7),
                   reads=["sq%d" % k, "ones_b"], writes=[pk])
            ACT(NS["stdt"][:, :n], pj[:, :n], AF.Sqrt, [pk, "eps_t"], ["stdt"], bias=eps_t[:, 0:1], scale=1.0 / 1024.0)
            S.op("dve", lambda e: e.reciprocal(out=NS["stdt"][:, :n], in_=NS["stdt"][:, :n]), ["stdt"], ["stdt"])
            for k in range(8):
                if final:
                    STT(out_f32[k], xk[k], small[:, 32 + k:33 + k], NS["stdt"][:, :n], ALU.mult, ALU.mult,
                        [xkeys[k], "stdt", "small_fn"], [out_f32_keys[k]])
                    continue
                STT(NS["t1"][:, k, :n], xk[k], mod1[:, base + 8 + k:base + 9 + k], NS["stdt"][:, :n], ALU.mult, ALU.mult,
                    [xkeys[k], "stdt", "mod1"], ["t1_%d" % k])
                if out_f32 is not None:
                    ACT(out_f32[k], NS["t1"][:, k, :n], AF.Identity, ["t1_%d" % k, "mod"], [out_f32_keys[k]],
                        bias=mod[:, base + k:base + k + 1])
                    COPY("pool", out_bf[k], out_f32[k], [out_f32_keys[k]], [out_bf_keys[k]])
                else:
                    ACT(out_bf[k], NS["t1"][:, k, :n], AF.Identity, ["t1_%d" % k, "mod"], [out_bf_keys[k]],
                        bias=mod[:, base + k:base + k + 1])

        def mixer_norm():
            with ExitStack() as PN:
                NS = {"sq": sbt(PN, "sq", [128, 8, 512], BF16), "stdt": sbt(PN, "stdt", [128, 512]),
                      "t1": sbt(PN, "t1", [128, 8, 512])}
                xt = [sbt(PN, "xt%d" % i, [128, 8, 512]) for i in range(2)]
                for ti, (t0, n) in enumerate(tiles(0, T)):
                    b = xt[ti % 2]
                    DMA("sp", b[:, :, :n], xTw[W["w"], :, t0:t0 + n].rearrange("(k p) t -> p k t", p=128), writes=["xt%d" % (ti % 2)])
                    norm_tile(NS, [b[:, k, :n] for k in range(8)], ["xt%d" % (ti % 2)] * 8, n, 0, False,
                              [hnT[:, k, t0:t0 + n] for k in range(8)], ["hn%d" % k for k in range(8)])
                S.barrier()

        def proj_fm(whb, qi, t0, n):
            wh, whk = whb
            pj, pk = ps_pj()
            for k in range(8):
                MM(pj[:, :n], wh[:, k, qi, :], hnT[:, k, t0:t0 + n], start=(k == 0), stop=(k == 7),
                   reads=[whk, "hn%d" % k], writes=[pk])
            return pj, pk

        def proj_v(whb, qi, vtok):
            wh, whk = whb
            for c0 in range(0, NCH, 4):
                pj, pk = ps_pj()
                ncs = min(4, NCH - c0)
                for ci in range(ncs):
                    c = c0 + ci
                    for k in range(8):
                        MM(pj[0:64, ci * 128:(ci + 1) * 128], hnT[:, k, c * 64:(c + 1) * 64], wh[:, k, qi, :],
                           start=(k == 0), stop=(k == 7), reads=[whk, "hn%d" % k], writes=[pk])
                ACT(vtok[:, c0:c0 + ncs, :], pj[0:64, 0:ncs * 128].rearrange("p (c e) -> p c e", e=128), AF.Identity,
                    [pk], ["vtok"])

        def k_transposes(kA, ktok, ksc):
            for c0 in range(0, NCH, 8):
                ncs = min(8, NCH - c0)
                for ci in range(ncs):
                    c = c0 + ci
                    TR(psb[0:64, ci * 128:(ci + 1) * 128], kA[:, c * 64:(c + 1) * 64], ident_b[:], ["kA", "ident_b"], ["psb"])
                TS("dve", ktok[:, c0:c0 + ncs, :], psb[0:64, 0:ncs * 128].rearrange("p (c e) -> p c e", e=128),
                   ksc, None, ALU.mult, reads=["psb", "kscale"], writes=["ktok"])

        def recurrence(h, HS, full, mask_ap):
            St, Sb = HS["S"], HS["Sb"]
            if not full:
                for c in range(NCH - 1):
                    MM(ps[6][:, 0:128], HS["ktok"][:, c, :], HS["vtok"][:, c, :], start=(c == 0), stop=(c == NCH - 2),
                       reads=["ktok", "vtok"], writes=["ps6"])
                COPY("act", (W["L"][:, h, :] if W["L"] is not None else Lst[:, h, :]), ps[6][:, 0:128], ["ps6"], ["Lst"])
                return
            if full:
                COPY("pool", St[:], Sin[:, h, :], ["Sin"], ["S"])
            else:
                MEMSET("pool", St[:], 0.0, ["S"])
            nch = NCH if full else NCH - 1
            ob = None
            for c in range(nch):
                cs = slice(c * 64, (c + 1) * 64)
                if full:
                    rot["at"] ^= 1
                    pa, pak = ps[2 + rot["at"]], "ps%d" % (2 + rot["at"])
                    MM(pa[0:64, 0:64], HS["kA"][:, cs], HS["qA"][:, cs], reads=["kA", "qA"], writes=[pak])
                    atm = HS["atm"][rot["at"]]
                    TT("dve", atm[:], pa[0:64, 0:64], mask_ap, ALU.mult, [pak, "masks"], ["atm%d" % rot["at"]])
                    TS("pool", Sb[:], St[:], HS["tabM"][:, c:c + 1], None, ALU.mult, reads=["S", "tabM"], writes=["Sb"])
                    if c % 8 == 0:
                        rot["o"] ^= 1
                        ob, obk = ps[4 + rot["o"]], "ps%d" % (4 + rot["o"])
                    oc = (c % 8) * 64
                    MM(ob[:, oc:oc + 64], HS["vtok"][:, c, :], atm[:], start=True, stop=False,
                       reads=["vtok", "atm%d" % rot["at"]], writes=[obk])
                    MM(ob[:, oc:oc + 64], Sb[:], HS["qB"][:, cs], start=False, stop=True, reads=["Sb", "qB"], writes=[obk])
                    if c % 8 == 7 or c == nch - 1:
                        c0 = (c // 8) * 8
                        headnorm(h, HS, ob, obk, c0 * 64, (c + 1 - c0) * 64)
                if c < NCH - 1:
                    rot["ds"] = (rot["ds"] + 1) % 4
                    pd = ps[6][:, rot["ds"] * 128:(rot["ds"] + 1) * 128]
                    pdk = "ps6_%d" % rot["ds"]
                    MM(pd, HS["ktok"][:, c, :], HS["vtok"][:, c, :], reads=["ktok", "vtok"], writes=[pdk])
                    ACT(HS["dst"][:], pd, AF.Identity, [pdk, "tabC"], ["dst"], scale=HS["tabC"][:, c:c + 1])
                    STT(St[:], St[:], HS["tabA"][:, c:c + 1], HS["dst"][:], ALU.mult, ALU.add, ["S", "tabA", "dst"], ["S"])
            if not full:
                COPY("pool", (W["L"][:, h, :] if W["L"] is not None else Lst[:, h, :]), St[:], ["S"], ["Lst"])

        def headnorm(h, HS, ob, obk, t0, n):
            ACT(HS["osq"][:, :n], ob[:, :n], AF.Square, [obk], ["osq"])
            pj, pk = ps_pj()
            MM(pj[:, :n], ones_b[:], HS["osq"][:, :n], reads=["osq", "ones_b"], writes=[pk])
            ACT(HS["ostd"][:, :n], pj[:, :n], AF.Sqrt, [pk, "eps_t"], ["ostd"], bias=eps_t[:, 0:1], scale=1.0 / 128.0)
            S.op("dve", lambda e: e.reciprocal(out=HS["ostd"][:, :n], in_=HS["ostd"][:, :n]), ["ostd"], ["ostd"])
            STT(HS["ot"][:, :n], ob[:, :n], small[:, 8 + h:9 + h], HS["ostd"][:, :n], ALU.mult, ALU.mult,
                [obk, "ostd", "small_g"], ["ot"])
            TT("pool", catT[:, h, t0:t0 + n], HS["ot"][:, :n], HS["gate"][:, t0:t0 + n], ALU.mult, ["ot", "gate"], ["cat%d" % h])

        def head_common(PH, full):
            HS = {"kA": sbt(PH, "kA", [128, T], BF16), "ktok": sbt(PH, "ktok", [64, NCH, 128], BF16),
                  "vtok": sbt(PH, "vtok", [64, NCH, 128], BF16), "S": sbt(PH, "S", [128, 128]),
                  "Sb": sbt(PH, "Sb", [128, 128], BF16), "dst": sbt(PH, "dst", [128, 128]),
                  "tabA": sbt(PH, "tabA", [128, NCH]), "tabC": sbt(PH, "tabC", [128, NCH]), "tabM": sbt(PH, "tabM", [128, NCH]),
                  "tmp": [sbt(PH, "tmp%d" % i, [128, 512]) for i in range(3)]}
            if full:
                HS.update({"qA": sbt(PH, "qA", [128, T], BF16), "gate": sbt(PH, "gate", [128, T], BF16),
                           "atm": [sbt(PH, "atm%d" % i, [64, 64], BF16) for i in range(2)],
                           "osq": sbt(PH, "osq", [128, 512], BF16), "ostd": sbt(PH, "ostd", [128, 512]),
                           "ot": sbt(PH, "ot", [128, 512])})
            return HS

        def hg_heads(full):
            with ExitStack() as PH:
                HS = head_common(PH, full)
                if full:
                    HS["qB"] = HS["qA"]
                whs = [sbt(PH, "wh%d" % i, [128, 8, 4, 128], BF16) for i in range(2)]

                def load_wh(h_):
                    for qi in (range(4) if full else (1, 2)):
                        DMA("pool", whs[h_ % 2][:, :, qi, :], w_in[:, qi * 512 + h_ * 128: qi * 512 + (h_ + 1) * 128]
                            .rearrange("(k p) c -> p k c", p=128), writes=["wh%d" % (h_ % 2)])
                ff = sbt(PH, "ff", [128, T])
                bA = sbt(PH, "bA", [128, T])
                bB = sbt(PH, "bB", [128, T])
                scanm = sbt(PH, "scanm", [128, T])
                masks = sbt(PH, "masks", [64, 64])
                ksc = sbt(PH, "ksc", [64, 8])
                tmpd = sbt(PH, "tmpd", [128, NCH])
                pref = sbt(PH, "pref", [128, NCH])
                Wt = sbt(PH, "Wt", [128, NCH])
                onesN = sbt(PH, "onesN", [128, NCH])
                MEMSET("pool", onesN[:], 1.0, ["onesN"])
                sumbl = sbt(PH, "sumbl", [128, 1])
                DMA("sp", scanm[:], scanm_d[:, :], writes=["scanm"])
                DMA("sp", masks[:], masks_d[:, 0:64], writes=["masks"])
                DMA("sp", ksc[:], kscale_d[:, :], writes=["kscale"])
                b3 = bB[:].rearrange("p (c s) -> p c s", s=64)
                a3 = bA[:].rearrange("p (c s) -> p c s", s=64)
                load_wh(0)
                for h in range(4):
                    if h + 1 < 4:
                        load_wh(h + 1)
                    wh = (whs[h % 2], "wh%d" % (h % 2))
                    for ti, (t0, n) in enumerate(tiles(0, T)):
                        pj, pk = proj_fm(wh, 1, t0, n)
                        tm = HS["tmp"][ti % 2]
                        ACT(tm[:, :n], pj[:, :n], AF.Sigmoid, [pk], ["tmp%d" % (ti % 2)])
                        TS("dve", ff[:, t0:t0 + n], tm[:, :n], small[:, 4 + h:5 + h], small[:, h:h + 1], ALU.mult, ALU.add,
                           reads=["tmp%d" % (ti % 2), "small_lb", "small_oml"], writes=["ff"])
                    if "h1" in dbg:
                        return
                    ACT(bA[:], ff[:], AF.Ln, ["ff"], ["bA"])
                    if "h1b" in dbg:
                        return
                    S.op("dve", lambda e: e.tensor_tensor_scan(out=bB[:], data0=scanm[:], data1=bA[:], initial=0.0,
                                                               op0=ALU.mult, op1=ALU.add), ["scanm", "bA"], ["bB"])
                    if "h1c" in dbg:
                        return
                    ACT(HS["tabA"][:], b3[:, :, 63], AF.Exp, ["bB"], ["tabA"])
                    TT("dve", tmpd[:], b3[:, :, 63], b3[:, :, 31], ALU.subtract, ["bB"], ["tmpd"])
                    ACT(HS["tabC"][:], tmpd[:], AF.Exp, ["tmpd"], ["tabC"])
                    ACT(HS["tabM"][:], b3[:, :, 31], AF.Exp, ["bB"], ["tabM"])
                    S.op("dve", lambda e: e.reduce_sum(out=sumbl[:], in_=b3[:, 0:NCH - 1, 63], axis=AX.X), ["bB"], ["sumbl"])
                    ACT((W["D"] if W["D"] is not None else Dst)[:, h:h + 1], sumbl[:], AF.Exp, ["sumbl"], ["Dst"])
                    TT("dve", HS["tabA"][:, 0:1], HS["tabA"][:, 0:1], flagw[:, W["w"]:W["w"] + 1], ALU.mult, ["tabA", "flag"], ["tabA"])
                    TT("dve", HS["tabC"][:, 0:1], HS["tabC"][:, 0:1], flagw[:, W["w"]:W["w"] + 1], ALU.mult, ["tabC", "flag"], ["tabC"])
                    if "h1d" in dbg:
                        return
                    if not full:
                        S.op("dve", lambda e: e.tensor_tensor_scan(out=pref[:], data0=onesN[:], data1=b3[:, :, 63], initial=0.0,
                                                                   op0=ALU.mult, op1=ALU.add), ["onesN", "bB"], ["pref"])
                        TT("dve", Wt[:, 0:NCH - 1], tmpd[:, 0:NCH - 1], pref[:, 0:NCH - 1], ALU.subtract, ["tmpd", "pref"], ["Wt"])
                        ACT(Wt[:, 0:NCH - 1], Wt[:, 0:NCH - 1], AF.Exp, ["Wt", "pref"], ["Wt"], bias=pref[:, NCH - 2:NCH - 1])
                        MEMSET("pool", Wt[:, NCH - 1:NCH], 0.0, ["Wt"])
                        TT("dve", Wt[:, 0:1], Wt[:, 0:1], flagw[:, W["w"]:W["w"] + 1], ALU.mult, ["Wt", "flag"], ["Wt"])
                    TT("dve", a3, b3, b3[:, :, 31:32].broadcast_to([128, NCH, 64]), ALU.subtract, ["bB", "bA"], ["bA"])
                    if "h1e" in dbg:
                        return
                    ACT(bB[:], bA[:], AF.Exp, ["bA"], ["bB"])
                    ACT(bA[:], bA[:], AF.Exp, ["bA"], ["bA"], scale=-1.0)
                    if not full:
                        TT("dve", a3, a3, Wt[:].unsqueeze(2).broadcast_to([128, NCH, 64]), ALU.mult, ["bA", "Wt"], ["bA"])
                    TS("pool", ff[:], ff[:], -1.0, 1.0, ALU.mult, ALU.add, reads=["ff"], writes=["ff"])
                    TT("dve", HS["kA"][:], ff[:], bA[:], ALU.mult, ["ff", "bA"], ["kA"])
                    if full:
                        for ti, (t0, n) in enumerate(tiles(0, T)):
                            pj, pk = proj_fm(wh, 0, t0, n)
                            tm = HS["tmp"][ti % 2]
                            ACT(tm[:, :n], pj[:, :n], AF.Silu, [pk], ["tmp%d" % (ti % 2)])
                            TT("dve", HS["qA"][:, t0:t0 + n], tm[:, :n], bB[:, t0:t0 + n], ALU.mult,
                               ["tmp%d" % (ti % 2), "bB"], ["qA"])
                            pj, pk = proj_fm(wh, 3, t0, n)
                            ACT(HS["gate"][:, t0:t0 + n], pj[:, :n], AF.Silu, [pk], ["gate"])
                    if "h2" in dbg:
                        return
                    proj_v(wh, 2, HS["vtok"])
                    if "h3" in dbg:
                        return
                    k_transposes(HS["kA"], HS["ktok"], ksc[:, h:h + 1])
                    if "h4" in dbg:
                        return
                    recurrence(h, HS, full, masks[:])
                    if "h5" in dbg:
                        return
                S.barrier()

        def ret_heads(full):
            with ExitStack() as PH:
                HS = head_common(PH, full)
                if full:
                    HS["qB"] = sbt(PH, "qB", [128, T], BF16)
                whs = [sbt(PH, "wh6_%d" % i, [128, 8, 6, 128], BF16) for i in range(2)]

                def load_wh(r_):
                    srcs = [w_in[:, 2048 + r_ * 128:2048 + (r_ + 1) * 128], w_in_sw[:, r_ * 128:(r_ + 1) * 128],
                            w_in[:, 2560 + r_ * 128:2560 + (r_ + 1) * 128], w_in_sw[:, 512 + r_ * 128:512 + (r_ + 1) * 128],
                            w_in[:, 3072 + r_ * 128:3072 + (r_ + 1) * 128], w_in[:, 3584 + r_ * 128:3584 + (r_ + 1) * 128]]
                    for qi in (range(6) if full else (2, 3, 4)):
                        DMA("pool", whs[r_ % 2][:, :, qi, :], srcs[qi].rearrange("(k p) c -> p k c", p=128), writes=["wh%d" % (r_ % 2)])
                cosT = sbt(PH, "cosT", [128, T])
                sinT = sbt(PH, "sinT", [128, T])
                gdec = sbt(PH, "gdec", [128, 4, 64])
                masks = sbt(PH, "dmasks", [64, 4, 64])
                ksc = sbt(PH, "ksc", [64, 8])
                Wr = sbt(PH, "Wr", [128, 4, NCH])
                DMA("sp", Wr[:], retW_d[:, :].rearrange("p (h c) -> p h c", c=NCH), writes=["Wr"])
                for r_ in range(4):
                    TT("dve", Wr[:, r_, 0:1], Wr[:, r_, 0:1], flagw[:, W["w"]:W["w"] + 1], ALU.mult, ["Wr", "flag"], ["Wr"])
                DMA("sp", cosT[:], cos_d[W["w"], :, :], writes=["cosT"])
                DMA("sp", sinT[:], sin_d[W["w"], :, :], writes=["sinT"])
                DMA("sp", gdec[:], gdec_d[:, :].rearrange("p (h s) -> p h s", s=64), writes=["gdec"])
                DMA("sp", masks[:], masks_d[:, 64:320].rearrange("p (h s) -> p h s", s=64), writes=["masks"])
                DMA("sp", ksc[:], kscale_d[:, :], writes=["kscale"])
                load_wh(0)
                for r in range(4):
                    h = 4 + r
                    if r + 1 < 4:
                        load_wh(r + 1)
                    wh = (whs[r % 2], "wh%d" % (r % 2))
                    MEMSET("pool", HS["tabA"][:], gam[r] ** 64, ["tabA"])
                    MEMSET("pool", HS["tabC"][:], 1.0, ["tabC"])
                    MEMSET("pool", HS["tabM"][:], 1.0, ["tabM"])
                    MEMSET("pool", (W["D"] if W["D"] is not None else Dst)[:, h:h + 1], gam[r] ** (64 * (NCH - 1)), ["Dst"])
                    TT("dve", HS["tabA"][:, 0:1], HS["tabA"][:, 0:1], flagw[:, W["w"]:W["w"] + 1], ALU.mult, ["tabA", "flag"], ["tabA"])
                    TT("dve", HS["tabC"][:, 0:1], HS["tabC"][:, 0:1], flagw[:, W["w"]:W["w"] + 1], ALU.mult, ["tabC", "flag"], ["tabC"])
                    for which in ((0, 1) if full else (1,)):
                        for ti, (t0, n) in enumerate(tiles(0, T)):
                            pa, pak = proj_fm(wh, 2 * which, t0, n)
                            t1, t2 = HS["tmp"][0], HS["tmp"][1]
                            TT("dve", t1[:, :n], pa[:, :n], cosT[:, t0:t0 + n], ALU.mult, [pak, "cosT"], ["tmp0"])
                            pb, pbk = proj_fm(wh, 2 * which + 1, t0, n)
                            TT("dve", t2[:, :n], pb[:, :n], sinT[:, t0:t0 + n], ALU.mult, [pbk, "sinT"], ["tmp1"])
                            TT("pool", t1[:, :n], t1[:, :n], t2[:, :n], ALU.add, ["tmp0", "tmp1"], ["tmp0"])
                            if which == 0:
                                ACT(HS["qA"][:, t0:t0 + n], t1[:, :n], AF.Identity, ["tmp0"], ["qA"])
                                TT("dve", HS["qB"][:, t0:t0 + n].rearrange("p (c s) -> p c s", s=64),
                                   t1[:, :n].rearrange("p (c s) -> p c s", s=64),
                                   gdec[:, r, :].unsqueeze(1).broadcast_to([128, n // 64, 64]), ALU.mult,
                                   ["tmp0", "gdec"], ["qB"])
                            else:
                                ACT(HS["kA"][:, t0:t0 + n], t1[:, :n], AF.Identity, ["tmp0"], ["kA"], scale=128.0 ** -0.5)
                    if not full:
                        k3 = HS["kA"][:].rearrange("p (c s) -> p c s", s=64)
                        TT("dve", k3, k3, Wr[:, r, :].unsqueeze(2).broadcast_to([128, NCH, 64]), ALU.mult, ["kA", "Wr"], ["kA"])
                    if full:
                        for ti, (t0, n) in enumerate(tiles(0, T)):
                            pj, pk = proj_fm(wh, 5, t0, n)
                            ACT(HS["gate"][:, t0:t0 + n], pj[:, :n], AF.Silu, [pk], ["gate"])
                    proj_v(wh, 4, HS["vtok"])
                    k_transposes(HS["kA"], HS["ktok"], ksc[:, h:h + 1])
                    recurrence(h, HS, full, masks[:, r, :])
                S.barrier()

        def chain_states(Lall_sb, Dall_sb):
            with ExitStack() as PC:
                tmp = sbt(PC, "ctmp", [128, 128])
                MEMSET("pool", Sin[:], 0.0, ["Sin"])
                for i in (range(ncores - 2, -1, -1) if mode == "F2" else range(ncores - 1)):
                    for h in range(8):
                        STT(tmp[:], Sin[:, h, :], Dall_sb[:, i, h:h + 1], Lall_sb[:, i, h, :], ALU.mult, ALU.add,
                            ["Sin", "Lall", "Dall"], ["ctmp"])
                        TT("dve", tmp[:], tmp[:], Sin[:, h, :], ALU.subtract, ["ctmp", "Sin"], ["ctmp"])
                        STT(Sin[:, h, :], tmp[:], seli[:, i:i + 1], Sin[:, h, :], ALU.mult, ALU.add,
                            ["ctmp", "seli", "Sin"], ["Sin"])
                S.barrier()

        def wout_residual():
            with ExitStack() as PW:
                wo = sbt(PW, "wo", [128, 8, 1024], BF16)
                xhi = sbt(PW, "xhi", [128, 4, T])
                xin = [sbt(PW, "xin%d" % i, [128, 512]) for i in range(2)]
                DMA("pool", wo[:], w_out[:, :].rearrange("(k p) c -> p k c", p=128), writes=["wo"])
                i = 0
                for dm in range(8):
                    for (t0, n) in tiles(0, T):
                        pj, pk = ps_pj()
                        for h in range(8):
                            MM(pj[:, :n], wo[:, h, dm * 128:(dm + 1) * 128], catT[:, h, t0:t0 + n], start=(h == 0), stop=(h == 7),
                               reads=["wo", "cat%d" % h], writes=[pk])
                        i ^= 1
                        DMA("sp", xin[i][:, :n], xTw[0, dm * 128:(dm + 1) * 128, t0:t0 + n], writes=["xin%d" % i])
                        dest = big[:, dm, t0:t0 + n] if dm < 4 else xhi[:, dm - 4, t0:t0 + n]
                        STT(dest, pj[:, :n], mod[:, 16 + dm:17 + dm], xin[i][:, :n], ALU.mult, ALU.add,
                            [pk, "mod", "xin%d" % i], ["xdest%d" % dm])
                S.barrier()
                for dm in range(4, 8):
                    COPY("pool" if dm % 2 else "act", big[:, dm, :], xhi[:, dm - 4, :], ["xdest%d" % dm], ["x%d" % dm])
                S.barrier()

        def moe(l, lo, hi):
            halves = [(lo, (lo + hi) // 2), ((lo + hi) // 2, hi)]
            for (h0, h1) in halves:
                NH = h1 - h0
                with ExitStack() as PM:
                    hnh = sbt(PM, "hnh", [128, 8, NH], BF16)
                    gatesT = sbt(PM, "gatesT", [32, NH])
                    with ExitStack() as PN:
                        NS = {"sq": sbt(PN, "sq", [128, 8, 512], BF16), "stdt": sbt(PN, "stdt", [128, 512]),
                              "t1": sbt(PN, "t1", [128, 8, 512])}
                        hn32 = sbt(PN, "hn32", [128, 8, 512])
                        wr = sbt(PN, "wr", [128, 8, 32])
                        br = sbt(PN, "br", [1, 32])
                        lg = sbt(PN, "lg", [128, 32])
                        mx8 = sbt(PN, "mx8", [128, 8])
                        msk = sbt(PN, "msk", [128, 32])
                        ex = sbt(PN, "ex", [128, 32])
                        den = sbt(PN, "den", [128, 2])
                        DMA("sp", wr[:], w_router[l].rearrange("(k p) e -> p k e", p=128), writes=["wr"])
                        DMA("sp", br[:], b_router[l:l + 1, :], writes=["br"])
                        for (t0, n) in tiles(h0, h1):
                            norm_tile(NS, [big[:, k, t0:t0 + n] for k in range(8)], ["x%d" % k for k in range(8)], n, l, True,
                                      [hnh[:, k, t0 - h0:t0 - h0 + n] for k in range(8)], ["hnh%d" % k for k in range(8)],
                                      [hn32[:, k, :n] for k in range(8)], ["hn32_%d" % k for k in range(8)])
                            for s0 in range(0, n, 128):
                                m = min(128, n - s0)
                                pl = ps[6]
                                for k in range(8):
                                    MM(pl[:m, 0:32], hn32[:, k, s0:s0 + m], wr[:, k, :], start=(k == 0), stop=False,
                                       reads=["hn32_%d" % k, "wr"], writes=["ps6"])
                                MM(pl[:m, 0:32], ones_f[0:1, 0:m], br[0:1, :], start=False, stop=True, reads=["ones_f", "br"], writes=["ps6"])
                                COPY("dve", lg[:m, :], pl[:m, 0:32], ["ps6"], ["lg"])
                                S.op("dve", lambda e, m=m: e.max(out=mx8[:m, :], in_=lg[:m, :]), ["lg"], ["mx8"])
                                TS("dve", msk[:m, :], lg[:m, :], mx8[:m, 3:4], None, ALU.is_ge, reads=["lg", "mx8"], writes=["msk"])
                                TS("dve", den[:m, 0:1], mx8[:m, 0:1], -1.0, None, ALU.mult, reads=["mx8"], writes=["den0"])
                                ACT(ex[:m, :], lg[:m, :], AF.Exp, ["lg", "den0"], ["ex"], bias=den[:m, 0:1])
                                TT("dve", ex[:m, :], ex[:m, :], msk[:m, :], ALU.mult, ["ex", "msk"], ["ex"])
                                S.op("dve", lambda e, m=m: e.reduce_sum(out=den[:m, 1:2], in_=ex[:m, :], axis=AX.X), ["ex"], ["den1"])
                                S.op("dve", lambda e, m=m: e.reciprocal(out=den[:m, 1:2], in_=den[:m, 1:2]), ["den1"], ["den1"])
                                TS("dve", ex[:m, :], ex[:m, :], den[:m, 1:2], None, ALU.mult, reads=["ex", "den1"], writes=["ex"])
                                TR(ps[6][0:32, 128:128 + m], ex[:m, :], ident_f[:m, :m], ["ex", "ident_f"], ["ps6"])
                                c0 = t0 - h0 + s0
                                COPY("act", gatesT[:, c0:c0 + m], ps[6][0:32, 128:128 + m], ["ps6"], ["gatesT"])
                        S.barrier()
                    if "gatesT" in dbg and h0 == lo and l == 0:
                        dump("gatesT", gatesT[:], [32, NH], "gatesT")
                    with ExitStack() as PE_:
                        act = sbt(PE_, "act", [128, 8, NH], BF16)
                        gbc = sbt(PE_, "gbc", [128, NH])
                        wg = [sbt(PE_, "wg%d" % i, [128, 8, 2, 512], BF16) for i in range(2)]
                        wd = [sbt(PE_, "wd%d" % i, [128, 8, 1024], BF16) for i in range(2)]
                        NTT = 3
                        tt = [[sbt(PE_, "tt%d_%d" % (i, j), [128, 512]) for j in range(3)] for i in range(NTT)]
                        bgu = sbt(PE_, "bgu", [128, 32 * 16])
                        bdn = sbt(PE_, "bdn", [32, 1024])
                        DMA("sp", bgu[:], b_guT_d[:, l * 512:(l + 1) * 512], writes=["bgu"])
                        bgl = bgu[:].rearrange("p (e c) -> p e c", c=16)[:, :, 8:16]
                        TS("dve", bgl, bgl, 1.0, None, ALU.add, reads=["bgu"], writes=["bgu"])
                        DMA("sp", bdn[:], b_down[l], writes=["bdn"])
                        cnt = {"g": 0, "t": 0, "y": 0, "d": 0}

                        def down_evac(py, pyk, dm, t0, n):
                            STT(big[:, dm, t0:t0 + n], py[:, :n], mod[:, l * 48 + 40 + dm:l * 48 + 41 + dm], big[:, dm, t0:t0 + n],
                                ALU.mult, ALU.add, [pyk, "mod", "x%d" % dm], ["x%d" % dm])

                        for dm in range(8):
                            for (t0, n) in tiles(h0, h1):
                                cnt["d"] ^= 1
                                py, pyk = ps[4 + cnt["d"]], "ps%d" % (4 + cnt["d"])
                                MM(py[:, :n], bdn[0:32, dm * 128:(dm + 1) * 128], gatesT[0:32, t0 - h0:t0 - h0 + n],
                                   reads=["bdn", "gatesT"], writes=[pyk])
                                down_evac(py, pyk, dm, t0, n)
                        def issue_wg(si):
                            e_, fq_ = si // 2, si % 2
                            for two in range(2):
                                DMA("pool", wg[si % 2][:, :, two, :],
                                    w_gu[l, e_, :, two * 1024 + fq_ * 512: two * 1024 + (fq_ + 1) * 512].rearrange("(k p) c -> p k c", p=128),
                                    writes=["wg%d" % (si % 2)])

                        def issue_wd(e_):
                            DMA("pool", wd[e_ % 2][:], w_down[l, e_].rearrange("(k p) c -> p k c", p=128), writes=["wd%d" % (e_ % 2)])

                        issue_wg(0)
                        issue_wd(0)
                        for e in range(32):
                            for (t0, n) in tiles(0, NH):
                                MM(ps[6][:, :n], ident_f[0:32, e:e + 1].broadcast_to([32, 128]), gatesT[0:32, t0:t0 + n],
                                   reads=["ident_f", "gatesT"], writes=["ps6"])
                                COPY("act", gbc[:, t0:t0 + n], ps[6][:, :n], ["ps6"], ["gbc"])
                            wdi = e % 2
                            if e + 1 < 32:
                                issue_wd(e + 1)
                            for fc in range(8):
                                si = e * 2 + fc // 4
                                fi = fc % 4
                                if fi == 0 and si + 1 < 64:
                                    issue_wg(si + 1)
                                wgi, wgk = wg[si % 2], "wg%d" % (si % 2)
                                bcol = e * 16 + fc
                                for (t0, n) in tiles(0, NH):
                                    pg, pgk = ps[0], "ps0"
                                    plin, plk = ps[1], "ps1"
                                    if (t0 // 512) % 2:
                                        pg, pgk, plin, plk = ps[2], "ps2", ps[3], "ps3"
                                    for k in range(8):
                                        MM(pg[:, :n], wgi[:, k, 0, fi * 128:(fi + 1) * 128], hnh[:, k, t0:t0 + n], start=(k == 0), stop=(k == 7),
                                           reads=[wgk, "hnh%d" % k], writes=[pgk])
                                    for k in range(8):
                                        MM(plin[:, :n], wgi[:, k, 1, fi * 128:(fi + 1) * 128], hnh[:, k, t0:t0 + n], start=(k == 0), stop=(k == 7),
                                           reads=[wgk, "hnh%d" % k], writes=[plk])
                                    cnt["t"] = (cnt["t"] + 1) % NTT
                                    t1, t2, t3 = tt[cnt["t"]]
                                    k1, k2, k3 = ["tt%d_%d" % (cnt["t"], j) for j in range(3)]
                                    TS("dve", t1[:, :n], pg[:, :n], bgu[:, bcol:bcol + 1], 7.0, ALU.add, ALU.min, reads=[pgk, "bgu"], writes=[k1])
                                    ACT(t2[:, :n], t1[:, :n], AF.Sigmoid, [k1], [k2], scale=1.702)
                                    TS("dve", t3[:, :n], plin[:, :n], bgu[:, bcol + 8:bcol + 9], 8.0, ALU.add, ALU.min, reads=[plk, "bgu"], writes=[k3])
                                    STT(t3[:, :n], t3[:, :n], -6.0, gbc[:, t0:t0 + n], ALU.max, ALU.mult, [k3, "gbc"], [k3])
                                    TT("pool", t1[:, :n], t1[:, :n], t2[:, :n], ALU.mult, [k1, k2], [k1])
                                    TT("pool", act[:, fc, t0:t0 + n], t1[:, :n], t3[:, :n], ALU.mult, [k1, k3], ["act%d" % fc])
                            for dm in range(8):
                                for (t0, n) in tiles(0, NH):
                                    cnt["d"] ^= 1
                                    py, pyk = ps[4 + cnt["d"]], "ps%d" % (4 + cnt["d"])
                                    for fc in range(8):
                                        MM(py[:, :n], wd[wdi][:, fc, dm * 128:(dm + 1) * 128], act[:, fc, t0:t0 + n],
                                           start=(fc == 0), stop=(fc == 7), reads=["wd%d" % wdi, "act%d" % fc], writes=[pyk])
                                    down_evac(py, pyk, dm, h0 + t0, n)
                        S.barrier()

        def pool_mixer():
            l = 1
            with ExitStack() as PP:
                NS = {"sq": sbt(PP, "sq", [128, 8, 512], BF16), "stdt": sbt(PP, "stdt", [128, 512])}
                rstd = sbt(PP, "rstd", [128, T])
                hb = sbt(PP, "hb", [128, T])
                s2 = sbt(PP, "s2", [128, T])
                s4 = sbt(PP, "s4", [128, T])
                pT = sbt(PP, "pT", [128, 8, T], BF16)
                pw = sbt(PP, "pw", [128, 4, 2, 256], BF16)
                invc = sbt(PP, "invc", [128, 4, 16])
                ptmp = [sbt(PP, "ptmp%d" % i, [128, 512]) for i in range(2)]
                DMA("sp", invc[:], invc_d[:, :].rearrange("p (g s) -> p g s", s=16), writes=["invc"])
                for g in range(4):
                    DMA("pool", pw[:, g, :, :], pool_w[g].rearrange("(i p) d -> p i d", p=128), writes=["pw"])
                for (t0, n) in tiles(0, T):
                    pj, pk = ps_pj()
                    for k in range(8):
                        ACT(NS["sq"][:, k, :n], big[:, k, t0:t0 + n], AF.Square, ["x%d" % k], ["sq%d" % k])
                    for k in range(8):
                        MM(pj[:, :n], ones_b[:], NS["sq"][:, k, :n], start=(k == 0), stop=(k == 7), reads=["sq%d" % k, "ones_b"], writes=[pk])
                    ACT(rstd[:, t0:t0 + n], pj[:, :n], AF.Sqrt, [pk, "eps_t"], ["rstd"], bias=eps_t[:, 0:1], scale=1.0 / 1024.0)
                S.op("dve", lambda e: e.reciprocal(out=rstd[:], in_=rstd[:]), ["rstd"], ["rstd"])
                for k in range(8):
                    g = k // 2
                    w = (2, 4, 8, 16)[g]
                    STT(hb[:], big[:, k, :], mod1[:, 48 + 8 + k:48 + 9 + k], rstd[:], ALU.mult, ALU.mult, ["x%d" % k, "rstd", "mod1"], ["hb"])
                    TS("dve", hb[:], hb[:], mod[:, 48 + k:48 + k + 1], None, ALU.add, reads=["hb", "mod"], writes=["hb"])
                    TS("dve", hb[:, 0:64], hb[:, 0:64], flagw[:, 0:1], None, ALU.mult, reads=["hb", "flag"], writes=["hb"])
                    TT("pool", s2[:, 1:T], hb[:, 1:T], hb[:, 0:T - 1], ALU.add, ["hb"], ["s2"])
                    ws, wsk = s2, "s2"
                    if w >= 4:
                        TT("pool", s4[:, 3:T], s2[:, 3:T], s2[:, 1:T - 2], ALU.add, ["s2"], ["s4"])
                        ws, wsk = s4, "s4"
                    if w >= 8:
                        TT("pool", s2[:, 7:T], s4[:, 7:T], s4[:, 3:T - 4], ALU.add, ["s4", "s2"], ["s2"])
                        ws, wsk = s2, "s2"
                    if w >= 16:
                        TT("pool", s4[:, 15:T], s2[:, 15:T], s2[:, 7:T - 8], ALU.add, ["s2", "s4"], ["s4"])
                        ws, wsk = s4, "s4"
                    STT(pT[:, k, 16:T], ws[:, 16:T], 1.0 / w, hb[:, 16:T], ALU.mult, ALU.subtract, [wsk, "hb"], ["pT%d" % k])
                    TT("dve", ws[:, 64:80], ws[:, 64:80], invc[:, g, :], ALU.mult, [wsk, "invc", "pT%d" % k], [wsk])
                    TT("dve", pT[:, k, 64:80], ws[:, 64:80], hb[:, 64:80], ALU.subtract, [wsk, "hb"], ["pT%d" % k])
                i = 0
                for k in range(8):
                    g, j = k // 2, k % 2
                    for (t0, n) in tiles(HALO, T):
                        pj, pk = ps_pj()
                        for ii in range(2):
                            MM(pj[:, :n], pw[:, g, ii, j * 128:(j + 1) * 128], pT[:, 2 * g + ii, t0:t0 + n], start=(ii == 0), stop=(ii == 1),
                               reads=["pw", "pT%d" % (2 * g + ii)], writes=[pk])
                        i ^= 1
                        TS("dve", ptmp[i][:, :n], pj[:, :n], small[:, 24 + k:25 + k], small[:, 16 + k:17 + k], ALU.add, ALU.mult,
                           reads=[pk, "small_pb", "small_pc"], writes=["ptmp%d" % i])
                        TT("pool", big[:, k, t0:t0 + n], big[:, k, t0:t0 + n], ptmp[i][:, :n], ALU.add, ["x%d" % k, "ptmp%d" % i], ["x%d" % k])
                S.barrier()

        def final_norm():
            with ExitStack() as PF:
                NS = {"sq": sbt(PF, "sq", [128, 8, 512], BF16), "stdt": sbt(PF, "stdt", [128, 512])}
                ob = [sbt(PF, "fo%d" % i, [128, 8, 512]) for i in range(2)]
                for ti, (t0, n) in enumerate(tiles(HALO, T)):
                    o = ob[ti % 2]
                    norm_tile(NS, [big[:, k, t0:t0 + n] for k in range(8)], ["x%d" % k for k in range(8)], n, 0, False,
                              None, None, [o[:, k, :n] for k in range(8)], ["fo%d_%d" % (ti % 2, k) for k in range(8)], final=True)
                    DMA("sp", yT[:, t0 - HALO:t0 - HALO + n].rearrange("(k p) t -> p k t", p=128), o[:, :, :n],
                        reads=["fo%d_%d" % (ti % 2, k) for k in range(8)], writes=["yT%d" % ti], is_out=True)

        if "s0" not in dbg and mode != "F2":
            mixer_norm()
        if mode in ("A", "F"):
            if "s0" not in dbg and "s1" not in dbg:
                try:
                    hg_heads(False)
                    if "s2" not in dbg:
                        ret_heads(False)
                except Stop:
                    pass
        if mode == "F2":
            with ExitStack() as PX:
                Lall_sb = sbt(PX, "Lall_sb", [128, ncores - 1, 8, 128])
                Dall_sb = sbt(PX, "Dall_sb", [128, ncores - 1, 8])
                for w in range(1, ncores):
                    W["w"], W["L"], W["D"] = w, Lall_sb[:, w - 1, :, :], Dall_sb[:, w - 1, :]
                    mixer_norm()
                    hg_heads(False)
                    ret_heads(False)
                W["w"], W["L"], W["D"] = 0, None, None
                chain_states(Lall_sb, Dall_sb)
            mixer_norm()
        if mode == "A":
            DMA("sp", Lst_o[:, :], Lst[:].rearrange("p h e -> p (h e)"), reads=["Lst"], writes=["Lst_o"], is_out=True)
            DMA("sp", Dst_o[:, :], Dst[:], reads=["Dst"], writes=["Dst_o"], is_out=True)
        else:
            for _once in ([1] if mode != "F2" else []):
              with ExitStack() as PX:
                Lall_sb = sbt(PX, "Lall_sb", [128, ncores, 8, 128])
                Dall_sb = sbt(PX, "Dall_sb", [128, ncores, 8])
                if mode == "B":
                    DMA("sp", Lall_sb[:], Lall_d.rearrange("c p (h e) -> p c h e", e=128), writes=["Lall"])
                    DMA("sp", Dall_sb[:], Dall_d.rearrange("c p h -> p c h"), writes=["Dall"])
                else:
                    bounce = nc.dram_tensor("st_bounce", [128, 1032], F32)
                    gath = nc.dram_tensor("st_gath", [ncores * 128, 1032], F32)
                    DMA("sp", bounce[:, 0:1024], Lst[:].rearrange("p h e -> p (h e)"), reads=["Lst"], writes=["bounce"])
                    DMA("sp", bounce[:, 1024:1032], Dst[:], reads=["Dst"], writes=["bounce"])
                    S.barrier()
                    S.op("pool", lambda e: e.collective_compute("AllGather", ALU.bypass, replica_groups=[list(range(ncores))],
                                                                ins=[bounce.ap().opt()], outs=[gath.ap().opt()]),
                         reads=["bounce"], writes=["gath"], dma_key="gath", dma_inc=1)
                    S.barrier()
                    gv = gath.ap().rearrange("(c p) w -> p c w", p=128)
                    DMA("sp", Lall_sb[:].rearrange("p c h e -> p c (h e)"), gv[:, :, 0:1024], reads=["gath"], writes=["Lall"])
                    DMA("sp", Dall_sb[:], gv[:, :, 1024:1032], reads=["gath"], writes=["Dall"])
                chain_states(Lall_sb, Dall_sb)
            dump("Sin", Sin[:].rearrange("p h e -> p (h e)"), [128, 1024], "Sin")
            hg_heads(True)
            ret_heads(True)
            if "cat" in dbg:
                cf = sbt(es, "catf", [128, 8, T])
                for h in range(8):
                    COPY("act", cf[:, h, :], catT[:, h, :], ["cat%d" % h], ["catf"])
                dump("cat", cf[:].rearrange("p h t -> p (h t)"), [128, 8 * T], "catf")
            wout_residual()
            dump("x1", big[:].rearrange("p k t -> p (k t)"), [128, 8 * T], "x0")
            if "stop1" not in dbg:
                moe(0, 0, T)
                dump("x2", big[:].rearrange("p k t -> p (k t)"), [128, 8 * T], "x0")
                pool_mixer()
                dump("x3", big[:].rearrange("p k t -> p (k t)"), [128, 8 * T], "x0")
                moe(1, HALO, T)
            final_norm()
        S.finish()
        S.emit()
    return nc, dbg_outs, in_names


def _consts(T, tok0):
    gam = [1.0 - 2.0 ** (-5 - h) for h in range(4)]
    idx = np.arange(64)
    rel = idx[None, :] - idx[:, None]
    masks = np.zeros((64, 5, 64), np.float64)
    masks[:, 0, :] = (rel >= 0)
    for h in range(4):
        masks[:, 1 + h, :] = np.where(rel >= 0, gam[h] ** np.maximum(rel, 0), 0.0)
    kscale = np.ones((64, 8), np.float64)
    for h in range(4):
        kscale[:, 4 + h] = gam[h] ** (63 - idx)
    gdec = np.zeros((128, 4, 64), np.float64)
    for h in range(4):
        gdec[:, h, :] = (gam[h] ** (idx + 1.0))[None, :]
    half = 64
    inv = (10000.0 ** (-np.arange(half, dtype=np.float32) / half)).astype(np.float32)
    pos = (tok0 + np.arange(T)).astype(np.float32)
    ang = (pos[None, :] * inv[:, None]).astype(np.float32)
    cos = np.cos(ang.astype(np.float64))
    sin = np.sin(ang.astype(np.float64))
    cosT = np.concatenate([cos, cos], 0)
    sinT = np.concatenate([-sin, sin], 0)
    nch = T // 64
    retW = np.zeros((128, 4, nch), np.float64)
    for h in range(4):
        for c in range(nch - 1):
            retW[:, h, c] = gam[h] ** (64.0 * (nch - 2 - c))
    scanm = np.ones((128, T), np.float32)
    scanm[:, ::64] = 0.0
    return dict(masks=masks.reshape(64, 320).astype(np.float32), kscale=kscale.astype(np.float32),
                gdec=gdec.reshape(128, 256).astype(np.float32), cosT=cosT.astype(np.float32),
                sinT=sinT.astype(np.float32), scanm=scanm, ident=np.eye(128, dtype=np.float32),
                retW=retW.reshape(128, 4 * nch).astype(np.float32))


def _pk(v, ncol):
    return np.ascontiguousarray(np.asarray(v, np.float32).reshape(ncol, 128).T)


def make_inputs(inp, ncores, nw=8):
    x = np.asarray(inp["x"], np.float32)[0]
    Sq = x.shape[0]
    TP = Sq // ncores
    T = TP + HALO
    w_in = np.asarray(inp["w_in"], np.float32)[0]
    perm = np.concatenate([np.arange(64, 128), np.arange(0, 64)])
    cols = []
    for base in (2048, 2560):
        for r in range(4):
            cols.append(base + r * 128 + perm)
    w_in_sw = np.ascontiguousarray(w_in[:, np.concatenate(cols)])
    shared = dict(
        cT=_pk(np.asarray(inp["c"])[0], 8),
        w_ada=np.asarray(inp["w_ada"], np.float32),
        b_adaT=np.concatenate([_pk(np.asarray(inp["b_ada"])[l], 48) for l in range(2)], 1),
        w_in=w_in, w_in_sw=w_in_sw,
        lbraw=np.concatenate([_pk(np.asarray(inp["hg_lower_bounds"])[r], 4) for r in range(2)], 1),
        gains=np.concatenate([_pk(np.asarray(inp["hg_norm"])[0].reshape(-1), 4),
                              _pk(np.asarray(inp["ret_norm"])[0].reshape(-1), 4)], 1),
        w_out=np.asarray(inp["w_out"], np.float32)[0],
        pool_w=np.asarray(inp["pool_w"], np.float32)[0],
        pool_bT=_pk(np.asarray(inp["pool_b"])[0].reshape(-1), 8),
        pool_sT=_pk(np.asarray(inp["pool_scale"])[0], 8),
        w_router=np.asarray(inp["w_router"], np.float32),
        b_router=np.asarray(inp["b_router"], np.float32),
        w_gu=np.asarray(inp["w_gu"], np.float32),
        b_guT=_pk(np.asarray(inp["b_gu"]).reshape(-1), 2 * 32 * 16),
        w_down=np.asarray(inp["w_down"], np.float32),
        b_down=np.asarray(inp["b_down"], np.float32),
        fnT=_pk(np.asarray(inp["final_norm"]), 8),
    )
    win = {}
    for c in range(ncores):
        tok0 = c * TP - HALO
        xs = np.zeros((T, 1024), np.float32)
        lo = max(tok0, 0)
        xs[lo - tok0:, :] = x[lo:tok0 + T, :]
        cst = _consts(T, tok0)
        win[c] = (np.ascontiguousarray(xs.T), cst["cosT"], cst["sinT"])
    zero_w = (np.zeros((1024, T), np.float32), np.zeros((128, T), np.float32), np.zeros((128, T), np.float32))
    maps = []
    for j in range(ncores):
        m = dict(shared)
        cst = _consts(T, j * TP - HALO)
        for k in ("masks", "kscale", "gdec", "scanm", "ident", "retW"):
            m[k] = cst[k]
        ws = [win[j - w] if j - w >= 0 else zero_w for w in range(nw)]
        m["xT"] = np.stack([w_[0] for w_ in ws], 0)
        m["cosT"] = np.stack([w_[1] for w_ in ws], 0)
        m["sinT"] = np.stack([w_[2] for w_ in ws], 0)
        flag = np.ones((128, 8), np.float32)
        valid = np.zeros((128, 8), np.float32)
        for w in range(8):
            if j - w <= 0:
                flag[:, w] = 0.0
            if w >= 1 and j - w >= 0:
                valid[:, w - 1] = 1.0
        m["flag"] = flag
        m["valid"] = valid
        seli = np.zeros((128, 8), np.float32)
        seli[:, :j] = 1.0
        m["seli"] = seli
        invc = np.zeros((128, 4, 16), np.float32)
        for g, w in enumerate((2, 4, 8, 16)):
            tg = j * TP + np.arange(16)
            invc[:, g, :] = (1.0 / np.minimum(tg + 1, w))[None, :]
        m["invc"] = invc.reshape(128, 64)
        maps.append(m)
    return maps, TP


_CACHE = {}


def _get(TP, mode, ncores):
    key = (TP, mode, ncores)
    if key not in _CACHE:
        r = build(TP, mode, ncores)
        _CACHE[key] = (r[0], r[2])
    return _CACHE[key]


def kernel(**inp):
    ncores = 8
    maps, TP = make_inputs(inp, ncores, 8)
    ncF, names = _get(TP, "F2", ncores)
    r = run_bass_kernel_spmd(ncF, [{k: m[k] for k in names} for m in maps], core_ids=list(range(ncores)))
    y = np.concatenate([r.results[j]["yT"].T for j in range(ncores)], 0)
    return np.ascontiguousarray(y[None].astype(np.float32))
```

```python
from contextlib import ExitStack
import numpy as np
import concourse.bass as bass
import concourse.mybir as mybir
from concourse.bass_utils import run_bass_kernel_spmd

F32 = mybir.dt.float32
BF16 = mybir.dt.bfloat16
ALU = mybir.AluOpType
AF = mybir.ActivationFunctionType
AX = mybir.AxisListType
ENGS = ("pe", "act", "dve", "pool", "sp")
EPS = 1e-6
HALO = 64


class Sched:
    def __init__(self, nc, es):
        self.nc, self.es = nc, es
        self.items = {e: [] for e in ENGS}
        self.cnt = {e: 0 for e in ENGS}
        self.clock = {e: {} for e in ENGS}
        self.snap, self.key_w, self.key_r, self.dma_cnt, self.sems = {}, {}, {}, {}, {}
        self.out_events = []
        self.epoch = 0
        self.ek = {e: "E:" + e for e in ENGS}
        for e in ENGS:
            self._sem(self.ek[e])

    def _sem(self, k):
        if k not in self.sems:
            self.sems[k] = self.es.enter_context(self.nc.semaphore("s%d" % len(self.sems)))
        return self.sems[k]

    def _need(self, eng, ev, deps):
        if ev is None:
            return
        sk, val = ev
        if eng == "pe" and sk.startswith("E:pe"):
            return
        if self.clock[eng].get(sk, 0) >= val:
            return
        if deps.get(sk, 0) < val:
            deps[sk] = val

    def _apply(self, eng, deps):
        clk = self.clock[eng]
        for sk, val in deps.items():
            self.items[eng].append(("w", sk, val))
            for a, b in self.snap.get((sk, val), {}).items():
                if clk.get(a, 0) < b:
                    clk[a] = b
            if clk.get(sk, 0) < val:
                clk[sk] = val

    def op(self, eng, fn, reads=(), writes=(), dma_key=None, is_out=False, dma_inc=16):
        writes = [k.split("_")[0] if k.startswith("ps") else k for k in writes]
        writes += [k.split("_")[0] for k in reads if k.startswith("ps")]
        reads = [k for k in reads if not k.startswith("ps")]
        deps = {}
        for k in reads:
            self._need(eng, self.key_w.get(k), deps)
        for k in writes:
            self._need(eng, self.key_w.get(k), deps)
            for ev in self.key_r.get(k, ()):
                self._need(eng, ev, deps)
        self._apply(eng, deps)
        if dma_key is not None:
            sk = "D:" + str(dma_key)
            self._sem(sk)
            self.dma_cnt[sk] = self.dma_cnt.get(sk, 0) + 1
            ev = (sk, self._dval(sk, dma_inc))
            self.items[eng].append(("i", fn, sk, dma_inc))
            if is_out:
                self.out_events.append(ev)
        else:
            self.cnt[eng] += 1
            ev = (self.ek[eng], self.cnt[eng])
            self.items[eng].append(("i", fn, self.ek[eng], 1))
        self.snap[ev] = dict(self.clock[eng])
        for k in writes:
            self.key_w[k] = ev
            self.key_r[k] = []
        for k in reads:
            self.key_r.setdefault(k, []).append(ev)
        return ev

    def _dval(self, sk, inc):
        self.dma_val = getattr(self, "dma_val", {})
        self.dma_val[sk] = self.dma_val.get(sk, 0) + inc
        return self.dma_val[sk]

    def barrier(self):
        evs = [(self.ek[e], self.cnt[e]) for e in ENGS if self.cnt[e] > 0]
        evs += [(sk, v) for sk, v in getattr(self, "dma_val", {}).items()]
        for eng in ENGS:
            deps = {}
            for sk, val in evs:
                if self.clock[eng].get(sk, 0) < val:
                    deps[sk] = val
            self._apply(eng, deps)
        self.key_w, self.key_r = {}, {}
        self.epoch += 1
        for e in ENGS:
            if self.cnt[e] > 12000:
                self.ek[e] = "E:%s:%d" % (e, self.epoch)
                self._sem(self.ek[e])
                self.cnt[e] = 0

    def finish(self):
        deps = {}
        for ev in self.out_events:
            self._need("sp", ev, deps)
        self._apply("sp", deps)

    def emit(self):
        nc = self.nc
        with nc.Block() as block:
            def run(name):
                def body(engine):
                    for it in self.items[name]:
                        if it[0] == "w":
                            engine.wait_ge(self.sems[it[1]], it[2])
                        else:
                            it[1](engine).then_inc(self.sems[it[2]], it[3])
                return body
            block.tensor(run("pe"))
            block.scalar(run("act"))
            block.vector(run("dve"))
            block.gpsimd(run("pool"))
            block.sync(run("sp"))


class Stop(Exception):
    pass


def tiles(lo, hi, step=512):
    return [(t, min(step, hi - t)) for t in range(lo, hi, step)]


def build(TP, mode, ncores=8, dbg=()):
    T = TP + HALO
    NCH = T // 64
    nc = bass.Bass("TRN2", target_bir_lowering=False)

    in_names = []

    def din(name, shape):
        in_names.append(name)
        return nc.dram_tensor(name, list(shape), F32, kind="ExternalInput").ap()

    def dout(name, shape):
        return nc.dram_tensor(name, list(shape), F32, kind="ExternalOutput").ap()

    NW = 8 if mode == "F2" else 1
    xTw = din("xT", [NW, 1024, T])
    flag_d = din("flag", [128, 8])
    valid_d = din("valid", [128, 8])
    cT_d = din("cT", [128, 8])
    w_ada = din("w_ada", [2, 1024, 6144])
    badaT_d = din("b_adaT", [128, 96])
    w_in = din("w_in", [1024, 4096])
    w_in_sw = din("w_in_sw", [1024, 1024])
    lbraw_d = din("lbraw", [128, 8])
    gains_d = din("gains", [128, 8])
    w_out = din("w_out", [1024, 1024]) if mode != "A" else None
    pool_w = din("pool_w", [4, 256, 256]) if mode != "A" else None
    pool_bT_d = din("pool_bT", [128, 8])
    pool_sT_d = din("pool_sT", [128, 8])
    w_router = din("w_router", [2, 1024, 32]) if mode != "A" else None
    b_router = din("b_router", [2, 32]) if mode != "A" else None
    w_gu = din("w_gu", [2, 32, 1024, 2048]) if mode != "A" else None
    b_guT_d = din("b_guT", [128, 2 * 32 * 16]) if mode != "A" else None
    w_down = din("w_down", [2, 32, 1024, 1024]) if mode != "A" else None
    b_down = din("b_down", [2, 32, 1024]) if mode != "A" else None
    fnT_d = din("fnT", [128, 8])
    ident_d = din("ident", [128, 128])
    masks_d = din("masks", [64, 5 * 64])
    kscale_d = din("kscale", [64, 8])
    cos_d = din("cosT", [NW, 128, T])
    sin_d = din("sinT", [NW, 128, T])
    gdec_d = din("gdec", [128, 4 * 64])
    scanm_d = din("scanm", [128, T])
    invc_d = din("invc", [128, 4 * 16])
    seli_d = din("seli", [128, 8])
    retW_d = din("retW", [128, 4 * NCH])
    if mode == "A":
        Lst_o = dout("Lst", [128, 8 * 128])
        Dst_o = dout("Dst", [128, 8])
    if mode == "B":
        Lall_d = din("Lall", [ncores, 128, 8 * 128])
        Dall_d = din("Dall", [ncores, 128, 8])
    if mode in ("B", "F", "F2"):
        yT = dout("yT", [1024, TP])
    dbg_outs = {}

    gam = [1.0 - 2.0 ** (-5 - h) for h in range(4)]

    with ExitStack() as es:
        S = Sched(nc, es)

        uniq = [0]

        def sbt(stack, name, shape, dt=F32):
            uniq[0] += 1
            return stack.enter_context(nc.sbuf_tensor("sb%d_%s" % (uniq[0], name), list(shape), dt))

        def DMA(eng, out, in_, reads=(), writes=(), key=None, is_out=False):
            S.op(eng, lambda e: e.dma_start(out=out, in_=in_), reads, writes, dma_key=key or writes[0], is_out=is_out)

        def MM(out, lhsT, rhs, start=True, stop=True, reads=(), writes=()):
            S.op("pe", lambda e: e.matmul(out, lhsT, rhs, start=start, stop=stop), reads, writes)

        def TR(out, in_, ident, reads=(), writes=()):
            S.op("pe", lambda e: e.transpose(out, in_, ident), reads, writes)

        def ACT(out, in_, func, reads=(), writes=(), bias=None, scale=None):
            kw = {}
            if bias is not None:
                kw["bias"] = bias
            if scale is not None:
                kw["scale"] = scale
            S.op("act", lambda e: e.activation(out=out, in_=in_, func=func, **kw), reads, writes)

        def TT(eng, out, in0, in1, op, reads=(), writes=()):
            S.op(eng, lambda e: e.tensor_tensor(out=out, in0=in0, in1=in1, op=op), reads, writes)

        def TS(eng, out, in0, s1, s2, op0, op1=None, reads=(), writes=()):
            if op1 is None:
                S.op(eng, lambda e: e.tensor_scalar(out=out, in0=in0, scalar1=s1, scalar2=None, op0=op0), reads, writes)
            else:
                S.op(eng, lambda e: e.tensor_scalar(out=out, in0=in0, scalar1=s1, scalar2=s2, op0=op0, op1=op1), reads, writes)

        def STT(out, in0, scalar, in1, op0, op1, reads=(), writes=()):
            S.op("dve", lambda e: e.scalar_tensor_tensor(out=out, in0=in0, scalar=scalar, in1=in1, op0=op0, op1=op1),
                 reads, writes)

        def MEMSET(eng, ap, val, writes=()):
            S.op(eng, lambda e: e.memset(ap, val), (), writes)

        def COPY(eng, out, in_, reads=(), writes=()):
            if eng == "act":
                ACT(out, in_, AF.Identity, reads, writes)
            else:
                S.op(eng, lambda e: e.tensor_copy(out=out, in_=in_), reads, writes)

        def dump(name, ap, shape, key):
            if name in dbg:
                d = dout("dbg_" + name, shape)
                dbg_outs[name] = d
                DMA("sp", d, ap, reads=[key], writes=["dbg_" + name], is_out=True)

        G = es
        big = sbt(G, "big", [128, 8, T])
        bigb = big[:].rearrange("p k t -> p (k t)").bitcast(BF16).rearrange("p (k t) -> p k t", k=16)
        hnT = bigb[:, 0:8, :]
        catT = bigb[:, 8:16, :]
        ps = [G.enter_context(nc.psum_tensor("ps%d" % i, [128, 512], F32)) for i in range(7)]
        psb = G.enter_context(nc.psum_tensor("psb", [128, 1024], BF16))
        ident_f = sbt(G, "ident_f", [128, 128])
        ident_b = sbt(G, "ident_b", [128, 128], BF16)
        ones_b = sbt(G, "ones_b", [128, 128], BF16)
        ones_f = sbt(G, "ones_f", [128, 128])
        eps_t = sbt(G, "eps_t", [128, 1])
        flagw = sbt(G, "flag_s", [128, 8])
        W = {"w": 0, "L": None, "D": None}
        mod = sbt(G, "mod", [128, 96])
        mod1 = sbt(G, "mod1", [128, 88])
        small = sbt(G, "small", [128, 56])
        Lst = sbt(G, "Lst_s", [128, 8, 128] if mode != "F2" else [128, 8, 1])
        Dst = sbt(G, "Dst_s", [128, 8])
        Sin = sbt(G, "Sin_s", [128, 8, 128])
        seli = sbt(G, "seli_s", [128, 8])

        DMA("sp", ident_f[:], ident_d[:, :], writes=["ident_f"])
        DMA("pool", ident_b[:], ident_d[:, :], writes=["ident_b"])
        DMA("sp", flagw[:], flag_d[:, :], writes=["flag"])
        DMA("sp", seli[:], (valid_d if mode == "F2" else seli_d)[:, :], writes=["seli"])
        MEMSET("pool", ones_b[:], 1.0, ["ones_b"])
        MEMSET("pool", ones_f[:], 1.0, ["ones_f"])
        MEMSET("pool", eps_t[:], EPS, ["eps_t"])
        DMA("sp", small[:, 40:48], lbraw_d[:, :], writes=["small_lbraw"])
        DMA("sp", small[:, 8:16], gains_d[:, :], writes=["small_g"])
        DMA("sp", small[:, 16:24], pool_sT_d[:, :], writes=["small_ps"])
        DMA("sp", small[:, 24:32], pool_bT_d[:, :], writes=["small_pb"])
        DMA("sp", small[:, 32:40], fnT_d[:, :], writes=["small_fn"])
        TT("dve", small[:, 48:52], small[:, 40:44], small[:, 44:48], ALU.subtract, ["small_lbraw"], ["small_t"])
        ACT(small[:, 0:4], small[:, 48:52], AF.Sigmoid, ["small_t"], ["small_lb"])
        TS("dve", small[:, 4:8], small[:, 0:4], -1.0, 1.0, ALU.mult, ALU.add, ["small_lb"], ["small_oml"])

        with ExitStack() as P0:
            cT = sbt(P0, "cT_s", [128, 8])
            cond = sbt(P0, "cond", [128, 8])
            badaT = sbt(P0, "badaT", [128, 96])
            wa = [sbt(P0, "wa%d" % i, [128, 8, 768]) for i in range(2)]
            DMA("sp", cT[:], cT_d[:, :], writes=["cT"])
            DMA("sp", badaT[:], badaT_d[:, :], writes=["badaT"])
            ACT(cond[:], cT[:], AF.Silu, ["cT"], ["cond"])
            for l in range(2):
                for blk in range(8):
                    i = (l * 8 + blk) % 2
                    DMA("sp", wa[i][:], w_ada[l, :, blk * 768:(blk + 1) * 768].rearrange("(k p) c -> p k c", p=128),
                        writes=["wa%d" % i])
                    for m in range(6):
                        col = l * 48 + blk * 6 + m
                        for k in range(8):
                            MM(ps[0][:, col:col + 1], wa[i][:, k, m * 128:(m + 1) * 128], cond[:, k:k + 1],
                               start=(k == 0), stop=(k == 7), reads=["wa%d" % i, "cond"], writes=["ps0"])
            TT("dve", mod[:], ps[0][:, 0:96], badaT[:], ALU.add, ["ps0", "badaT"], ["mod"])
            TS("dve", mod1[:], mod[:, 0:88], 1.0, None, ALU.add, reads=["mod"], writes=["mod1"])
            TT("dve", small[:, 16:24], small[:, 16:24], mod[:, 48 + 16:48 + 24], ALU.mult, ["small_ps", "mod"], ["small_pc"])
            S.barrier()
        dump("mod", mod[:], [128, 96], "mod")

        rot = {"pj": 0, "at": 0, "o": 0, "ds": 0}

        def ps_pj():
            rot["pj"] ^= 1
            return ps[rot["pj"]], "ps%d" % rot["pj"]

        def norm_tile(NS, xk, xkeys, n, l, ffn, out_bf, out_bf_keys, out_f32=None, out_f32_keys=None, final=False):
            base = l * 48 + (24 if ffn else 0)
            pj, pk = ps_pj()
            for k in range(8):
                ACT(NS["sq"][:, k, :n], xk[k], AF.Square, [xkeys[k]], ["sq%d" % k])
            for k in range(8):
                MM(pj[:, :n], ones_b[:], NS["sq"][:, k, :n], start=(k == 0), stop=(k == 7),
                   reads=["sq%d" % k, "ones_b"], writes=[pk])
            ACT(NS["stdt"][:, :n], pj[:, :n], AF.Sqrt, [pk, "eps_t"], ["stdt"], bias=eps_t[:, 0:1], scale=1.0 / 1024.0)
            S.op("dve", lambda e: e.reciprocal(out=NS["stdt"][:, :n], in_=NS["stdt"][:, :n]), ["stdt"], ["stdt"])
            for k in range(8):
                if final:
                    STT(out_f32[k], xk[k], small[:, 32 + k:33 + k], NS["stdt"][:, :n], ALU.mult, ALU.mult,
                        [xkeys[k], "stdt", "small_fn"], [out_f32_keys[k]])
                    continue
                STT(NS["t1"][:, k, :n], xk[k], mod1[:, base + 8 + k:base + 9 + k], NS["stdt"][:, :n], ALU.mult, ALU.mult,
                    [xkeys[k], "stdt", "mod1"], ["t1_%d" % k])
                if out_f32 is not None:
                    ACT(out_f32[k], NS["t1"][:, k, :n], AF.Identity, ["t1_%d" % k, "mod"], [out_f32_keys[k]],
                        bias=mod[:, base + k:base + k + 1])
                    COPY("pool", out_bf[k], out_f32[k], [out_f32_keys[k]], [out_bf_keys[k]])
                else:
                    ACT(out_bf[k], NS["t1"][:, k, :n], AF.Identity, ["t1_%d" % k, "mod"], [out_bf_keys[k]],
                        bias=mod[:, base + k:base + k + 1])

        def mixer_norm():
            with ExitStack() as PN:
                NS = {"sq": sbt(PN, "sq", [128, 8, 512], BF16), "stdt": sbt(PN, "stdt", [128, 512]),
                      "t1": sbt(PN, "t1", [128, 8, 512])}
                xt = [sbt(PN, "xt%d" % i, [128, 8, 512]) for i in range(2)]
                for ti, (t0, n) in enumerate(tiles(0, T)):
                    b = xt[ti % 2]
                    DMA("sp", b[:, :, :n], xTw[W["w"], :, t0:t0 + n].rearrange("(k p) t -> p k t", p=128), writes=["xt%d" % (ti % 2)])
                    norm_tile(NS, [b[:, k, :n] for k in range(8)], ["xt%d" % (ti % 2)] * 8, n, 0, False,
                              [hnT[:, k, t0:t0 + n] for k in range(8)], ["hn%d" % k for k in range(8)])
                S.barrier()

        def proj_fm(whb, qi, t0, n):
            wh, whk = whb
            pj, pk = ps_pj()
            for k in range(8):
                MM(pj[:, :n], wh[:, k, qi, :], hnT[:, k, t0:t0 + n], start=(k == 0), stop=(k == 7),
                   reads=[whk, "hn%d" % k], writes=[pk])
            return pj, pk

        def proj_v(whb, qi, vtok):
            wh, whk = whb
            for c0 in range(0, NCH, 4):
                pj, pk = ps_pj()
                ncs = min(4, NCH - c0)
                for ci in range(ncs):
                    c = c0 + ci
                    for k in range(8):
                        MM(pj[0:64, ci * 128:(ci + 1) * 128], hnT[:, k, c * 64:(c + 1) * 64], wh[:, k, qi, :],
                           start=(k == 0), stop=(k == 7), reads=[whk, "hn%d" % k], writes=[pk])
                ACT(vtok[:, c0:c0 + ncs, :], pj[0:64, 0:ncs * 128].rearrange("p (c e) -> p c e", e=128), AF.Identity,
                    [pk], ["vtok"])

        def k_transposes(kA, ktok, ksc):
            for c0 in range(0, NCH, 8):
                ncs = min(8, NCH - c0)
                for ci in range(ncs):
                    c = c0 + ci
                    TR(psb[0:64, ci * 128:(ci + 1) * 128], kA[:, c * 64:(c + 1) * 64], ident_b[:], ["kA", "ident_b"], ["psb"])
                TS("dve", ktok[:, c0:c0 + ncs, :], psb[0:64, 0:ncs * 128].rearrange("p (c e) -> p c e", e=128),
                   ksc, None, ALU.mult, reads=["psb", "kscale"], writes=["ktok"])

        def recurrence(h, HS, full, mask_ap):
            St, Sb = HS["S"], HS["Sb"]
            if not full:
                for c in range(NCH - 1):
                    MM(ps[6][:, 0:128], HS["ktok"][:, c, :], HS["vtok"][:, c, :], start=(c == 0), stop=(c == NCH - 2),
                       reads=["ktok", "vtok"], writes=["ps6"])
                COPY("act", (W["L"][:, h, :] if W["L"] is not None else Lst[:, h, :]), ps[6][:, 0:128], ["ps6"], ["Lst"])
                return
            if full:
                COPY("pool", St[:], Sin[:, h, :], ["Sin"], ["S"])
            else:
                MEMSET("pool", St[:], 0.0, ["S"])
            nch = NCH if full else NCH - 1
            ob = None
            for c in range(nch):
                cs = slice(c * 64, (c + 1) * 64)
                if full:
                    rot["at"] ^= 1
                    pa, pak = ps[2 + rot["at"]], "ps%d" % (2 + rot["at"])
                    MM(pa[0:64, 0:64], HS["kA"][:, cs], HS["qA"][:, cs], reads=["kA", "qA"], writes=[pak])
                    atm = HS["atm"][rot["at"]]
                    TT("dve", atm[:], pa[0:64, 0:64], mask_ap, ALU.mult, [pak, "masks"], ["atm%d" % rot["at"]])
                    TS("pool", Sb[:], St[:], HS["tabM"][:, c:c + 1], None, ALU.mult, reads=["S", "tabM"], writes=["Sb"])
                    if c % 8 == 0:
                        rot["o"] ^= 1
                        ob, obk = ps[4 + rot["o"]], "ps%d" % (4 + rot["o"])
                    oc = (c % 8) * 64
                    MM(ob[:, oc:oc + 64], HS["vtok"][:, c, :], atm[:], start=True, stop=False,
                       reads=["vtok", "atm%d" % rot["at"]], writes=[obk])
                    MM(ob[:, oc:oc + 64], Sb[:], HS["qB"][:, cs], start=False, stop=True, reads=["Sb", "qB"], writes=[obk])
                    if c % 8 == 7 or c == nch - 1:
                        c0 = (c // 8) * 8
                        headnorm(h, HS, ob, obk, c0 * 64, (c + 1 - c0) * 64)
                if c < NCH - 1:
                    rot["ds"] = (rot["ds"] + 1) % 4
                    pd = ps[6][:, rot["ds"] * 128:(rot["ds"] + 1) * 128]
                    pdk = "ps6_%d" % rot["ds"]
                    MM(pd, HS["ktok"][:, c, :], HS["vtok"][:, c, :], reads=["ktok", "vtok"], writes=[pdk])
                    ACT(HS["dst"][:], pd, AF.Identity, [pdk, "tabC"], ["dst"], scale=HS["tabC"][:, c:c + 1])
                    STT(St[:], St[:], HS["tabA"][:, c:c + 1], HS["dst"][:], ALU.mult, ALU.add, ["S", "tabA", "dst"], ["S"])
            if not full:
                COPY("pool", (W["L"][:, h, :] if W["L"] is not None else Lst[:, h, :]), St[:], ["S"], ["Lst"])

        def headnorm(h, HS, ob, obk, t0, n):
            ACT(HS["osq"][:, :n], ob[:, :n], AF.Square, [obk], ["osq"])
            pj, pk = ps_pj()
            MM(pj[:, :n], ones_b[:], HS["osq"][:, :n], reads=["osq", "ones_b"], writes=[pk])
            ACT(HS["ostd"][:, :n], pj[:, :n], AF.Sqrt, [pk, "eps_t"], ["ostd"], bias=eps_t[:, 0:1], scale=1.0 / 128.0)
            S.op("dve", lambda e: e.reciprocal(out=HS["ostd"][:, :n], in_=HS["ostd"][:, :n]), ["ostd"], ["ostd"])
            STT(HS["ot"][:, :n], ob[:, :n], small[:, 8 + h:9 + h], HS["ostd"][:, :n], ALU.mult, ALU.mult,
                [obk, "ostd", "small_g"], ["ot"])
            TT("pool", catT[:, h, t0:t0 + n], HS["ot"][:, :n], HS["gate"][:, t0:t0 + n], ALU.mult, ["ot", "gate"], ["cat%d" % h])

        def head_common(PH, full):
            HS = {"kA": sbt(PH, "kA", [128, T], BF16), "ktok": sbt(PH, "ktok", [64, NCH, 128], BF16),
                  "vtok": sbt(PH, "vtok", [64, NCH, 128], BF16), "S": sbt(PH, "S", [128, 128]),
                  "Sb": sbt(PH, "Sb", [128, 128], BF16), "dst": sbt(PH, "dst", [128, 128]),
                  "tabA": sbt(PH, "tabA", [128, NCH]), "tabC": sbt(PH, "tabC", [128, NCH]), "tabM": sbt(PH, "tabM", [128, NCH]),
                  "tmp": [sbt(PH, "tmp%d" % i, [128, 512]) for i in range(3)]}
            if full:
                HS.update({"qA": sbt(PH, "qA", [128, T], BF16), "gate": sbt(PH, "gate", [128, T], BF16),
                           "atm": [sbt(PH, "atm%d" % i, [64, 64], BF16) for i in range(2)],
                           "osq": sbt(PH, "osq", [128, 512], BF16), "ostd": sbt(PH, "ostd", [128, 512]),
                           "ot": sbt(PH, "ot", [128, 512])})
            return HS

        def hg_heads(full):
            with ExitStack() as PH:
                HS = head_common(PH, full)
                if full:
                    HS["qB"] = HS["qA"]
                whs = [sbt(PH, "wh%d" % i, [128, 8, 4, 128], BF16) for i in range(2)]

                def load_wh(h_):
                    for qi in (range(4) if full else (1, 2)):
                        DMA("pool", whs[h_ % 2][:, :, qi, :], w_in[:, qi * 512 + h_ * 128: qi * 512 + (h_ + 1) * 128]
                            .rearrange("(k p) c -> p k c", p=128), writes=["wh%d" % (h_ % 2)])
                ff = sbt(PH, "ff", [128, T])
                bA = sbt(PH, "bA", [128, T])
                bB = sbt(PH, "bB", [128, T])
                scanm = sbt(PH, "scanm", [128, T])
                masks = sbt(PH, "masks", [64, 64])
                ksc = sbt(PH, "ksc", [64, 8])
                tmpd = sbt(PH, "tmpd", [128, NCH])
                pref = sbt(PH, "pref", [128, NCH])
                Wt = sbt(PH, "Wt", [128, NCH])
                onesN = sbt(PH, "onesN", [128, NCH])
                MEMSET("pool", onesN[:], 1.0, ["onesN"])
                sumbl = sbt(PH, "sumbl", [128, 1])
                DMA("sp", scanm[:], scanm_d[:, :], writes=["scanm"])
                DMA("sp", masks[:], masks_d[:, 0:64], writes=["masks"])
                DMA("sp", ksc[:], kscale_d[:, :], writes=["kscale"])
                b3 = bB[:].rearrange("p (c s) -> p c s", s=64)
                a3 = bA[:].rearrange("p (c s) -> p c s", s=64)
                load_wh(0)
                for h in range(4):
                    if h + 1 < 4:
                        load_wh(h + 1)
                    wh = (whs[h % 2], "wh%d" % (h % 2))
                    for ti, (t0, n) in enumerate(tiles(0, T)):
                        pj, pk = proj_fm(wh, 1, t0, n)
                        tm = HS["tmp"][ti % 2]
                        ACT(tm[:, :n], pj[:, :n], AF.Sigmoid, [pk], ["tmp%d" % (ti % 2)])
                        TS("dve", ff[:, t0:t0 + n], tm[:, :n], small[:, 4 + h:5 + h], small[:, h:h + 1], ALU.mult, ALU.add,
                           reads=["tmp%d" % (ti % 2), "small_lb", "small_oml"], writes=["ff"])
                    if "h1" in dbg:
                        return
                    ACT(bA[:], ff[:], AF.Ln, ["ff"], ["bA"])
                    if "h1b" in dbg:
                        return
                    S.op("dve", lambda e: e.tensor_tensor_scan(out=bB[:], data0=scanm[:], data1=bA[:], initial=0.0,
                                                               op0=ALU.mult, op1=ALU.add), ["scanm", "bA"], ["bB"])
                    if "h1c" in dbg:
                        return
                    ACT(HS["tabA"][:], b3[:, :, 63], AF.Exp, ["bB"], ["tabA"])
                    TT("dve", tmpd[:], b3[:, :, 63], b3[:, :, 31], ALU.subtract, ["bB"], ["tmpd"])
                    ACT(HS["tabC"][:], tmpd[:], AF.Exp, ["tmpd"], ["tabC"])
                    ACT(HS["tabM"][:], b3[:, :, 31], AF.Exp, ["bB"], ["tabM"])
                    S.op("dve", lambda e: e.reduce_sum(out=sumbl[:], in_=b3[:, 0:NCH - 1, 63], axis=AX.X), ["bB"], ["sumbl"])
                    ACT((W["D"] if W["D"] is not None else Dst)[:, h:h + 1], sumbl[:], AF.Exp, ["sumbl"], ["Dst"])
                    TT("dve", HS["tabA"][:, 0:1], HS["tabA"][:, 0:1], flagw[:, W["w"]:W["w"] + 1], ALU.mult, ["tabA", "flag"], ["tabA"])
                    TT("dve", HS["tabC"][:, 0:1], HS["tabC"][:, 0:1], flagw[:, W["w"]:W["w"] + 1], ALU.mult, ["tabC", "flag"], ["tabC"])
                    if "h1d" in dbg:
                        return
                    if not full:
                        S.op("dve", lambda e: e.tensor_tensor_scan(out=pref[:], data0=onesN[:], data1=b3[:, :, 63], initial=0.0,
                                                                   op0=ALU.mult, op1=ALU.add), ["onesN", "bB"], ["pref"])
                        TT("dve", Wt[:, 0:NCH - 1], tmpd[:, 0:NCH - 1], pref[:, 0:NCH - 1], ALU.subtract, ["tmpd", "pref"], ["Wt"])
                        ACT(Wt[:, 0:NCH - 1], Wt[:, 0:NCH - 1], AF.Exp, ["Wt", "pref"], ["Wt"], bias=pref[:, NCH - 2:NCH - 1])
                        MEMSET("pool", Wt[:, NCH - 1:NCH], 0.0, ["Wt"])
                        TT("dve", Wt[:, 0:1], Wt[:, 0:1], flagw[:, W["w"]:W["w"] + 1], ALU.mult, ["Wt", "flag"], ["Wt"])
                    TT("dve", a3, b3, b3[:, :, 31:32].broadcast_to([128, NCH, 64]), ALU.subtract, ["bB", "bA"], ["bA"])
                    if "h1e" in dbg:
                        return
                    ACT(bB[:], bA[:], AF.Exp, ["bA"], ["bB"])
                    ACT(bA[:], bA[:], AF.Exp, ["bA"], ["bA"], scale=-1.0)
                    if not full:
                        TT("dve", a3, a3, Wt[:].unsqueeze(2).broadcast_to([128, NCH, 64]), ALU.mult, ["bA", "Wt"], ["bA"])
                    TS("pool", ff[:], ff[:], -1.0, 1.0, ALU.mult, ALU.add, reads=["ff"], writes=["ff"])
                    TT("dve", HS["kA"][:], ff[:], bA[:], ALU.mult, ["ff", "bA"], ["kA"])
                    if full:
                        for ti, (t0, n) in enumerate(tiles(0, T)):
                            pj, pk = proj_fm(wh, 0, t0, n)
                            tm = HS["tmp"][ti % 2]
                            ACT(tm[:, :n], pj[:, :n], AF.Silu, [pk], ["tmp%d" % (ti % 2)])
                            TT("dve", HS["qA"][:, t0:t0 + n], tm[:, :n], bB[:, t0:t0 + n], ALU.mult,
                               ["tmp%d" % (ti % 2), "bB"], ["qA"])
                            pj, pk = proj_fm(wh, 3, t0, n)
                            ACT(HS["gate"][:, t0:t0 + n], pj[:, :n], AF.Silu, [pk], ["gate"])
                    if "h2" in dbg:
                        return
                    proj_v(wh, 2, HS["vtok"])
                    if "h3" in dbg:
                        return
                    k_transposes(HS["kA"], HS["ktok"], ksc[:, h:h + 1])
                    if "h4" in dbg:
                        return
                    recurrence(h, HS, full, masks[:])
                    if "h5" in dbg:
                        return
                S.barrier()

        def ret_heads(full):
            with ExitStack() as PH:
                HS = head_common(PH, full)
                if full:
                    HS["qB"] = sbt(PH, "qB", [128, T], BF16)
                whs = [sbt(PH, "wh6_%d" % i, [128, 8, 6, 128], BF16) for i in range(2)]

                def load_wh(r_):
                    srcs = [w_in[:, 2048 + r_ * 128:2048 + (r_ + 1) * 128], w_in_sw[:, r_ * 128:(r_ + 1) * 128],
                            w_in[:, 2560 + r_ * 128:2560 + (r_ + 1) * 128], w_in_sw[:, 512 + r_ * 128:512 + (r_ + 1) * 128],
                            w_in[:, 3072 + r_ * 128:3072 + (r_ + 1) * 128], w_in[:, 3584 + r_ * 128:3584 + (r_ + 1) * 128]]
                    for qi in (range(6) if full else (2, 3, 4)):
                        DMA("pool", whs[r_ % 2][:, :, qi, :], srcs[qi].rearrange("(k p) c -> p k c", p=128), writes=["wh%d" % (r_ % 2)])
                cosT = sbt(PH, "cosT", [128, T])
                sinT = sbt(PH, "sinT", [128, T])
                gdec = sbt(PH, "gdec", [128, 4, 64])
                masks = sbt(PH, "dmasks", [64, 4, 64])
                ksc = sbt(PH, "ksc", [64, 8])
                Wr = sbt(PH, "Wr", [128, 4, NCH])
                DMA("sp", Wr[:], retW_d[:, :].rearrange("p (h c) -> p h c", c=NCH), writes=["Wr"])
                for r_ in range(4):
                    TT("dve", Wr[:, r_, 0:1], Wr[:, r_, 0:1], flagw[:, W["w"]:W["w"] + 1], ALU.mult, ["Wr", "flag"], ["Wr"])
                DMA("sp", cosT[:], cos_d[W["w"], :, :], writes=["cosT"])
                DMA("sp", sinT[:], sin_d[W["w"], :, :], writes=["sinT"])
                DMA("sp", gdec[:], gdec_d[:, :].rearrange("p (h s) -> p h s", s=64), writes=["gdec"])
                DMA("sp", masks[:], masks_d[:, 64:320].rearrange("p (h s) -> p h s", s=64), writes=["masks"])
                DMA("sp", ksc[:], kscale_d[:, :], writes=["kscale"])
                load_wh(0)
                for r in range(4):
                    h = 4 + r
                    if r + 1 < 4:
                        load_wh(r + 1)
                    wh = (whs[r % 2], "wh%d" % (r % 2))
                    MEMSET("pool", HS["tabA"][:], gam[r] ** 64, ["tabA"])
                    MEMSET("pool", HS["tabC"][:], 1.0, ["tabC"])
                    MEMSET("pool", HS["tabM"][:], 1.0, ["tabM"])
                    MEMSET("pool", (W["D"] if W["D"] is not None else Dst)[:, h:h + 1], gam[r] ** (64 * (NCH - 1)), ["Dst"])
                    TT("dve", HS["tabA"][:, 0:1], HS["tabA"][:, 0:1], flagw[:, W["w"]:W["w"] + 1], ALU.mult, ["tabA", "flag"], ["tabA"])
                    TT("dve", HS["tabC"][:, 0:1], HS["tabC"][:, 0:1], flagw[:, W["w"]:W["w"] + 1], ALU.mult, ["tabC", "flag"], ["tabC"])
                    for which in ((0, 1) if full else (1,)):
                        for ti, (t0, n) in enumerate(tiles(0, T)):
                            pa, pak = proj_fm(wh, 2 * which, t0, n)
                            t1, t2 = HS["tmp"][0], HS["tmp"][1]
                            TT("dve", t1[:, :n], pa[:, :n], cosT[:, t0:t0 + n], ALU.mult, [pak, "cosT"], ["tmp0"])
                            pb, pbk = proj_fm(wh, 2 * which + 1, t0, n)
                            TT("dve", t2[:, :n], pb[:, :n], sinT[:, t0:t0 + n], ALU.mult, [pbk, "sinT"], ["tmp1"])
                            TT("pool", t1[:, :n], t1[:, :n], t2[:, :n], ALU.add, ["tmp0", "tmp1"], ["tmp0"])
                            if which == 0:
                                ACT(HS["qA"][:, t0:t0 + n], t1[:, :n], AF.Identity, ["tmp0"], ["qA"])
                                TT("dve", HS["qB"][:, t0:t0 + n].rearrange("p (c s) -> p c s", s=64),
                                   t1[:, :n].rearrange("p (c s) -> p c s", s=64),
                                   gdec[:, r, :].unsqueeze(1).broadcast_to([128, n // 64, 64]), ALU.mult,
                                   ["tmp0", "gdec"], ["qB"])
                            else:
                                ACT(HS["kA"][:, t0:t0 + n], t1[:, :n], AF.Identity, ["tmp0"], ["kA"], scale=128.0 ** -0.5)
                    if not full:
                        k3 = HS["kA"][:].rearrange("p (c s) -> p c s", s=64)
                        TT("dve", k3, k3, Wr[:, r, :].unsqueeze(2).broadcast_to([128, NCH, 64]), ALU.mult, ["kA", "Wr"], ["kA"])
                    if full:
                        for ti, (t0, n) in enumerate(tiles(0, T)):
                            pj, pk = proj_fm(wh, 5, t0, n)
                            ACT(HS["gate"][:, t0:t0 + n], pj[:, :n], AF.Silu, [pk], ["gate"])
                    proj_v(wh, 4, HS["vtok"])
                    k_transposes(HS["kA"], HS["ktok"], ksc[:, h:h + 1])
                    recurrence(h, HS, full, masks[:, r, :])
                S.barrier()

        def chain_states(Lall_sb, Dall_sb):
            with ExitStack() as PC:
                tmp = sbt(PC, "ctmp", [128, 128])
                MEMSET("pool", Sin[:], 0.0, ["Sin"])
                for i in (range(ncores - 2, -1, -1) if mode == "F2" else range(ncores - 1)):
                    for h in range(8):
                        STT(tmp[:], Sin[:, h, :], Dall_sb[:, i, h:h + 1], Lall_sb[:, i, h, :], ALU.mult, ALU.add,
                            ["Sin", "Lall", "Dall"], ["ctmp"])
                        TT("dve", tmp[:], tmp[:], Sin[:, h, :], ALU.subtract, ["ctmp", "Sin"], ["ctmp"])
                        STT(Sin[:, h, :], tmp[:], seli[:, i:i + 1], Sin[:, h, :], ALU.mult, ALU.add,
                            ["ctmp", "seli", "Sin"], ["Sin"])
                S.barrier()

        def wout_residual():
            with ExitStack() as PW:
                wo = sbt(PW, "wo", [128, 8, 1024], BF16)
                xhi = sbt(PW, "xhi", [128, 4, T])
                xin = [sbt(PW, "xin%d" % i, [128, 512]) for i in range(2)]
                DMA("pool", wo[:], w_out[:, :].rearrange("(k p) c -> p k c", p=128), writes=["wo"])
                i = 0
                for dm in range(8):
                    for (t0, n) in tiles(0, T):
                        pj, pk = ps_pj()
                        for h in range(8):
                            MM(pj[:, :n], wo[:, h, dm * 128:(dm + 1) * 128], catT[:, h, t0:t0 + n], start=(h == 0), stop=(h == 7),
                               reads=["wo", "cat%d" % h], writes=[pk])
                        i ^= 1
                        DMA("sp", xin[i][:, :n], xTw[0, dm * 128:(dm + 1) * 128, t0:t0 + n], writes=["xin%d" % i])
                        dest = big[:, dm, t0:t0 + n] if dm < 4 else xhi[:, dm - 4, t0:t0 + n]
                        STT(dest, pj[:, :n], mod[:, 16 + dm:17 + dm], xin[i][:, :n], ALU.mult, ALU.add,
                            [pk, "mod", "xin%d" % i], ["xdest%d" % dm])
                S.barrier()
                for dm in range(4, 8):
                    COPY("pool" if dm % 2 else "act", big[:, dm, :], xhi[:, dm - 4, :], ["xdest%d" % dm], ["x%d" % dm])
                S.barrier()

        def moe(l, lo, hi):
            halves = [(lo, (lo + hi) // 2), ((lo + hi) // 2, hi)]
            for (h0, h1) in halves:
                NH = h1 - h0
                with ExitStack() as PM:
                    hnh = sbt(PM, "hnh", [128, 8, NH], BF16)
                    gatesT = sbt(PM, "gatesT", [32, NH])
                    with ExitStack() as PN:
                        NS = {"sq": sbt(PN, "sq", [128, 8, 512], BF16), "stdt": sbt(PN, "stdt", [128, 512]),
                              "t1": sbt(PN, "t1", [128, 8, 512])}
                        hn32 = sbt(PN, "hn32", [128, 8, 512])
                        wr = sbt(PN, "wr", [128, 8, 32])
                        br = sbt(PN, "br", [1, 32])
                        lg = sbt(PN, "lg", [128, 32])
                        mx8 = sbt(PN, "mx8", [128, 8])
                        msk = sbt(PN, "msk", [128, 32])
                        ex = sbt(PN, "ex", [128, 32])
                        den = sbt(PN, "den", [128, 2])
                        DMA("sp", wr[:], w_router[l].rearrange("(k p) e -> p k e", p=128), writes=["wr"])
                        DMA("sp", br[:], b_router[l:l + 1, :], writes=["br"])
                        for (t0, n) in tiles(h0, h1):
                            norm_tile(NS, [big[:, k, t0:t0 + n] for k in range(8)], ["x%d" % k for k in range(8)], n, l, True,
                                      [hnh[:, k, t0 - h0:t0 - h0 + n] for k in range(8)], ["hnh%d" % k for k in range(8)],
                                      [hn32[:, k, :n] for k in range(8)], ["hn32_%d" % k for k in range(8)])
                            for s0 in range(0, n, 128):
                                m = min(128, n - s0)
                                pl = ps[6]
                                for k in range(8):
                                    MM(pl[:m, 0:32], hn32[:, k, s0:s0 + m], wr[:, k, :], start=(k == 0), stop=False,
                                       reads=["hn32_%d" % k, "wr"], writes=["ps6"])
                                MM(pl[:m, 0:32], ones_f[0:1, 0:m], br[0:1, :], start=False, stop=True, reads=["ones_f", "br"], writes=["ps6"])
                                COPY("dve", lg[:m, :], pl[:m, 0:32], ["ps6"], ["lg"])
                                S.op("dve", lambda e, m=m: e.max(out=mx8[:m, :], in_=lg[:m, :]), ["lg"], ["mx8"])
                                TS("dve", msk[:m, :], lg[:m, :], mx8[:m, 3:4], None, ALU.is_ge, reads=["lg", "mx8"], writes=["msk"])
                                TS("dve", den[:m, 0:1], mx8[:m, 0:1], -1.0, None, ALU.mult, reads=["mx8"], writes=["den0"])
                                ACT(ex[:m, :], lg[:m, :], AF.Exp, ["lg", "den0"], ["ex"], bias=den[:m, 0:1])
                                TT("dve", ex[:m, :], ex[:m, :], msk[:m, :], ALU.mult, ["ex", "msk"], ["ex"])
                                S.op("dve", lambda e, m=m: e.reduce_sum(out=den[:m, 1:2], in_=ex[:m, :], axis=AX.X), ["ex"], ["den1"])
                                S.op("dve", lambda e, m=m: e.reciprocal(out=den[:m, 1:2], in_=den[:m, 1:2]), ["den1"], ["den1"])
                                TS("dve", ex[:m, :], ex[:m, :], den[:m, 1:2], None, ALU.mult, reads=["ex", "den1"], writes=["ex"])
                                TR(ps[6][0:32, 128:128 + m], ex[:m, :], ident_f[:m, :m], ["ex", "ident_f"], ["ps6"])
                                c0 = t0 - h0 + s0
                                COPY("act", gatesT[:, c0:c0 + m], ps[6][0:32, 128:128 + m], ["ps6"], ["gatesT"])
                        S.barrier()
                    if "gatesT" in dbg and h0 == lo and l == 0:
                        dump("gatesT", gatesT[:], [32, NH], "gatesT")
                    with ExitStack() as PE_:
                        act = sbt(PE_, "act", [128, 8, NH], BF16)
                        gbc = sbt(PE_, "gbc", [128, NH])
                        wg = [sbt(PE_, "wg%d" % i, [128, 8, 2, 512], BF16) for i in range(2)]
                        wd = [sbt(PE_, "wd%d" % i, [128, 8, 1024], BF16) for i in range(2)]
                        NTT = 4
                        tt = [[sbt(PE_, "tt%d_%d" % (i, j), [128, 512]) for j in range(3)] for i in range(NTT)]
                        bgu = sbt(PE_, "bgu", [128, 32 * 16])
                        bdn = sbt(PE_, "bdn", [32, 1024])
                        DMA("sp", bgu[:], b_guT_d[:, l * 512:(l + 1) * 512], writes=["bgu"])
                        bgl = bgu[:].rearrange("p (e c) -> p e c", c=16)[:, :, 8:16]
                        TS("dve", bgl, bgl, 1.0, None, ALU.add, reads=["bgu"], writes=["bgu"])
                        DMA("sp", bdn[:], b_down[l], writes=["bdn"])
                        cnt = {"g": 0, "t": 0, "y": 0, "d": 0}

                        def down_evac(py, pyk, dm, t0, n):
                            STT(big[:, dm, t0:t0 + n], py[:, :n], mod[:, l * 48 + 40 + dm:l * 48 + 41 + dm], big[:, dm, t0:t0 + n],
                                ALU.mult, ALU.add, [pyk, "mod", "x%d" % dm], ["x%d" % dm])

                        for dm in range(8):
                            for (t0, n) in tiles(h0, h1):
                                cnt["d"] ^= 1
                                py, pyk = ps[4 + cnt["d"]], "ps%d" % (4 + cnt["d"])
                                MM(py[:, :n], bdn[0:32, dm * 128:(dm + 1) * 128], gatesT[0:32, t0 - h0:t0 - h0 + n],
                                   reads=["bdn", "gatesT"], writes=[pyk])
                                down_evac(py, pyk, dm, t0, n)
                        def issue_wg(si):
                            e_, fq_ = si // 2, si % 2
                            for two in range(2):
                                DMA("pool", wg[si % 2][:, :, two, :],
                                    w_gu[l, e_, :, two * 1024 + fq_ * 512: two * 1024 + (fq_ + 1) * 512].rearrange("(k p) c -> p k c", p=128),
                                    writes=["wg%d" % (si % 2)])

                        def issue_wd(e_):
                            DMA("pool", wd[e_ % 2][:], w_down[l, e_].rearrange("(k p) c -> p k c", p=128), writes=["wd%d" % (e_ % 2)])

                        issue_wg(0)
                        issue_wd(0)
                        for e in range(32):
                            for (t0, n) in tiles(0, NH):
                                MM(ps[6][:, :n], ident_f[0:32, e:e + 1].broadcast_to([32, 128]), gatesT[0:32, t0:t0 + n],
                                   reads=["ident_f", "gatesT"], writes=["ps6"])
                                COPY("act", gbc[:, t0:t0 + n], ps[6][:, :n], ["ps6"], ["gbc"])
                            wdi = e % 2
                            if e + 1 < 32:
                                issue_wd(e + 1)
                            for fc in range(8):
                                si = e * 2 + fc // 4
                                fi = fc % 4
                                if fi == 0 and si + 1 < 64:
                                    issue_wg(si + 1)
                                wgi, wgk = wg[si % 2], "wg%d" % (si % 2)
                                bcol = e * 16 + fc
                                for (t0, n) in tiles(0, NH):
                                    pg, pgk = ps[0], "ps0"
                                    plin, plk = ps[1], "ps1"
                                    if (t0 // 512) % 2:
                                        pg, pgk, plin, plk = ps[2], "ps2", ps[3], "ps3"
                                    for k in range(8):
                                        MM(pg[:, :n], wgi[:, k, 0, fi * 128:(fi + 1) * 128], hnh[:, k, t0:t0 + n], start=(k == 0), stop=(k == 7),
                                           reads=[wgk, "hnh%d" % k], writes=[pgk])
                                    for k in range(8):
                                        MM(plin[:, :n], wgi[:, k, 1, fi * 128:(fi + 1) * 128], hnh[:, k, t0:t0 + n], start=(k == 0), stop=(k == 7),
                                           reads=[wgk, "hnh%d" % k], writes=[plk])
                                    cnt["t"] = (cnt["t"] + 1) % NTT
                                    t1, t2, t3 = tt[cnt["t"]]
                                    k1, k2, k3 = ["tt%d_%d" % (cnt["t"], j) for j in range(3)]
                                    TS("dve", t1[:, :n], pg[:, :n], bgu[:, bcol:bcol + 1], 7.0, ALU.add, ALU.min, reads=[pgk, "bgu"], writes=[k1])
                                    ACT(t2[:, :n], t1[:, :n], AF.Sigmoid, [k1], [k2], scale=1.702)
                                    TS("dve", t3[:, :n], plin[:, :n], bgu[:, bcol + 8:bcol + 9], 8.0, ALU.add, ALU.min, reads=[plk, "bgu"], writes=[k3])
                                    STT(t3[:, :n], t3[:, :n], -6.0, gbc[:, t0:t0 + n], ALU.max, ALU.mult, [k3, "gbc"], [k3])
                                    TT("pool", t1[:, :n], t1[:, :n], t2[:, :n], ALU.mult, [k1, k2], [k1])
                                    TT("pool", act[:, fc, t0:t0 + n], t1[:, :n], t3[:, :n], ALU.mult, [k1, k3], ["act%d" % fc])
                            for dm in range(8):
                                for (t0, n) in tiles(0, NH):
                                    cnt["d"] ^= 1
                                    py, pyk = ps[4 + cnt["d"]], "ps%d" % (4 + cnt["d"])
                                    for fc in range(8):
                                        MM(py[:, :n], wd[wdi][:, fc, dm * 128:(dm + 1) * 128], act[:, fc, t0:t0 + n],
                                           start=(fc == 0), stop=(fc == 7), reads=["wd%d" % wdi, "act%d" % fc], writes=[pyk])
                                    down_evac(py, pyk, dm, h0 + t0, n)
                        S.barrier()

        def pool_mixer():
            l = 1
            with ExitStack() as PP:
                NS = {"sq": sbt(PP, "sq", [128, 8, 512], BF16), "stdt": sbt(PP, "stdt", [128, 512])}
                rstd = sbt(PP, "rstd", [128, T])
                hb = sbt(PP, "hb", [128, T])
                s2 = sbt(PP, "s2", [128, T])
                s4 = sbt(PP, "s4", [128, T])
                pT = sbt(PP, "pT", [128, 8, T], BF16)
                pw = sbt(PP, "pw", [128, 4, 2, 256], BF16)
                invc = sbt(PP, "invc", [128, 4, 16])
                ptmp = [sbt(PP, "ptmp%d" % i, [128, 512]) for i in range(2)]
                DMA("sp", invc[:], invc_d[:, :].rearrange("p (g s) -> p g s", s=16), writes=["invc"])
                for g in range(4):
                    DMA("pool", pw[:, g, :, :], pool_w[g].rearrange("(i p) d -> p i d", p=128), writes=["pw"])
                for (t0, n) in tiles(0, T):
                    pj, pk = ps_pj()
                    for k in range(8):
                        ACT(NS["sq"][:, k, :n], big[:, k, t0:t0 + n], AF.Square, ["x%d" % k], ["sq%d" % k])
                    for k in range(8):
                        MM(pj[:, :n], ones_b[:], NS["sq"][:, k, :n], start=(k == 0), stop=(k == 7), reads=["sq%d" % k, "ones_b"], writes=[pk])
                    ACT(rstd[:, t0:t0 + n], pj[:, :n], AF.Sqrt, [pk, "eps_t"], ["rstd"], bias=eps_t[:, 0:1], scale=1.0 / 1024.0)
                S.op("dve", lambda e: e.reciprocal(out=rstd[:], in_=rstd[:]), ["rstd"], ["rstd"])
                for k in range(8):
                    g = k // 2
                    w = (2, 4, 8, 16)[g]
                    STT(hb[:], big[:, k, :], mod1[:, 48 + 8 + k:48 + 9 + k], rstd[:], ALU.mult, ALU.mult, ["x%d" % k, "rstd", "mod1"], ["hb"])
                    TS("dve", hb[:], hb[:], mod[:, 48 + k:48 + k + 1], None, ALU.add, reads=["hb", "mod"], writes=["hb"])
                    TS("dve", hb[:, 0:64], hb[:, 0:64], flagw[:, 0:1], None, ALU.mult, reads=["hb", "flag"], writes=["hb"])
                    TT("pool", s2[:, 1:T], hb[:, 1:T], hb[:, 0:T - 1], ALU.add, ["hb"], ["s2"])
                    ws, wsk = s2, "s2"
                    if w >= 4:
                        TT("pool", s4[:, 3:T], s2[:, 3:T], s2[:, 1:T - 2], ALU.add, ["s2"], ["s4"])
                        ws, wsk = s4, "s4"
                    if w >= 8:
                        TT("pool", s2[:, 7:T], s4[:, 7:T], s4[:, 3:T - 4], ALU.add, ["s4", "s2"], ["s2"])
                        ws, wsk = s2, "s2"
                    if w >= 16:
                        TT("pool", s4[:, 15:T], s2[:, 15:T], s2[:, 7:T - 8], ALU.add, ["s2", "s4"], ["s4"])
                        ws, wsk = s4, "s4"
                    STT(pT[:, k, 16:T], ws[:, 16:T], 1.0 / w, hb[:, 16:T], ALU.mult, ALU.subtract, [wsk, "hb"], ["pT%d" % k])
                    TT("dve", ws[:, 64:80], ws[:, 64:80], invc[:, g, :], ALU.mult, [wsk, "invc", "pT%d" % k], [wsk])
                    TT("dve", pT[:, k, 64:80], ws[:, 64:80], hb[:, 64:80], ALU.subtract, [wsk, "hb"], ["pT%d" % k])
                i = 0
                for k in range(8):
                    g, j = k // 2, k % 2
                    for (t0, n) in tiles(HALO, T):
                        pj, pk = ps_pj()
                        for ii in range(2):
                            MM(pj[:, :n], pw[:, g, ii, j * 128:(j + 1) * 128], pT[:, 2 * g + ii, t0:t0 + n], start=(ii == 0), stop=(ii == 1),
                               reads=["pw", "pT%d" % (2 * g + ii)], writes=[pk])
                        i ^= 1
                        TS("dve", ptmp[i][:, :n], pj[:, :n], small[:, 24 + k:25 + k], small[:, 16 + k:17 + k], ALU.add, ALU.mult,
                           reads=[pk, "small_pb", "small_pc"], writes=["ptmp%d" % i])
                        TT("pool", big[:, k, t0:t0 + n], big[:, k, t0:t0 + n], ptmp[i][:, :n], ALU.add, ["x%d" % k, "ptmp%d" % i], ["x%d" % k])
                S.barrier()

        def final_norm():
            with ExitStack() as PF:
                NS = {"sq": sbt(PF, "sq", [128, 8, 512], BF16), "stdt": sbt(PF, "stdt", [128, 512])}
                ob = [sbt(PF, "fo%d" % i, [128, 8, 512]) for i in range(2)]
                for ti, (t0, n) in enumerate(tiles(HALO, T)):
                    o = ob[ti % 2]
                    norm_tile(NS, [big[:, k, t0:t0 + n] for k in range(8)], ["x%d" % k for k in range(8)], n, 0, False,
                              None, None, [o[:, k, :n] for k in range(8)], ["fo%d_%d" % (ti % 2, k) for k in range(8)], final=True)
                    DMA("sp", yT[:, t0 - HALO:t0 - HALO + n].rearrange("(k p) t -> p k t", p=128), o[:, :, :n],
                        reads=["fo%d_%d" % (ti % 2, k) for k in range(8)], writes=["yT%d" % ti], is_out=True)

        if "s0" not in dbg and mode != "F2":
            mixer_norm()
        if mode in ("A", "F"):
            if "s0" not in dbg and "s1" not in dbg:
                try:
                    hg_heads(False)
                    if "s2" not in dbg:
                        ret_heads(False)
                except Stop:
                    pass
        if mode == "F2":
            with ExitStack() as PX:
                Lall_sb = sbt(PX, "Lall_sb", [128, ncores - 1, 8, 128])
                Dall_sb = sbt(PX, "Dall_sb", [128, ncores - 1, 8])
                for w in range(1, ncores):
                    W["w"], W["L"], W["D"] = w, Lall_sb[:, w - 1, :, :], Dall_sb[:, w - 1, :]
                    mixer_norm()
                    hg_heads(False)
                    ret_heads(False)
                W["w"], W["L"], W["D"] = 0, None, None
                chain_states(Lall_sb, Dall_sb)
            mixer_norm()
        if mode == "A":
            DMA("sp", Lst_o[:, :], Lst[:].rearrange("p h e -> p (h e)"), reads=["Lst"], writes=["Lst_o"], is_out=True)
            DMA("sp", Dst_o[:, :], Dst[:], reads=["Dst"], writes=["Dst_o"], is_out=True)
        else:
            for _once in ([1] if mode != "F2" else []):
              with ExitStack() as PX:
                Lall_sb = sbt(PX, "Lall_sb", [128, ncores, 8, 128])
                Dall_sb = sbt(PX, "Dall_sb", [128, ncores, 8])
                if mode == "B":
                    DMA("sp", Lall_sb[:], Lall_d.rearrange("c p (h e) -> p c h e", e=128), writes=["Lall"])
                    DMA("sp", Dall_sb[:], Dall_d.rearrange("c p h -> p c h"), writes=["Dall"])
                else:
                    bounce = nc.dram_tensor("st_bounce", [128, 1032], F32)
                    gath = nc.dram_tensor("st_gath", [ncores * 128, 1032], F32)
                    DMA("sp", bounce[:, 0:1024], Lst[:].rearrange("p h e -> p (h e)"), reads=["Lst"], writes=["bounce"])
                    DMA("sp", bounce[:, 1024:1032], Dst[:], reads=["Dst"], writes=["bounce"])
                    S.barrier()
                    S.op("pool", lambda e: e.collective_compute("AllGather", ALU.bypass, replica_groups=[list(range(ncores))],
                                                                ins=[bounce.ap().opt()], outs=[gath.ap().opt()]),
                         reads=["bounce"], writes=["gath"], dma_key="gath", dma_inc=1)
                    S.barrier()
                    gv = gath.ap().rearrange("(c p) w -> p c w", p=128)
                    DMA("sp", Lall_sb[:].rearrange("p c h e -> p c (h e)"), gv[:, :, 0:1024], reads=["gath"], writes=["Lall"])
                    DMA("sp", Dall_sb[:], gv[:, :, 1024:1032], reads=["gath"], writes=["Dall"])
                chain_states(Lall_sb, Dall_sb)
            dump("Sin", Sin[:].rearrange("p h e -> p (h e)"), [128, 1024], "Sin")
            hg_heads(True)
            ret_heads(True)
            if "cat" in dbg:
                cf = sbt(es, "catf", [128, 8, T])
                for h in range(8):
                    COPY("act", cf[:, h, :], catT[:, h, :], ["cat%d" % h], ["catf"])
                dump("cat", cf[:].rearrange("p h t -> p (h t)"), [128, 8 * T], "catf")
            wout_residual()
            dump("x1", big[:].rearrange("p k t -> p (k t)"), [128, 8 * T], "x0")
            if "stop1" not in dbg:
                moe(0, 0, T)
                dump("x2", big[:].rearrange("p k t -> p (k t)"), [128, 8 * T], "x0")
                pool_mixer()
                dump("x3", big[:].rearrange("p k t -> p (k t)"), [128, 8 * T], "x0")
                moe(1, HALO, T)
            final_norm()
        S.finish()
        S.emit()
    return nc, dbg_outs, in_names


def _consts(T, tok0):
    gam = [1.0 - 2.0 ** (-5 - h) for h in range(4)]
    idx = np.arange(64)
    rel = idx[None, :] - idx[:, None]
    masks = np.zeros((64, 5, 64), np.float64)
    masks[:, 0, :] = (rel >= 0)
    for h in range(4):
        masks[:, 1 + h, :] = np.where(rel >= 0, gam[h] ** np.maximum(rel, 0), 0.0)
    kscale = np.ones((64, 8), np.float64)
    for h in range(4):
        kscale[:, 4 + h] = gam[h] ** (63 - idx)
    gdec = np.zeros((128, 4, 64), np.float64)
    for h in range(4):
        gdec[:, h, :] = (gam[h] ** (idx + 1.0))[None, :]
    half = 64
    inv = (10000.0 ** (-np.arange(half, dtype=np.float32) / half)).astype(np.float32)
    pos = (tok0 + np.arange(T)).astype(np.float32)
    ang = (pos[None, :] * inv[:, None]).astype(np.float32)
    cos = np.cos(ang.astype(np.float64))
    sin = np.sin(ang.astype(np.float64))
    cosT = np.concatenate([cos, cos], 0)
    sinT = np.concatenate([-sin, sin], 0)
    nch = T // 64
    retW = np.zeros((128, 4, nch), np.float64)
    for h in range(4):
        for c in range(nch - 1):
            retW[:, h, c] = gam[h] ** (64.0 * (nch - 2 - c))
    scanm = np.ones((128, T), np.float32)
    scanm[:, ::64] = 0.0
    return dict(masks=masks.reshape(64, 320).astype(np.float32), kscale=kscale.astype(np.float32),
                gdec=gdec.reshape(128, 256).astype(np.float32), cosT=cosT.astype(np.float32),
                sinT=sinT.astype(np.float32), scanm=scanm, ident=np.eye(128, dtype=np.float32),
                retW=retW.reshape(128, 4 * nch).astype(np.float32))


def _pk(v, ncol):
    return np.ascontiguousarray(np.asarray(v, np.float32).reshape(ncol, 128).T)


def make_inputs(inp, ncores, nw=8):
    x = np.asarray(inp["x"], np.float32)[0]
    Sq = x.shape[0]
    TP = Sq // ncores
    T = TP + HALO
    w_in = np.asarray(inp["w_in"], np.float32)[0]
    perm = np.concatenate([np.arange(64, 128), np.arange(0, 64)])
    cols = []
    for base in (2048, 2560):
        for r in range(4):
            cols.append(base + r * 128 + perm)
    w_in_sw = np.ascontiguousarray(w_in[:, np.concatenate(cols)])
    shared = dict(
        cT=_pk(np.asarray(inp["c"])[0], 8),
        w_ada=np.asarray(inp["w_ada"], np.float32),
        b_adaT=np.concatenate([_pk(np.asarray(inp["b_ada"])[l], 48) for l in range(2)], 1),
        w_in=w_in, w_in_sw=w_in_sw,
        lbraw=np.concatenate([_pk(np.asarray(inp["hg_lower_bounds"])[r], 4) for r in range(2)], 1),
        gains=np.concatenate([_pk(np.asarray(inp["hg_norm"])[0].reshape(-1), 4),
                              _pk(np.asarray(inp["ret_norm"])[0].reshape(-1), 4)], 1),
        w_out=np.asarray(inp["w_out"], np.float32)[0],
        pool_w=np.asarray(inp["pool_w"], np.float32)[0],
        pool_bT=_pk(np.asarray(inp["pool_b"])[0].reshape(-1), 8),
        pool_sT=_pk(np.asarray(inp["pool_scale"])[0], 8),
        w_router=np.asarray(inp["w_router"], np.float32),
        b_router=np.asarray(inp["b_router"], np.float32),
        w_gu=np.asarray(inp["w_gu"], np.float32),
        b_guT=_pk(np.asarray(inp["b_gu"]).reshape(-1), 2 * 32 * 16),
        w_down=np.asarray(inp["w_down"], np.float32),
        b_down=np.asarray(inp["b_down"], np.float32),
        fnT=_pk(np.asarray(inp["final_norm"]), 8),
    )
    win = {}
    for c in range(ncores):
        tok0 = c * TP - HALO
        xs = np.zeros((T, 1024), np.float32)
        lo = max(tok0, 0)
        xs[lo - tok0:, :] = x[lo:tok0 + T, :]
        cst = _consts(T, tok0)
        win[c] = (np.ascontiguousarray(xs.T), cst["cosT"], cst["sinT"])
    zero_w = (np.zeros((1024, T), np.float32), np.zeros((128, T), np.float32), np.zeros((128, T), np.float32))
    maps = []
    for j in range(ncores):
        m = dict(shared)
        cst = _consts(T, j * TP - HALO)
        for k in ("masks", "kscale", "gdec", "scanm", "ident", "retW"):
            m[k] = cst[k]
        ws = [win[j - w] if j - w >= 0 else zero_w for w in range(nw)]
        m["xT"] = np.stack([w_[0] for w_ in ws], 0)
        m["cosT"] = np.stack([w_[1] for w_ in ws], 0)
        m["sinT"] = np.stack([w_[2] for w_ in ws], 0)
        flag = np.ones((128, 8), np.float32)
        valid = np.zeros((128, 8), np.float32)
        for w in range(8):
            if j - w <= 0:
                flag[:, w] = 0.0
            if w >= 1 and j - w >= 0:
                valid[:, w - 1] = 1.0
        m["flag"] = flag
        m["valid"] = valid
        seli = np.zeros((128, 8), np.float32)
        seli[:, :j] = 1.0
        m["seli"] = seli
        invc = np.zeros((128, 4, 16), np.float32)
        for g, w in enumerate((2, 4, 8, 16)):
            tg = j * TP + np.arange(16)
            invc[:, g, :] = (1.0 / np.minimum(tg + 1, w))[None, :]
        m["invc"] = invc.reshape(128, 64)
        maps.append(m)
    return maps, TP


_CACHE = {}


def _get(TP, mode, ncores):
    key = (TP, mode, ncores)
    if key not in _CACHE:
        r = build(TP, mode, ncores)
        _CACHE[key] = (r[0], r[2])
    return _CACHE[key]


def kernel(**inp):
    ncores = 8
    maps, TP = make_inputs(inp, ncores, 8)
    ncF, names = _get(TP, "F2", ncores)
    r = run_bass_kernel_spmd(ncF, [{k: m[k] for k in names} for m in maps], core_ids=list(range(ncores)))
    y = np.concatenate([r.results[j]["yT"].T for j in range(ncores)], 0)
    return np.ascontiguousarray(y[None].astype(np.float32))
```
